# Optimizing a Trainium2 kernel written in Bass

```python
import math
import jax
import jax.numpy as jnp
from jax import lax
import numpy as np

D_MODEL = 1024
BATCH = 16
SEQ = 2048
DEPTH = 4

GRID_W = 64
CTX_LEN = 256
N_MOD = 6
NORM_EPS = 1e-6
GN_EPS = 64e-5

BRANCH_WIDTH = 512
N_BRANCH = 3

SSM_WIDTH = BRANCH_WIDTH
SSM_GROUP = 16
SSM_GROUPS = SSM_WIDTH // SSM_GROUP
SSM_STATE = 64
SSM_DT_MIN = 1e-3
SSM_DT_MAX = 1e-1

RWKV_WIDTH = BRANCH_WIDTH
RWKV_HEAD = 64
RWKV_HEADS = RWKV_WIDTH // RWKV_HEAD
DECAY_LORA = 64
ICLR_LORA = 64
GATE_LORA = 128
RWKV_IN = 3 * RWKV_WIDTH + 2 * DECAY_LORA + 2 * ICLR_LORA + GATE_LORA
RWKV_SPLITS = (RWKV_WIDTH, 2 * RWKV_WIDTH, 3 * RWKV_WIDTH,
               3 * RWKV_WIDTH + 2 * DECAY_LORA,
               3 * RWKV_WIDTH + 2 * DECAY_LORA + 2 * ICLR_LORA)

MLA_HEADS = 8
MLA_NOPE = 64
MLA_ROPE = 32
MLA_V = BRANCH_WIDTH // MLA_HEADS
Q_LORA = 384
KV_LORA = 256
MLA_IN = Q_LORA + KV_LORA + MLA_ROPE
MLA_SCALE = 1.0 / math.sqrt(MLA_NOPE + MLA_ROPE)
ROPE_AXIS_DIMS = MLA_ROPE // 2
ROPE_BASE = 10000.0
Q_BLOCK = 128

RWKV_OFF = SSM_WIDTH
MLA_OFF = RWKV_OFF + RWKV_IN
GATE_OFF = MLA_OFF + MLA_IN
N_IN = GATE_OFF + N_BRANCH * D_MODEL

N_EXPERTS = 16
EXPERT_FF = 1536
EC_CAPACITY = 2

kernel_name = 'hybrid_s5_rwkv7_mla_ecmoe_diffusion_trunk'


def rmsnorm(x, g):
    xf = x.astype(jnp.float32)
    y = xf * lax.rsqrt(jnp.mean(xf * xf, axis=-1, keepdims=True) + NORM_EPS)
    return (y * g.astype(jnp.float32)).astype(x.dtype)


def modulate(h, shift, scale):
    return h * (1.0 + scale) + shift


def centred_shift(p):
    prev = jnp.pad(p[:, :-1], ((0, 0), (1, 0), (0, 0)))
    nxt = jnp.pad(p[:, 1:], ((0, 0), (0, 1), (0, 0)))
    return 0.5 * (prev + nxt)


def axial_rope(rows):
    row = jnp.repeat(jnp.arange(rows), GRID_W).astype(jnp.float32)
    col = jnp.tile(jnp.arange(GRID_W), rows).astype(jnp.float32)
    inv = ROPE_BASE ** (-jnp.arange(0, ROPE_AXIS_DIMS, 2, dtype=jnp.float32) / ROPE_AXIS_DIMS)
    ang = jnp.concatenate([row[:, None] * inv, col[:, None] * inv], axis=-1)
    return jnp.cos(ang), jnp.sin(ang)


def apply_rope(x, cos, sin):
    half = MLA_ROPE // 2
    x1, x2 = x[..., :half], x[..., half:]
    return jnp.concatenate([x1 * cos - x2 * sin, x1 * sin + x2 * cos], axis=-1).astype(x.dtype)


def ssm_discretise(lam_re, lam_im, log_dt, b_re, b_im):
    f32 = jnp.float32
    lr, li = lam_re.astype(f32), lam_im.astype(f32)
    dt = jnp.exp(log_dt.astype(f32))[:, None]
    mag = jnp.exp(lr * dt)
    abar_re, abar_im = mag * jnp.cos(li * dt), mag * jnp.sin(li * dt)
    den = lr * lr + li * li
    nr, ni = abar_re - 1.0, abar_im
    coef_re = (nr * lr + ni * li) / den
    coef_im = (ni * lr - nr * li) / den
    br, bi = b_re.astype(f32), b_im.astype(f32)
    bbar_re = coef_re[..., None] * br - coef_im[..., None] * bi
    bbar_im = coef_re[..., None] * bi + coef_im[..., None] * br
    return abar_re, abar_im, bbar_re, bbar_im


def _ssm_combine(e1, e2):
    a1r, a1i, b1r, b1i = e1
    a2r, a2i, b2r, b2i = e2
    return (a2r * a1r - a2i * a1i, a2r * a1i + a2i * a1r,
            a2r * b1r - a2i * b1i + b2r, a2r * b1i + a2i * b1r + b2i)


def ssm_scan(u, abar_re, abar_im, bbar_re, bbar_im, reverse, s0_re=None, s0_im=None):
    T = u.shape[1]
    bu_re = jnp.einsum('btgc,gpc->btgp', u, bbar_re)
    bu_im = jnp.einsum('btgc,gpc->btgp', u, bbar_im)
    a_re = jnp.broadcast_to(abar_re, (1, T) + abar_re.shape)
    a_im = jnp.broadcast_to(abar_im, (1, T) + abar_im.shape)
    acc_re, acc_im, x_re, x_im = lax.associative_scan(
        _ssm_combine, (a_re, a_im, bu_re, bu_im), reverse=reverse, axis=1)
    if s0_re is not None:
        s_re, s_im = s0_re[:, None], s0_im[:, None]
        x_re = x_re + acc_re * s_re - acc_im * s_im
        x_im = x_im + acc_re * s_im + acc_im * s_re
    return x_re, x_im


def ssm_readout(x_re, x_im, c_re, c_im):
    return (jnp.einsum('btgp,gcp->btgc', x_re, c_re.astype(jnp.float32))
            - jnp.einsum('btgp,gcp->btgc', x_im, c_im.astype(jnp.float32)))


def ssm_glu(y, glu_w, glu_b):
    B, T = y.shape[:2]
    y = jax.nn.gelu(y.reshape(B, T, SSM_WIDTH))
    return y * jax.nn.sigmoid(y @ glu_w + glu_b)


def ssm_branch(u_ctx, u_lat, lam_re, lam_im, log_dt, b_re, b_im, c_re, c_im, d_skip,
               glu_w, glu_b, with_ctx):
    def groups(u):
        return u.astype(jnp.float32).reshape(u.shape[0], u.shape[1], SSM_GROUPS, SSM_GROUP)
    uc, ul = groups(u_ctx), groups(u_lat)
    d = d_skip.astype(jnp.float32).reshape(SSM_GROUPS, SSM_GROUP)
    y_lat = ul * d
    y_ctx = uc * d if with_ctx else None
    for j, reverse in enumerate((False, True)):
        disc = ssm_discretise(lam_re[j], lam_im[j], log_dt[j], b_re, b_im)
        xc_re, xc_im = ssm_scan(uc, *disc, reverse=reverse)
        end = 0 if reverse else -1
        xl_re, xl_im = ssm_scan(ul, *disc, reverse=reverse,
                                s0_re=xc_re[:, end], s0_im=xc_im[:, end])
        y_lat = y_lat + ssm_readout(xl_re, xl_im, c_re[j], c_im[j])
        if with_ctx:
            y_ctx = y_ctx + ssm_readout(xc_re, xc_im, c_re[j], c_im[j])
    out_lat = ssm_glu(y_lat, glu_w, glu_b)
    out_ctx = ssm_glu(y_ctx, glu_w, glu_b) if with_ctx else None
    return out_ctx, out_lat


def rwkv_prepare(p, mu, w0, w2, a0, a2, g2, k_k, k_a):
    B, T, _ = p.shape
    p = p.astype(jnp.float32)
    p = p + mu * (centred_shift(p) - p)
    r, k, v, pw, pa, pg = jnp.split(p, RWKV_SPLITS, axis=-1)
    pw = pw.reshape(B, T, 2, DECAY_LORA)
    pa = pa.reshape(B, T, 2, ICLR_LORA)
    w_log = -jax.nn.softplus(-(w0 + jnp.einsum('btjl,jlc->btjc', jnp.tanh(pw), w2))) - 0.5
    decay = jnp.exp(-jnp.exp(w_log))
    iclr = jax.nn.sigmoid(a0 + jnp.einsum('btjl,jlc->btjc', pa, a2))
    k_dir = k[:, :, None] * (1.0 + (iclr - 1.0) * k_a)
    g = jax.nn.sigmoid(pg) @ g2

    def heads(t):
        return t.reshape(t.shape[:-1] + (RWKV_HEADS, RWKV_HEAD))
    kk = heads(k * k_k)
    kk = kk * lax.rsqrt(jnp.sum(kk * kk, axis=-1, keepdims=True) + 1e-12)
    return heads(r), heads(v), kk, g, heads(decay), heads(iclr), heads(k_dir)


def rwkv_scan(s0, r, w, k, v, kk, a, reverse):
    xs = tuple(jnp.moveaxis(t, 1, 0) for t in (r, w, k, v, kk, a))

    def step(S, inp):
        r_t, w_t, k_t, v_t, kk_t, a_t = inp
        sa = jnp.einsum('bhvk,bhk->bhv', S, -kk_t)
        S = (S * w_t[:, :, None, :] + sa[..., None] * (kk_t * a_t)[:, :, None, :]
             + v_t[..., None] * k_t[:, :, None, :])
        return S, jnp.einsum('bhvk,bhk->bhv', S, r_t)

    s_end, ys = lax.scan(step, s0, xs, reverse=reverse)
    return s_end, jnp.moveaxis(ys, 0, 1)


def rwkv_readout(y, prep, r_k, ln_w, ln_b):
    r, v, _, g, _, _, k_dir = prep
    B, T = y.shape[:2]
    mean = jnp.mean(y, axis=-1, keepdims=True)
    var = jnp.mean(jnp.square(y - mean), axis=-1, keepdims=True)
    yn = ((y - mean) * lax.rsqrt(var + GN_EPS)).reshape(B, T, RWKV_WIDTH) * ln_w + ln_b
    bonus = jnp.sum(r * jnp.sum(k_dir, axis=2) * r_k, axis=-1, keepdims=True) * v
    return (yn + bonus.reshape(B, T, RWKV_WIDTH)) * g


def rwkv_branch(p_ctx, p_lat, mu, w0, w2, a0, a2, g2, k_k, k_a, r_k, ln_w, ln_b, with_ctx):
    pc = rwkv_prepare(p_ctx, mu, w0, w2, a0, a2, g2, k_k, k_a)
    pl = rwkv_prepare(p_lat, mu, w0, w2, a0, a2, g2, k_k, k_a)
    rc, vc, kkc, _, dc, ac, kc = pc
    rl, vl, kkl, _, dl, al, kl = pl
    s_zero = jnp.zeros((p_lat.shape[0], RWKV_HEADS, RWKV_HEAD, RWKV_HEAD), jnp.float32)
    y_lat = jnp.zeros_like(rl)
    y_ctx = jnp.zeros_like(rc) if with_ctx else None
    for j, reverse in enumerate((False, True)):
        s_ctx, yc = rwkv_scan(s_zero, rc, dc[:, :, j], kc[:, :, j], vc, kkc, ac[:, :, j], reverse)
        _, yl = rwkv_scan(s_ctx, rl, dl[:, :, j], kl[:, :, j], vl, kkl, al[:, :, j], reverse)
        y_lat = y_lat + yl
        if with_ctx:
            y_ctx = y_ctx + yc
    out_lat = rwkv_readout(y_lat, pl, r_k, ln_w, ln_b)
    out_ctx = rwkv_readout(y_ctx, pc, r_k, ln_w, ln_b) if with_ctx else None
    return out_ctx, out_lat


def mla_keys(p, kv_norm, w_ukv, kn_nope, kn_rope, cos, sin):
    B, T, _ = p.shape
    ckv = rmsnorm(p[..., Q_LORA:Q_LORA + KV_LORA], kv_norm)
    k_rope = rmsnorm(p[..., Q_LORA + KV_LORA:], kn_rope)
    kv = (ckv @ w_ukv).reshape(B, T, MLA_HEADS, MLA_NOPE + MLA_V)
    k_nope = rmsnorm(kv[..., :MLA_NOPE], kn_nope)
    v = kv[..., MLA_NOPE:]
    if cos is not None:
        k_rope = apply_rope(k_rope, cos, sin)
    return k_nope, k_rope, v


def mla_queries(p, q_norm, w_uq, qn_nope, qn_rope, cos, sin):
    B, T, _ = p.shape
    cq = rmsnorm(p[..., :Q_LORA], q_norm)
    q = (cq @ w_uq).reshape(B, T, MLA_HEADS, MLA_NOPE + MLA_ROPE)
    q_nope = rmsnorm(q[..., :MLA_NOPE], qn_nope)
    q_rope = rmsnorm(q[..., MLA_NOPE:], qn_rope)
    if cos is not None:
        q_rope = apply_rope(q_rope, cos[:, None], sin[:, None])
    return q_nope, q_rope


def mla_attend(q_nope, q_rope, k_nope, k_rope, v):
    s = (jnp.einsum('bqhd,bkhd->bhqk', q_nope, k_nope)
         + jnp.einsum('bqhr,bkr->bhqk', q_rope, k_rope))
    pr = jax.nn.softmax(s.astype(jnp.float32) * MLA_SCALE, axis=-1).astype(v.dtype)
    return jnp.einsum('bhqk,bkhd->bqhd', pr, v)


def mla_branch(p_ctx, p_lat, cos, sin, q_norm, kv_norm, w_uq, w_ukv, qn_nope, kn_nope,
               qn_rope, kn_rope, with_ctx):
    B, T, _ = p_lat.shape
    kn_c, kr_c, v_c = mla_keys(p_ctx, kv_norm, w_ukv, kn_nope, kn_rope, None, None)
    kn_l, kr_l, v_l = mla_keys(p_lat, kv_norm, w_ukv, kn_nope, kn_rope, cos, sin)
    qn_l, qr_l = mla_queries(p_lat, q_norm, w_uq, qn_nope, qn_rope, cos, sin)
    kn = jnp.concatenate([kn_c, kn_l], axis=1)
    kr = jnp.concatenate([kr_c, kr_l], axis=1)
    v = jnp.concatenate([v_c, v_l], axis=1)
    nb = T // Q_BLOCK

    def to_blocks(t):
        return jnp.swapaxes(t.reshape((B, nb, Q_BLOCK) + t.shape[2:]), 0, 1)

    out = lax.map(lambda qs: mla_attend(qs[0], qs[1], kn, kr, v), (to_blocks(qn_l), to_blocks(qr_l)))
    out_lat = jnp.swapaxes(out, 0, 1).reshape(B, T, MLA_HEADS * MLA_V)
    out_ctx = None
    if with_ctx:
        qn_c, qr_c = mla_queries(p_ctx, q_norm, w_uq, qn_nope, qn_rope, None, None)
        out_ctx = mla_attend(qn_c, qr_c, kn_c, kr_c, v_c).reshape(B, p_ctx.shape[1], MLA_HEADS * MLA_V)
    return out_ctx, out_lat


def merge_branches(gate_logits, ys, w_branch, w_out):
    B, T, _ = gate_logits.shape
    gates = jax.nn.sigmoid(gate_logits.reshape(B, T, N_BRANCH, D_MODEL))
    y = jnp.stack([t.astype(gate_logits.dtype) for t in ys], axis=2)
    br = jnp.einsum('btjn,jnd->btjd', y, w_branch)
    return jnp.sum(gates * br, axis=2) @ w_out


def token_mixer(h_ctx, h_lat, cos, sin, with_ctx, w_in, ssm_p, rwkv_p, mla_p, w_branch, w_out):
    p_ctx = h_ctx @ w_in
    p_lat = h_lat @ w_in
    s_c, s_l = ssm_branch(p_ctx[..., :RWKV_OFF], p_lat[..., :RWKV_OFF], *ssm_p, with_ctx=with_ctx)
    r_c, r_l = rwkv_branch(p_ctx[..., RWKV_OFF:MLA_OFF], p_lat[..., RWKV_OFF:MLA_OFF], *rwkv_p,
                           with_ctx=with_ctx)
    m_c, m_l = mla_branch(p_ctx[..., MLA_OFF:GATE_OFF], p_lat[..., MLA_OFF:GATE_OFF], cos, sin,
                          *mla_p, with_ctx=with_ctx)
    out_lat = merge_branches(p_lat[..., GATE_OFF:], (s_l, r_l, m_l), w_branch, w_out)
    out_ctx = merge_branches(p_ctx[..., GATE_OFF:], (s_c, r_c, m_c), w_branch, w_out) if with_ctx else None
    return out_ctx, out_lat


def ec_moe(h, router_w, w1, w3, w2):
    B, T, _ = h.shape
    cap = EC_CAPACITY * T // N_EXPERTS
    aff = jax.nn.softmax((h @ router_w).astype(jnp.float32), axis=-1)
    gate, idx = lax.top_k(jnp.swapaxes(aff, 1, 2), cap)
    bidx = jnp.arange(B)[:, None, None]
    xs = h[bidx, idx]
    hid = jax.nn.silu(jnp.einsum('becd,edf->becf', xs, w1)) * jnp.einsum('becd,edf->becf', xs, w3)
    y = jnp.einsum('becf,efd->becd', hid, w2) * gate[..., None].astype(h.dtype)
    return jnp.zeros_like(h).at[bidx, idx].add(y)


def setup_inputs(seed: int = 0) -> dict:
    key = jax.random.key(seed)
    ks = iter(jax.random.split(key, 64))
    f32 = jnp.float32

    def nrm(shape, scale):
        return scale * jax.random.normal(next(ks), shape, f32)

    def gain(shape):
        return 1.0 + nrm(shape, 0.02)

    L, D = DEPTH, D_MODEL
    G, P, GC = SSM_GROUPS, SSM_STATE, SSM_GROUP
    W, H, N = RWKV_WIDTH, RWKV_HEADS, RWKV_HEAD
    E, F = N_EXPERTS, EXPERT_FF
    return {
        'x': nrm((BATCH, SEQ, D), 1.0),
        'c': nrm((BATCH, D), 1.0),
        'ctx': nrm((BATCH, CTX_LEN, D), 1.0),
        'c_ctx': nrm((D,), 1.0),
        'ada_w': nrm((L, D, N_MOD * D), 0.5 * D ** -0.5),
        'ada_b': nrm((L, N_MOD * D), 0.02),
        'norm1_g': gain((L, D)),
        'norm2_g': gain((L, D)),
        'w_in': nrm((L, D, N_IN), D ** -0.5),
        'ssm_lambda_re': -0.5 + nrm((L, 2, G, P), 0.01),
        'ssm_lambda_im': math.pi * jnp.arange(P, dtype=f32) + nrm((L, 2, G, P), 0.01),
        'ssm_log_dt': jax.random.uniform(next(ks), (L, 2, G), f32,
                                         math.log(SSM_DT_MIN), math.log(SSM_DT_MAX)),
        'ssm_b_re': nrm((L, G, P, GC), (2.0 * GC) ** -0.5),
        'ssm_b_im': nrm((L, G, P, GC), (2.0 * GC) ** -0.5),
        'ssm_c_re': nrm((L, 2, G, GC, P), (2.0 * P) ** -0.5),
        'ssm_c_im': nrm((L, 2, G, GC, P), (2.0 * P) ** -0.5),
        'ssm_d': nrm((L, SSM_WIDTH), 1.0),
        'ssm_glu_w': nrm((L, SSM_WIDTH, SSM_WIDTH), SSM_WIDTH ** -0.5),
        'ssm_glu_b': nrm((L, SSM_WIDTH), 0.02),
        'rwkv_mu': jax.random.uniform(next(ks), (L, RWKV_IN), f32, 0.0, 1.0),
        'rwkv_w0': jax.random.uniform(next(ks), (L, 2, W), f32, -6.0, 0.0),
        'rwkv_w2': nrm((L, 2, DECAY_LORA, W), 0.5 * DECAY_LORA ** -0.5),
        'rwkv_a0': nrm((L, 2, W), 0.1),
        'rwkv_a2': nrm((L, 2, ICLR_LORA, W), 0.5 * ICLR_LORA ** -0.5),
        'rwkv_g2': nrm((L, GATE_LORA, W), GATE_LORA ** -0.5),
        'rwkv_k_k': 0.85 + nrm((L, W), 0.02),
        'rwkv_k_a': 1.0 + nrm((L, W), 0.02),
        'rwkv_r_k': nrm((L, H, N), 0.1),
        'rwkv_ln_w': gain((L, W)),
        'rwkv_ln_b': nrm((L, W), 0.02),
        'mla_q_norm': gain((L, Q_LORA)),
        'mla_kv_norm': gain((L, KV_LORA)),
        'mla_w_uq': nrm((L, Q_LORA, MLA_HEADS * (MLA_NOPE + MLA_ROPE)), Q_LORA ** -0.5),
        'mla_w_ukv': nrm((L, KV_LORA, MLA_HEADS * (MLA_NOPE + MLA_V)), KV_LORA ** -0.5),
        'mla_qn_nope': gain((L, MLA_NOPE)),
        'mla_kn_nope': gain((L, MLA_NOPE)),
        'mla_qn_rope': gain((L, MLA_ROPE)),
        'mla_kn_rope': gain((L, MLA_ROPE)),
        'w_branch': nrm((L, N_BRANCH, BRANCH_WIDTH, D), BRANCH_WIDTH ** -0.5),
        'w_out': nrm((L, D, D), D ** -0.5),
        'router_w': nrm((L, D, E), D ** -0.5),
        'moe_w1': nrm((L, E, D, F), D ** -0.5),
        'moe_w3': nrm((L, E, D, F), D ** -0.5),
        'moe_w2': nrm((L, E, F, D), F ** -0.5),
    }


def reference(x, c, ctx, c_ctx, ada_w, ada_b, norm1_g, norm2_g, w_in,
              ssm_lambda_re, ssm_lambda_im, ssm_log_dt, ssm_b_re, ssm_b_im, ssm_c_re, ssm_c_im,
              ssm_d, ssm_glu_w, ssm_glu_b,
              rwkv_mu, rwkv_w0, rwkv_w2, rwkv_a0, rwkv_a2, rwkv_g2, rwkv_k_k, rwkv_k_a, rwkv_r_k,
              rwkv_ln_w, rwkv_ln_b,
              mla_q_norm, mla_kv_norm, mla_w_uq, mla_w_ukv, mla_qn_nope, mla_kn_nope,
              mla_qn_rope, mla_kn_rope,
              w_branch, w_out, router_w, moe_w1, moe_w3, moe_w2):
    B, T, _ = x.shape
    rows = T // GRID_W
    cos, sin = axial_rope(rows)
    silu_c = jax.nn.silu(c)
    silu_cc = jax.nn.silu(c_ctx)
    for l in range(DEPTH):
        with_ctx = l < DEPTH - 1
        mod_lat = (silu_c @ ada_w[l] + ada_b[l]).reshape(B, N_MOD, 1, D_MODEL)
        mod_ctx = (silu_cc @ ada_w[l] + ada_b[l]).reshape(N_MOD, D_MODEL)
        h_lat = modulate(rmsnorm(x, norm1_g[l]), mod_lat[:, 0], mod_lat[:, 1])
        h_ctx = modulate(rmsnorm(ctx, norm1_g[l]), mod_ctx[0], mod_ctx[1])
        ssm_p = (ssm_lambda_re[l], ssm_lambda_im[l], ssm_log_dt[l], ssm_b_re[l], ssm_b_im[l],
                 ssm_c_re[l], ssm_c_im[l], ssm_d[l], ssm_glu_w[l], ssm_glu_b[l])
        rwkv_p = (rwkv_mu[l], rwkv_w0[l], rwkv_w2[l], rwkv_a0[l], rwkv_a2[l], rwkv_g2[l],
                  rwkv_k_k[l], rwkv_k_a[l], rwkv_r_k[l], rwkv_ln_w[l], rwkv_ln_b[l])
        mla_p = (mla_q_norm[l], mla_kv_norm[l], mla_w_uq[l], mla_w_ukv[l], mla_qn_nope[l],
                 mla_kn_nope[l], mla_qn_rope[l], mla_kn_rope[l])
        mix_ctx, mix_lat = token_mixer(h_ctx, h_lat, cos, sin, with_ctx, w_in[l], ssm_p, rwkv_p,
                                       mla_p, w_branch[l], w_out[l])
        x = x + mod_lat[:, 2] * mix_lat
        h2 = modulate(rmsnorm(x, norm2_g[l]), mod_lat[:, 3], mod_lat[:, 4])
        x = x + mod_lat[:, 5] * ec_moe(h2, router_w[l], moe_w1[l], moe_w3[l], moe_w2[l])
        if with_ctx:
            ctx = ctx + mod_ctx[2] * mix_ctx
            h2c = modulate(rmsnorm(ctx, norm2_g[l]), mod_ctx[3], mod_ctx[4])
            ctx = ctx + mod_ctx[5] * ec_moe(h2c, router_w[l], moe_w1[l], moe_w3[l], moe_w2[l])
    return x
```

```python
import contextlib
import numpy as np
import concourse.bass as bass
import concourse.mybir as mybir
from concourse.bass_utils import run_bass_kernel_spmd

F32 = mybir.dt.float32
BF16 = mybir.dt.bfloat16
AF = mybir.ActivationFunctionType
ALU = mybir.AluOpType
AX = mybir.AxisListType

NCORES = 8
NB = 2
D = 1024
KC = 8
TC = 256
TL = 2048
TT = TC + TL
DEPTH = 4
N_IN = 6176
NCH = 49
NORM_EPS = 1e-6


def chunk_cols(ci):
    if ci < 24:
        return ci * 128, 128
    if ci == 24:
        return 3072, 32
    return 3104 + (ci - 25) * 128, 128


class Buf:
    __slots__ = ("name", "w", "r")

    def __init__(self, name=""):
        self.name = name
        self.w = None
        self.r = []


class K:
    NDMA = 12

    def __init__(self, nc):
        self.nc = nc
        self.eng = {"pe": nc.tensor, "dve": nc.vector, "act": nc.scalar, "pool": nc.gpsimd, "sp": nc.sync}
        self.sem = {}
        self.cnt = {}
        self.seen = {e: {} for e in self.eng}
        self._stack = []
        for e in self.eng:
            self.sem[e] = self._mksem("s_" + e)
            self.cnt[e] = 0
        self.dq = {}
        for q in ("sp", "act", "pool"):
            self.dq[q] = {"n": 0}
            for i in range(self.NDMA):
                self.sem[(q, i)] = self._mksem(f"d_{q}{i}")
        self.n_instr = 0
        self.n_wait = 0

    def _mksem(self, name):
        g = self.nc.semaphore(name)
        s = g.__enter__()
        self._stack.append(g)
        return s

    def _wait(self, e, dep):
        if dep is None:
            return
        key, val = dep
        if key == "pe" and e == "pe":
            return
        if self.seen[e].get(key, 0) >= val:
            return
        self.eng[e].wait_ge(self.sem[key], val)
        self.seen[e][key] = val
        self.n_wait += 1

    def _deps(self, e, reads, writes):
        for b in reads:
            self._wait(e, b.w)
        for b in writes:
            self._wait(e, b.w)
            for r in b.r:
                self._wait(e, r)

    def _mark(self, tok, reads, writes):
        for b in reads:
            b.r = [r for r in b.r if r[0] != tok[0]]
            b.r.append(tok)
        for b in writes:
            b.w = tok
            b.r = []

    def op(self, e, fn, reads=(), writes=()):
        self._deps(e, reads, writes)
        ins = fn(self.eng[e])
        self.cnt[e] += 1
        ins.then_inc(self.sem[e], 1)
        self._mark((e, self.cnt[e]), reads, writes)
        self.n_instr += 1
        return ins

    def dma(self, q, out, in_, reads=(), writes=(), **kw):
        d = self.dq[q]
        i = d["n"] % self.NDMA
        gen = d["n"] // self.NDMA
        key = (q, i)
        if gen > 0:
            self._wait(q, (key, 16 * gen))
        self._deps(q, reads, writes)
        ins = self.eng[q].dma_start(out=out, in_=in_, **kw)
        ins.then_inc(self.sem[key], 16)
        d["n"] += 1
        self._mark((key, 16 * (gen + 1)), reads, writes)
        self.n_instr += 1

    def barrier(self):
        toks = [(e, self.cnt[e]) for e in self.eng if self.cnt[e] > 0]
        for q, d in self.dq.items():
            n = d["n"]
            for i in range(self.NDMA):
                c = (n - i + self.NDMA - 1) // self.NDMA
                if c > 0:
                    toks.append(((q, i), 16 * c))
        for e in self.eng:
            for t in toks:
                self._wait(e, t)


class Ring:
    def __init__(self, P, name, n, shape, dtype, psum=False):
        self.items = []
        for i in range(n):
            t = P.ps(f"{name}{i}", shape, dtype) if psum else P.sb(f"{name}{i}", shape, dtype)
            self.items.append((t, Buf(f"{name}{i}")))
        self.i = 0

    def next(self):
        it = self.items[self.i % len(self.items)]
        self.i += 1
        return it


class Prog:
    def __init__(self, n_layers=DEPTH, debug=()):
        self.nc = nc = bass.Bass("TRN2", target_bir_lowering=False)
        self.k = K(nc)
        self.debug = set(debug)
        self.n_layers = n_layers
        self._scopes = []
        self.uid = 0
        self.inputs = {}
        self.outputs = {}
        self.dbufs = {}

    def din(self, name, shape, dtype=F32):
        t = self.nc.dram_tensor(name, list(shape), dtype, kind="ExternalInput").ap()
        self.inputs[name] = t
        return t

    def dscr(self, name, shape, dtype, out=False):
        kind = "ExternalOutput" if (out or name in self.debug) else "Internal"
        t = self.nc.dram_tensor(name, list(shape), dtype, kind=kind).ap()
        if kind == "ExternalOutput":
            self.outputs[name] = t
        self.dbufs[name] = Buf(name)
        return t

    @contextlib.contextmanager
    def scope(self):
        st = contextlib.ExitStack()
        self._scopes.append(st)
        try:
            yield
        finally:
            self.k.barrier()
            self._scopes.pop()
            st.close()

    def sb(self, name, shape, dtype):
        self.uid += 1
        g = self.nc.sbuf_tensor(f"{name}_{self.uid}", list(shape), dtype)
        return self._scopes[-1].enter_context(g)

    def ps(self, name, shape, dtype=F32):
        self.uid += 1
        g = self.nc.psum_tensor(f"{name}_{self.uid}", list(shape), dtype)
        return self._scopes[-1].enter_context(g)


MLA_SCALE = 1.0 / float(np.sqrt(96.0))


def declare_inputs(P, C):
    C.mla_w_uq = P.din("mla_w_uq", [DEPTH, 384, 768])
    C.mla_w_ukv = P.din("mla_w_ukv", [DEPTH, 256, 1024])
    C.mla_mats = P.din("mla_mats", [128, 3, 128])
    C.mla_vec = P.din("mla_vec", [DEPTH, 128, 8])
    C.rope_tab = P.din("rope_tab", [96, 2, TT])
    C.ssm_iota = P.din("ssm_iota", [128, 2, TT])
    C.ssm_sv = P.din("ssm_sv", [DEPTH, 128, 2, 16, 3])
    C.ssm_BT = P.din("ssm_BT", [DEPTH, 2, 16, 128, 128])
    C.ssm_CT = P.din("ssm_CT", [DEPTH, 2, 2, 16, 128, 128])
    C.ssm_vec = P.din("ssm_vec", [DEPTH, 128, 2, 4])
    C.ssm_glu_w = P.din("ssm_glu_w", [DEPTH, 512, 512])
    C.w_branch = P.din("w_branch", [DEPTH, 3, 512, D])
    C.w_out = P.din("w_out", [DEPTH, D, D])
    C.router_w = P.din("router_w", [DEPTH, D, 16])
    C.ident = P.din("ident", [128, 128])
    C.LG = P.dscr("LG", [NB, 16, TT], F32)
    C.bLG = Buf("LG")
    C.H2 = P.dscr("H2", [NB, TT, D], BF16)
    C.bH2 = Buf("H2")
    C.moe_w1 = P.din("moe_w1", [DEPTH, NE, D, FF])
    C.moe_w3 = P.din("moe_w3", [DEPTH, NE, D, FF])
    C.moe_w2 = P.din("moe_w2", [DEPTH, NE, FF, D])
    C.moe_cst = P.din("moe_cst", [128, 3, 256])
    C.moe_jcol = P.din("moe_jcol", [128, 2])
    C.POSD = P.dscr("POSD", [NB, NE, TT], F32)
    C.MGD = P.dscr("MGD", [NB, NE, TT], F32)
    C.bPOSD = Buf("POSD")
    C.XS = P.dscr("XS", [NE, D, NJ], BF16)
    C.bXS = Buf("XS")
    C.YE = P.dscr("YE", [NE, NJ, D], BF16)
    C.bYE = Buf("YE")
    C.OUT = P.dscr("OUT", [NB, D, TL], F32, out=True)
    C.bOUT = Buf("OUT")
    C.rw_vec = P.din("rw_vec", [DEPTH, 128, 51])
    C.rwkv_w2 = P.din("rwkv_w2", [DEPTH, 2, 64, 512])
    C.rwkv_a2 = P.din("rwkv_a2", [DEPTH, 2, 64, 512])
    C.rwkv_g2 = P.din("rwkv_g2", [DEPTH, 128, 512])
    C.rw_bd = P.din("rw_bd", [128, 128])
    C.rw_rst = P.din("rw_rst", [128, TT + 1])
    C.rw_masks = P.din("rw_masks", [128, 2, 640])
    C.rw_lm = P.din("rw_lm", [128, 4, 128])
    C.RWT = P.dscr("RWT", [NB, 2, 4, 512, TT], BF16)
    C.RWV = P.dscr("RWV", [NB, 512, TT], BF16)
    C.RWG = P.dscr("RWG", [NB, 512, TT], BF16)
    C.RWB = P.dscr("RWB", [NB, 512, TT], F32)
    C.RWPL = P.dscr("RWPL", [NB, 2, 512, NCK], F32)
    C.bRW = Buf("RW")
    if "YTOK" in P.debug:
        C.YTOK = P.dscr("YTOK", [NB, 128, NCK, 512], F32)
    C.YG = P.dscr("YG", [NB, 512, TT], BF16)
    C.bYG = Buf("YG")


def rstd_from(k, ss_ap, out_ap, scale, bias, reads, bout):
    k.op("act", lambda e: e.activation(out_ap, ss_ap, AF.Sqrt, bias=bias, scale=scale), reads=reads, writes=[bout])
    k.op("dve", lambda e: e.reciprocal(out_ap, out_ap), reads=[bout], writes=[bout])


def phase_mla(P, C, l):
    k = P.k
    PT, YT = C.PT, C.YT
    blocks = [(0, TC)] + [(TC + i * 512, 512) for i in range(4)]
    with P.scope():
        mats = P.sb("mla_mats", [128, 3, 128], BF16)
        b_mats = Buf("mats")
        k.dma("pool", mats[:], C.mla_mats, writes=[b_mats])
        onesb = P.sb("onesb", [128, 128], BF16)
        b_onesb = Buf("onesb")
        k.op("dve", lambda e: e.memset(onesb[:], 1.0), writes=[b_onesb])
        vec = P.sb("mla_vec", [128, 8], F32)
        b_vec = Buf("vec")
        k.dma("sp", vec[:], C.mla_vec[l], writes=[b_vec])
        epsc = P.sb("epsc", [128, 1], F32)
        k.op("dve", lambda e: e.memset(epsc[:], NORM_EPS), writes=[b_vec])
        tab = P.sb("rope_tab", [96, 2, TT], F32)
        b_tab = Buf("tab")
        k.dma("sp", tab[:], C.rope_tab, writes=[b_tab])
        wuq = P.sb("wuq", [128, 3, 768], BF16)
        b_wuq = Buf("wuq")
        k.dma("pool", wuq[:], C.mla_w_uq[l].rearrange("(kc p) n -> p kc n", p=128), writes=[b_wuq])
        wk = P.sb("wk", [128, 2, 8, 64], BF16)
        wv = P.sb("wv", [128, 2, 8, 64], BF16)
        b_wkv = Buf("wkv")
        ukv = C.mla_w_ukv[l].rearrange("(kc p) (h x) -> p kc h x", p=128, x=128)
        for kc in range(2):
            k.dma("pool", wk[:, kc], ukv[:, kc, :, 0:64], writes=[b_wkv])
            k.dma("pool", wv[:, kc], ukv[:, kc, :, 64:128], writes=[b_wkv])
        QT = P.sb("QT", [96, 8, TT], BF16)
        KT = P.sb("KT", [96, 8, TT], BF16)
        Vt = P.sb("Vt", [128, 18, 512], BF16)
        for b in range(NB):
            b_Q = [Buf(f"Q{i}") for i in range(5)]
            b_K = [Buf(f"K{i}") for i in range(5)]
            b_V = [Buf(f"V{i}") for i in range(5)]
            with P.scope():
                x5r = Ring(P, "x5", 2, [128, 5, 512], BF16)
                krr = Ring(P, "kr", 2, [96, 512], BF16)
                sq5r = Ring(P, "sq5", 1, [128, 5, 512], BF16)
                cqnr = Ring(P, "cqn", 2, [128, 5, 512], BF16)
                psA = Ring(P, "psA", 3, [128, 512], F32, psum=True)
                psB = Ring(P, "psB", 3, [128, 512], F32, psum=True)
                rsr = Ring(P, "rs", 3, [128, 512], F32)
                f32r = Ring(P, "f32t", 3, [128, 512], F32)
                f32r2 = Ring(P, "f32u", 3, [128, 512], F32)
                bfr = Ring(P, "bft", 3, [128, 512], BF16)
                krf = P.sb("krf", [96, 512], BF16)
                b_krf = Buf("krf")
                for bi, (t0, n) in enumerate(blocks):
                    x5, bx5 = x5r.next()
                    k.dma("sp", x5[:, :, :n], PT[b, 19 * 128:24 * 128, t0:t0 + n].rearrange("(c p) n -> p c n", p=128),
                          reads=[C.bPT], writes=[bx5])
                    kr, bkr = krr.next()
                    k.dma("sp", kr[64:96, :n], PT[b, 24 * 128:24 * 128 + 32, t0:t0 + n], reads=[C.bPT], writes=[bkr])
                    sq5, bsq5 = sq5r.next()
                    k.op("act", lambda e: e.activation(sq5[:, :, :n], x5[:, :, :n], AF.Square), reads=[bx5], writes=[bsq5])
                    cqn, bcqn = cqnr.next()
                    for (c0, nc_, col0, dim) in ((0, 3, 0, 384.0), (3, 2, 3, 256.0)):
                        ps, bps = psA.next()
                        for c in range(nc_):
                            k.op("pe", lambda e: e.matmul(ps[:, :n], onesb[:], sq5[:, c0 + c, :n],
                                                          start=(c == 0), stop=(c == nc_ - 1)),
                                 reads=[bsq5, b_onesb], writes=[bps])
                        rs, brs = rsr.next()
                        rstd_from(k, ps[:, :n], rs[:, :n], 1.0 / dim, epsc[:, 0:1], [bps, b_vec], brs)
                        for c in range(nc_):
                            k.op("dve", lambda e: e.scalar_tensor_tensor(
                                cqn[:, c0 + c, :n], x5[:, c0 + c, :n], vec[:, col0 + c:col0 + c + 1], rs[:, :n],
                                ALU.mult, ALU.mult), reads=[bx5, brs, b_vec], writes=[bcqn])
                    sqk, bsqk = bfr.next()
                    k.op("act", lambda e: e.activation(sqk[64:96, :n], kr[64:96, :n], AF.Square), reads=[bkr], writes=[bsqk])
                    ps, bps = psA.next()
                    k.op("pe", lambda e: e.matmul(ps[64:96, :n], mats[64:96, 0, 64:96], sqk[64:96, :n], start=True, stop=True),
                         reads=[bsqk, b_mats], writes=[bps])
                    rs, brs = rsr.next()
                    rstd_from(k, ps[64:96, :n], rs[64:96, :n], 1.0 / 32.0, epsc[64:96, 0:1], [bps, b_vec], brs)
                    krn, bkrn = bfr.next()
                    k.op("dve", lambda e: e.scalar_tensor_tensor(
                        krn[64:96, :n], kr[64:96, :n], vec[64:96, 6:7], rs[64:96, :n], ALU.mult, ALU.mult),
                        reads=[bkr, brs, b_vec], writes=[bkrn])
                    ps2, bps2 = psB.next()
                    k.op("pe", lambda e: e.matmul(ps2[64:96, :n], mats[64:96, 2, 64:96], krn[64:96, :n], start=True, stop=True),
                         reads=[bkrn, b_mats], writes=[bps2])
                    t1, bt1 = f32r.next()
                    k.op("dve", lambda e: e.tensor_tensor(t1[64:96, :n], krn[64:96, :n], tab[64:96, 0, t0:t0 + n], ALU.mult),
                         reads=[bkrn, b_tab], writes=[bt1])
                    t2, bt2 = f32r2.next()
                    k.op("dve", lambda e: e.tensor_tensor(t2[64:96, :n], ps2[64:96, :n], tab[64:96, 1, t0:t0 + n], ALU.mult),
                         reads=[bps2, b_tab], writes=[bt2])
                    k.op("pool", lambda e: e.tensor_tensor(krf[64:96, :n], t1[64:96, :n], t2[64:96, :n], ALU.add),
                         reads=[bt1, bt2], writes=[b_krf])
                    k.op("pool", lambda e: e.tensor_copy(
                        KT[64:96, :, t0:t0 + n], krf[64:96, :n].unsqueeze(1).to_broadcast([32, 8, n])),
                        reads=[b_krf], writes=[b_K[bi]])
                    for h in range(8):
                        ps, bps = psA.next()
                        for c in range(3):
                            k.op("pe", lambda e: e.matmul(ps[:96, :n], wuq[:, c, h * 96:(h + 1) * 96], cqn[:, c, :n],
                                                          start=(c == 0), stop=(c == 2)),
                                 reads=[bcqn, b_wuq], writes=[bps])
                        qs, bqs = f32r.next()
                        k.op("act", lambda e: e.activation(qs[:96, :n], ps[:96, :n], AF.Copy), reads=[bps], writes=[bqs])
                        sq, bsq = bfr.next()
                        k.op("act", lambda e: e.activation(sq[:96, :n], ps[:96, :n], AF.Square), reads=[bps], writes=[bsq])
                        ps2, bps2 = psB.next()
                        k.op("pe", lambda e: e.matmul(ps2[:96, :n], mats[:96, 0, :96], sq[:96, :n], start=True, stop=True),
                             reads=[bsq, b_mats], writes=[bps2])
                        rs, brs = rsr.next()
                        rstd_from(k, ps2[:96, :n], rs[:96, :n], vec[:96, 7:8], epsc[:96, 0:1], [bps2, b_vec], brs)
                        qn, bqn = bfr.next()
                        k.op("dve", lambda e: e.scalar_tensor_tensor(
                            qn[:96, :n], qs[:96, :n], vec[:96, 5:6], rs[:96, :n], ALU.mult, ALU.mult),
                            reads=[bqs, brs, b_vec], writes=[bqn])
                        ps3, bps3 = psB.next()
                        k.op("pe", lambda e: e.matmul(ps3[:96, :n], mats[:96, 2, :96], qn[:96, :n], start=True, stop=True),
                             reads=[bqn, b_mats], writes=[bps3])
                        t1, bt1 = f32r.next()
                        k.op("pool", lambda e: e.tensor_tensor(t1[:96, :n], qn[:96, :n], tab[:, 0, t0:t0 + n], ALU.mult),
                             reads=[bqn, b_tab], writes=[bt1])
                        t2, bt2 = f32r2.next()
                        k.op("dve", lambda e: e.tensor_tensor(t2[:96, :n], ps3[:96, :n], tab[:, 1, t0:t0 + n], ALU.mult),
                             reads=[bps3, b_tab], writes=[bt2])
                        k.op("pool", lambda e: e.tensor_tensor(QT[:, h, t0:t0 + n], t1[:96, :n], t2[:96, :n], ALU.add),
                             reads=[bt1, bt2], writes=[b_Q[bi]])
                        ps, bps = psA.next()
                        for c in range(2):
                            k.op("pe", lambda e: e.matmul(ps[:64, :n], wk[:, c, h, :], cqn[:, 3 + c, :n],
                                                          start=(c == 0), stop=(c == 1)),
                                 reads=[bcqn, b_wkv], writes=[bps])
                        ks_, bks = f32r.next()
                        k.op("act", lambda e: e.activation(ks_[:64, :n], ps[:64, :n], AF.Copy), reads=[bps], writes=[bks])
                        sq, bsq = bfr.next()
                        k.op("act", lambda e: e.activation(sq[:64, :n], ps[:64, :n], AF.Square), reads=[bps], writes=[bsq])
                        ps2, bps2 = psB.next()
                        k.op("pe", lambda e: e.matmul(ps2[:64, :n], onesb[:64, :64], sq[:64, :n], start=True, stop=True),
                             reads=[bsq, b_onesb], writes=[bps2])
                        rs, brs = rsr.next()
                        rstd_from(k, ps2[:64, :n], rs[:64, :n], 1.0 / 64.0, epsc[:64, 0:1], [bps2, b_vec], brs)
                        k.op("dve", lambda e: e.scalar_tensor_tensor(
                            KT[0:64, h, t0:t0 + n], ks_[:64, :n], vec[:64, 6:7], rs[:64, :n], ALU.mult, ALU.mult),
                            reads=[bks, brs, b_vec], writes=[b_K[bi]])
                    for ti in range(n // 128):
                        tile_i = (t0 // 128) + ti
                        ps, bps = psA.next()
                        for c in range(2):
                            k.op("pe", lambda e: e.matmul(
                                ps[:, :], cqn[:, 3 + c, ti * 128:(ti + 1) * 128],
                                wv[:, c].rearrange("p h x -> p (h x)"), start=(c == 0), stop=(c == 1)),
                                reads=[bcqn, b_wkv], writes=[bps])
                        k.op("act", lambda e: e.activation(Vt[:, tile_i, :], ps[:, :], AF.Copy), reads=[bps], writes=[b_V[bi]])
            with P.scope():
                psS = Ring(P, "psS", 3, [128, 512], F32, psum=True)
                psO = Ring(P, "psO", 2, [64, 512], F32, psum=True)
                psD = Ring(P, "psD", 2, [64, 512], F32, psum=True)
                ptr = Ring(P, "pT", 3, [128, 512], BF16)
                rdr = Ring(P, "rden", 2, [64, 512], F32)
                osr = Ring(P, "ost", 3, [64, 512], BF16)
                allK = b_K + b_V
                for h in range(8):
                    for bi, (q0, n) in enumerate(blocks):
                        nkt = 2 if bi == 0 else 18
                        po, bpo = psO.next()
                        pd, bpd = psD.next()
                        for kt in range(nkt):
                            pS, bpS = psS.next()
                            k.op("pe", lambda e: e.matmul(pS[:, :n], KT[:, h, kt * 128:(kt + 1) * 128], QT[:, h, q0:q0 + n],
                                                          start=True, stop=True),
                                 reads=allK + [b_Q[bi]], writes=[bpS])
                            pT, bpT = ptr.next()
                            k.op("act", lambda e: e.activation(pT[:, :n], pS[:, :n], AF.Exp, scale=MLA_SCALE),
                                 reads=[bpS], writes=[bpT])
                            k.op("pe", lambda e: e.matmul(po[:, :n], Vt[:, kt, h * 64:(h + 1) * 64], pT[:, :n],
                                                          start=(kt == 0), stop=(kt == nkt - 1)),
                                 reads=[bpT] + b_V, writes=[bpo])
                            k.op("pe", lambda e: e.matmul(pd[:, :n], onesb[:, :64], pT[:, :n],
                                                          start=(kt == 0), stop=(kt == nkt - 1)),
                                 reads=[bpT, b_onesb], writes=[bpd])
                        rd, brd = rdr.next()
                        k.op("dve", lambda e: e.reciprocal(rd[:, :n], pd[:, :n]), reads=[bpd], writes=[brd])
                        os_, bos = osr.next()
                        k.op("dve", lambda e: e.tensor_tensor(os_[:, :n], po[:, :n], rd[:, :n], ALU.mult),
                             reads=[bpo, brd], writes=[bos])
                        k.dma("sp", YT[b, 2, h * 64:(h + 1) * 64, q0:q0 + n], os_[:, :n], reads=[bos], writes=[C.bYT])


PI = float(np.pi)


def rev_ap(ap2d, lo, hi):
    from concourse.ap import AP
    a = ap2d[:, lo:hi]
    return AP(a.tensor, a.offset + (hi - lo - 1) * a.ap[1][0], [list(a.ap[0]), [-a.ap[1][0], hi - lo]])


MAGIC = 12582912.0
TWO_PI = 2.0 * PI


def sin_reduced(k, out, tmp, in0, th, th2pi, shift, hp_tile, reads, bout, btmp):
    k.op("dve", lambda e: e.tensor_scalar(tmp, in0, th2pi, MAGIC + shift / TWO_PI, ALU.mult, ALU.add),
         reads=reads + [btmp], writes=[btmp])
    k.op("dve", lambda e: e.tensor_scalar(tmp, tmp, -MAGIC, -TWO_PI, ALU.add, ALU.mult), reads=[btmp], writes=[btmp])
    k.op("dve", lambda e: e.scalar_tensor_tensor(tmp, in0, th, tmp, ALU.mult, ALU.add), reads=reads + [btmp], writes=[btmp])
    if shift == 0.0:
        k.op("act", lambda e: e.activation(out, tmp, AF.Sin, scale=0.999999), reads=[btmp, bout], writes=[bout])
    else:
        k.op("act", lambda e: e.activation(out, tmp, AF.Sin, scale=0.999999, bias=hp_tile), reads=[btmp, bout], writes=[bout])


def phase_ssm(P, C, l):
    k = P.k
    PT, YT = C.PT, C.YT
    blocks = [(0, TC)] + [(TC + i * 512, 512) for i in range(4)]
    YG = C.YG
    with P.scope():
        iota = P.sb("iota", [128, 2, TT], F32)
        b_c = Buf("ssmconst")
        k.dma("sp", iota[:], C.ssm_iota, writes=[b_c])
        sv = P.sb("sv", [128, 2, 16, 3], F32)
        k.dma("sp", sv[:], C.ssm_sv[l], writes=[b_c])
        BT = P.sb("BT", [128, 2, 16, 128], BF16)
        k.dma("pool", BT[:], C.ssm_BT[l].rearrange("r s p n -> p r s n"), writes=[b_c])
        CT = P.sb("CT", [128, 2, 2, 16, 128], BF16)
        for j in range(2):
            k.dma("pool", CT[:, j], C.ssm_CT[l, j].rearrange("r s p n -> p r s n"), writes=[b_c])
        nCT = P.sb("nCT", [128, 2, 2, 16, 128], BF16)
        k.op("dve", lambda e: e.tensor_scalar(nCT[:].rearrange("p a b c d -> p (a b c d)"),
                                              CT[:].rearrange("p a b c d -> p (a b c d)"), -1.0, None, ALU.mult),
             reads=[b_c], writes=[b_c])
        dsk = P.sb("dsk", [128, 2, 4], F32)
        k.dma("sp", dsk[:], C.ssm_vec[l], writes=[b_c])
        hpi = P.sb("hpi", [128, 1], F32)
        k.op("dve", lambda e: e.memset(hpi[:], 0.5 * PI * 0.999999), writes=[b_c])
        shp = [128, 2, 16]
        names = "dt rho th th2 sn cs are aim den t1 t2 cre cim ncre".split()
        Tl = {n_: P.sb("d_" + n_, shp, F32) for n_ in names}
        bd = Buf("disc")
        lr, li, ldt = sv[:, :, :, 0], sv[:, :, :, 1], sv[:, :, :, 2]
        A_ = lambda e_, fn: k.op(e_, fn, reads=[b_c, bd], writes=[bd])
        A_("act", lambda e: e.activation(Tl["dt"][:], ldt, AF.Exp))
        A_("dve", lambda e: e.tensor_tensor(Tl["t1"][:], lr, Tl["dt"][:], ALU.mult))
        A_("act", lambda e: e.activation(Tl["rho"][:], Tl["t1"][:], AF.Exp))
        A_("dve", lambda e: e.tensor_tensor(Tl["th"][:], li, Tl["dt"][:], ALU.mult))
        sin_reduced(k, Tl["sn"][:], Tl["t1"][:], Tl["th"][:], 1.0, 1.0 / TWO_PI, 0.0, None, [b_c, bd], bd, bd)
        sin_reduced(k, Tl["cs"][:], Tl["t1"][:], Tl["th"][:], 1.0, 1.0 / TWO_PI, 0.5 * PI, hpi[:, 0:1], [b_c, bd], bd, bd)
        A_("dve", lambda e: e.tensor_scalar(Tl["th2"][:], Tl["th"][:], 1.0 / TWO_PI, None, ALU.mult))
        A_("dve", lambda e: e.tensor_tensor(Tl["are"][:], Tl["rho"][:], Tl["cs"][:], ALU.mult))
        A_("dve", lambda e: e.tensor_tensor(Tl["aim"][:], Tl["rho"][:], Tl["sn"][:], ALU.mult))
        A_("dve", lambda e: e.tensor_scalar(Tl["are"][:], Tl["are"][:], -1.0, None, ALU.add))
        A_("dve", lambda e: e.tensor_tensor(Tl["t1"][:], lr, lr, ALU.mult))
        A_("dve", lambda e: e.tensor_tensor(Tl["t2"][:], li, li, ALU.mult))
        A_("dve", lambda e: e.tensor_tensor(Tl["den"][:], Tl["t1"][:], Tl["t2"][:], ALU.add))
        A_("dve", lambda e: e.reciprocal(Tl["den"][:], Tl["den"][:]))
        A_("dve", lambda e: e.tensor_tensor(Tl["t1"][:], Tl["are"][:], lr, ALU.mult))
        A_("dve", lambda e: e.tensor_tensor(Tl["t2"][:], Tl["aim"][:], li, ALU.mult))
        A_("dve", lambda e: e.tensor_tensor(Tl["cre"][:], Tl["t1"][:], Tl["t2"][:], ALU.add))
        A_("dve", lambda e: e.tensor_tensor(Tl["cre"][:], Tl["cre"][:], Tl["den"][:], ALU.mult))
        A_("dve", lambda e: e.tensor_tensor(Tl["t1"][:], Tl["aim"][:], lr, ALU.mult))
        A_("dve", lambda e: e.tensor_tensor(Tl["t2"][:], Tl["are"][:], li, ALU.mult))
        A_("dve", lambda e: e.tensor_tensor(Tl["cim"][:], Tl["t1"][:], Tl["t2"][:], ALU.subtract))
        A_("dve", lambda e: e.tensor_tensor(Tl["cim"][:], Tl["cim"][:], Tl["den"][:], ALU.mult))
        A_("dve", lambda e: e.tensor_scalar(Tl["ncre"][:], Tl["cre"][:], -1.0, None, ALU.mult))
        big = lambda n_: P.sb(n_, [128, TT], F32)
        CS, SN, ERE, EIM = big("CS"), big("SN"), big("ERE"), big("EIM")
        BUR, BUI, T1, T2, ZR, ZI = big("BUR"), big("BUI"), big("T1"), big("T2"), big("ZR"), big("ZI")
        b_tab, b_bu, b_t1, b_t2, b_zr, b_zi = [Buf(x) for x in "tab bu t1 t2 zr zi".split()]
        Q = [P.sb(f"Q{i}", [128, TT], BF16) for i in range(4)]
        b_q = [Buf(f"q{i}") for i in range(4)]
        U = [P.sb(f"U{b}", [128, TT], BF16) for b in range(NB)]
        b_u = [Buf(f"u{b}") for b in range(NB)]
        YA = [P.sb(f"YA{b}", [128, TT], F32) for b in range(NB)]
        b_ya = [Buf(f"ya{b}") for b in range(NB)]
        psr = Ring(P, "ssmps", 4, [128, 512], F32, psum=True)
        psy = Ring(P, "ssmpy", 3, [128, 512], F32, psum=True)
        for oc in range(4):
            for b in range(NB):
                k.dma("sp", U[b][:], PT[b, oc * 128:(oc + 1) * 128, :], reads=[C.bPT], writes=[b_u[b]])
                k.op("pool", lambda e: e.memset(YA[b][:], 0.0), writes=[b_ya[b]])
            for j in range(2):
                for s4 in range(4):
                    sc = oc * 4 + s4
                    th = Tl["th"][:, j, sc:sc + 1]
                    th2 = Tl["th2"][:, j, sc:sc + 1]
                    sin_reduced(k, SN[:], T1[:], iota[:, j, :], th, th2, 0.0, None, [b_c, bd], b_tab, b_t1)
                    sin_reduced(k, CS[:], T2[:], iota[:, j, :], th, th2, 0.5 * PI, hpi[:, 0:1], [b_c, bd], b_tab, b_t2)
                    cre, cim, ncre = (Tl[x][:, j, sc:sc + 1] for x in ("cre", "cim", "ncre"))
                    k.op("dve", lambda e: e.tensor_scalar(ERE[:], CS[:], cre, None, ALU.mult), reads=[b_tab, bd], writes=[b_tab])
                    k.op("dve", lambda e: e.scalar_tensor_tensor(ERE[:], SN[:], cim, ERE[:], ALU.mult, ALU.add),
                         reads=[b_tab, bd], writes=[b_tab])
                    k.op("pool", lambda e: e.tensor_scalar(EIM[:], CS[:], cim, None, ALU.mult), reads=[b_tab, bd], writes=[b_tab])
                    k.op("dve", lambda e: e.scalar_tensor_tensor(EIM[:], SN[:], ncre, EIM[:], ALU.mult, ALU.add),
                         reads=[b_tab, bd], writes=[b_tab])
                    rho = Tl["rho"][:, j, sc:sc + 1]
                    for b in range(NB):
                        for (t0, n) in blocks:
                            for ri, dst in ((0, BUR), (1, BUI)):
                                ps, bps = psr.next()
                                k.op("pe", lambda e: e.matmul(ps[:, :n], BT[:, ri, sc, :], U[b][:, t0:t0 + n], start=True, stop=True),
                                     reads=[b_c, b_u[b]], writes=[bps])
                                k.op("act", lambda e: e.activation(dst[:, t0:t0 + n], ps[:, :n], AF.Copy),
                                     reads=[bps], writes=[b_bu])
                        k.op("dve", lambda e: e.tensor_tensor(T1[:], ERE[:], BUR[:], ALU.mult), reads=[b_tab, b_bu], writes=[b_t1])
                        k.op("pool", lambda e: e.tensor_tensor(T2[:], EIM[:], BUI[:], ALU.mult), reads=[b_tab, b_bu], writes=[b_t2])
                        k.op("dve", lambda e: e.tensor_tensor(ZR[:], T1[:], T2[:], ALU.subtract), reads=[b_t1, b_t2], writes=[b_zr])
                        k.op("pool", lambda e: e.tensor_tensor(T1[:], ERE[:], BUI[:], ALU.mult), reads=[b_tab, b_bu, b_zr], writes=[b_t1])
                        k.op("dve", lambda e: e.tensor_tensor(T2[:], EIM[:], BUR[:], ALU.mult), reads=[b_tab, b_bu, b_zr], writes=[b_t2])
                        k.op("pool", lambda e: e.tensor_tensor(ZI[:], T1[:], T2[:], ALU.add), reads=[b_t1, b_t2], writes=[b_zi])
                        for Z, bz, eng in ((ZR, b_zr, "dve"), (ZI, b_zi, "dve")):
                            if j == 0:
                                k.op(eng, lambda e: e.tensor_tensor_scan(Z[:], rho.to_broadcast([128, TT]), Z[:], 0.0, ALU.mult, ALU.add),
                                     reads=[bz, bd], writes=[bz])
                            else:
                                r0 = rev_ap(Z[:], 0, TC)
                                k.op(eng, lambda e: e.tensor_tensor_scan(r0, rho.to_broadcast([128, TC]), r0, 0.0, ALU.mult, ALU.add),
                                     reads=[bz, bd], writes=[bz])
                                r1 = rev_ap(Z[:], TC, TT)
                                k.op(eng, lambda e: e.tensor_tensor_scan(r1, rho.to_broadcast([128, TL]), r1, Z[:, 0:1], ALU.mult, ALU.add),
                                     reads=[bz, bd], writes=[bz])
                        for qi, (tabl, Z, bz, eng) in enumerate(((CS, ZR, b_zr, "dve"), (SN, ZI, b_zi, "pool"),
                                                                 (SN, ZR, b_zr, "dve"), (CS, ZI, b_zi, "pool"))):
                            k.op(eng, lambda e: e.tensor_tensor(Q[qi][:], tabl[:], Z[:], ALU.mult), reads=[b_tab, bz], writes=[b_q[qi]])
                        lhs = (CT[:, j, 0, sc, :], nCT[:, j, 0, sc, :], nCT[:, j, 1, sc, :], nCT[:, j, 1, sc, :])
                        for (t0, n) in blocks:
                            ps, bps = psy.next()
                            for qi in range(4):
                                k.op("pe", lambda e: e.matmul(ps[:, :n], lhs[qi], Q[qi][:, t0:t0 + n], start=(qi == 0), stop=(qi == 3)),
                                     reads=[b_c, b_q[qi]], writes=[bps])
                            k.op("dve", lambda e: e.tensor_tensor(YA[b][:, t0:t0 + n], YA[b][:, t0:t0 + n], ps[:, :n], ALU.add),
                                 reads=[bps, b_ya[b]], writes=[b_ya[b]])
            for b in range(NB):
                k.op("dve", lambda e: e.scalar_tensor_tensor(T1[:], U[b][:], dsk[:, 0, oc:oc + 1], YA[b][:], ALU.mult, ALU.add),
                     reads=[b_u[b], b_ya[b], b_c, b_t1], writes=[b_t1])
                k.op("act", lambda e: e.activation(T2[:], T1[:], AF.Square), reads=[b_t1, b_t2], writes=[b_t2])
                k.op("dve", lambda e: e.tensor_scalar(T2[:], T2[:], 0.044715, 1.0, ALU.mult, ALU.add), reads=[b_t2], writes=[b_t2])
                k.op("pool", lambda e: e.tensor_tensor(T2[:], T2[:], T1[:], ALU.mult), reads=[b_t1, b_t2], writes=[b_t2])
                k.op("act", lambda e: e.activation(T2[:], T2[:], AF.Sigmoid, scale=1.5957691216057308), reads=[b_t2], writes=[b_t2])
                k.op("dve", lambda e: e.tensor_tensor(Q[b][:], T1[:], T2[:], ALU.mult), reads=[b_t1, b_t2, b_q[b]], writes=[b_q[b]])
                k.dma("sp", YG[b, oc * 128:(oc + 1) * 128, :], Q[b][:], reads=[b_q[b]], writes=[C.bYG])
    with P.scope():
        gw = P.sb("gluw", [128, 4, 512], BF16)
        b_gw = Buf("gluw")
        k.dma("pool", gw[:], C.ssm_glu_w[l].rearrange("(kc p) n -> p kc n", p=128), writes=[b_gw])
        dsk = P.sb("dsk2", [128, 2, 4], F32)
        k.dma("sp", dsk[:], C.ssm_vec[l], writes=[b_gw])
        ygr = Ring(P, "yg", 2, [128, 4, 512], BF16)
        psr = Ring(P, "glups", 4, [128, 512], F32, psum=True)
        sgr = Ring(P, "sg", 3, [128, 512], F32)
        str_ = Ring(P, "gst", 3, [128, 512], BF16)
        for b in range(NB):
            for (t0, n) in blocks:
                yg, byg = ygr.next()
                k.dma("sp", yg[:, :, :n], YG[b, :, t0:t0 + n].rearrange("(c p) n -> p c n", p=128), reads=[C.bYG], writes=[byg])
                for oc in range(4):
                    ps, bps = psr.next()
                    for kc in range(4):
                        k.op("pe", lambda e: e.matmul(ps[:, :n], gw[:, kc, oc * 128:(oc + 1) * 128], yg[:, kc, :n],
                                                      start=(kc == 0), stop=(kc == 3)), reads=[b_gw, byg], writes=[bps])
                    sg, bsg = sgr.next()
                    k.op("act", lambda e: e.activation(sg[:, :n], ps[:, :n], AF.Sigmoid, bias=dsk[:, 1, oc:oc + 1]),
                         reads=[bps, b_gw], writes=[bsg])
                    st, bst = str_.next()
                    k.op("dve", lambda e: e.tensor_tensor(st[:, :n], yg[:, oc, :n], sg[:, :n], ALU.mult), reads=[byg, bsg], writes=[bst])
                    k.dma("sp", YT[b, 0, oc * 128:(oc + 1) * 128, t0:t0 + n], st[:, :n], reads=[bst], writes=[C.bYT])


def phase_merge(P, C, l):
    k = P.k
    PT, YT, XT = C.PT, C.YT, C.XT
    src_x = C.xt0 if l == 0 else XT
    mod, gs = C.mod, C.gs
    blocks = [(i * 256, 256) for i in range(9)]
    with P.scope():
        wb = P.sb("wb", [128, 3, 4, D], BF16)
        b_w = Buf("mw")
        for j in range(3):
            k.dma("pool", wb[:, j], C.w_branch[l, j].rearrange("(kc p) n -> p kc n", p=128), writes=[b_w])
        wo = P.sb("wo", [128, KC, D], BF16)
        k.dma("pool", wo[:], C.w_out[l].rearrange("(kc p) n -> p kc n", p=128), writes=[b_w])
        rw = P.sb("rw", [128, KC, 16], F32)
        k.dma("sp", rw[:], C.router_w[l].rearrange("(kc p) n -> p kc n", p=128), writes=[b_w])
        ident = P.sb("ident", [128, 128], BF16)
        k.dma("pool", ident[:], C.ident, writes=[b_w])
        y3r = Ring(P, "y3", 2, [128, 3, 4, 256], BF16)
        gr = Ring(P, "g", 2, [128, 24, 256], BF16)
        xr = Ring(P, "mx", 2, [128, KC, 256], F32)
        mTr = Ring(P, "mT", 1, [128, KC, 256], BF16)
        mr = Ring(P, "m", 6, [128, 256], F32)
        x1r = Ring(P, "x1", 1, [128, KC, 256], F32)
        sqr = Ring(P, "msq", 1, [128, KC, 256], F32)
        h2fr = Ring(P, "h2f", 1, [128, KC, 256], F32)
        h2br = Ring(P, "h2b", 1, [128, KC, 256], BF16)
        tsr = Ring(P, "tst", 2, [128, D], BF16)
        rsr = Ring(P, "mrs", 2, [128, 256], F32)
        lgr = Ring(P, "lgs", 2, [16, 256], F32)
        psb = Ring(P, "psb", 3, [128, 256], F32, psum=True)
        pso = Ring(P, "pso", 2, [128, 256], F32, psum=True)
        pss = Ring(P, "pss", 2, [128, 256], F32, psum=True)
        pst = Ring(P, "pst", 1, [128, D], BF16, psum=True)
        for b in range(NB):
            for (t0, n) in blocks:
                j = 2 if t0 < TC else b
                y3, by3 = y3r.next()
                k.dma("sp", y3[:], YT[b, :, :, t0:t0 + n].rearrange("j (c p) n -> p j c n", p=128), reads=[C.bYT], writes=[by3])
                g, bg = gr.next()
                k.dma("act", g[:], PT[b, 25 * 128:49 * 128, t0:t0 + n].rearrange("(c p) n -> p c n", p=128), reads=[C.bPT], writes=[bg])
                x, bx = xr.next()
                k.dma("sp", x[:], src_x[b][:, t0:t0 + n].rearrange("(kc p) n -> p kc n", p=128), reads=[C.bXT[b]], writes=[bx])
                mT, bmT = mTr.next()
                for dc in range(KC):
                    ms = []
                    for jj in range(3):
                        ps, bps = psb.next()
                        for kc in range(4):
                            k.op("pe", lambda e: e.matmul(ps[:], wb[:, jj, kc, dc * 128:(dc + 1) * 128], y3[:, jj, kc, :],
                                                          start=(kc == 0), stop=(kc == 3)), reads=[b_w, by3], writes=[bps])
                        m, bm = mr.next()
                        k.op("dve", lambda e: e.tensor_tensor(m[:], ps[:], g[:, jj * 8 + dc, :], ALU.mult), reads=[bps, bg], writes=[bm])
                        ms.append((m, bm))
                    k.op("pool", lambda e: e.tensor_tensor(ms[0][0][:], ms[0][0][:], ms[1][0][:], ALU.add),
                         reads=[ms[0][1], ms[1][1]], writes=[ms[0][1]])
                    k.op("pool", lambda e: e.tensor_tensor(mT[:, dc, :], ms[0][0][:], ms[2][0][:], ALU.add),
                         reads=[ms[0][1], ms[2][1]], writes=[bmT])
                x1, bx1 = x1r.next()
                for dc in range(KC):
                    ps, bps = pso.next()
                    for kc in range(KC):
                        k.op("pe", lambda e: e.matmul(ps[:], wo[:, kc, dc * 128:(dc + 1) * 128], mT[:, kc, :],
                                                      start=(kc == 0), stop=(kc == KC - 1)), reads=[b_w, bmT], writes=[bps])
                    k.op("dve", lambda e: e.scalar_tensor_tensor(x1[:, dc, :], ps[:], mod[:, 2 * 8 + dc, j:j + 1], x[:, dc, :],
                                                                 ALU.mult, ALU.add), reads=[bps, bx, C.b_mod], writes=[bx1])
                k.dma("sp", XT[b][:, t0:t0 + n].rearrange("(kc p) n -> p kc n", p=128), x1[:], reads=[bx1], writes=[C.bXT[b]])
                sq, bsq = sqr.next()
                k.op("act", lambda e: e.activation(sq[:], x1[:], AF.Square), reads=[bx1], writes=[bsq])
                ss, bss = pss.next()
                for kc in range(KC):
                    k.op("pe", lambda e: e.matmul(ss[:], C.ones[:], sq[:, kc, :], start=(kc == 0), stop=(kc == KC - 1)),
                         reads=[bsq, C.b_ones], writes=[bss])
                rs, brs = rsr.next()
                k.op("act", lambda e: e.activation(rs[:], ss[:], AF.Sqrt, bias=C.epsc[:, 0:1], scale=1.0 / D), reads=[bss], writes=[brs])
                k.op("dve", lambda e: e.reciprocal(rs[:], rs[:]), reads=[brs], writes=[brs])
                k.op("dve", lambda e: e.tensor_tensor(sq[:], x1[:], rs[:].unsqueeze(1).to_broadcast([128, KC, n]), ALU.mult),
                     reads=[bx1, brs, bsq], writes=[bsq])
                h2f, bh2f = h2fr.next()
                for kc in range(KC):
                    k.op("act", lambda e: e.activation(h2f[:, kc, :], sq[:, kc, :], AF.Identity,
                                                       bias=mod[:, 3 * 8 + kc, j:j + 1], scale=gs[:, 1, kc, j:j + 1]),
                         reads=[bsq, C.b_mod, C.b_gs], writes=[bh2f])
                h2b, bh2b = h2br.next()
                k.op("pool", lambda e: e.tensor_copy(h2b[:], h2f[:]), reads=[bh2f], writes=[bh2b])
                lp, blp = pss.next()
                for kc in range(KC):
                    k.op("pe", lambda e: e.matmul(lp[:16, :], rw[:, kc, :], h2f[:, kc, :], start=(kc == 0), stop=(kc == KC - 1)),
                         reads=[b_w, bh2f], writes=[blp])
                lg, blg = lgr.next()
                k.op("act", lambda e: e.activation(lg[:], lp[:16, :], AF.Copy), reads=[blp], writes=[blg])
                k.dma("sp", C.LG[b, :, t0:t0 + n], lg[:], reads=[blg], writes=[C.bLG])
                for tt in range(n // 128):
                    pt, bpt = pst.next()
                    for kc in range(KC):
                        k.op("pe", lambda e: e.transpose(pt[:, kc * 128:(kc + 1) * 128], h2b[:, kc, tt * 128:(tt + 1) * 128], ident[:]),
                             reads=[bh2b, b_w], writes=[bpt])
                    ts, bts = tsr.next()
                    k.op("act", lambda e: e.activation(ts[:], pt[:], AF.Copy), reads=[bpt], writes=[bts])
                    k.dma("sp", C.H2[b, t0 + tt * 128:t0 + (tt + 1) * 128, :], ts[:], reads=[bts], writes=[C.bH2])


NE = 16
FF = 1536
CAPL = 256
CAPC = 32
NJ = 2 * CAPL + 2 * CAPC


def phase_moe(P, C, l, last):
    k = P.k
    XT = C.XT
    mod = C.mod
    with P.scope():
        cst = P.sb("moecst", [128, 3, 256], F32)
        b_c = Buf("moecst")
        k.dma("sp", cst[:], C.moe_cst, writes=[b_c])
        A = P.sb("rA", [32, TT], F32)
        AFF = P.sb("rAFF", [32, TT], F32)
        W = P.sb("rW", [32, TT], F32)
        MG = P.sb("rMG", [32, TT], F32)
        MK = P.sb("rMK", [32, TT], F32)
        PS_ = P.sb("rPOS", [32, TT], F32)
        mx = P.sb("rmx", [32, 8], F32)
        bA, bAFF, bW, bMG, bMK, bPOS, bmx = [Buf(x) for x in "A AFF W MG MK POS mx".split()]
        for b in range(NB):
            k.dma("sp", A[b * 16:(b + 1) * 16, :], C.LG[b], reads=[C.bLG], writes=[bA])
        k.op("act", lambda e: e.activation(A[:], A[:], AF.Exp), reads=[bA], writes=[bA])
        psr = Ring(P, "rps", 2, [128, 512], F32, psum=True)
        for t0 in range(0, TT, 512):
            n = min(512, TT - t0)
            ps, bps = psr.next()
            k.op("pe", lambda e: e.matmul(ps[:32, :n], cst[:32, 1, :32], A[:, t0:t0 + n], start=True, stop=True),
                 reads=[bA, b_c], writes=[bps])
            k.op("dve", lambda e: e.reciprocal(W[:, t0:t0 + n], ps[:32, :n]), reads=[bps], writes=[bW])
        k.op("dve", lambda e: e.tensor_tensor(AFF[:], A[:], W[:], ALU.mult), reads=[bA, bW], writes=[bAFF])
        k.op("dve", lambda e: e.tensor_copy(W[:], AFF[:]), reads=[bAFF, bW], writes=[bW])
        for (lo, hi, cap) in ((0, TC, CAPC), (TC, TT, CAPL)):
            for it in range(cap // 8):
                k.op("dve", lambda e: e.max(out=mx[:], in_=W[:, lo:hi]), reads=[bW, bmx], writes=[bmx])
                k.op("dve", lambda e: e.match_replace(out=W[:, lo:hi], in_to_replace=mx[:], in_values=W[:, lo:hi], imm_value=0.0),
                     reads=[bmx, bW], writes=[bW])
        k.op("dve", lambda e: e.tensor_tensor(MG[:], AFF[:], W[:], ALU.subtract), reads=[bAFF, bW], writes=[bMG])
        k.op("dve", lambda e: e.tensor_single_scalar(MK[:], MG[:], 0.0, ALU.is_gt), reads=[bMG], writes=[bMK])
        for (lo, hi) in ((0, TC), (TC, TT)):
            k.op("dve", lambda e: e.tensor_tensor_scan(PS_[:, lo:hi], cst[:32, 0, 0:1].to_broadcast([32, hi - lo]), MK[:, lo:hi],
                                                       0.0, ALU.mult, ALU.add), reads=[bMK, b_c], writes=[bPOS])
        for b in range(NB):
            k.dma("sp", C.POSD[b], PS_[b * 16:(b + 1) * 16, :], reads=[bPOS], writes=[C.bPOSD])
            k.dma("sp", C.MGD[b], MG[b * 16:(b + 1) * 16, :], reads=[bMG], writes=[C.bPOSD])
        posT = P.sb("posT", [128, 18, 32], F32)
        mkT = P.sb("mkT", [128, 18, 32], F32)
        b_pT = Buf("posT")
        for tt in range(18):
            ps, bps = psr.next()
            k.op("pe", lambda e: e.transpose(ps[:, 0:32], PS_[:, tt * 128:(tt + 1) * 128], cst[:32, 2, :32]),
                 reads=[bPOS, b_c], writes=[bps])
            k.op("pe", lambda e: e.transpose(ps[:, 32:64], MK[:, tt * 128:(tt + 1) * 128], cst[:32, 2, :32]),
                 reads=[bMK, b_c], writes=[bps])
            k.op("act", lambda e: e.activation(posT[:, tt, :], ps[:, 0:32], AF.Copy), reads=[bps], writes=[b_pT])
            k.op("act", lambda e: e.activation(mkT[:, tt, :], ps[:, 32:64], AF.Copy), reads=[bps], writes=[b_pT])
        H2s = P.sb("H2s", [128, 18, D], BF16)
        bH = Buf("H2s")
        selr = Ring(P, "sel", 2, [128, 18, 256], BF16)
        gps = Ring(P, "gps", 3, [128, 256], F32, psum=True)
        gpc = Ring(P, "gpc", 2, [128, 32], F32, psum=True)
        xsr = Ring(P, "xs", 2, [128, KC, CAPL + CAPC], BF16)
        for b in range(NB):
            k.dma("sp", H2s[:], C.H2[b].rearrange("(tt p) d -> p tt d", p=128), reads=[C.bH2], writes=[bH])
            for ex in range(NE):
                col = b * 16 + ex
                sel, bsel = selr.next()
                for tt in range(18):
                    ncap = CAPC if tt < 2 else CAPL
                    k.op("dve" if tt % 2 == 0 else "pool", lambda e: e.tensor_scalar(
                        sel[:, tt, :ncap], cst[:, 0, :ncap], posT[:, tt, col:col + 1], mkT[:, tt, col:col + 1],
                        ALU.is_equal, ALU.mult), reads=[b_c, b_pT], writes=[bsel])
                xs, bxs = xsr.next()
                for kc in range(KC):
                    ps, bps = gps.next()
                    for tt in range(16):
                        k.op("pe", lambda e: e.matmul(ps[:], H2s[:, 2 + tt, kc * 128:(kc + 1) * 128], sel[:, 2 + tt, :],
                                                      start=(tt == 0), stop=(tt == 15)), reads=[bH, bsel], writes=[bps])
                    k.op("act" if kc % 2 == 0 else "dve",
                         (lambda e: e.activation(xs[:, kc, :CAPL], ps[:], AF.Copy)) if kc % 2 == 0 else
                         (lambda e: e.tensor_copy(xs[:, kc, :CAPL], ps[:])), reads=[bps], writes=[bxs])
                    pc, bpc = gpc.next()
                    for tt in range(2):
                        k.op("pe", lambda e: e.matmul(pc[:], H2s[:, tt, kc * 128:(kc + 1) * 128], sel[:, tt, :CAPC],
                                                      start=(tt == 0), stop=(tt == 1)), reads=[bH, bsel], writes=[bpc])
                    k.op("act", lambda e: e.activation(xs[:, kc, CAPL:], pc[:], AF.Copy), reads=[bpc], writes=[bxs])
                k.dma("sp", C.XS[ex, :, b * CAPL:(b + 1) * CAPL].rearrange("(kc p) j -> p kc j", p=128), xs[:, :, :CAPL],
                      reads=[bxs], writes=[C.bXS])
                k.dma("sp", C.XS[ex, :, 2 * CAPL + b * CAPC:2 * CAPL + (b + 1) * CAPC].rearrange("(kc p) j -> p kc j", p=128),
                      xs[:, :, CAPL:], reads=[bxs], writes=[C.bXS])
    with P.scope():
        w1r = Ring(P, "w1", 2, [128, KC, FF], BF16)
        w3r = Ring(P, "w3", 2, [128, KC, FF], BF16)
        w2r = Ring(P, "w2", 2, [128, 12, D], BF16)
        xsr = Ring(P, "xsb", 2, [128, KC, NJ], BF16)
        hr = Ring(P, "hid", 2, [128, 12, NJ], BF16)
        slr = Ring(P, "silu", 3, [128, 288], F32)
        yer = Ring(P, "ye", 3, [128, D], BF16)
        ps1 = Ring(P, "ps1", 2, [128, 288], F32, psum=True)
        ps3 = Ring(P, "ps3", 2, [128, 288], F32, psum=True)
        psy = Ring(P, "psy", 3, [128, 512], F32, psum=True)
        for ex in range(NE):
            w1, bw1 = w1r.next()
            w3, bw3 = w3r.next()
            w2, bw2 = w2r.next()
            k.dma("pool", w1[:], C.moe_w1[l, ex].rearrange("(kc p) f -> p kc f", p=128), writes=[bw1])
            k.dma("pool", w3[:], C.moe_w3[l, ex].rearrange("(kc p) f -> p kc f", p=128), writes=[bw3])
            k.dma("pool", w2[:], C.moe_w2[l, ex].rearrange("(fc p) d -> p fc d", p=128), writes=[bw2])
            xs, bxs = xsr.next()
            k.dma("sp", xs[:], C.XS[ex].rearrange("(kc p) j -> p kc j", p=128), reads=[C.bXS], writes=[bxs])
            hid, bh = hr.next()
            for fc in range(12):
                for half in range(2):
                    c0 = half * 288
                    p1, bp1 = ps1.next()
                    p3, bp3 = ps3.next()
                    for kc in range(KC):
                        k.op("pe", lambda e: e.matmul(p1[:], w1[:, kc, fc * 128:(fc + 1) * 128], xs[:, kc, c0:c0 + 288],
                                                      start=(kc == 0), stop=(kc == KC - 1)), reads=[bw1, bxs], writes=[bp1])
                    for kc in range(KC):
                        k.op("pe", lambda e: e.matmul(p3[:], w3[:, kc, fc * 128:(fc + 1) * 128], xs[:, kc, c0:c0 + 288],
                                                      start=(kc == 0), stop=(kc == KC - 1)), reads=[bw3, bxs], writes=[bp3])
                    sl, bsl = slr.next()
                    k.op("act", lambda e: e.activation(sl[:], p1[:], AF.Silu), reads=[bp1], writes=[bsl])
                    k.op("dve", lambda e: e.tensor_tensor(hid[:, fc, c0:c0 + 288], sl[:], p3[:], ALU.mult),
                         reads=[bsl, bp3], writes=[bh])
            for jt in range(5):
                nj = 128 if jt < 4 else NJ - 512
                ye, bye = yer.next()
                for dh in range(2):
                    py, bpy = psy.next()
                    for fc in range(12):
                        k.op("pe", lambda e: e.matmul(py[:nj, :], hid[:, fc, jt * 128:jt * 128 + nj], w2[:, fc, dh * 512:(dh + 1) * 512],
                                                      start=(fc == 0), stop=(fc == 11)), reads=[bh, bw2], writes=[bpy])
                    k.op("act" if dh == 0 else "dve",
                         (lambda e: e.activation(ye[:nj, dh * 512:(dh + 1) * 512], py[:nj, :], AF.Copy)) if dh == 0 else
                         (lambda e: e.tensor_copy(ye[:nj, dh * 512:(dh + 1) * 512], py[:nj, :])), reads=[bpy], writes=[bye])
                k.dma("sp", C.YE[ex, jt * 128:jt * 128 + nj, :], ye[:nj, :], reads=[bye], writes=[C.bYE])
    with P.scope():
        jcol = P.sb("jcol", [128, 2], F32)
        b_c = Buf("jcol")
        k.dma("sp", jcol[:], C.moe_jcol, writes=[b_c])
        yel = P.sb("yel", [128, NE, 2, D], BF16)
        yec = P.sb("yec", [32, NE, D], BF16)
        b_ye = Buf("yel")
        posb = P.sb("posb", [128, NE, 512], F32)
        mgb = P.sb("mgb", [128, NE, 512], F32)
        b_pb = Buf("posb")
        sgr = Ring(P, "selg", 4, [128, 512], BF16)
        tmr = Ring(P, "seltmp", 3, [128, 512], F32)
        x1r = Ring(P, "cx1", 2, [128, KC, 512], F32)
        pso = Ring(P, "cps", 8, [128, 512], F32, psum=True)
        for b in range(NB):
            for jt in range(2):
                k.dma("sp", yel[:, :, jt, :], C.YE[:, b * CAPL + jt * 128:b * CAPL + (jt + 1) * 128, :].rearrange("e p d -> p e d"),
                      reads=[C.bYE], writes=[b_ye])
            k.dma("sp", yec[:], C.YE[:, 2 * CAPL + b * CAPC:2 * CAPL + (b + 1) * CAPC, :].rearrange("e p d -> p e d"),
                  reads=[C.bYE], writes=[b_ye])
            for (t0, n) in [(0, TC)] + [(TC + i * 512, 512) for i in range(4)]:
                isctx = t0 < TC
                j = 2 if isctx else b
                k.dma("sp", posb[:, :, :n], C.POSD[b, :, t0:t0 + n].partition_broadcast(128), reads=[C.bPOSD], writes=[b_pb])
                k.dma("act", mgb[:, :, :n], C.MGD[b, :, t0:t0 + n].partition_broadcast(128), reads=[C.bPOSD], writes=[b_pb])
                x1, bx1 = x1r.next()
                k.dma("sp", x1[:, :, :n], XT[b][:, t0:t0 + n].rearrange("(kc p) n -> p kc n", p=128), reads=[C.bXT[b]], writes=[bx1])
                acc = [pso.next() for _ in range(KC)]
                njt = 1 if isctx else 2
                for ex in range(NE):
                    for jt in range(njt):
                        tm, btm = tmr.next()
                        k.op("pool", lambda e: e.tensor_scalar(tm[:, :n], posb[:, ex, :n], jcol[:, jt:jt + 1], None, ALU.is_equal),
                             reads=[b_pb, b_c], writes=[btm])
                        sg, bsg = sgr.next()
                        k.op("dve", lambda e: e.tensor_tensor(sg[:, :n], tm[:, :n], mgb[:, ex, :n], ALU.mult),
                             reads=[btm, b_pb], writes=[bsg])
                        first = (ex == 0 and jt == 0)
                        lastm = (ex == NE - 1 and jt == njt - 1)
                        for dc in range(KC):
                            pa, bpa = acc[dc]
                            if isctx:
                                k.op("pe", lambda e: e.matmul(pa[:, :n], yec[:, ex, dc * 128:(dc + 1) * 128], sg[:32, :n],
                                                              start=first, stop=lastm), reads=[b_ye, bsg], writes=[bpa])
                            else:
                                k.op("pe", lambda e: e.matmul(pa[:, :n], yel[:, ex, jt, dc * 128:(dc + 1) * 128], sg[:, :n],
                                                              start=first, stop=lastm), reads=[b_ye, bsg], writes=[bpa])
                for dc in range(KC):
                    pa, bpa = acc[dc]
                    k.op("dve", lambda e: e.scalar_tensor_tensor(x1[:, dc, :n], pa[:, :n], mod[:, 5 * 8 + dc, j:j + 1], x1[:, dc, :n],
                                                                 ALU.mult, ALU.add), reads=[bpa, bx1, C.b_mod], writes=[bx1])
                if last:
                    if not isctx:
                        k.dma("sp", C.OUT[b][:, t0 - TC:t0 - TC + n].rearrange("(kc p) n -> p kc n", p=128), x1[:, :, :n],
                              reads=[bx1], writes=[C.bOUT])
                else:
                    k.dma("sp", XT[b][:, t0:t0 + n].rearrange("(kc p) n -> p kc n", p=128), x1[:, :, :n],
                          reads=[bx1], writes=[C.bXT[b]])


LAM = float(np.exp(-0.5))
GN_EPS = 64e-5
NCK = TT // 128


def phase_rwkv_prep(P, C, l):
    k = P.k
    PT = C.PT
    blocks = [(0, TC)] + [(TC + i * 512, 512) for i in range(4)]
    with P.scope():
        vec = P.sb("rwvec", [128, 51], F32)
        b_c = Buf("rwc")
        k.dma("sp", vec[:], C.rw_vec[l], writes=[b_c])
        MU, W0, A0, KK_, KA, RK = 0, 15, 23, 31, 35, 39
        der = P.sb("rwder", [128, 15 + 15 + 4], F32)
        k.op("dve", lambda e: e.tensor_scalar(der[:, 0:15], vec[:, MU:MU + 15], -1.0, 1.0, ALU.mult, ALU.add), reads=[b_c], writes=[b_c])
        k.op("dve", lambda e: e.tensor_scalar(der[:, 15:30], vec[:, MU:MU + 15], 0.5, None, ALU.mult), reads=[b_c], writes=[b_c])
        k.op("dve", lambda e: e.tensor_scalar(der[:, 30:34], vec[:, KA:KA + 4], -1.0, 1.0, ALU.mult, ALU.add), reads=[b_c], writes=[b_c])
        tiny = P.sb("rwtiny", [128, 1], F32)
        k.op("dve", lambda e: e.memset(tiny[:], 1e-12), writes=[b_c])
        w2 = P.sb("rw_w2", [128, 512], BF16)
        a2 = P.sb("rw_a2", [128, 512], BF16)
        g2 = P.sb("rw_g2", [128, 512], BF16)
        k.dma("pool", w2[:], C.rwkv_w2[l].rearrange("j l c -> (j l) c"), writes=[b_c])
        k.dma("pool", a2[:], C.rwkv_a2[l].rearrange("j l c -> (j l) c"), writes=[b_c])
        k.dma("pool", g2[:], C.rwkv_g2[l], writes=[b_c])
        bd = P.sb("rw_bd", [128, 128], BF16)
        k.dma("pool", bd[:], C.rw_bd, writes=[b_c])
        rst = P.sb("rw_rst", [128, TT + 1], F32)
        k.dma("sp", rst[:], C.rw_rst, writes=[b_c])
        big = lambda n_, dt=F32: P.sb(n_, [128, TT], dt)
        X = big("rX", BF16)
        S = big("rS")
        KP = [big(f"rKP{i}", BF16) for i in range(4)]
        KAP = [big(f"rKAP{i}", BF16) for i in range(4)]
        KS = big("rKSUM")
        bKS = Buf("KS")
        RP = [big(f"rRP{i}", BF16) for i in range(4)]
        VP = [big(f"rVP{i}", BF16) for i in range(4)]
        TW, PA, SG = big("rTW", BF16), big("rPA", BF16), big("rSG", BF16)
        T1, T2, T3, T4 = big("rT1"), big("rT2"), big("rT3"), big("rT4")
        O = [big(f"rO{i}", BF16) for i in range(4)]
        bX, bS, bT1, bT2, bT3, bT4 = [Buf(x) for x in "X S T1 T2 T3 T4".split()]
        bKP = [Buf(f"KP{i}") for i in range(4)]
        bKAP = [Buf(f"KAP{i}") for i in range(4)]
        bRP = [Buf(f"RP{i}") for i in range(4)]
        bVP = [Buf(f"VP{i}") for i in range(4)]
        bTW, bPA, bSG = Buf("TW"), Buf("PA"), Buf("SG")
        bO = [Buf(f"O{i}") for i in range(4)]
        psr = Ring(P, "rwps", 4, [128, 512], F32, psum=True)
        stg = Ring(P, "rwstg", 3, [128, 512], BF16)
        plr = Ring(P, "rwpl", 2, [128, NCK], F32)
        for b in range(NB):
            for ci in range(15):
                k.dma("sp", X[:], PT[b, (4 + ci) * 128:(5 + ci) * 128, :], reads=[C.bPT], writes=[bX])
                k.op("pool", lambda e: e.tensor_tensor(S[:, 1:TT - 1], X[:, 0:TT - 2], X[:, 2:TT], ALU.add), reads=[bX], writes=[bS])
                for (d_, s_) in ((0, 1), (TC - 1, TC - 2), (TC, TC + 1), (TT - 1, TT - 2)):
                    k.op("pool", lambda e: e.tensor_copy(S[:, d_:d_ + 1], X[:, s_:s_ + 1]), reads=[bX, bS], writes=[bS])
                k.op("dve", lambda e: e.tensor_scalar(T1[:], X[:], der[:, ci:ci + 1], None, ALU.mult), reads=[bX, b_c], writes=[bT1])
                if ci < 4:
                    dst, bdst = RP[ci], bRP[ci]
                elif ci < 8:
                    dst, bdst = KP[ci - 4], bKP[ci - 4]
                elif ci < 12:
                    dst, bdst = VP[ci - 8], bVP[ci - 8]
                else:
                    dst, bdst = T2, bT2
                k.op("dve", lambda e: e.scalar_tensor_tensor(dst[:], S[:], der[:, 15 + ci:16 + ci], T1[:], ALU.mult, ALU.add),
                     reads=[bS, bT1, b_c], writes=[bdst])
                if ci == 12:
                    k.op("act", lambda e: e.activation(TW[:], T2[:], AF.Tanh), reads=[bT2], writes=[bTW])
                elif ci == 13:
                    k.op("act", lambda e: e.activation(PA[:], T2[:], AF.Copy), reads=[bT2], writes=[bPA])
                elif ci == 14:
                    k.op("act", lambda e: e.activation(SG[:], T2[:], AF.Sigmoid), reads=[bT2], writes=[bSG])
            for cc in range(4):
                k.dma("sp", C.RWV[b, cc * 128:(cc + 1) * 128, :], VP[cc][:], reads=[bVP[cc]], writes=[C.bRW])
            for cc in range(4):
                for (t0, n) in blocks:
                    ps, bps = psr.next()
                    k.op("pe", lambda e: e.matmul(ps[:, :n], g2[:, cc * 128:(cc + 1) * 128], SG[:, t0:t0 + n], start=True, stop=True),
                         reads=[bSG, b_c], writes=[bps])
                    st, bst = stg.next()
                    k.op("act", lambda e: e.activation(st[:, :n], ps[:, :n], AF.Copy), reads=[bps], writes=[bst])
                    k.dma("sp", C.RWG[b, cc * 128:(cc + 1) * 128, t0:t0 + n], st[:, :n], reads=[bst], writes=[C.bRW])
            for cc in range(4):
                k.op("dve", lambda e: e.tensor_scalar(T1[:], KP[cc][:], vec[:, KK_ + cc:KK_ + cc + 1], None, ALU.mult),
                     reads=[bKP[cc], b_c, bT1], writes=[bT1])
                k.op("act", lambda e: e.activation(O[0][:], T1[:], AF.Square), reads=[bT1, bO[0]], writes=[bO[0]])
                for (t0, n) in blocks:
                    ps, bps = psr.next()
                    k.op("pe", lambda e: e.matmul(ps[:, :n], bd[:], O[0][:, t0:t0 + n], start=True, stop=True),
                         reads=[bO[0], b_c], writes=[bps])
                    k.op("act", lambda e: e.activation(T2[:, t0:t0 + n], ps[:, :n], AF.Sqrt, bias=tiny[:, 0:1]), reads=[bps, bT2, b_c], writes=[bT2])
                k.op("dve", lambda e: e.reciprocal(T2[:], T2[:]), reads=[bT2], writes=[bT2])
                k.op("dve", lambda e: e.tensor_tensor(KAP[cc][:], T1[:], T2[:], ALU.mult), reads=[bT1, bT2], writes=[bKAP[cc]])
            for cc in range(4):
                for j in range(2):
                    jr = slice(j * 64, (j + 1) * 64)
                    for (t0, n) in blocks:
                        ps, bps = psr.next()
                        k.op("pe", lambda e: e.matmul(ps[:, :n], w2[jr, cc * 128:(cc + 1) * 128], TW[jr, t0:t0 + n], start=True, stop=True),
                             reads=[bTW, b_c], writes=[bps])
                        k.op("act", lambda e: e.activation(T1[:, t0:t0 + n], ps[:, :n], AF.Sigmoid, bias=vec[:, W0 + j * 4 + cc:W0 + j * 4 + cc + 1]),
                             reads=[bps, b_c, bT1], writes=[bT1])
                        ps2, bps2 = psr.next()
                        k.op("pe", lambda e: e.matmul(ps2[:, :n], a2[jr, cc * 128:(cc + 1) * 128], PA[jr, t0:t0 + n], start=True, stop=True),
                             reads=[bPA, b_c], writes=[bps2])
                        k.op("act", lambda e: e.activation(T2[:, t0:t0 + n], ps2[:, :n], AF.Sigmoid, bias=vec[:, A0 + j * 4 + cc:A0 + j * 4 + cc + 1]),
                             reads=[bps2, b_c, bT2], writes=[bT2])
                    if j == 0:
                        k.op("dve", lambda e: e.tensor_tensor_scan(T3[:], rst[:, 0:TT], T1[:], 0.0, ALU.mult, ALU.add),
                             reads=[bT1, b_c, bT3], writes=[bT3])
                    else:
                        k.op("dve", lambda e: e.tensor_tensor_scan(rev_ap(T3[:], 0, TT), rev_ap(rst[:], 1, TT + 1), rev_ap(T1[:], 0, TT),
                                                                   0.0, ALU.mult, ALU.add), reads=[bT1, b_c, bT3], writes=[bT3])
                    k.op("dve", lambda e: e.tensor_scalar(T4[:], T2[:], vec[:, KA + cc:KA + cc + 1], der[:, 30 + cc:31 + cc], ALU.mult, ALU.add),
                         reads=[bT2, b_c, bT4], writes=[bT4])
                    k.op("pool", lambda e: e.tensor_tensor(T4[:], T4[:], KP[cc][:], ALU.mult), reads=[bT4, bKP[cc]], writes=[bT4])
                    k.op("pool", lambda e: e.tensor_tensor(T2[:], T2[:], KAP[cc][:], ALU.mult), reads=[bT2, bKAP[cc]], writes=[bT2])
                    k.op("pool", lambda e: e.tensor_tensor(T1[:], T3[:], T1[:], ALU.subtract), reads=[bT1, bT3], writes=[bT1])
                    k.op("act", lambda e: e.activation(T1[:], T1[:], AF.Exp, scale=-LAM), reads=[bT1], writes=[bT1])
                    k.op("act", lambda e: e.activation(S[:], T3[:], AF.Exp, scale=LAM), reads=[bT3, bS], writes=[bS])
                    k.op("act", lambda e: e.activation(T3[:], T3[:], AF.Exp, scale=-LAM), reads=[bT3], writes=[bT3])
                    pl, bpl = plr.next()
                    off = 127 if j == 0 else 0
                    k.op("dve", lambda e: e.tensor_copy(pl[:], T3[:, off:TT:128]), reads=[bT3], writes=[bpl])
                    k.dma("sp", C.RWPL[b, j, cc * 128:(cc + 1) * 128, :], pl[:], reads=[bpl], writes=[C.bRW])
                    k.op("dve", lambda e: e.tensor_tensor(O[0][:], RP[cc][:], T3[:], ALU.mult), reads=[bRP[cc], bT3, bO[0]], writes=[bO[0]])
                    k.op("pool", lambda e: e.tensor_tensor(O[1][:], KAP[cc][:], T1[:], ALU.mult), reads=[bKAP[cc], bT1, bO[1]], writes=[bO[1]])
                    k.op("dve", lambda e: e.tensor_tensor(O[2][:], T4[:], S[:], ALU.mult), reads=[bT4, bS, bO[2]], writes=[bO[2]])
                    k.op("pool", lambda e: e.tensor_tensor(O[3][:], T2[:], S[:], ALU.mult), reads=[bT2, bS, bO[3]], writes=[bO[3]])
                    for q in range(4):
                        k.dma("sp" if q % 2 == 0 else "act", C.RWT[b, j, q, cc * 128:(cc + 1) * 128, :], O[q][:], reads=[bO[q]], writes=[C.bRW])
                    if j == 0:
                        k.op("dve", lambda e: e.tensor_copy(KS[:], T4[:]), reads=[bT4, bKS], writes=[bKS])
                    else:
                        k.op("dve", lambda e: e.tensor_tensor(KS[:], KS[:], T4[:], ALU.add), reads=[bT4, bKS], writes=[bKS])
                k.op("dve", lambda e: e.scalar_tensor_tensor(KS[:], KS[:], vec[:, RK + cc:RK + cc + 1], RP[cc][:], ALU.mult, ALU.mult),
                     reads=[bKS, bRP[cc], b_c], writes=[bKS])
                k.op("act", lambda e: e.activation(O[0][:], KS[:], AF.Copy), reads=[bKS, bO[0]], writes=[bO[0]])
                for (t0, n) in blocks:
                    ps, bps = psr.next()
                    k.op("pe", lambda e: e.matmul(ps[:, :n], bd[:], O[0][:, t0:t0 + n], start=True, stop=True), reads=[bO[0], b_c], writes=[bps])
                    k.op("dve", lambda e: e.tensor_tensor(T1[:, t0:t0 + n], ps[:, :n], VP[cc][:, t0:t0 + n], ALU.mult),
                         reads=[bps, bVP[cc], bT1], writes=[bT1])
                k.dma("sp", C.RWB[b, cc * 128:(cc + 1) * 128, :], T1[:], reads=[bT1], writes=[C.bRW])


def phase_rwkv_scan(P, C, l):
    k = P.k
    YT = C.YT
    with P.scope():
        masks = P.sb("rwmask", [128, 2, 640], BF16)
        b_c = Buf("rwc2")
        k.dma("pool", masks[:], C.rw_masks, writes=[b_c])
        ident = P.sb("rwident", [128, 128], BF16)
        k.dma("pool", ident[:], C.ident, writes=[b_c])
        identf = P.sb("rwidentf", [128, 128], F32)
        k.dma("sp", identf[:], C.ident, writes=[b_c])
        bdm = P.sb("rwbdm", [128, 128], F32)
        k.dma("sp", bdm[:], C.rw_bd, writes=[b_c])
        vec = P.sb("rwvec2", [128, 51], F32)
        k.dma("sp", vec[:], C.rw_vec[l], writes=[b_c])
        lmf = P.sb("rwlm", [128, 4, 128], F32)
        k.dma("sp", lmf[:], C.rw_lm, writes=[b_c])
        gne = P.sb("gne", [128, 1], F32)
        k.op("dve", lambda e: e.memset(gne[:], GN_EPS), writes=[b_c])
        Ytok = P.sb("Ytok", [128, NCK, 512], F32)
        for b in range(NB):
            bY = [[Buf(f"Y{c}_{hp}") for hp in range(4)] for c in range(NCK)]
            with P.scope():
                KR = P.sb("KR", [128, NCK, 2, 128], BF16)
                KF = P.sb("KF", [128, TT], BF16)
                BF_ = P.sb("BF", [128, TT], BF16)
                VF = P.sb("VF", [128, TT], BF16)
                PLt = P.sb("PLt", [128, NCK], F32)
                TOK = P.sb("TOK", [128, NCK, 3, 128], BF16)
                SC = P.sb("SC", [128, NCK, 2, 512], BF16)
                NM = P.sb("NM", [128, NCK, 2, 128], BF16)
                Tt = P.sb("Tt", [128, NCK * 2, 128], BF16)
                SC36 = SC[:].rearrange("p c h x -> p (c h) x")
                NM36 = NM[:].rearrange("p c h x -> p (c h) x")
                NFr = Ring(P, "NF", 2, [128, 4, 2, 128], F32)
                F4 = Ring(P, "F4", 10, [128, 4, 128], F32)
                B4 = Ring(P, "B4", 12, [128, 4, 128], BF16)
                H = P.sb("Hst", [128, 128], F32)
                Ht = P.sb("Htmp", [128, 128], F32)
                Hb = P.sb("Hb", [128, 128], BF16)
                Wr = Ring(P, "Wsb", 2, [128, 128], BF16)
                Ur = Ring(P, "Un", 2, [128, 128], BF16)
                psT = P.ps("pstr", [128, 3, 128], BF16)
                bpsT = Buf("pstr")
                psL = Ring(P, "psL", 4, [128, 512], F32, psum=True)
                psS = Ring(P, "psS", 3, [128, 128], F32, psum=True)
                b_in, b_tok, b_sc, b_tt, b_H = Buf("in"), Buf("tok"), Buf("sc"), Buf("tt"), Buf("H")
                for j in ([0] if "rw_j0" in P.debug else [1] if "rw_j1" in P.debug else [0, 1]):
                    for hp in range(4):
                        rows = slice(hp * 128, (hp + 1) * 128)
                        k.dma("sp", KR[:, :, 0, :], C.RWT[b, j, 1, rows, :].rearrange("p (c t) -> p c t", t=128), reads=[C.bRW], writes=[b_in])
                        k.dma("act", KR[:, :, 1, :], C.RWT[b, j, 0, rows, :].rearrange("p (c t) -> p c t", t=128), reads=[C.bRW], writes=[b_in])
                        k.dma("sp", KF[:], C.RWT[b, j, 2, rows, :], reads=[C.bRW], writes=[b_in])
                        k.dma("act", BF_[:], C.RWT[b, j, 3, rows, :], reads=[C.bRW], writes=[b_in])
                        k.dma("sp", VF[:], C.RWV[b, rows, :], reads=[C.bRW], writes=[b_in])
                        k.dma("sp", PLt[:], C.RWPL[b, j, rows, :], reads=[C.bRW], writes=[b_in])
                        for c in range(NCK):
                            cs = slice(c * 128, (c + 1) * 128)
                            for q, src in enumerate((KF, BF_, VF)):
                                k.op("pe", lambda e: e.transpose(psT[:, q, :], src[:, cs], ident[:]), reads=[b_in, b_c], writes=[bpsT])
                            k.op("act", lambda e: e.activation(TOK[:, c], psT[:], AF.Copy), reads=[bpsT], writes=[b_tok])
                        for g in range(NCK // 2):
                            NF, bNF = NFr.next()
                            for i in range(4):
                                c, h = 2 * g + i // 2, i % 2
                                cs = slice(c * 128, (c + 1) * 128)
                                hr = slice(h * 64, (h + 1) * 64)
                                kr2 = KR[hr, c].rearrange("p x t -> p (x t)")
                                pX, bpX = psL.next()
                                k.op("pe", lambda e: e.matmul(pX[:, 0:256], KF[hr, cs], kr2, start=True, stop=True), reads=[b_in], writes=[bpX])
                                k.op("pe", lambda e: e.matmul(pX[:, 256:512], BF_[hr, cs], kr2, start=True, stop=True), reads=[b_in], writes=[bpX])
                                pY, bpY = psS.next()
                                k.op("pe", lambda e: e.matmul(pY[:], KR[hr, c, 0, :], BF_[hr, cs], start=True, stop=True), reads=[b_in], writes=[bpY])
                                k.op("dve", lambda e: e.tensor_tensor(SC[:, c, h, :], pX[:], masks[:, j, 0:512], ALU.mult), reads=[bpX, b_c], writes=[b_sc])
                                k.op("dve", lambda e: e.tensor_tensor(NF[:, i, 1, :], pX[:, 256:384], masks[:, j, 256:384], ALU.mult), reads=[bpX, b_c], writes=[bNF])
                                k.op("dve", lambda e: e.tensor_tensor(NF[:, i, 0, :], pY[:], masks[:, j, 512:640], ALU.mult), reads=[bpY, b_c], writes=[bNF])
                            bl = slice(g * 4, (g + 1) * 4)
                            bc4 = lambda m_: m_.unsqueeze(1).to_broadcast([128, 4, 128])
                            Mk, bMk = F4.next()
                            Mtk, bMtk = F4.next()
                            Tf, bTf = F4.next()
                            Ttf, bTtf = F4.next()
                            k.op("pool", lambda e: e.tensor_tensor(Mk[:], NF[:, :, 0, :], bc4(lmf[:, 0, :]), ALU.mult), reads=[bNF, b_c], writes=[bMk])
                            k.op("pool", lambda e: e.tensor_tensor(Mtk[:], NF[:, :, 1, :], bc4(lmf[:, 0, :]), ALU.mult), reads=[bNF, b_c], writes=[bMtk])
                            k.op("pool", lambda e: e.tensor_tensor(Tf[:], Mk[:], bc4(identf[:]), ALU.add), reads=[bMk, b_c], writes=[bTf])
                            k.op("pool", lambda e: e.tensor_tensor(Ttf[:], Mtk[:], bc4(identf[:]), ALU.add), reads=[bMtk, b_c], writes=[bTtf])
                            for lev in range(1, 4):
                                M2, bM2 = F4.next()
                                Mt2, bMt2 = F4.next()
                                p1, bp1 = psL.next()
                                p2, bp2 = psL.next()
                                for i in range(4):
                                    k.op("pe", lambda e: e.matmul(p1[:, i * 128:(i + 1) * 128], Mk[:, i, :], Mtk[:, i, :], start=True, stop=True),
                                         reads=[bMk, bMtk], writes=[bp1])
                                    k.op("pe", lambda e: e.matmul(p2[:, i * 128:(i + 1) * 128], Mtk[:, i, :], Mk[:, i, :], start=True, stop=True),
                                         reads=[bMk, bMtk], writes=[bp2])
                                k.op("act", lambda e: e.activation(Mt2[:].rearrange("p a b -> p (a b)"), p1[:], AF.Copy), reads=[bp1], writes=[bMt2])
                                k.op("dve", lambda e: e.tensor_copy(M2[:].rearrange("p a b -> p (a b)"), p2[:]), reads=[bp2], writes=[bM2])
                                p3, bp3 = psL.next()
                                p4, bp4 = psL.next()
                                for i in range(4):
                                    k.op("pe", lambda e: e.matmul(p3[:, i * 128:(i + 1) * 128], Mt2[:, i, :], Tf[:, i, :], start=True, stop=True),
                                         reads=[bMt2, bTf], writes=[bp3])
                                    k.op("pe", lambda e: e.matmul(p4[:, i * 128:(i + 1) * 128], M2[:, i, :], Ttf[:, i, :], start=True, stop=True),
                                         reads=[bM2, bTtf], writes=[bp4])
                                k.op("dve", lambda e: e.tensor_tensor(Tf[:].rearrange("p a b -> p (a b)"), Tf[:].rearrange("p a b -> p (a b)"), p3[:], ALU.add),
                                     reads=[bp3, bTf], writes=[bTf])
                                k.op("dve", lambda e: e.tensor_tensor(Ttf[:].rearrange("p a b -> p (a b)"), Ttf[:].rearrange("p a b -> p (a b)"), p4[:], ALU.add),
                                     reads=[bp4, bTtf], writes=[bTtf])
                                Mk, bMk, Mtk, bMtk = M2, bM2, Mt2, bMt2
                            Tb, bTb = B4.next()
                            Ttb, bTtb = B4.next()
                            k.op("act", lambda e: e.activation(Tb[:], Tf[:], AF.Copy), reads=[bTf], writes=[bTb])
                            k.op("act", lambda e: e.activation(Ttb[:], Ttf[:], AF.Copy), reads=[bTtf], writes=[bTtb])
                            for li in range(1, 4):
                                lastl = (li == 3)
                                Cm, bCm = B4.next()
                                k.op("pool", lambda e: e.tensor_tensor(Cm[:], NF[:, :, 0, :], bc4(lmf[:, li, :]), ALU.mult), reads=[bNF, b_c], writes=[bCm])
                                p2, bp2 = psL.next()
                                for i in range(4):
                                    k.op("pe", lambda e: e.matmul(p2[:, i * 128:(i + 1) * 128], Cm[:, i, :], Ttb[:, i, :], start=True, stop=True),
                                         reads=[bCm, bTtb], writes=[bp2])
                                Z2, bZ2 = B4.next()
                                k.op("act", lambda e: e.activation(Z2[:].rearrange("p a b -> p (a b)"), p2[:], AF.Copy), reads=[bp2], writes=[bZ2])
                                if not lastl:
                                    Cmt, bCmt = B4.next()
                                    k.op("pool", lambda e: e.tensor_tensor(Cmt[:], NF[:, :, 1, :], bc4(lmf[:, li, :]), ALU.mult), reads=[bNF, b_c], writes=[bCmt])
                                    p1, bp1 = psL.next()
                                    for i in range(4):
                                        k.op("pe", lambda e: e.matmul(p1[:, i * 128:(i + 1) * 128], Cmt[:, i, :], Tb[:, i, :], start=True, stop=True),
                                             reads=[bCmt, bTb], writes=[bp1])
                                    Z1, bZ1 = B4.next()
                                    k.op("dve", lambda e: e.tensor_copy(Z1[:].rearrange("p a b -> p (a b)"), p1[:]), reads=[bp1], writes=[bZ1])
                                p4, bp4 = psL.next()
                                for i in range(4):
                                    k.op("pe", lambda e: e.matmul(p4[:, i * 128:(i + 1) * 128], Tb[:, i, :], Z2[:, i, :], start=True, stop=True),
                                         reads=[bTb, bZ2], writes=[bp4])
                                if not lastl:
                                    p3, bp3 = psL.next()
                                    for i in range(4):
                                        k.op("pe", lambda e: e.matmul(p3[:, i * 128:(i + 1) * 128], Ttb[:, i, :], Z1[:, i, :], start=True, stop=True),
                                             reads=[bTtb, bZ1], writes=[bp3])
                                    Tn, bTn = B4.next()
                                    Ttn, bTtn = B4.next()
                                    k.op("dve", lambda e: e.tensor_tensor(Tn[:].rearrange("p a b -> p (a b)"), Tb[:].rearrange("p a b -> p (a b)"), p3[:], ALU.add),
                                         reads=[bp3, bTb], writes=[bTn])
                                    k.op("dve", lambda e: e.tensor_tensor(Ttn[:].rearrange("p a b -> p (a b)"), Ttb[:].rearrange("p a b -> p (a b)"), p4[:], ALU.add),
                                         reads=[bp4, bTtb], writes=[bTtn])
                                    Tb, bTb, Ttb, bTtb = Tn, bTn, Ttn, bTtn
                                else:
                                    k.op("dve", lambda e: e.tensor_tensor(Tt[:, bl, :], Ttb[:], p4[:].rearrange("p (a b) -> p a b", b=128), ALU.add),
                                         reads=[bp4, bTtb, b_tt], writes=[b_tt])
                        k.op("pool", lambda e: e.memset(H[:], 0.0), reads=[b_H], writes=[b_H])
                        k.op("pool", lambda e: e.memset(Hb[:], 0.0), reads=[b_H], writes=[b_H])
                        order = list(range(NCK)) if j == 0 else [1, 0] + list(range(NCK - 1, 1, -1))
                        for c in order:
                            pW, bpW = psS.next()
                            k.op("pe", lambda e: e.matmul(pW[:], KR[:, c, 0, :], Hb[:], start=True, stop=False, skip_group_check=True),
                                 reads=[b_in, b_H], writes=[bpW])
                            for h in range(2):
                                hc = slice(h * 64, (h + 1) * 64)
                                k.op("pe", lambda e: e.matmul(pW[:, hc], SC[:, c, h, 0:128], TOK[:, c, 2, hc], start=False, stop=True, skip_group_check=True),
                                     reads=[b_sc, b_tok], writes=[bpW])
                            Wsb, bW = Wr.next()
                            k.op("act", lambda e: e.activation(Wsb[:], pW[:], AF.Copy), reads=[bpW], writes=[bW])
                            pU, bpU = psS.next()
                            for h in range(2):
                                hc = slice(h * 64, (h + 1) * 64)
                                k.op("pe", lambda e: e.matmul(pU[:, hc], Tt[:, c * 2 + h, :], Wsb[:, hc], start=True, stop=True),
                                     reads=[b_tt, bW], writes=[bpU])
                            Un, bUn = Ur.next()
                            k.op("act", lambda e: e.activation(Un[:], pU[:], AF.Copy, scale=-1.0), reads=[bpU], writes=[bUn])
                            pYy, bpYy = psS.next()
                            k.op("pe", lambda e: e.matmul(pYy[:], KR[:, c, 1, :], Hb[:], start=True, stop=False, skip_group_check=True),
                                 reads=[b_in, b_H], writes=[bpYy])
                            for h in range(2):
                                hc = slice(h * 64, (h + 1) * 64)
                                k.op("pe", lambda e: e.matmul(pYy[:, hc], SC[:, c, h, 128:256], TOK[:, c, 2, hc], start=False, stop=False, skip_group_check=True),
                                     reads=[b_sc, b_tok], writes=[bpYy])
                                k.op("pe", lambda e: e.matmul(pYy[:, hc], SC[:, c, h, 384:512], Un[:, hc], start=False, stop=True, skip_group_check=True),
                                     reads=[b_sc, bUn], writes=[bpYy])
                            ysl = Ytok[:, c, hp * 128:(hp + 1) * 128]
                            if j == 0 or "rw_j1" in P.debug:
                                k.op("act", lambda e: e.activation(ysl, pYy[:], AF.Copy), reads=[bpYy], writes=[bY[c][hp]])
                            else:
                                k.op("dve", lambda e: e.tensor_tensor(ysl, ysl, pYy[:], ALU.add), reads=[bpYy, bY[c][hp]], writes=[bY[c][hp]])
                            pH, bpH = psS.next()
                            k.op("pe", lambda e: e.matmul(pH[:], TOK[:, c, 0, :], TOK[:, c, 2, :], start=True, stop=False), reads=[b_tok], writes=[bpH])
                            k.op("pe", lambda e: e.matmul(pH[:], TOK[:, c, 1, :], Un[:], start=False, stop=True), reads=[b_tok, bUn], writes=[bpH])
                            k.op("dve", lambda e: e.tensor_tensor(Ht[:], H[:], pH[:], ALU.add), reads=[bpH, b_H], writes=[b_H])
                            k.op("dve", lambda e: e.scalar_tensor_tensor(H[:], Ht[:], PLt[:, c:c + 1], bdm[:], ALU.mult, ALU.mult),
                                 reads=[b_H, b_in, b_c], writes=[b_H])
                            k.op("act", lambda e: e.activation(Hb[:], H[:], AF.Copy), reads=[b_H], writes=[b_H])
            if "YTOK" in P.debug:
                k.dma("sp", C.YTOK[b], Ytok[:], reads=[x for row in bY for x in row], writes=[C.bRW])
            with P.scope():
                BON = P.sb("BON", [128, 4, TT], F32)
                G = P.sb("G", [128, 4, TT], BF16)
                b_l = Buf("rdl")
                k.dma("sp", BON[:], C.RWB[b].rearrange("(c p) t -> p c t", p=128), reads=[C.bRW], writes=[b_l])
                k.dma("act", G[:], C.RWG[b].rearrange("(c p) t -> p c t", p=128), reads=[C.bRW], writes=[b_l])
                st8 = Ring(P, "st8", 2, [128, 6, 8], F32)
                ysq = Ring(P, "ysq", 2, [128, 512], F32)
                ynr = Ring(P, "yn", 2, [128, 512], F32)
                psR = Ring(P, "psR", 2, [128, 4, 128], F32, psum=True)
                ofr = Ring(P, "of", 3, [128, 128], F32)
                obr = Ring(P, "ob", 3, [128, 128], BF16)
                for c in range(NCK):
                    allY = bY[c]
                    y = Ytok[:, c, :]
                    y3 = y.rearrange("p (h x) -> p h x", x=64)
                    s, bs = st8.next()
                    k.op("dve", lambda e: e.reduce_sum(s[:, 0, :], y3, AX.X), reads=allY, writes=[bs])
                    q, bq = ysq.next()
                    k.op("pool", lambda e: e.tensor_tensor(q[:], y, y, ALU.mult), reads=allY, writes=[bq])
                    k.op("dve", lambda e: e.reduce_sum(s[:, 1, :], q[:].rearrange("p (h x) -> p h x", x=64), AX.X), reads=[bq, bs], writes=[bs])
                    k.op("dve", lambda e: e.tensor_scalar(s[:, 2, :], s[:, 0, :], 1.0 / 64.0, None, ALU.mult), reads=[bs], writes=[bs])
                    k.op("dve", lambda e: e.tensor_tensor(s[:, 3, :], s[:, 2, :], s[:, 2, :], ALU.mult), reads=[bs], writes=[bs])
                    k.op("dve", lambda e: e.scalar_tensor_tensor(s[:, 4, :], s[:, 1, :], 1.0 / 64.0, s[:, 3, :], ALU.mult, ALU.subtract),
                         reads=[bs], writes=[bs])
                    k.op("act", lambda e: e.activation(s[:, 5, :], s[:, 4, :], AF.Sqrt, bias=gne[:, 0:1]), reads=[bs, b_c], writes=[bs])
                    k.op("dve", lambda e: e.reciprocal(s[:, 5, :], s[:, 5, :]), reads=[bs], writes=[bs])
                    yn, byn = ynr.next()
                    yn3 = yn[:].rearrange("p (h x) -> p h x", x=64)
                    k.op("dve", lambda e: e.tensor_tensor(yn3, y3, s[:, 2, :].unsqueeze(2).to_broadcast([128, 8, 64]), ALU.subtract),
                         reads=allY + [bs], writes=[byn])
                    k.op("pool", lambda e: e.tensor_tensor(yn3, yn3, s[:, 5, :].unsqueeze(2).to_broadcast([128, 8, 64]), ALU.mult),
                         reads=[byn, bs], writes=[byn])
                    pr, bpr = psR.next()
                    for hp in range(4):
                        k.op("pe", lambda e: e.transpose(pr[:, hp, :], yn[:, hp * 128:(hp + 1) * 128], identf[:]), reads=[byn, b_c], writes=[bpr])
                    cs = slice(c * 128, (c + 1) * 128)
                    for hp in range(4):
                        of, bof = ofr.next()
                        k.op("act", lambda e: e.activation(of[:], pr[:, hp, :], AF.Identity, bias=vec[:, 47 + hp:48 + hp], scale=vec[:, 43 + hp:44 + hp]),
                             reads=[bpr, b_c], writes=[bof])
                        k.op("dve", lambda e: e.tensor_tensor(of[:], of[:], BON[:, hp, cs], ALU.add), reads=[bof, b_l], writes=[bof])
                        ob, bob = obr.next()
                        k.op("pool", lambda e: e.tensor_tensor(ob[:], of[:], G[:, hp, cs], ALU.mult), reads=[bof, b_l], writes=[bob])
                        k.dma("sp", YT[b, 1, hp * 128:(hp + 1) * 128, cs], ob[:], reads=[bob], writes=[C.bYT])


def build(n_layers=DEPTH, debug=(), stop_after=None):
    P = Prog(n_layers, debug)
    nc, k = P.nc, P.k
    xt0 = P.din("xt0", [NB, D, TT])
    cT = P.din("cT", [128, KC, 3])
    cst_ones = P.din("ones", [128, 128])
    ada_w = P.din("ada_w", [DEPTH, D, 6 * D])
    ada_b = P.din("ada_b_r", [DEPTH, 128, 48])
    ng_r = P.din("ng_r", [DEPTH, 128, 2, KC])
    w_in = P.din("w_in", [DEPTH, D, N_IN])
    XT = P.dscr("XT", [NB, D, TT], F32)
    PT = P.dscr("PT", [NB, NCH * 128, TT], BF16)
    bXT = [Buf("XT0"), Buf("XT1")]
    bPT = Buf("PT")
    if "yt_in" in P.debug:
        YT = P.din("YT3", [NB, 3, 512, TT], BF16)
        P.dbufs["YT3"] = Buf("YT3")
    else:
        YT = P.dscr("YT3", [NB, 3, 512, TT], BF16)
    C = type("Ctx", (), {})()
    C.XT, C.PT, C.YT, C.bXT, C.bPT, C.bYT = XT, PT, YT, bXT, bPT, Buf("YT3")
    C.xt0 = xt0
    declare_inputs(P, C)

    with P.scope():
        ones = P.sb("ones", [128, 128], F32)
        b_ones = Buf("ones")
        k.dma("sp", ones[:], cst_ones, writes=[b_ones])
        silu_c = P.sb("silu_c", [128, KC, 3], F32)
        b_silu = Buf("silu_c")
        k.dma("sp", silu_c[:], cT, writes=[b_silu])
        k.op("act", lambda e: e.activation(silu_c[:], silu_c[:], AF.Silu), reads=[b_silu], writes=[b_silu])
        C.epsc = P.sb("epsc_g", [128, 1], F32)
        k.op("dve", lambda e: e.memset(C.epsc[:], NORM_EPS), writes=[b_ones])
        mod = P.sb("mod", [128, 48, 3], F32)
        b_mod = Buf("mod")
        gs = P.sb("gs", [128, 2, KC, 3], F32)
        b_gs = Buf("gs")

        for l in range(n_layers):
            src_x = xt0 if l == 0 else XT
            with P.scope():
                adab = P.sb("adab", [128, 48], F32)
                b_adab = Buf("adab")
                k.dma("sp", adab[:], ada_b[l], writes=[b_adab])
                ng = P.sb("ng", [128, 2, KC], F32)
                b_ng = Buf("ng")
                k.dma("sp", ng[:], ng_r[l], writes=[b_ng])
                wr = Ring(P, "adaw", 2, [128, KC, 512], F32)
                pr = Ring(P, "modps", 2, [128, 4, 3], F32, psum=True)
                for g in range(12):
                    wt, bw = wr.next()
                    k.dma("sp" if g % 2 == 0 else "act", wt[:],
                          ada_w[l][:, g * 512:(g + 1) * 512].rearrange("(kc p) n -> p kc n", p=128), writes=[bw])
                    pt, bp = pr.next()
                    for c4 in range(4):
                        for kc in range(KC):
                            k.op("pe", lambda e, c4=c4, kc=kc: e.matmul(
                                pt[:, c4, :], wt[:, kc, c4 * 128:(c4 + 1) * 128], silu_c[:, kc, :],
                                start=(kc == 0), stop=(kc == KC - 1)),
                                reads=[bw, b_silu], writes=[bp])
                    k.op("dve", lambda e: e.tensor_tensor(
                        mod[:, g * 4:(g + 1) * 4, :], pt[:],
                        adab[:, g * 4:(g + 1) * 4].unsqueeze(2).to_broadcast([128, 4, 3]), ALU.add),
                        reads=[bp, b_adab], writes=[b_mod])
                for n, mi in ((0, 1), (1, 4)):
                    k.op("dve", lambda e, n=n, mi=mi: e.tensor_scalar(
                        gs[:, n, :, :], mod[:, mi * 8:(mi + 1) * 8, :], 1.0, None, ALU.add),
                        reads=[b_mod], writes=[b_gs])
                    k.op("dve", lambda e, n=n: e.tensor_tensor(
                        gs[:, n, :, :], gs[:, n, :, :],
                        ng[:, n, :].unsqueeze(2).to_broadcast([128, KC, 3]), ALU.mult),
                        reads=[b_gs, b_ng], writes=[b_gs])
            if stop_after == "mod":
                break

            with P.scope():
                hT = P.sb("hT", [128, NB, KC, TT], BF16)
                b_h = [[Buf(f"h{b}_{i}") for i in range(5)] for b in range(NB)]
                blocks = [(0, TC)] + [(TC + i * 512, 512) for i in range(4)]
                xr = Ring(P, "xin", 2, [128, KC, 512], F32)
                sqr = Ring(P, "sq", 1, [128, KC, 512], F32)
                ssr = Ring(P, "ssps", 2, [128, 512], F32, psum=True)
                rsr = Ring(P, "rstd", 2, [128, 512], F32)
                for b in range(NB):
                    for bi, (t0, n) in enumerate(blocks):
                        j = 2 if bi == 0 else b
                        xt, bx = xr.next()
                        k.dma("sp", xt[:, :, :n], src_x[b][:, t0:t0 + n].rearrange("(kc p) n -> p kc n", p=128),
                              reads=[bXT[b]], writes=[bx])
                        sq, bs = sqr.next()
                        k.op("act", lambda e: e.activation(sq[:, :, :n], xt[:, :, :n], AF.Square),
                             reads=[bx], writes=[bs])
                        ss, bss = ssr.next()
                        for kc in range(KC):
                            k.op("pe", lambda e, kc=kc: e.matmul(ss[:, :n], ones[:], sq[:, kc, :n],
                                                                 start=(kc == 0), stop=(kc == KC - 1)),
                                 reads=[bs, b_ones], writes=[bss])
                        rs, brs = rsr.next()
                        k.op("act", lambda e: e.activation(rs[:, :n], ss[:, :n], AF.Sqrt, bias=NORM_EPS, scale=1.0 / D),
                             reads=[bss], writes=[brs])
                        k.op("dve", lambda e: e.reciprocal(rs[:, :n], rs[:, :n]), reads=[brs], writes=[brs])
                        k.op("dve", lambda e: e.tensor_tensor(
                            sq[:, :, :n], xt[:, :, :n], rs[:, :n].unsqueeze(1).to_broadcast([128, KC, n]), ALU.mult),
                            reads=[bx, brs, bs], writes=[bs])
                        for kc in range(KC):
                            k.op("act", lambda e, kc=kc: e.activation(
                                hT[:, b, kc, t0:t0 + n], sq[:, kc, :n], AF.Identity,
                                bias=mod[:, 0 * 8 + kc, j:j + 1], scale=gs[:, 0, kc, j:j + 1]),
                                reads=[bs, b_mod, b_gs], writes=[b_h[b][bi]])
                groups = [list(range(g * 4, g * 4 + 4)) for g in range(6)] + [[24]] + \
                         [list(range(25 + g * 4, 29 + g * 4)) for g in range(6)]
                wr = Ring(P, "win", 2, [128, KC, 512], BF16)
                pr = Ring(P, "inps", 4, [128, 512], F32, psum=True)
                sr = Ring(P, "instage", 4, [128, 512], BF16)
                ev = 0
                for grp in groups:
                    c0 = chunk_cols(grp[0])[0]
                    ncols = sum(chunk_cols(ci)[1] for ci in grp)
                    wt, bw = wr.next()
                    k.dma("pool", wt[:, :, :ncols],
                          w_in[l][:, c0:c0 + ncols].rearrange("(kc p) n -> p kc n", p=128), writes=[bw])
                    for b in range(NB):
                        for bi, (t0, n) in enumerate(blocks):
                            for ci in grp:
                                cc0, cn = chunk_cols(ci)
                                o = cc0 - c0
                                pt, bp = pr.next()
                                for kc in range(KC):
                                    k.op("pe", lambda e, kc=kc: e.matmul(
                                        pt[:cn, :n], wt[:, kc, o:o + cn], hT[:, b, kc, t0:t0 + n],
                                        start=(kc == 0), stop=(kc == KC - 1)),
                                        reads=[bw, b_h[b][bi]], writes=[bp])
                                st, bst = sr.next()
                                if ci >= 25:
                                    k.op("act", lambda e: e.activation(st[:cn, :n], pt[:cn, :n], AF.Sigmoid),
                                         reads=[bp], writes=[bst])
                                elif ev % 2 == 0:
                                    k.op("act", lambda e: e.activation(st[:cn, :n], pt[:cn, :n], AF.Copy),
                                         reads=[bp], writes=[bst])
                                else:
                                    k.op("dve", lambda e: e.tensor_copy(st[:cn, :n], pt[:cn, :n]),
                                         reads=[bp], writes=[bst])
                                ev += 1
                                k.dma("sp", PT[b, ci * 128:ci * 128 + cn, t0:t0 + n], st[:cn, :n],
                                      reads=[bst], writes=[bPT])
            if stop_after == "in":
                break
            C.mod, C.b_mod, C.gs, C.b_gs, C.ones, C.b_ones = mod, b_mod, gs, b_gs, ones, b_ones
            if "nomla" not in P.debug and "yt_in" not in P.debug:
                phase_mla(P, C, l)
            if stop_after == "mla":
                break
            if "nossm" not in P.debug and "yt_in" not in P.debug:
                phase_ssm(P, C, l)
            if stop_after == "ssm":
                break
            if "yt_in" not in P.debug:
                phase_rwkv_prep(P, C, l)
                if stop_after == "rwprep":
                    break
                phase_rwkv_scan(P, C, l)
                if stop_after == "rwkv":
                    break
            phase_merge(P, C, l)
            if stop_after == "merge":
                break
            phase_moe(P, C, l, last=(l == n_layers - 1))
            if stop_after == "moe":
                break
        k.barrier()
    return P


def host_inputs(inputs, core):
    b0 = core * NB
    x = inputs["x"][b0:b0 + NB]
    ctx = inputs["ctx"][b0:b0 + NB]
    xt0 = np.ascontiguousarray(np.concatenate([ctx, x], axis=1).transpose(0, 2, 1))
    cT = np.stack([inputs["c"][b0], inputs["c"][b0 + 1], inputs["c_ctx"]], axis=1)
    cT = np.ascontiguousarray(cT.reshape(KC, 128, 3).transpose(1, 0, 2))
    m = {"xt0": xt0, "cT": cT}
    return m


def pj(v, n):
    return np.ascontiguousarray(v.reshape(v.shape[:-1] + (n, 128)).swapaxes(-1, -2))


def host_shared(inputs):
    m = {"ones": np.ones((128, 128), np.float32)}
    for name in ("ada_w", "w_in"):
        m[name] = inputs[name]
    for name in ("mla_w_uq", "mla_w_ukv"):
        m[name] = inputs[name]
    mats = np.zeros((128, 3, 128), np.float32)
    mats[:64, 0, :64] = 1.0
    mats[64:96, 0, 64:96] = 1.0
    mats[:64, 1, :64] = 1.0
    for i in range(16):
        mats[64 + 16 + i, 2, 64 + i] = -1.0
        mats[64 + i, 2, 64 + 16 + i] = 1.0
    m["mla_mats"] = mats
    vec = np.zeros((DEPTH, 128, 8), np.float32)
    vec[:, :, 0:3] = pj(inputs["mla_q_norm"], 3)
    vec[:, :, 3:5] = pj(inputs["mla_kv_norm"], 2)
    vec[:, :64, 5] = inputs["mla_qn_nope"]
    vec[:, 64:96, 5] = inputs["mla_qn_rope"]
    vec[:, :64, 6] = inputs["mla_kn_nope"]
    vec[:, 64:96, 6] = inputs["mla_kn_rope"]
    vec[:, :64, 7] = 1.0 / 64.0
    vec[:, 64:96, 7] = 1.0 / 32.0
    m["mla_vec"] = vec
    tt = np.arange(TL)
    inv = (10000.0 ** (-np.arange(0, 16, 2, dtype=np.float32) / 16.0)).astype(np.float32)
    ang = np.concatenate([(tt // 64).astype(np.float32)[:, None] * inv, (tt % 64).astype(np.float32)[:, None] * inv], axis=-1)
    tab = np.zeros((96, 2, TT), np.float32)
    tab[:, 0, :] = 1.0
    tab[64:80, 0, TC:] = np.cos(ang).T
    tab[80:96, 0, TC:] = np.cos(ang).T
    tab[64:80, 1, TC:] = np.sin(ang).T
    tab[80:96, 1, TC:] = np.sin(ang).T
    m["rope_tab"] = tab
    it = np.zeros((128, 2, TT), np.float32)
    it[:, 0, :] = np.arange(TT)
    it[:, 1, :TC] = TC - 1 - np.arange(TC)
    it[:, 1, TC:] = TC + (TL - 1 - np.arange(TL))
    m["ssm_iota"] = it
    L_ = DEPTH
    lre = inputs["ssm_lambda_re"].reshape(L_, 2, 16, 128)
    lim = inputs["ssm_lambda_im"].reshape(L_, 2, 16, 128)
    ldt = np.repeat(inputs["ssm_log_dt"], 64, axis=-1).reshape(L_, 2, 16, 128)
    m["ssm_sv"] = np.ascontiguousarray(np.stack([lre, lim, ldt], axis=-1).transpose(0, 3, 1, 2, 4))
    BT = np.zeros((L_, 2, 16, 128, 128), np.float32)
    CT = np.zeros((L_, 2, 2, 16, 128, 128), np.float32)
    for ri, (bn, cn) in enumerate((("ssm_b_re", "ssm_c_re"), ("ssm_b_im", "ssm_c_im"))):
        bb = inputs[bn]
        cc = inputs[cn]
        for g in range(32):
            sc, gg = g // 2, g % 2
            r0 = (sc % 4) * 32 + gg * 16
            BT[:, ri, sc, r0:r0 + 16, gg * 64:(gg + 1) * 64] = bb[:, g].transpose(0, 2, 1)
            CT[:, :, ri, sc, gg * 64:(gg + 1) * 64, r0:r0 + 16] = cc[:, :, g].transpose(0, 1, 3, 2)
    m["ssm_BT"] = BT
    m["ssm_CT"] = CT
    m["ssm_vec"] = np.ascontiguousarray(np.stack([pj(inputs["ssm_d"], 4), pj(inputs["ssm_glu_b"], 4)], axis=2))
    m["ssm_glu_w"] = inputs["ssm_glu_w"]
    for name in ("w_branch", "w_out", "router_w"):
        m[name] = inputs[name]
    for name in ("moe_w1", "moe_w3", "moe_w2"):
        m[name] = inputs[name]
    cst = np.zeros((128, 3, 256), np.float32)
    cst[:, 0, :] = np.arange(1, 257)
    cst[:16, 1, :16] = 1.0
    cst[16:32, 1, 16:32] = 1.0
    cst[:, 2, :128] = np.eye(128)
    m["moe_cst"] = cst
    m["moe_jcol"] = np.stack([np.arange(1, 129), np.arange(129, 257)], axis=1).astype(np.float32)
    L_ = DEPTH
    rv = np.zeros((L_, 128, 51), np.float32)
    rv[:, :, 0:15] = pj(inputs["rwkv_mu"], 15)
    rv[:, :, 15:23] = pj(inputs["rwkv_w0"], 4).transpose(0, 2, 1, 3).reshape(L_, 128, 8)
    rv[:, :, 23:31] = pj(inputs["rwkv_a0"], 4).transpose(0, 2, 1, 3).reshape(L_, 128, 8)
    rv[:, :, 31:35] = pj(inputs["rwkv_k_k"], 4)
    rv[:, :, 35:39] = pj(inputs["rwkv_k_a"], 4)
    rv[:, :, 39:43] = pj(inputs["rwkv_r_k"].reshape(L_, 512), 4)
    rv[:, :, 43:47] = pj(inputs["rwkv_ln_w"], 4)
    rv[:, :, 47:51] = pj(inputs["rwkv_ln_b"], 4)
    m["rw_vec"] = rv
    for name in ("rwkv_w2", "rwkv_a2", "rwkv_g2"):
        m[name] = inputs[name]
    bd = np.zeros((128, 128), np.float32)
    bd[:64, :64] = 1.0
    bd[64:, 64:] = 1.0
    m["rw_bd"] = bd
    rst = np.ones((128, TT + 1), np.float32)
    rst[:, 0::128] = 0.0
    m["rw_rst"] = rst
    ii = np.arange(128)
    mk = np.zeros((128, 2, 640), np.float32)
    for j_ in range(2):
        if j_ == 0:
            strict = (ii[None, :] > ii[:, None]).astype(np.float32)
            incl = (ii[None, :] >= ii[:, None]).astype(np.float32)
        else:
            strict = (ii[None, :] < ii[:, None]).astype(np.float32)
            incl = (ii[None, :] <= ii[:, None]).astype(np.float32)
        mk[:, j_, 0:128] = strict
        mk[:, j_, 128:256] = incl
        mk[:, j_, 256:384] = -strict
        mk[:, j_, 384:512] = incl
        mk[:, j_, 512:640] = -strict.T
    m["rw_masks"] = mk
    lm = np.zeros((128, 4, 128), np.float32)
    ti, si = ii[:, None], ii[None, :]
    lm[:, 0, :] = (ti // 16 == si // 16)
    for q_, sz in enumerate((16, 32, 64)):
        lm[:, 1 + q_, :] = (ti // (2 * sz) == si // (2 * sz)) & (ti // sz != si // sz)
    m["rw_lm"] = lm
    m["ident"] = np.eye(128, dtype=np.float32)
    m["ada_b_r"] = pj(inputs["ada_b"], 48)
    m["ng_r"] = np.ascontiguousarray(np.stack([pj(inputs["norm1_g"], KC), pj(inputs["norm2_g"], KC)], axis=2))
    return m


def kernel(**inputs):
    inputs = {k_: np.asarray(v) for k_, v in inputs.items()}
    P = build()
    shared = host_shared(inputs)
    in_maps = []
    for c in range(NCORES):
        m = host_inputs(inputs, c)
        m.update(shared)
        in_maps.append({n: m[n] for n in P.inputs})
    res = run_bass_kernel_spmd(P.nc, in_maps, core_ids=list(range(NCORES)))
    outs = [r["OUT"] for r in res.results]
    y = np.concatenate(outs, axis=0)
    return np.ascontiguousarray(y.transpose(0, 2, 1)).astype(np.float32)
```

```python
import contextlib
import numpy as np
import concourse.bass as bass
import concourse.mybir as mybir
from concourse.bass_utils import run_bass_kernel_spmd

F32 = mybir.dt.float32
BF16 = mybir.dt.bfloat16
AF = mybir.ActivationFunctionType
ALU = mybir.AluOpType
AX = mybir.AxisListType

NCORES = 8
NB = 2
D = 1024
KC = 8
TC = 256
TL = 2048
TT = TC + TL
DEPTH = 4
N_IN = 6176
NCH = 49
NORM_EPS = 1e-6


def chunk_cols(ci):
    if ci < 24:
        return ci * 128, 128
    if ci == 24:
        return 3072, 32
    return 3104 + (ci - 25) * 128, 128


class Buf:
    __slots__ = ("name", "w", "r")

    def __init__(self, name=""):
        self.name = name
        self.w = None
        self.r = []


class K:
    NDMA = 12

    def __init__(self, nc):
        self.nc = nc
        self.eng = {"pe": nc.tensor, "dve": nc.vector, "act": nc.scalar, "pool": nc.gpsimd, "sp": nc.sync}
        self.sem = {}
        self.cnt = {}
        self.seen = {e: {} for e in self.eng}
        self._stack = []
        for e in self.eng:
            self.sem[e] = self._mksem("s_" + e)
            self.cnt[e] = 0
        self.dq = {}
        for q in ("sp", "act", "pool"):
            self.dq[q] = {"n": 0}
            for i in range(self.NDMA):
                self.sem[(q, i)] = self._mksem(f"d_{q}{i}")
        self.n_instr = 0
        self.n_wait = 0

    def _mksem(self, name):
        g = self.nc.semaphore(name)
        s = g.__enter__()
        self._stack.append(g)
        return s

    def _wait(self, e, dep):
        if dep is None:
            return
        key, val = dep
        if key == "pe" and e == "pe":
            return
        if self.seen[e].get(key, 0) >= val:
            return
        self.eng[e].wait_ge(self.sem[key], val)
        self.seen[e][key] = val
        self.n_wait += 1

    def _deps(self, e, reads, writes):
        for b in reads:
            self._wait(e, b.w)
        for b in writes:
            self._wait(e, b.w)
            for r in b.r:
                self._wait(e, r)

    def _mark(self, tok, reads, writes):
        for b in reads:
            b.r = [r for r in b.r if r[0] != tok[0]]
            b.r.append(tok)
        for b in writes:
            b.w = tok
            b.r = []

    def op(self, e, fn, reads=(), writes=()):
        self._deps(e, reads, writes)
        ins = fn(self.eng[e])
        self.cnt[e] += 1
        ins.then_inc(self.sem[e], 1)
        self._mark((e, self.cnt[e]), reads, writes)
        self.n_instr += 1
        return ins

    def dma(self, q, out, in_, reads=(), writes=(), **kw):
        d = self.dq[q]
        i = d["n"] % self.NDMA
        gen = d["n"] // self.NDMA
        key = (q, i)
        if gen > 0:
            self._wait(q, (key, 16 * gen))
        self._deps(q, reads, writes)
        ins = self.eng[q].dma_start(out=out, in_=in_, **kw)
        ins.then_inc(self.sem[key], 16)
        d["n"] += 1
        self._mark((key, 16 * (gen + 1)), reads, writes)
        self.n_instr += 1

    def barrier(self):
        toks = [(e, self.cnt[e]) for e in self.eng if self.cnt[e] > 0]
        for q, d in self.dq.items():
            n = d["n"]
            for i in range(self.NDMA):
                c = (n - i + self.NDMA - 1) // self.NDMA
                if c > 0:
                    toks.append(((q, i), 16 * c))
        for e in self.eng:
            for t in toks:
                self._wait(e, t)


class Ring:
    def __init__(self, P, name, n, shape, dtype, psum=False):
        self.items = []
        for i in range(n):
            t = P.ps(f"{name}{i}", shape, dtype) if psum else P.sb(f"{name}{i}", shape, dtype)
            self.items.append((t, Buf(f"{name}{i}")))
        self.i = 0

    def next(self):
        it = self.items[self.i % len(self.items)]
        self.i += 1
        return it


class Prog:
    def __init__(self, n_layers=DEPTH, debug=()):
        self.nc = nc = bass.Bass("TRN2", target_bir_lowering=False)
        self.k = K(nc)
        self.debug = set(debug)
        self.n_layers = n_layers
        self._scopes = []
        self.uid = 0
        self.inputs = {}
        self.outputs = {}
        self.dbufs = {}

    def din(self, name, shape, dtype=F32):
        t = self.nc.dram_tensor(name, list(shape), dtype, kind="ExternalInput").ap()
        self.inputs[name] = t
        return t

    def dscr(self, name, shape, dtype, out=False):
        kind = "ExternalOutput" if (out or name in self.debug) else "Internal"
        t = self.nc.dram_tensor(name, list(shape), dtype, kind=kind).ap()
        if kind == "ExternalOutput":
            self.outputs[name] = t
        self.dbufs[name] = Buf(name)
        return t

    @contextlib.contextmanager
    def scope(self):
        st = contextlib.ExitStack()
        self._scopes.append(st)
        try:
            yield
        finally:
            self.k.barrier()
            self._scopes.pop()
            st.close()

    def sb(self, name, shape, dtype):
        self.uid += 1
        g = self.nc.sbuf_tensor(f"{name}_{self.uid}", list(shape), dtype)
        return self._scopes[-1].enter_context(g)

    def ps(self, name, shape, dtype=F32):
        self.uid += 1
        g = self.nc.psum_tensor(f"{name}_{self.uid}", list(shape), dtype)
        return self._scopes[-1].enter_context(g)


MLA_SCALE = 1.0 / float(np.sqrt(96.0))


def declare_inputs(P, C):
    C.mla_w_uq = P.din("mla_w_uq", [DEPTH, 384, 768])
    C.mla_w_ukv = P.din("mla_w_ukv", [DEPTH, 256, 1024])
    C.mla_mats = P.din("mla_mats", [128, 3, 128])
    C.mla_vec = P.din("mla_vec", [DEPTH, 128, 8])
    C.rope_tab = P.din("rope_tab", [96, 2, TT])
    C.ssm_iota = P.din("ssm_iota", [128, 2, TT])
    C.ssm_sv = P.din("ssm_sv", [DEPTH, 128, 2, 16, 3])
    C.ssm_BT = P.din("ssm_BT", [DEPTH, 2, 16, 128, 128])
    C.ssm_CT = P.din("ssm_CT", [DEPTH, 2, 2, 16, 128, 128])
    C.ssm_vec = P.din("ssm_vec", [DEPTH, 128, 2, 4])
    C.ssm_glu_w = P.din("ssm_glu_w", [DEPTH, 512, 512])
    C.w_branch = P.din("w_branch", [DEPTH, 3, 512, D])
    C.w_out = P.din("w_out", [DEPTH, D, D])
    C.router_w = P.din("router_w", [DEPTH, D, 16])
    C.ident = P.din("ident", [128, 128])
    C.LG = P.dscr("LG", [NB, 16, TT], F32)
    C.bLG = Buf("LG")
    C.H2 = P.dscr("H2", [NB, TT, D], BF16)
    C.bH2 = Buf("H2")
    C.moe_w1 = P.din("moe_w1", [DEPTH, NE, D, FF])
    C.moe_w3 = P.din("moe_w3", [DEPTH, NE, D, FF])
    C.moe_w2 = P.din("moe_w2", [DEPTH, NE, FF, D])
    C.moe_cst = P.din("moe_cst", [128, 3, 256])
    C.moe_jcol = P.din("moe_jcol", [128, 2])
    C.POSD = P.dscr("POSD", [NB, NE, TT], F32)
    C.MGD = P.dscr("MGD", [NB, NE, TT], F32)
    C.bPOSD = Buf("POSD")
    C.XS = P.dscr("XS", [NE, D, NJ], BF16)
    C.bXS = Buf("XS")
    C.YE = P.dscr("YE", [NE, NJ, D], BF16)
    C.bYE = Buf("YE")
    C.OUT = P.dscr("OUT", [NB, D, TL], F32, out=True)
    C.bOUT = Buf("OUT")
    C.rw_vec = P.din("rw_vec", [DEPTH, 128, 51])
    C.rwkv_w2 = P.din("rwkv_w2", [DEPTH, 2, 64, 512])
    C.rwkv_a2 = P.din("rwkv_a2", [DEPTH, 2, 64, 512])
    C.rwkv_g2 = P.din("rwkv_g2", [DEPTH, 128, 512])
    C.rw_bd = P.din("rw_bd", [128, 128])
    C.rw_rst = P.din("rw_rst", [128, TT + 1])
    C.rw_masks = P.din("rw_masks", [128, 2, 640])
    C.rw_lm = P.din("rw_lm", [128, 4, 128])
    C.RWT = P.dscr("RWT", [NB, 2, 4, 512, TT], BF16)
    C.RWV = P.dscr("RWV", [NB, 512, TT], BF16)
    C.RWG = P.dscr("RWG", [NB, 512, TT], BF16)
    C.RWB = P.dscr("RWB", [NB, 512, TT], F32)
    C.RWPL = P.dscr("RWPL", [NB, 2, 512, NCK], F32)
    C.bRW = Buf("RW")
    if "YTOK" in P.debug:
        C.YTOK = P.dscr("YTOK", [NB, 128, NCK, 512], F32)
    C.YG = P.dscr("YG", [NB, 512, TT], BF16)
    C.bYG = Buf("YG")


def rstd_from(k, ss_ap, out_ap, scale, bias, reads, bout):
    k.op("act", lambda e: e.activation(out_ap, ss_ap, AF.Sqrt, bias=bias, scale=scale), reads=reads, writes=[bout])
    k.op("dve", lambda e: e.reciprocal(out_ap, out_ap), reads=[bout], writes=[bout])


def phase_mla(P, C, l):
    k = P.k
    PT, YT = C.PT, C.YT
    blocks = [(0, TC)] + [(TC + i * 512, 512) for i in range(4)]
    with P.scope():
        mats = P.sb("mla_mats", [128, 3, 128], BF16)
        b_mats = Buf("mats")
        k.dma("pool", mats[:], C.mla_mats, writes=[b_mats])
        onesb = P.sb("onesb", [128, 128], BF16)
        b_onesb = Buf("onesb")
        k.op("dve", lambda e: e.memset(onesb[:], 1.0), writes=[b_onesb])
        vec = P.sb("mla_vec", [128, 8], F32)
        b_vec = Buf("vec")
        k.dma("sp", vec[:], C.mla_vec[l], writes=[b_vec])
        epsc = P.sb("epsc", [128, 1], F32)
        k.op("dve", lambda e: e.memset(epsc[:], NORM_EPS), writes=[b_vec])
        tab = P.sb("rope_tab", [96, 2, TT], F32)
        b_tab = Buf("tab")
        k.dma("sp", tab[:], C.rope_tab, writes=[b_tab])
        wuq = P.sb("wuq", [128, 3, 768], BF16)
        b_wuq = Buf("wuq")
        k.dma("pool", wuq[:], C.mla_w_uq[l].rearrange("(kc p) n -> p kc n", p=128), writes=[b_wuq])
        wk = P.sb("wk", [128, 2, 8, 64], BF16)
        wv = P.sb("wv", [128, 2, 8, 64], BF16)
        b_wkv = Buf("wkv")
        ukv = C.mla_w_ukv[l].rearrange("(kc p) (h x) -> p kc h x", p=128, x=128)
        for kc in range(2):
            k.dma("pool", wk[:, kc], ukv[:, kc, :, 0:64], writes=[b_wkv])
            k.dma("pool", wv[:, kc], ukv[:, kc, :, 64:128], writes=[b_wkv])
        QT = P.sb("QT", [96, 8, TT], BF16)
        KT = P.sb("KT", [96, 8, TT], BF16)
        Vt = P.sb("Vt", [128, 18, 512], BF16)
        for b in range(NB):
            b_Q = [Buf(f"Q{i}") for i in range(5)]
            b_K = [Buf(f"K{i}") for i in range(5)]
            b_V = [Buf(f"V{i}") for i in range(5)]
            with P.scope():
                x5r = Ring(P, "x5", 2, [128, 5, 512], BF16)
                krr = Ring(P, "kr", 2, [96, 512], BF16)
                sq5r = Ring(P, "sq5", 1, [128, 5, 512], BF16)
                cqnr = Ring(P, "cqn", 2, [128, 5, 512], BF16)
                psA = Ring(P, "psA", 3, [128, 512], F32, psum=True)
                psB = Ring(P, "psB", 3, [128, 512], F32, psum=True)
                rsr = Ring(P, "rs", 3, [128, 512], F32)
                f32r = Ring(P, "f32t", 3, [128, 512], F32)
                f32r2 = Ring(P, "f32u", 3, [128, 512], F32)
                bfr = Ring(P, "bft", 3, [128, 512], BF16)
                krf = P.sb("krf", [96, 512], BF16)
                b_krf = Buf("krf")
                for bi, (t0, n) in enumerate(blocks):
                    x5, bx5 = x5r.next()
                    k.dma("sp", x5[:, :, :n], PT[b, 19 * 128:24 * 128, t0:t0 + n].rearrange("(c p) n -> p c n", p=128),
                          reads=[C.bPT], writes=[bx5])
                    kr, bkr = krr.next()
                    k.dma("sp", kr[64:96, :n], PT[b, 24 * 128:24 * 128 + 32, t0:t0 + n], reads=[C.bPT], writes=[bkr])
                    sq5, bsq5 = sq5r.next()
                    k.op("act", lambda e: e.activation(sq5[:, :, :n], x5[:, :, :n], AF.Square), reads=[bx5], writes=[bsq5])
                    cqn, bcqn = cqnr.next()
                    for (c0, nc_, col0, dim) in ((0, 3, 0, 384.0), (3, 2, 3, 256.0)):
                        ps, bps = psA.next()
                        for c in range(nc_):
                            k.op("pe", lambda e: e.matmul(ps[:, :n], onesb[:], sq5[:, c0 + c, :n],
                                                          start=(c == 0), stop=(c == nc_ - 1)),
                                 reads=[bsq5, b_onesb], writes=[bps])
                        rs, brs = rsr.next()
                        rstd_from(k, ps[:, :n], rs[:, :n], 1.0 / dim, epsc[:, 0:1], [bps, b_vec], brs)
                        for c in range(nc_):
                            k.op("dve", lambda e: e.scalar_tensor_tensor(
                                cqn[:, c0 + c, :n], x5[:, c0 + c, :n], vec[:, col0 + c:col0 + c + 1], rs[:, :n],
                                ALU.mult, ALU.mult), reads=[bx5, brs, b_vec], writes=[bcqn])
                    sqk, bsqk = bfr.next()
                    k.op("act", lambda e: e.activation(sqk[64:96, :n], kr[64:96, :n], AF.Square), reads=[bkr], writes=[bsqk])
                    ps, bps = psA.next()
                    k.op("pe", lambda e: e.matmul(ps[64:96, :n], mats[64:96, 0, 64:96], sqk[64:96, :n], start=True, stop=True),
                         reads=[bsqk, b_mats], writes=[bps])
                    rs, brs = rsr.next()
                    rstd_from(k, ps[64:96, :n], rs[64:96, :n], 1.0 / 32.0, epsc[64:96, 0:1], [bps, b_vec], brs)
                    krn, bkrn = bfr.next()
                    k.op("dve", lambda e: e.scalar_tensor_tensor(
                        krn[64:96, :n], kr[64:96, :n], vec[64:96, 6:7], rs[64:96, :n], ALU.mult, ALU.mult),
                        reads=[bkr, brs, b_vec], writes=[bkrn])
                    ps2, bps2 = psB.next()
                    k.op("pe", lambda e: e.matmul(ps2[64:96, :n], mats[64:96, 2, 64:96], krn[64:96, :n], start=True, stop=True),
                         reads=[bkrn, b_mats], writes=[bps2])
                    t1, bt1 = f32r.next()
                    k.op("dve", lambda e: e.tensor_tensor(t1[64:96, :n], krn[64:96, :n], tab[64:96, 0, t0:t0 + n], ALU.mult),
                         reads=[bkrn, b_tab], writes=[bt1])
                    t2, bt2 = f32r2.next()
                    k.op("dve", lambda e: e.tensor_tensor(t2[64:96, :n], ps2[64:96, :n], tab[64:96, 1, t0:t0 + n], ALU.mult),
                         reads=[bps2, b_tab], writes=[bt2])
                    k.op("pool", lambda e: e.tensor_tensor(krf[64:96, :n], t1[64:96, :n], t2[64:96, :n], ALU.add),
                         reads=[bt1, bt2], writes=[b_krf])
                    k.op("pool", lambda e: e.tensor_copy(
                        KT[64:96, :, t0:t0 + n], krf[64:96, :n].unsqueeze(1).to_broadcast([32, 8, n])),
                        reads=[b_krf], writes=[b_K[bi]])
                    for h in range(8):
                        ps, bps = psA.next()
                        for c in range(3):
                            k.op("pe", lambda e: e.matmul(ps[:96, :n], wuq[:, c, h * 96:(h + 1) * 96], cqn[:, c, :n],
                                                          start=(c == 0), stop=(c == 2)),
                                 reads=[bcqn, b_wuq], writes=[bps])
                        qs, bqs = f32r.next()
                        k.op("act", lambda e: e.activation(qs[:96, :n], ps[:96, :n], AF.Copy), reads=[bps], writes=[bqs])
                        sq, bsq = bfr.next()
                        k.op("act", lambda e: e.activation(sq[:96, :n], ps[:96, :n], AF.Square), reads=[bps], writes=[bsq])
                        ps2, bps2 = psB.next()
                        k.op("pe", lambda e: e.matmul(ps2[:96, :n], mats[:96, 0, :96], sq[:96, :n], start=True, stop=True),
                             reads=[bsq, b_mats], writes=[bps2])
                        rs, brs = rsr.next()
                        rstd_from(k, ps2[:96, :n], rs[:96, :n], vec[:96, 7:8], epsc[:96, 0:1], [bps2, b_vec], brs)
                        qn, bqn = bfr.next()
                        k.op("dve", lambda e: e.scalar_tensor_tensor(
                            qn[:96, :n], qs[:96, :n], vec[:96, 5:6], rs[:96, :n], ALU.mult, ALU.mult),
                            reads=[bqs, brs, b_vec], writes=[bqn])
                        ps3, bps3 = psB.next()
                        k.op("pe", lambda e: e.matmul(ps3[:96, :n], mats[:96, 2, :96], qn[:96, :n], start=True, stop=True),
                             reads=[bqn, b_mats], writes=[bps3])
                        t1, bt1 = f32r.next()
                        k.op("pool", lambda e: e.tensor_tensor(t1[:96, :n], qn[:96, :n], tab[:, 0, t0:t0 + n], ALU.mult),
                             reads=[bqn, b_tab], writes=[bt1])
                        t2, bt2 = f32r2.next()
                        k.op("dve", lambda e: e.tensor_tensor(t2[:96, :n], ps3[:96, :n], tab[:, 1, t0:t0 + n], ALU.mult),
                             reads=[bps3, b_tab], writes=[bt2])
                        k.op("pool", lambda e: e.tensor_tensor(QT[:, h, t0:t0 + n], t1[:96, :n], t2[:96, :n], ALU.add),
                             reads=[bt1, bt2], writes=[b_Q[bi]])
                        ps, bps = psA.next()
                        for c in range(2):
                            k.op("pe", lambda e: e.matmul(ps[:64, :n], wk[:, c, h, :], cqn[:, 3 + c, :n],
                                                          start=(c == 0), stop=(c == 1)),
                                 reads=[bcqn, b_wkv], writes=[bps])
                        ks_, bks = f32r.next()
                        k.op("act", lambda e: e.activation(ks_[:64, :n], ps[:64, :n], AF.Copy), reads=[bps], writes=[bks])
                        sq, bsq = bfr.next()
                        k.op("act", lambda e: e.activation(sq[:64, :n], ps[:64, :n], AF.Square), reads=[bps], writes=[bsq])
                        ps2, bps2 = psB.next()
                        k.op("pe", lambda e: e.matmul(ps2[:64, :n], onesb[:64, :64], sq[:64, :n], start=True, stop=True),
                             reads=[bsq, b_onesb], writes=[bps2])
                        rs, brs = rsr.next()
                        rstd_from(k, ps2[:64, :n], rs[:64, :n], 1.0 / 64.0, epsc[:64, 0:1], [bps2, b_vec], brs)
                        k.op("dve", lambda e: e.scalar_tensor_tensor(
                            KT[0:64, h, t0:t0 + n], ks_[:64, :n], vec[:64, 6:7], rs[:64, :n], ALU.mult, ALU.mult),
                            reads=[bks, brs, b_vec], writes=[b_K[bi]])
                    for ti in range(n // 128):
                        tile_i = (t0 // 128) + ti
                        ps, bps = psA.next()
                        for c in range(2):
                            k.op("pe", lambda e: e.matmul(
                                ps[:, :], cqn[:, 3 + c, ti * 128:(ti + 1) * 128],
                                wv[:, c].rearrange("p h x -> p (h x)"), start=(c == 0), stop=(c == 1)),
                                reads=[bcqn, b_wkv], writes=[bps])
                        k.op("act", lambda e: e.activation(Vt[:, tile_i, :], ps[:, :], AF.Copy), reads=[bps], writes=[b_V[bi]])
            if "stop_mlaprep" in P.debug:
                continue
            with P.scope():
                psS = Ring(P, "psS", 4, [128, 512], F32, psum=True)
                psO = Ring(P, "psO", 2, [64, 512], F32, psum=True)
                psD = Ring(P, "psD", 2, [64, 512], F32, psum=True)
                ptr = Ring(P, "pT", 4, [128, 512], BF16)
                rdr = Ring(P, "rden", 2, [64, 512], F32)
                osr = Ring(P, "ost", 3, [64, 512], BF16)
                allK = b_K + b_V
                LOOK = 2
                for h in range(8):
                    for bi, (q0, n) in enumerate(blocks):
                        nkt = 2 if bi == 0 else 18
                        po, bpo = psO.next()
                        pd, bpd = psD.next()
                        pend = []
                        for kt in range(nkt + LOOK):
                            if kt < nkt:
                                pS, bpS = psS.next()
                                k.op("pe", lambda e: e.matmul(pS[:, :n], KT[:, h, kt * 128:(kt + 1) * 128], QT[:, h, q0:q0 + n],
                                                              start=True, stop=True),
                                     reads=allK + [b_Q[bi]], writes=[bpS])
                                pT, bpT = ptr.next()
                                k.op("act", lambda e: e.activation(pT[:, :n], pS[:, :n], AF.Exp, scale=MLA_SCALE),
                                     reads=[bpS], writes=[bpT])
                                pend.append((kt, pT, bpT))
                            if kt >= LOOK:
                                k2, pT2, bpT2 = pend.pop(0)
                                k.op("pe", lambda e: e.matmul(po[:, :n], Vt[:, k2, h * 64:(h + 1) * 64], pT2[:, :n],
                                                              start=(k2 == 0), stop=(k2 == nkt - 1)),
                                     reads=[bpT2] + b_V, writes=[bpo])
                                k.op("pe", lambda e: e.matmul(pd[:, :n], onesb[:, :64], pT2[:, :n],
                                                              start=(k2 == 0), stop=(k2 == nkt - 1)),
                                     reads=[bpT2, b_onesb], writes=[bpd])
                        rd, brd = rdr.next()
                        k.op("dve", lambda e: e.reciprocal(rd[:, :n], pd[:, :n]), reads=[bpd], writes=[brd])
                        os_, bos = osr.next()
                        k.op("dve", lambda e: e.tensor_tensor(os_[:, :n], po[:, :n], rd[:, :n], ALU.mult),
                             reads=[bpo, brd], writes=[bos])
                        k.dma("sp", YT[b, 2, h * 64:(h + 1) * 64, q0:q0 + n], os_[:, :n], reads=[bos], writes=[C.bYT])


PI = float(np.pi)


def rev_ap(ap2d, lo, hi):
    from concourse.ap import AP
    a = ap2d[:, lo:hi]
    return AP(a.tensor, a.offset + (hi - lo - 1) * a.ap[1][0], [list(a.ap[0]), [-a.ap[1][0], hi - lo]])


MAGIC = 12582912.0
TWO_PI = 2.0 * PI


def sin_reduced(k, out, tmp, in0, th, th2pi, shift, hp_tile, reads, bout, btmp):
    k.op("dve", lambda e: e.tensor_scalar(tmp, in0, th2pi, MAGIC + shift / TWO_PI, ALU.mult, ALU.add),
         reads=reads + [btmp], writes=[btmp])
    k.op("dve", lambda e: e.tensor_scalar(tmp, tmp, -MAGIC, -TWO_PI, ALU.add, ALU.mult), reads=[btmp], writes=[btmp])
    k.op("dve", lambda e: e.scalar_tensor_tensor(tmp, in0, th, tmp, ALU.mult, ALU.add), reads=reads + [btmp], writes=[btmp])
    if shift == 0.0:
        k.op("act", lambda e: e.activation(out, tmp, AF.Sin, scale=0.999999), reads=[btmp, bout], writes=[bout])
    else:
        k.op("act", lambda e: e.activation(out, tmp, AF.Sin, scale=0.999999, bias=hp_tile), reads=[btmp, bout], writes=[bout])


def phase_ssm(P, C, l):
    k = P.k
    PT, YT = C.PT, C.YT
    blocks = [(0, TC)] + [(TC + i * 512, 512) for i in range(4)]
    YG = C.YG
    with P.scope():
        iota = P.sb("iota", [128, 2, TT], F32)
        b_c = Buf("ssmconst")
        k.dma("sp", iota[:], C.ssm_iota, writes=[b_c])
        sv = P.sb("sv", [128, 2, 16, 3], F32)
        k.dma("sp", sv[:], C.ssm_sv[l], writes=[b_c])
        BT = P.sb("BT", [128, 2, 16, 128], BF16)
        k.dma("pool", BT[:], C.ssm_BT[l].rearrange("r s p n -> p r s n"), writes=[b_c])
        CT = P.sb("CT", [128, 2, 2, 16, 128], BF16)
        for j in range(2):
            k.dma("pool", CT[:, j], C.ssm_CT[l, j].rearrange("r s p n -> p r s n"), writes=[b_c])
        dsk = P.sb("dsk", [128, 2, 4], F32)
        k.dma("sp", dsk[:], C.ssm_vec[l], writes=[b_c])
        hpi = P.sb("hpi", [128, 1], F32)
        k.op("dve", lambda e: e.memset(hpi[:], 0.5 * PI * 0.999999), writes=[b_c])
        shp = [128, 2, 16]
        names = "dt rho th th2 sn cs are aim den t1 t2 cre cim ncre".split()
        Tl = {n_: P.sb("d_" + n_, shp, F32) for n_ in names}
        bd = Buf("disc")
        lr, li, ldt = sv[:, :, :, 0], sv[:, :, :, 1], sv[:, :, :, 2]
        A_ = lambda e_, fn: k.op(e_, fn, reads=[b_c, bd], writes=[bd])
        A_("act", lambda e: e.activation(Tl["dt"][:], ldt, AF.Exp))
        A_("dve", lambda e: e.tensor_tensor(Tl["t1"][:], lr, Tl["dt"][:], ALU.mult))
        A_("act", lambda e: e.activation(Tl["rho"][:], Tl["t1"][:], AF.Exp))
        A_("dve", lambda e: e.tensor_tensor(Tl["th"][:], li, Tl["dt"][:], ALU.mult))
        sin_reduced(k, Tl["sn"][:], Tl["t1"][:], Tl["th"][:], 1.0, 1.0 / TWO_PI, 0.0, None, [b_c, bd], bd, bd)
        sin_reduced(k, Tl["cs"][:], Tl["t1"][:], Tl["th"][:], 1.0, 1.0 / TWO_PI, 0.5 * PI, hpi[:, 0:1], [b_c, bd], bd, bd)
        A_("dve", lambda e: e.tensor_scalar(Tl["th2"][:], Tl["th"][:], 1.0 / TWO_PI, None, ALU.mult))
        A_("dve", lambda e: e.tensor_tensor(Tl["are"][:], Tl["rho"][:], Tl["cs"][:], ALU.mult))
        A_("dve", lambda e: e.tensor_tensor(Tl["aim"][:], Tl["rho"][:], Tl["sn"][:], ALU.mult))
        A_("dve", lambda e: e.tensor_scalar(Tl["are"][:], Tl["are"][:], -1.0, None, ALU.add))
        A_("dve", lambda e: e.tensor_tensor(Tl["t1"][:], lr, lr, ALU.mult))
        A_("dve", lambda e: e.tensor_tensor(Tl["t2"][:], li, li, ALU.mult))
        A_("dve", lambda e: e.tensor_tensor(Tl["den"][:], Tl["t1"][:], Tl["t2"][:], ALU.add))
        A_("dve", lambda e: e.reciprocal(Tl["den"][:], Tl["den"][:]))
        A_("dve", lambda e: e.tensor_tensor(Tl["t1"][:], Tl["are"][:], lr, ALU.mult))
        A_("dve", lambda e: e.tensor_tensor(Tl["t2"][:], Tl["aim"][:], li, ALU.mult))
        A_("dve", lambda e: e.tensor_tensor(Tl["cre"][:], Tl["t1"][:], Tl["t2"][:], ALU.add))
        A_("dve", lambda e: e.tensor_tensor(Tl["cre"][:], Tl["cre"][:], Tl["den"][:], ALU.mult))
        A_("dve", lambda e: e.tensor_tensor(Tl["t1"][:], Tl["aim"][:], lr, ALU.mult))
        A_("dve", lambda e: e.tensor_tensor(Tl["t2"][:], Tl["are"][:], li, ALU.mult))
        A_("dve", lambda e: e.tensor_tensor(Tl["cim"][:], Tl["t1"][:], Tl["t2"][:], ALU.subtract))
        A_("dve", lambda e: e.tensor_tensor(Tl["cim"][:], Tl["cim"][:], Tl["den"][:], ALU.mult))
        A_("dve", lambda e: e.tensor_scalar(Tl["ncre"][:], Tl["cre"][:], -1.0, None, ALU.mult))
        big = lambda n_: P.sb(n_, [128, TT], F32)
        bigb = lambda n_: P.sb(n_, [128, TT], BF16)
        CS, SN, ERE, EIM = bigb("CS"), bigb("SN"), bigb("ERE"), bigb("EIM")
        BUR, BUI = bigb("BUR"), bigb("BUI")
        WK = [[big(f"{x}{b}") for x in ("T1", "T2", "ZR", "ZI")] for b in range(NB)]
        bWK = [[Buf(f"{x}{b}") for x in ("t1", "t2", "zr", "zi")] for b in range(NB)]
        T1, T2, ZR, ZI = WK[0]
        b_t1, b_t2, b_zr, b_zi = bWK[0]
        b_tab, b_bu = Buf("tab"), Buf("bu")
        Q = [P.sb(f"Q{i}", [128, TT], BF16) for i in range(4)]
        b_q = [Buf(f"q{i}") for i in range(4)]
        U = [P.sb(f"U{b}", [128, TT], BF16) for b in range(NB)]
        b_u = [Buf(f"u{b}") for b in range(NB)]
        YA = [P.sb(f"YA{b}", [128, TT], F32) for b in range(NB)]
        b_ya = [Buf(f"ya{b}") for b in range(NB)]
        psr = Ring(P, "ssmps", 4, [128, 512], F32, psum=True)
        psy = Ring(P, "ssmpy", 3, [128, 512], F32, psum=True)
        for oc in range(4):
            for b in range(NB):
                k.dma("sp", U[b][:], PT[b, oc * 128:(oc + 1) * 128, :], reads=[C.bPT], writes=[b_u[b]])
                k.op("pool", lambda e: e.memset(YA[b][:], 0.0), writes=[b_ya[b]])
            for j in range(2):
                for s4 in range(4):
                    sc = oc * 4 + s4
                    th = Tl["th"][:, j, sc:sc + 1]
                    th2 = Tl["th2"][:, j, sc:sc + 1]
                    sin_reduced(k, SN[:], T1[:], iota[:, j, :], th, th2, 0.0, None, [b_c, bd], b_tab, b_t1)
                    sin_reduced(k, CS[:], T2[:], iota[:, j, :], th, th2, 0.5 * PI, hpi[:, 0:1], [b_c, bd], b_tab, b_t2)
                    cre, cim, ncre = (Tl[x][:, j, sc:sc + 1] for x in ("cre", "cim", "ncre"))
                    k.op("dve", lambda e: e.tensor_scalar(ERE[:], CS[:], cre, None, ALU.mult), reads=[b_tab, bd], writes=[b_tab])
                    k.op("dve", lambda e: e.scalar_tensor_tensor(ERE[:], SN[:], cim, ERE[:], ALU.mult, ALU.add),
                         reads=[b_tab, bd], writes=[b_tab])
                    k.op("pool", lambda e: e.tensor_scalar(EIM[:], CS[:], cim, None, ALU.mult), reads=[b_tab, bd], writes=[b_tab])
                    k.op("dve", lambda e: e.scalar_tensor_tensor(EIM[:], SN[:], ncre, EIM[:], ALU.mult, ALU.add),
                         reads=[b_tab, bd], writes=[b_tab])
                    rho = Tl["rho"][:, j, sc:sc + 1]
                    for b in range(NB):
                        T1, T2, ZR, ZI = WK[b]
                        b_t1, b_t2, b_zr, b_zi = bWK[b]
                        for (t0, n) in blocks:
                            for ri, dst in ((0, BUR), (1, BUI)):
                                ps, bps = psr.next()
                                k.op("pe", lambda e: e.matmul(ps[:, :n], BT[:, ri, sc, :], U[b][:, t0:t0 + n], start=True, stop=True),
                                     reads=[b_c, b_u[b]], writes=[bps])
                                k.op("act", lambda e: e.activation(dst[:, t0:t0 + n], ps[:, :n], AF.Copy),
                                     reads=[bps], writes=[b_bu])
                        k.op("dve", lambda e: e.tensor_tensor(T1[:], ERE[:], BUR[:], ALU.mult), reads=[b_tab, b_bu], writes=[b_t1])
                        k.op("pool", lambda e: e.tensor_tensor(T2[:], EIM[:], BUI[:], ALU.mult), reads=[b_tab, b_bu], writes=[b_t2])
                        k.op("dve", lambda e: e.tensor_tensor(ZR[:], T1[:], T2[:], ALU.subtract), reads=[b_t1, b_t2], writes=[b_zr])
                        k.op("pool", lambda e: e.tensor_tensor(T1[:], ERE[:], BUI[:], ALU.mult), reads=[b_tab, b_bu, b_zr], writes=[b_t1])
                        k.op("dve", lambda e: e.tensor_tensor(T2[:], EIM[:], BUR[:], ALU.mult), reads=[b_tab, b_bu, b_zr], writes=[b_t2])
                        k.op("pool", lambda e: e.tensor_tensor(ZI[:], T1[:], T2[:], ALU.add), reads=[b_t1, b_t2], writes=[b_zi])
                        for Z, bz, eng in ((ZR, b_zr, "dve"), (ZI, b_zi, "dve")):
                            if j == 0:
                                k.op(eng, lambda e: e.tensor_tensor_scan(Z[:], rho.to_broadcast([128, TT]), Z[:], 0.0, ALU.mult, ALU.add),
                                     reads=[bz, bd], writes=[bz])
                            else:
                                r0 = rev_ap(Z[:], 0, TC)
                                k.op(eng, lambda e: e.tensor_tensor_scan(r0, rho.to_broadcast([128, TC]), r0, 0.0, ALU.mult, ALU.add),
                                     reads=[bz, bd], writes=[bz])
                                r1 = rev_ap(Z[:], TC, TT)
                                k.op(eng, lambda e: e.tensor_tensor_scan(r1, rho.to_broadcast([128, TL]), r1, Z[:, 0:1], ALU.mult, ALU.add),
                                     reads=[bz, bd], writes=[bz])
                        k.op("pool", lambda e: e.tensor_tensor(Q[0][:], CS[:], ZR[:], ALU.mult), reads=[b_tab, b_zr], writes=[b_q[0]])
                        for qi, (tabl, Z, bz) in ((1, (SN, ZI, b_zi)), (2, (SN, ZR, b_zr)), (3, (CS, ZI, b_zi))):
                            k.op("dve", lambda e: e.scalar_tensor_tensor(Q[qi][:], tabl[:], -1.0, Z[:], ALU.mult, ALU.mult),
                                 reads=[b_tab, bz], writes=[b_q[qi]])
                        lhs = (CT[:, j, 0, sc, :], CT[:, j, 0, sc, :], CT[:, j, 1, sc, :], CT[:, j, 1, sc, :])
                        for (t0, n) in blocks:
                            ps, bps = psy.next()
                            for qi in range(4):
                                k.op("pe", lambda e: e.matmul(ps[:, :n], lhs[qi], Q[qi][:, t0:t0 + n], start=(qi == 0), stop=(qi == 3)),
                                     reads=[b_c, b_q[qi]], writes=[bps])
                            k.op("dve", lambda e: e.tensor_tensor(YA[b][:, t0:t0 + n], YA[b][:, t0:t0 + n], ps[:, :n], ALU.add),
                                 reads=[bps, b_ya[b]], writes=[b_ya[b]])
            for b in range(NB):
                T1, T2, ZR, ZI = WK[b]
                b_t1, b_t2, b_zr, b_zi = bWK[b]
                k.op("dve", lambda e: e.scalar_tensor_tensor(T1[:], U[b][:], dsk[:, 0, oc:oc + 1], YA[b][:], ALU.mult, ALU.add),
                     reads=[b_u[b], b_ya[b], b_c, b_t1], writes=[b_t1])
                k.op("act", lambda e: e.activation(T2[:], T1[:], AF.Square), reads=[b_t1, b_t2], writes=[b_t2])
                k.op("dve", lambda e: e.tensor_scalar(T2[:], T2[:], 0.044715, 1.0, ALU.mult, ALU.add), reads=[b_t2], writes=[b_t2])
                k.op("pool", lambda e: e.tensor_tensor(T2[:], T2[:], T1[:], ALU.mult), reads=[b_t1, b_t2], writes=[b_t2])
                k.op("act", lambda e: e.activation(T2[:], T2[:], AF.Sigmoid, scale=1.5957691216057308), reads=[b_t2], writes=[b_t2])
                k.op("dve", lambda e: e.tensor_tensor(Q[b][:], T1[:], T2[:], ALU.mult), reads=[b_t1, b_t2, b_q[b]], writes=[b_q[b]])
                k.dma("sp", YG[b, oc * 128:(oc + 1) * 128, :], Q[b][:], reads=[b_q[b]], writes=[C.bYG])
    with P.scope():
        gw = P.sb("gluw", [128, 4, 512], BF16)
        b_gw = Buf("gluw")
        k.dma("pool", gw[:], C.ssm_glu_w[l].rearrange("(kc p) n -> p kc n", p=128), writes=[b_gw])
        dsk = P.sb("dsk2", [128, 2, 4], F32)
        k.dma("sp", dsk[:], C.ssm_vec[l], writes=[b_gw])
        ygr = Ring(P, "yg", 2, [128, 4, 512], BF16)
        psr = Ring(P, "glups", 4, [128, 512], F32, psum=True)
        sgr = Ring(P, "sg", 3, [128, 512], F32)
        str_ = Ring(P, "gst", 3, [128, 512], BF16)
        for b in range(NB):
            for (t0, n) in blocks:
                yg, byg = ygr.next()
                k.dma("sp", yg[:, :, :n], YG[b, :, t0:t0 + n].rearrange("(c p) n -> p c n", p=128), reads=[C.bYG], writes=[byg])
                for oc in range(4):
                    ps, bps = psr.next()
                    for kc in range(4):
                        k.op("pe", lambda e: e.matmul(ps[:, :n], gw[:, kc, oc * 128:(oc + 1) * 128], yg[:, kc, :n],
                                                      start=(kc == 0), stop=(kc == 3)), reads=[b_gw, byg], writes=[bps])
                    sg, bsg = sgr.next()
                    k.op("act", lambda e: e.activation(sg[:, :n], ps[:, :n], AF.Sigmoid, bias=dsk[:, 1, oc:oc + 1]),
                         reads=[bps, b_gw], writes=[bsg])
                    st, bst = str_.next()
                    k.op("dve", lambda e: e.tensor_tensor(st[:, :n], yg[:, oc, :n], sg[:, :n], ALU.mult), reads=[byg, bsg], writes=[bst])
                    k.dma("sp", YT[b, 0, oc * 128:(oc + 1) * 128, t0:t0 + n], st[:, :n], reads=[bst], writes=[C.bYT])


def phase_merge(P, C, l):
    k = P.k
    PT, YT, XT = C.PT, C.YT, C.XT
    src_x = C.xt0 if l == 0 else XT
    mod, gs = C.mod, C.gs
    blocks = [(i * 256, 256) for i in range(9)]
    with P.scope():
        wb = P.sb("wb", [128, 3, 4, D], BF16)
        b_w = Buf("mw")
        for j in range(3):
            k.dma("pool", wb[:, j], C.w_branch[l, j].rearrange("(kc p) n -> p kc n", p=128), writes=[b_w])
        wo = P.sb("wo", [128, KC, D], BF16)
        k.dma("pool", wo[:], C.w_out[l].rearrange("(kc p) n -> p kc n", p=128), writes=[b_w])
        rw = P.sb("rw", [128, KC, 16], F32)
        k.dma("sp", rw[:], C.router_w[l].rearrange("(kc p) n -> p kc n", p=128), writes=[b_w])
        ident = P.sb("ident", [128, 128], BF16)
        k.dma("pool", ident[:], C.ident, writes=[b_w])
        y3r = Ring(P, "y3", 2, [128, 3, 4, 256], BF16)
        gr = Ring(P, "g", 2, [128, 24, 256], BF16)
        xr = Ring(P, "mx", 2, [128, KC, 256], F32)
        mTr = Ring(P, "mT", 1, [128, KC, 256], BF16)
        mr = Ring(P, "m", 6, [128, 256], F32)
        x1r = Ring(P, "x1", 1, [128, KC, 256], F32)
        sqr = Ring(P, "msq", 1, [128, KC, 256], F32)
        h2fr = Ring(P, "h2f", 1, [128, KC, 256], F32)
        h2br = Ring(P, "h2b", 1, [128, KC, 256], BF16)
        tsr = Ring(P, "tst", 2, [128, D], BF16)
        rsr = Ring(P, "mrs", 2, [128, 256], F32)
        lgr = Ring(P, "lgs", 2, [16, 256], F32)
        psb = Ring(P, "psb", 3, [128, 256], F32, psum=True)
        pso = Ring(P, "pso", 2, [128, 256], F32, psum=True)
        pss = Ring(P, "pss", 2, [128, 256], F32, psum=True)
        pst = Ring(P, "pst", 1, [128, D], BF16, psum=True)
        for b in range(NB):
            for (t0, n) in blocks:
                j = 2 if t0 < TC else b
                y3, by3 = y3r.next()
                k.dma("sp", y3[:], YT[b, :, :, t0:t0 + n].rearrange("j (c p) n -> p j c n", p=128), reads=[C.bYT], writes=[by3])
                g, bg = gr.next()
                k.dma("act", g[:], PT[b, 25 * 128:49 * 128, t0:t0 + n].rearrange("(c p) n -> p c n", p=128), reads=[C.bPT], writes=[bg])
                x, bx = xr.next()
                k.dma("sp", x[:], src_x[b][:, t0:t0 + n].rearrange("(kc p) n -> p kc n", p=128), reads=[C.bXT[b]], writes=[bx])
                mT, bmT = mTr.next()
                for dc in range(KC):
                    ms = []
                    for jj in range(3):
                        ps, bps = psb.next()
                        for kc in range(4):
                            k.op("pe", lambda e: e.matmul(ps[:], wb[:, jj, kc, dc * 128:(dc + 1) * 128], y3[:, jj, kc, :],
                                                          start=(kc == 0), stop=(kc == 3)), reads=[b_w, by3], writes=[bps])
                        m, bm = mr.next()
                        k.op("dve", lambda e: e.tensor_tensor(m[:], ps[:], g[:, jj * 8 + dc, :], ALU.mult), reads=[bps, bg], writes=[bm])
                        ms.append((m, bm))
                    k.op("pool", lambda e: e.tensor_tensor(ms[0][0][:], ms[0][0][:], ms[1][0][:], ALU.add),
                         reads=[ms[0][1], ms[1][1]], writes=[ms[0][1]])
                    k.op("pool", lambda e: e.tensor_tensor(mT[:, dc, :], ms[0][0][:], ms[2][0][:], ALU.add),
                         reads=[ms[0][1], ms[2][1]], writes=[bmT])
                x1, bx1 = x1r.next()
                for dc in range(KC):
                    ps, bps = pso.next()
                    for kc in range(KC):
                        k.op("pe", lambda e: e.matmul(ps[:], wo[:, kc, dc * 128:(dc + 1) * 128], mT[:, kc, :],
                                                      start=(kc == 0), stop=(kc == KC - 1)), reads=[b_w, bmT], writes=[bps])
                    k.op("dve", lambda e: e.scalar_tensor_tensor(x1[:, dc, :], ps[:], mod[:, 2 * 8 + dc, j:j + 1], x[:, dc, :],
                                                                 ALU.mult, ALU.add), reads=[bps, bx, C.b_mod], writes=[bx1])
                k.dma("sp", XT[b][:, t0:t0 + n].rearrange("(kc p) n -> p kc n", p=128), x1[:], reads=[bx1], writes=[C.bXT[b]])
                sq, bsq = sqr.next()
                k.op("act", lambda e: e.activation(sq[:], x1[:], AF.Square), reads=[bx1], writes=[bsq])
                ss, bss = pss.next()
                for kc in range(KC):
                    k.op("pe", lambda e: e.matmul(ss[:], C.ones[:], sq[:, kc, :], start=(kc == 0), stop=(kc == KC - 1)),
                         reads=[bsq, C.b_ones], writes=[bss])
                rs, brs = rsr.next()
                k.op("act", lambda e: e.activation(rs[:], ss[:], AF.Sqrt, bias=C.epsc[:, 0:1], scale=1.0 / D), reads=[bss], writes=[brs])
                k.op("dve", lambda e: e.reciprocal(rs[:], rs[:]), reads=[brs], writes=[brs])
                k.op("dve", lambda e: e.tensor_tensor(sq[:], x1[:], rs[:].unsqueeze(1).to_broadcast([128, KC, n]), ALU.mult),
                     reads=[bx1, brs, bsq], writes=[bsq])
                h2f, bh2f = h2fr.next()
                for kc in range(KC):
                    k.op("act", lambda e: e.activation(h2f[:, kc, :], sq[:, kc, :], AF.Identity,
                                                       bias=mod[:, 3 * 8 + kc, j:j + 1], scale=gs[:, 1, kc, j:j + 1]),
                         reads=[bsq, C.b_mod, C.b_gs], writes=[bh2f])
                h2b, bh2b = h2br.next()
                k.op("pool", lambda e: e.tensor_copy(h2b[:], h2f[:]), reads=[bh2f], writes=[bh2b])
                lp, blp = pss.next()
                for kc in range(KC):
                    k.op("pe", lambda e: e.matmul(lp[:16, :], rw[:, kc, :], h2f[:, kc, :], start=(kc == 0), stop=(kc == KC - 1)),
                         reads=[b_w, bh2f], writes=[blp])
                lg, blg = lgr.next()
                k.op("act", lambda e: e.activation(lg[:], lp[:16, :], AF.Copy), reads=[blp], writes=[blg])
                k.dma("sp", C.LG[b, :, t0:t0 + n], lg[:], reads=[blg], writes=[C.bLG])
                for tt in range(n // 128):
                    pt, bpt = pst.next()
                    for kc in range(KC):
                        k.op("pe", lambda e: e.transpose(pt[:, kc * 128:(kc + 1) * 128], h2b[:, kc, tt * 128:(tt + 1) * 128], ident[:]),
                             reads=[bh2b, b_w], writes=[bpt])
                    ts, bts = tsr.next()
                    k.op("act", lambda e: e.activation(ts[:], pt[:], AF.Copy), reads=[bpt], writes=[bts])
                    k.dma("sp", C.H2[b, t0 + tt * 128:t0 + (tt + 1) * 128, :], ts[:], reads=[bts], writes=[C.bH2])


NE = 16
FF = 1536
CAPL = 256
CAPC = 32
NJ = 2 * CAPL + 2 * CAPC


def phase_moe(P, C, l, last):
    k = P.k
    XT = C.XT
    mod = C.mod
    with P.scope():
        cst = P.sb("moecst", [128, 3, 256], F32)
        b_c = Buf("moecst")
        k.dma("sp", cst[:], C.moe_cst, writes=[b_c])
        A = P.sb("rA", [32, TT], F32)
        AFF = P.sb("rAFF", [32, TT], F32)
        W = P.sb("rW", [32, TT], F32)
        MG = P.sb("rMG", [32, TT], F32)
        MK = P.sb("rMK", [32, TT], F32)
        PS_ = P.sb("rPOS", [32, TT], F32)
        mx = P.sb("rmx", [32, 8], F32)
        bA, bAFF, bW, bMG, bMK, bPOS, bmx = [Buf(x) for x in "A AFF W MG MK POS mx".split()]
        for b in range(NB):
            k.dma("sp", A[b * 16:(b + 1) * 16, :], C.LG[b], reads=[C.bLG], writes=[bA])
        k.op("act", lambda e: e.activation(A[:], A[:], AF.Exp), reads=[bA], writes=[bA])
        psr = Ring(P, "rps", 2, [128, 512], F32, psum=True)
        for t0 in range(0, TT, 512):
            n = min(512, TT - t0)
            ps, bps = psr.next()
            k.op("pe", lambda e: e.matmul(ps[:32, :n], cst[:32, 1, :32], A[:, t0:t0 + n], start=True, stop=True),
                 reads=[bA, b_c], writes=[bps])
            k.op("dve", lambda e: e.reciprocal(W[:, t0:t0 + n], ps[:32, :n]), reads=[bps], writes=[bW])
        k.op("dve", lambda e: e.tensor_tensor(AFF[:], A[:], W[:], ALU.mult), reads=[bA, bW], writes=[bAFF])
        k.op("dve", lambda e: e.tensor_copy(W[:], AFF[:]), reads=[bAFF, bW], writes=[bW])
        for (lo, hi, cap) in ((0, TC, CAPC), (TC, TT, CAPL)):
            for it in range(cap // 8):
                k.op("dve", lambda e: e.max(out=mx[:], in_=W[:, lo:hi]), reads=[bW, bmx], writes=[bmx])
                k.op("dve", lambda e: e.match_replace(out=W[:, lo:hi], in_to_replace=mx[:], in_values=W[:, lo:hi], imm_value=0.0),
                     reads=[bmx, bW], writes=[bW])
        k.op("dve", lambda e: e.tensor_tensor(MG[:], AFF[:], W[:], ALU.subtract), reads=[bAFF, bW], writes=[bMG])
        k.op("dve", lambda e: e.tensor_single_scalar(MK[:], MG[:], 0.0, ALU.is_gt), reads=[bMG], writes=[bMK])
        for (lo, hi) in ((0, TC), (TC, TT)):
            k.op("dve", lambda e: e.tensor_tensor_scan(PS_[:, lo:hi], cst[:32, 0, 0:1].to_broadcast([32, hi - lo]), MK[:, lo:hi],
                                                       0.0, ALU.mult, ALU.add), reads=[bMK, b_c], writes=[bPOS])
        for b in range(NB):
            k.dma("sp", C.POSD[b], PS_[b * 16:(b + 1) * 16, :], reads=[bPOS], writes=[C.bPOSD])
            k.dma("sp", C.MGD[b], MG[b * 16:(b + 1) * 16, :], reads=[bMG], writes=[C.bPOSD])
        posT = P.sb("posT", [128, 18, 32], F32)
        mkT = P.sb("mkT", [128, 18, 32], F32)
        b_pT = Buf("posT")
        for tt in range(18):
            ps, bps = psr.next()
            k.op("pe", lambda e: e.transpose(ps[:, 0:32], PS_[:, tt * 128:(tt + 1) * 128], cst[:32, 2, :32]),
                 reads=[bPOS, b_c], writes=[bps])
            k.op("pe", lambda e: e.transpose(ps[:, 32:64], MK[:, tt * 128:(tt + 1) * 128], cst[:32, 2, :32]),
                 reads=[bMK, b_c], writes=[bps])
            k.op("act", lambda e: e.activation(posT[:, tt, :], ps[:, 0:32], AF.Copy), reads=[bps], writes=[b_pT])
            k.op("act", lambda e: e.activation(mkT[:, tt, :], ps[:, 32:64], AF.Copy), reads=[bps], writes=[b_pT])
        H2s = P.sb("H2s", [128, 18, D], BF16)
        bH = Buf("H2s")
        selr = Ring(P, "sel", 2, [128, 18, 256], BF16)
        gps = Ring(P, "gps", 3, [128, 256], F32, psum=True)
        gpc = Ring(P, "gpc", 2, [128, 32], F32, psum=True)
        xsr = Ring(P, "xs", 2, [128, KC, CAPL + CAPC], BF16)
        for b in range(NB):
            k.dma("sp", H2s[:], C.H2[b].rearrange("(tt p) d -> p tt d", p=128), reads=[C.bH2], writes=[bH])
            for ex in range(NE):
                col = b * 16 + ex
                sel, bsel = selr.next()
                for tt in range(18):
                    ncap = CAPC if tt < 2 else CAPL
                    k.op("dve" if tt % 2 == 0 else "pool", lambda e: e.tensor_scalar(
                        sel[:, tt, :ncap], cst[:, 0, :ncap], posT[:, tt, col:col + 1], mkT[:, tt, col:col + 1],
                        ALU.is_equal, ALU.mult), reads=[b_c, b_pT], writes=[bsel])
                xs, bxs = xsr.next()
                for kc in range(KC):
                    ps, bps = gps.next()
                    for tt in range(16):
                        k.op("pe", lambda e: e.matmul(ps[:], H2s[:, 2 + tt, kc * 128:(kc + 1) * 128], sel[:, 2 + tt, :],
                                                      start=(tt == 0), stop=(tt == 15)), reads=[bH, bsel], writes=[bps])
                    k.op("act" if kc % 2 == 0 else "dve",
                         (lambda e: e.activation(xs[:, kc, :CAPL], ps[:], AF.Copy)) if kc % 2 == 0 else
                         (lambda e: e.tensor_copy(xs[:, kc, :CAPL], ps[:])), reads=[bps], writes=[bxs])
                    pc, bpc = gpc.next()
                    for tt in range(2):
                        k.op("pe", lambda e: e.matmul(pc[:], H2s[:, tt, kc * 128:(kc + 1) * 128], sel[:, tt, :CAPC],
                                                      start=(tt == 0), stop=(tt == 1)), reads=[bH, bsel], writes=[bpc])
                    k.op("act", lambda e: e.activation(xs[:, kc, CAPL:], pc[:], AF.Copy), reads=[bpc], writes=[bxs])
                k.dma("sp", C.XS[ex, :, b * CAPL:(b + 1) * CAPL].rearrange("(kc p) j -> p kc j", p=128), xs[:, :, :CAPL],
                      reads=[bxs], writes=[C.bXS])
                k.dma("sp", C.XS[ex, :, 2 * CAPL + b * CAPC:2 * CAPL + (b + 1) * CAPC].rearrange("(kc p) j -> p kc j", p=128),
                      xs[:, :, CAPL:], reads=[bxs], writes=[C.bXS])
    if "stop_moeA" in P.debug:
        return
    with P.scope():
        w1r = Ring(P, "w1", 2, [128, KC, FF], BF16)
        w3r = Ring(P, "w3", 2, [128, KC, FF], BF16)
        w2r = Ring(P, "w2", 2, [128, 12, D], BF16)
        xsr = Ring(P, "xsb", 2, [128, KC, NJ], BF16)
        hr = Ring(P, "hid", 2, [128, 12, NJ], BF16)
        slr = Ring(P, "silu", 3, [128, 288], F32)
        yer = Ring(P, "ye", 3, [128, D], BF16)
        ps1 = Ring(P, "ps1", 2, [128, 288], F32, psum=True)
        ps3 = Ring(P, "ps3", 2, [128, 288], F32, psum=True)
        psy = Ring(P, "psy", 3, [128, 512], F32, psum=True)
        for ex in range(NE):
            w1, bw1 = w1r.next()
            w3, bw3 = w3r.next()
            w2, bw2 = w2r.next()
            k.dma("pool", w1[:], C.moe_w1[l, ex].rearrange("(kc p) f -> p kc f", p=128), writes=[bw1])
            k.dma("pool", w3[:], C.moe_w3[l, ex].rearrange("(kc p) f -> p kc f", p=128), writes=[bw3])
            k.dma("pool", w2[:], C.moe_w2[l, ex].rearrange("(fc p) d -> p fc d", p=128), writes=[bw2])
            xs, bxs = xsr.next()
            k.dma("sp", xs[:], C.XS[ex].rearrange("(kc p) j -> p kc j", p=128), reads=[C.bXS], writes=[bxs])
            hid, bh = hr.next()
            for fc in range(12):
                for half in range(2):
                    c0 = half * 288
                    p1, bp1 = ps1.next()
                    p3, bp3 = ps3.next()
                    for kc in range(KC):
                        k.op("pe", lambda e: e.matmul(p1[:], w1[:, kc, fc * 128:(fc + 1) * 128], xs[:, kc, c0:c0 + 288],
                                                      start=(kc == 0), stop=(kc == KC - 1)), reads=[bw1, bxs], writes=[bp1])
                    for kc in range(KC):
                        k.op("pe", lambda e: e.matmul(p3[:], w3[:, kc, fc * 128:(fc + 1) * 128], xs[:, kc, c0:c0 + 288],
                                                      start=(kc == 0), stop=(kc == KC - 1)), reads=[bw3, bxs], writes=[bp3])
                    sl, bsl = slr.next()
                    k.op("act", lambda e: e.activation(sl[:], p1[:], AF.Silu), reads=[bp1], writes=[bsl])
                    k.op("dve", lambda e: e.tensor_tensor(hid[:, fc, c0:c0 + 288], sl[:], p3[:], ALU.mult),
                         reads=[bsl, bp3], writes=[bh])
            for jt in range(5):
                nj = 128 if jt < 4 else NJ - 512
                ye, bye = yer.next()
                for dh in range(2):
                    py, bpy = psy.next()
                    for fc in range(12):
                        k.op("pe", lambda e: e.matmul(py[:nj, :], hid[:, fc, jt * 128:jt * 128 + nj], w2[:, fc, dh * 512:(dh + 1) * 512],
                                                      start=(fc == 0), stop=(fc == 11)), reads=[bh, bw2], writes=[bpy])
                    k.op("act" if dh == 0 else "dve",
                         (lambda e: e.activation(ye[:nj, dh * 512:(dh + 1) * 512], py[:nj, :], AF.Copy)) if dh == 0 else
                         (lambda e: e.tensor_copy(ye[:nj, dh * 512:(dh + 1) * 512], py[:nj, :])), reads=[bpy], writes=[bye])
                k.dma("sp", C.YE[ex, jt * 128:jt * 128 + nj, :], ye[:nj, :], reads=[bye], writes=[C.bYE])
    if "stop_moeB" in P.debug:
        return
    with P.scope():
        jcol = P.sb("jcol", [128, 2], F32)
        b_c = Buf("jcol")
        k.dma("sp", jcol[:], C.moe_jcol, writes=[b_c])
        yel = P.sb("yel", [128, NE, 2, D], BF16)
        yec = P.sb("yec", [32, NE, D], BF16)
        b_ye = Buf("yel")
        posb = P.sb("posb", [128, NE, 512], F32)
        mgb = P.sb("mgb", [128, NE, 512], F32)
        b_pb = Buf("posb")
        sgr = Ring(P, "selg", 4, [128, 512], BF16)
        x1r = Ring(P, "cx1", 2, [128, KC, 512], F32)
        pso = Ring(P, "cps", 8, [128, 512], F32, psum=True)
        for b in range(NB):
            for jt in range(2):
                k.dma("sp", yel[:, :, jt, :], C.YE[:, b * CAPL + jt * 128:b * CAPL + (jt + 1) * 128, :].rearrange("e p d -> p e d"),
                      reads=[C.bYE], writes=[b_ye])
            k.dma("sp", yec[:], C.YE[:, 2 * CAPL + b * CAPC:2 * CAPL + (b + 1) * CAPC, :].rearrange("e p d -> p e d"),
                  reads=[C.bYE], writes=[b_ye])
            for (t0, n) in [(0, TC)] + [(TC + i * 512, 512) for i in range(4)]:
                isctx = t0 < TC
                j = 2 if isctx else b
                k.dma("sp", posb[:, :, :n], C.POSD[b, :, t0:t0 + n].partition_broadcast(128), reads=[C.bPOSD], writes=[b_pb])
                k.dma("act", mgb[:, :, :n], C.MGD[b, :, t0:t0 + n].partition_broadcast(128), reads=[C.bPOSD], writes=[b_pb])
                x1, bx1 = x1r.next()
                k.dma("sp", x1[:, :, :n], XT[b][:, t0:t0 + n].rearrange("(kc p) n -> p kc n", p=128), reads=[C.bXT[b]], writes=[bx1])
                acc = [pso.next() for _ in range(KC)]
                njt = 1 if isctx else 2
                for ex in range(NE):
                    for jt in range(njt):
                        sg, bsg = sgr.next()
                        k.op("dve", lambda e: e.scalar_tensor_tensor(sg[:, :n], posb[:, ex, :n], jcol[:, jt:jt + 1], mgb[:, ex, :n],
                                                                     ALU.is_equal, ALU.mult), reads=[b_pb, b_c], writes=[bsg])
                        first = (ex == 0 and jt == 0)
                        lastm = (ex == NE - 1 and jt == njt - 1)
                        for dc in range(KC):
                            pa, bpa = acc[dc]
                            if isctx:
                                k.op("pe", lambda e: e.matmul(pa[:, :n], yec[:, ex, dc * 128:(dc + 1) * 128], sg[:32, :n],
                                                              start=first, stop=lastm), reads=[b_ye, bsg], writes=[bpa])
                            else:
                                k.op("pe", lambda e: e.matmul(pa[:, :n], yel[:, ex, jt, dc * 128:(dc + 1) * 128], sg[:, :n],
                                                              start=first, stop=lastm), reads=[b_ye, bsg], writes=[bpa])
                for dc in range(KC):
                    pa, bpa = acc[dc]
                    k.op("dve", lambda e: e.scalar_tensor_tensor(x1[:, dc, :n], pa[:, :n], mod[:, 5 * 8 + dc, j:j + 1], x1[:, dc, :n],
                                                                 ALU.mult, ALU.add), reads=[bpa, bx1, C.b_mod], writes=[bx1])
                if last:
                    if not isctx:
                        k.dma("sp", C.OUT[b][:, t0 - TC:t0 - TC + n].rearrange("(kc p) n -> p kc n", p=128), x1[:, :, :n],
                              reads=[bx1], writes=[C.bOUT])
                else:
                    k.dma("sp", XT[b][:, t0:t0 + n].rearrange("(kc p) n -> p kc n", p=128), x1[:, :, :n],
                          reads=[bx1], writes=[C.bXT[b]])


LAM = float(np.exp(-0.5))
GN_EPS = 64e-5
NCK = TT // 128


def phase_rwkv_prep(P, C, l):
    k = P.k
    PT = C.PT
    blocks = [(0, TC)] + [(TC + i * 512, 512) for i in range(4)]
    with P.scope():
        vec = P.sb("rwvec", [128, 51], F32)
        b_c = Buf("rwc")
        k.dma("sp", vec[:], C.rw_vec[l], writes=[b_c])
        MU, W0, A0, KK_, KA, RK = 0, 15, 23, 31, 35, 39
        der = P.sb("rwder", [128, 15 + 15 + 4], F32)
        k.op("dve", lambda e: e.tensor_scalar(der[:, 0:15], vec[:, MU:MU + 15], -1.0, 1.0, ALU.mult, ALU.add), reads=[b_c], writes=[b_c])
        k.op("dve", lambda e: e.tensor_scalar(der[:, 15:30], vec[:, MU:MU + 15], 0.5, None, ALU.mult), reads=[b_c], writes=[b_c])
        k.op("dve", lambda e: e.tensor_scalar(der[:, 30:34], vec[:, KA:KA + 4], -1.0, 1.0, ALU.mult, ALU.add), reads=[b_c], writes=[b_c])
        tiny = P.sb("rwtiny", [128, 1], F32)
        k.op("dve", lambda e: e.memset(tiny[:], 1e-12), writes=[b_c])
        w2 = P.sb("rw_w2", [128, 512], BF16)
        a2 = P.sb("rw_a2", [128, 512], BF16)
        g2 = P.sb("rw_g2", [128, 512], BF16)
        k.dma("pool", w2[:], C.rwkv_w2[l].rearrange("j l c -> (j l) c"), writes=[b_c])
        k.dma("pool", a2[:], C.rwkv_a2[l].rearrange("j l c -> (j l) c"), writes=[b_c])
        k.dma("pool", g2[:], C.rwkv_g2[l], writes=[b_c])
        bd = P.sb("rw_bd", [128, 128], BF16)
        k.dma("pool", bd[:], C.rw_bd, writes=[b_c])
        rst = P.sb("rw_rst", [128, TT + 1], F32)
        k.dma("sp", rst[:], C.rw_rst, writes=[b_c])
        big = lambda n_, dt=F32: P.sb(n_, [128, TT], dt)
        X = big("rX", BF16)
        S = big("rS")
        KP = [big(f"rKP{i}", BF16) for i in range(4)]
        KAP = [big(f"rKAP{i}", BF16) for i in range(4)]
        KS = big("rKSUM")
        bKS = Buf("KS")
        RP = [big(f"rRP{i}", BF16) for i in range(4)]
        VP = [big(f"rVP{i}", BF16) for i in range(4)]
        TW, PA, SG = big("rTW", BF16), big("rPA", BF16), big("rSG", BF16)
        T1, T2, T3, T4 = big("rT1"), big("rT2"), big("rT3"), big("rT4")
        O = [big(f"rO{i}", BF16) for i in range(4)]
        bX, bS, bT1, bT2, bT3, bT4 = [Buf(x) for x in "X S T1 T2 T3 T4".split()]
        bKP = [Buf(f"KP{i}") for i in range(4)]
        bKAP = [Buf(f"KAP{i}") for i in range(4)]
        bRP = [Buf(f"RP{i}") for i in range(4)]
        bVP = [Buf(f"VP{i}") for i in range(4)]
        bTW, bPA, bSG = Buf("TW"), Buf("PA"), Buf("SG")
        bO = [Buf(f"O{i}") for i in range(4)]
        psr = Ring(P, "rwps", 4, [128, 512], F32, psum=True)
        stg = Ring(P, "rwstg", 3, [128, 512], BF16)
        plr = Ring(P, "rwpl", 2, [128, NCK], F32)
        for b in range(NB):
            for ci in range(15):
                k.dma("sp", X[:], PT[b, (4 + ci) * 128:(5 + ci) * 128, :], reads=[C.bPT], writes=[bX])
                k.op("pool", lambda e: e.tensor_tensor(S[:, 1:TT - 1], X[:, 0:TT - 2], X[:, 2:TT], ALU.add), reads=[bX], writes=[bS])
                for (d_, s_) in ((0, 1), (TC - 1, TC - 2), (TC, TC + 1), (TT - 1, TT - 2)):
                    k.op("pool", lambda e: e.tensor_copy(S[:, d_:d_ + 1], X[:, s_:s_ + 1]), reads=[bX, bS], writes=[bS])
                k.op("dve", lambda e: e.tensor_scalar(T1[:], X[:], der[:, ci:ci + 1], None, ALU.mult), reads=[bX, b_c], writes=[bT1])
                if ci < 4:
                    dst, bdst = RP[ci], bRP[ci]
                elif ci < 8:
                    dst, bdst = KP[ci - 4], bKP[ci - 4]
                elif ci < 12:
                    dst, bdst = VP[ci - 8], bVP[ci - 8]
                else:
                    dst, bdst = T2, bT2
                k.op("dve", lambda e: e.scalar_tensor_tensor(dst[:], S[:], der[:, 15 + ci:16 + ci], T1[:], ALU.mult, ALU.add),
                     reads=[bS, bT1, b_c], writes=[bdst])
                if ci == 12:
                    k.op("act", lambda e: e.activation(TW[:], T2[:], AF.Tanh), reads=[bT2], writes=[bTW])
                elif ci == 13:
                    k.op("act", lambda e: e.activation(PA[:], T2[:], AF.Copy), reads=[bT2], writes=[bPA])
                elif ci == 14:
                    k.op("act", lambda e: e.activation(SG[:], T2[:], AF.Sigmoid), reads=[bT2], writes=[bSG])
            for cc in range(4):
                k.dma("sp", C.RWV[b, cc * 128:(cc + 1) * 128, :], VP[cc][:], reads=[bVP[cc]], writes=[C.bRW])
            for cc in range(4):
                for (t0, n) in blocks:
                    ps, bps = psr.next()
                    k.op("pe", lambda e: e.matmul(ps[:, :n], g2[:, cc * 128:(cc + 1) * 128], SG[:, t0:t0 + n], start=True, stop=True),
                         reads=[bSG, b_c], writes=[bps])
                    st, bst = stg.next()
                    k.op("act", lambda e: e.activation(st[:, :n], ps[:, :n], AF.Copy), reads=[bps], writes=[bst])
                    k.dma("sp", C.RWG[b, cc * 128:(cc + 1) * 128, t0:t0 + n], st[:, :n], reads=[bst], writes=[C.bRW])
            for cc in range(4):
                k.op("dve", lambda e: e.tensor_scalar(T1[:], KP[cc][:], vec[:, KK_ + cc:KK_ + cc + 1], None, ALU.mult),
                     reads=[bKP[cc], b_c, bT1], writes=[bT1])
                k.op("act", lambda e: e.activation(O[0][:], T1[:], AF.Square), reads=[bT1, bO[0]], writes=[bO[0]])
                for (t0, n) in blocks:
                    ps, bps = psr.next()
                    k.op("pe", lambda e: e.matmul(ps[:, :n], bd[:], O[0][:, t0:t0 + n], start=True, stop=True),
                         reads=[bO[0], b_c], writes=[bps])
                    k.op("act", lambda e: e.activation(T2[:, t0:t0 + n], ps[:, :n], AF.Sqrt, bias=tiny[:, 0:1]), reads=[bps, bT2, b_c], writes=[bT2])
                k.op("dve", lambda e: e.reciprocal(T2[:], T2[:]), reads=[bT2], writes=[bT2])
                k.op("dve", lambda e: e.tensor_tensor(KAP[cc][:], T1[:], T2[:], ALU.mult), reads=[bT1, bT2], writes=[bKAP[cc]])
            for cc in range(4):
                for j in range(2):
                    jr = slice(j * 64, (j + 1) * 64)
                    for (t0, n) in blocks:
                        ps, bps = psr.next()
                        k.op("pe", lambda e: e.matmul(ps[:, :n], w2[jr, cc * 128:(cc + 1) * 128], TW[jr, t0:t0 + n], start=True, stop=True),
                             reads=[bTW, b_c], writes=[bps])
                        k.op("act", lambda e: e.activation(T1[:, t0:t0 + n], ps[:, :n], AF.Sigmoid, bias=vec[:, W0 + j * 4 + cc:W0 + j * 4 + cc + 1]),
                             reads=[bps, b_c, bT1], writes=[bT1])
                        ps2, bps2 = psr.next()
                        k.op("pe", lambda e: e.matmul(ps2[:, :n], a2[jr, cc * 128:(cc + 1) * 128], PA[jr, t0:t0 + n], start=True, stop=True),
                             reads=[bPA, b_c], writes=[bps2])
                        k.op("act", lambda e: e.activation(T2[:, t0:t0 + n], ps2[:, :n], AF.Sigmoid, bias=vec[:, A0 + j * 4 + cc:A0 + j * 4 + cc + 1]),
                             reads=[bps2, b_c, bT2], writes=[bT2])
                    if j == 0:
                        k.op("dve", lambda e: e.tensor_tensor_scan(T3[:], rst[:, 0:TT], T1[:], 0.0, ALU.mult, ALU.add),
                             reads=[bT1, b_c, bT3], writes=[bT3])
                    else:
                        k.op("dve", lambda e: e.tensor_tensor_scan(rev_ap(T3[:], 0, TT), rev_ap(rst[:], 1, TT + 1), rev_ap(T1[:], 0, TT),
                                                                   0.0, ALU.mult, ALU.add), reads=[bT1, b_c, bT3], writes=[bT3])
                    k.op("dve", lambda e: e.tensor_scalar(T4[:], T2[:], vec[:, KA + cc:KA + cc + 1], der[:, 30 + cc:31 + cc], ALU.mult, ALU.add),
                         reads=[bT2, b_c, bT4], writes=[bT4])
                    k.op("pool", lambda e: e.tensor_tensor(T4[:], T4[:], KP[cc][:], ALU.mult), reads=[bT4, bKP[cc]], writes=[bT4])
                    k.op("pool", lambda e: e.tensor_tensor(T2[:], T2[:], KAP[cc][:], ALU.mult), reads=[bT2, bKAP[cc]], writes=[bT2])
                    k.op("pool", lambda e: e.tensor_tensor(T1[:], T3[:], T1[:], ALU.subtract), reads=[bT1, bT3], writes=[bT1])
                    k.op("act", lambda e: e.activation(T1[:], T1[:], AF.Exp, scale=-LAM), reads=[bT1], writes=[bT1])
                    k.op("act", lambda e: e.activation(S[:], T3[:], AF.Exp, scale=LAM), reads=[bT3, bS], writes=[bS])
                    k.op("act", lambda e: e.activation(T3[:], T3[:], AF.Exp, scale=-LAM), reads=[bT3], writes=[bT3])
                    pl, bpl = plr.next()
                    off = 127 if j == 0 else 0
                    k.op("dve", lambda e: e.tensor_copy(pl[:], T3[:, off:TT:128]), reads=[bT3], writes=[bpl])
                    k.dma("sp", C.RWPL[b, j, cc * 128:(cc + 1) * 128, :], pl[:], reads=[bpl], writes=[C.bRW])
                    k.op("dve", lambda e: e.tensor_tensor(O[0][:], RP[cc][:], T3[:], ALU.mult), reads=[bRP[cc], bT3, bO[0]], writes=[bO[0]])
                    k.op("pool", lambda e: e.tensor_tensor(O[1][:], KAP[cc][:], T1[:], ALU.mult), reads=[bKAP[cc], bT1, bO[1]], writes=[bO[1]])
                    k.op("dve", lambda e: e.tensor_tensor(O[2][:], T4[:], S[:], ALU.mult), reads=[bT4, bS, bO[2]], writes=[bO[2]])
                    k.op("pool", lambda e: e.tensor_tensor(O[3][:], T2[:], S[:], ALU.mult), reads=[bT2, bS, bO[3]], writes=[bO[3]])
                    for q in range(4):
                        k.dma("sp" if q % 2 == 0 else "act", C.RWT[b, j, q, cc * 128:(cc + 1) * 128, :], O[q][:], reads=[bO[q]], writes=[C.bRW])
                    if j == 0:
                        k.op("dve", lambda e: e.tensor_copy(KS[:], T4[:]), reads=[bT4, bKS], writes=[bKS])
                    else:
                        k.op("dve", lambda e: e.tensor_tensor(KS[:], KS[:], T4[:], ALU.add), reads=[bT4, bKS], writes=[bKS])
                k.op("dve", lambda e: e.scalar_tensor_tensor(KS[:], KS[:], vec[:, RK + cc:RK + cc + 1], RP[cc][:], ALU.mult, ALU.mult),
                     reads=[bKS, bRP[cc], b_c], writes=[bKS])
                k.op("act", lambda e: e.activation(O[0][:], KS[:], AF.Copy), reads=[bKS, bO[0]], writes=[bO[0]])
                for (t0, n) in blocks:
                    ps, bps = psr.next()
                    k.op("pe", lambda e: e.matmul(ps[:, :n], bd[:], O[0][:, t0:t0 + n], start=True, stop=True), reads=[bO[0], b_c], writes=[bps])
                    k.op("dve", lambda e: e.tensor_tensor(T1[:, t0:t0 + n], ps[:, :n], VP[cc][:, t0:t0 + n], ALU.mult),
                         reads=[bps, bVP[cc], bT1], writes=[bT1])
                k.dma("sp", C.RWB[b, cc * 128:(cc + 1) * 128, :], T1[:], reads=[bT1], writes=[C.bRW])


def phase_rwkv_scan(P, C, l):
    k = P.k
    YT = C.YT
    with P.scope():
        masks = P.sb("rwmask", [128, 2, 640], BF16)
        b_c = Buf("rwc2")
        k.dma("pool", masks[:], C.rw_masks, writes=[b_c])
        ident = P.sb("rwident", [128, 128], BF16)
        k.dma("pool", ident[:], C.ident, writes=[b_c])
        identf = P.sb("rwidentf", [128, 128], F32)
        k.dma("sp", identf[:], C.ident, writes=[b_c])
        bdm = P.sb("rwbdm", [128, 128], F32)
        k.dma("sp", bdm[:], C.rw_bd, writes=[b_c])
        vec = P.sb("rwvec2", [128, 51], F32)
        k.dma("sp", vec[:], C.rw_vec[l], writes=[b_c])
        lmf = P.sb("rwlm", [128, 4, 128], F32)
        k.dma("sp", lmf[:], C.rw_lm, writes=[b_c])
        gne = P.sb("gne", [128, 1], F32)
        k.op("dve", lambda e: e.memset(gne[:], GN_EPS), writes=[b_c])
        Ytok = P.sb("Ytok", [128, NCK, 512], F32)
        for b in range(NB):
            bY = [[Buf(f"Y{c}_{hp}") for hp in range(4)] for c in range(NCK)]
            with P.scope():
                KR = P.sb("KR", [128, NCK, 2, 128], BF16)
                KF = P.sb("KF", [128, TT], BF16)
                BF_ = P.sb("BF", [128, TT], BF16)
                VF = P.sb("VF", [128, TT], BF16)
                PLt = P.sb("PLt", [128, NCK], F32)
                TOK = P.sb("TOK", [128, NCK, 3, 128], BF16)
                SC = P.sb("SC", [128, NCK, 2, 512], BF16)
                NM = P.sb("NM", [128, NCK, 2, 128], BF16)
                Tt = P.sb("Tt", [128, NCK * 2, 128], BF16)
                SC36 = SC[:].rearrange("p c h x -> p (c h) x")
                NM36 = NM[:].rearrange("p c h x -> p (c h) x")
                NFr = Ring(P, "NF", 2, [128, 4, 2, 128], F32)
                F4 = Ring(P, "F4", 10, [128, 4, 128], F32)
                B4 = Ring(P, "B4", 12, [128, 4, 128], BF16)
                H = P.sb("Hst", [128, 128], F32)
                Ht = P.sb("Htmp", [128, 128], F32)
                Hb = P.sb("Hb", [128, 128], BF16)
                Wr = Ring(P, "Wsb", 2, [128, 128], BF16)
                Ur = Ring(P, "Un", 2, [128, 128], BF16)
                psT = P.ps("pstr", [128, 3, 128], BF16)
                bpsT = Buf("pstr")
                psL = Ring(P, "psL", 4, [128, 512], F32, psum=True)
                psS = Ring(P, "psS", 3, [128, 128], F32, psum=True)
                b_in, b_tok, b_sc, b_tt, b_H = Buf("in"), Buf("tok"), Buf("sc"), Buf("tt"), Buf("H")
                for j in ([0] if "rw_j0" in P.debug else [1] if "rw_j1" in P.debug else [0, 1]):
                    for hp in range(4):
                        rows = slice(hp * 128, (hp + 1) * 128)
                        k.dma("sp", KR[:, :, 0, :], C.RWT[b, j, 1, rows, :].rearrange("p (c t) -> p c t", t=128), reads=[C.bRW], writes=[b_in])
                        k.dma("act", KR[:, :, 1, :], C.RWT[b, j, 0, rows, :].rearrange("p (c t) -> p c t", t=128), reads=[C.bRW], writes=[b_in])
                        k.dma("sp", KF[:], C.RWT[b, j, 2, rows, :], reads=[C.bRW], writes=[b_in])
                        k.dma("act", BF_[:], C.RWT[b, j, 3, rows, :], reads=[C.bRW], writes=[b_in])
                        k.dma("sp", VF[:], C.RWV[b, rows, :], reads=[C.bRW], writes=[b_in])
                        k.dma("sp", PLt[:], C.RWPL[b, j, rows, :], reads=[C.bRW], writes=[b_in])
                        for c in range(NCK):
                            cs = slice(c * 128, (c + 1) * 128)
                            for q, src in enumerate((KF, BF_, VF)):
                                k.op("pe", lambda e: e.transpose(psT[:, q, :], src[:, cs], ident[:]), reads=[b_in, b_c], writes=[bpsT])
                            k.op("act", lambda e: e.activation(TOK[:, c], psT[:], AF.Copy), reads=[bpsT], writes=[b_tok])
                        for g in range(NCK // 2):
                            NF, bNF = NFr.next()
                            for i in range(4):
                                c, h = 2 * g + i // 2, i % 2
                                cs = slice(c * 128, (c + 1) * 128)
                                hr = slice(h * 64, (h + 1) * 64)
                                kr2 = KR[hr, c].rearrange("p x t -> p (x t)")
                                pX, bpX = psL.next()
                                k.op("pe", lambda e: e.matmul(pX[:, 0:256], KF[hr, cs], kr2, start=True, stop=True), reads=[b_in], writes=[bpX])
                                k.op("pe", lambda e: e.matmul(pX[:, 256:512], BF_[hr, cs], kr2, start=True, stop=True), reads=[b_in], writes=[bpX])
                                pY, bpY = psS.next()
                                k.op("pe", lambda e: e.matmul(pY[:], KR[hr, c, 0, :], BF_[hr, cs], start=True, stop=True), reads=[b_in], writes=[bpY])
                                k.op("dve", lambda e: e.tensor_tensor(SC[:, c, h, :], pX[:], masks[:, j, 0:512], ALU.mult), reads=[bpX, b_c], writes=[b_sc])
                                k.op("dve", lambda e: e.tensor_tensor(NF[:, i, 1, :], pX[:, 256:384], masks[:, j, 256:384], ALU.mult), reads=[bpX, b_c], writes=[bNF])
                                k.op("dve", lambda e: e.tensor_tensor(NF[:, i, 0, :], pY[:], masks[:, j, 512:640], ALU.mult), reads=[bpY, b_c], writes=[bNF])
                            bl = slice(g * 4, (g + 1) * 4)
                            bc4 = lambda m_: m_.unsqueeze(1).to_broadcast([128, 4, 128])
                            Mk, bMk = F4.next()
                            Mtk, bMtk = F4.next()
                            Tf, bTf = F4.next()
                            Ttf, bTtf = F4.next()
                            k.op("pool", lambda e: e.tensor_tensor(Mk[:], NF[:, :, 0, :], bc4(lmf[:, 0, :]), ALU.mult), reads=[bNF, b_c], writes=[bMk])
                            k.op("pool", lambda e: e.tensor_tensor(Mtk[:], NF[:, :, 1, :], bc4(lmf[:, 0, :]), ALU.mult), reads=[bNF, b_c], writes=[bMtk])
                            k.op("pool", lambda e: e.tensor_tensor(Tf[:], Mk[:], bc4(identf[:]), ALU.add), reads=[bMk, b_c], writes=[bTf])
                            k.op("pool", lambda e: e.tensor_tensor(Ttf[:], Mtk[:], bc4(identf[:]), ALU.add), reads=[bMtk, b_c], writes=[bTtf])
                            for lev in range(1, 4):
                                M2, bM2 = F4.next()
                                Mt2, bMt2 = F4.next()
                                p1, bp1 = psL.next()
                                p2, bp2 = psL.next()
                                for i in range(4):
                                    k.op("pe", lambda e: e.matmul(p1[:, i * 128:(i + 1) * 128], Mk[:, i, :], Mtk[:, i, :], start=True, stop=True),
                                         reads=[bMk, bMtk], writes=[bp1])
                                    k.op("pe", lambda e: e.matmul(p2[:, i * 128:(i + 1) * 128], Mtk[:, i, :], Mk[:, i, :], start=True, stop=True),
                                         reads=[bMk, bMtk], writes=[bp2])
                                k.op("act", lambda e: e.activation(Mt2[:].rearrange("p a b -> p (a b)"), p1[:], AF.Copy), reads=[bp1], writes=[bMt2])
                                k.op("dve", lambda e: e.tensor_copy(M2[:].rearrange("p a b -> p (a b)"), p2[:]), reads=[bp2], writes=[bM2])
                                p3, bp3 = psL.next()
                                p4, bp4 = psL.next()
                                for i in range(4):
                                    k.op("pe", lambda e: e.matmul(p3[:, i * 128:(i + 1) * 128], Mt2[:, i, :], Tf[:, i, :], start=True, stop=True),
                                         reads=[bMt2, bTf], writes=[bp3])
                                    k.op("pe", lambda e: e.matmul(p4[:, i * 128:(i + 1) * 128], M2[:, i, :], Ttf[:, i, :], start=True, stop=True),
                                         reads=[bM2, bTtf], writes=[bp4])
                                k.op("dve", lambda e: e.tensor_tensor(Tf[:].rearrange("p a b -> p (a b)"), Tf[:].rearrange("p a b -> p (a b)"), p3[:], ALU.add),
                                     reads=[bp3, bTf], writes=[bTf])
                                k.op("dve", lambda e: e.tensor_tensor(Ttf[:].rearrange("p a b -> p (a b)"), Ttf[:].rearrange("p a b -> p (a b)"), p4[:], ALU.add),
                                     reads=[bp4, bTtf], writes=[bTtf])
                                Mk, bMk, Mtk, bMtk = M2, bM2, Mt2, bMt2
                            Tb, bTb = B4.next()
                            Ttb, bTtb = B4.next()
                            k.op("act", lambda e: e.activation(Tb[:], Tf[:], AF.Copy), reads=[bTf], writes=[bTb])
                            k.op("act", lambda e: e.activation(Ttb[:], Ttf[:], AF.Copy), reads=[bTtf], writes=[bTtb])
                            for li in range(1, 4):
                                lastl = (li == 3)
                                Cm, bCm = B4.next()
                                k.op("pool", lambda e: e.tensor_tensor(Cm[:], NF[:, :, 0, :], bc4(lmf[:, li, :]), ALU.mult), reads=[bNF, b_c], writes=[bCm])
                                p2, bp2 = psL.next()
                                for i in range(4):
                                    k.op("pe", lambda e: e.matmul(p2[:, i * 128:(i + 1) * 128], Cm[:, i, :], Ttb[:, i, :], start=True, stop=True),
                                         reads=[bCm, bTtb], writes=[bp2])
                                Z2, bZ2 = B4.next()
                                k.op("act", lambda e: e.activation(Z2[:].rearrange("p a b -> p (a b)"), p2[:], AF.Copy), reads=[bp2], writes=[bZ2])
                                if not lastl:
                                    Cmt, bCmt = B4.next()
                                    k.op("pool", lambda e: e.tensor_tensor(Cmt[:], NF[:, :, 1, :], bc4(lmf[:, li, :]), ALU.mult), reads=[bNF, b_c], writes=[bCmt])
                                    p1, bp1 = psL.next()
                                    for i in range(4):
                                        k.op("pe", lambda e: e.matmul(p1[:, i * 128:(i + 1) * 128], Cmt[:, i, :], Tb[:, i, :], start=True, stop=True),
                                             reads=[bCmt, bTb], writes=[bp1])
                                    Z1, bZ1 = B4.next()
                                    k.op("dve", lambda e: e.tensor_copy(Z1[:].rearrange("p a b -> p (a b)"), p1[:]), reads=[bp1], writes=[bZ1])
                                p4, bp4 = psL.next()
                                for i in range(4):
                                    k.op("pe", lambda e: e.matmul(p4[:, i * 128:(i + 1) * 128], Tb[:, i, :], Z2[:, i, :], start=True, stop=True),
                                         reads=[bTb, bZ2], writes=[bp4])
                                if not lastl:
                                    p3, bp3 = psL.next()
                                    for i in range(4):
                                        k.op("pe", lambda e: e.matmul(p3[:, i * 128:(i + 1) * 128], Ttb[:, i, :], Z1[:, i, :], start=True, stop=True),
                                             reads=[bTtb, bZ1], writes=[bp3])
                                    Tn, bTn = B4.next()
                                    Ttn, bTtn = B4.next()
                                    k.op("dve", lambda e: e.tensor_tensor(Tn[:].rearrange("p a b -> p (a b)"), Tb[:].rearrange("p a b -> p (a b)"), p3[:], ALU.add),
                                         reads=[bp3, bTb], writes=[bTn])
                                    k.op("dve", lambda e: e.tensor_tensor(Ttn[:].rearrange("p a b -> p (a b)"), Ttb[:].rearrange("p a b -> p (a b)"), p4[:], ALU.add),
                                         reads=[bp4, bTtb], writes=[bTtn])
                                    Tb, bTb, Ttb, bTtb = Tn, bTn, Ttn, bTtn
                                else:
                                    k.op("dve", lambda e: e.tensor_tensor(Tt[:, bl, :], Ttb[:], p4[:].rearrange("p (a b) -> p a b", b=128), ALU.add),
                                         reads=[bp4, bTtb, b_tt], writes=[b_tt])
                        k.op("pool", lambda e: e.memset(H[:], 0.0), reads=[b_H], writes=[b_H])
                        k.op("pool", lambda e: e.memset(Hb[:], 0.0), reads=[b_H], writes=[b_H])
                        order = list(range(NCK)) if j == 0 else [1, 0] + list(range(NCK - 1, 1, -1))
                        for c in order:
                            pW, bpW = psS.next()
                            k.op("pe", lambda e: e.matmul(pW[:], KR[:, c, 0, :], Hb[:], start=True, stop=False, skip_group_check=True),
                                 reads=[b_in, b_H], writes=[bpW])
                            for h in range(2):
                                hc = slice(h * 64, (h + 1) * 64)
                                k.op("pe", lambda e: e.matmul(pW[:, hc], SC[:, c, h, 0:128], TOK[:, c, 2, hc], start=False, stop=True, skip_group_check=True),
                                     reads=[b_sc, b_tok], writes=[bpW])
                            Wsb, bW = Wr.next()
                            k.op("act", lambda e: e.activation(Wsb[:], pW[:], AF.Copy), reads=[bpW], writes=[bW])
                            pU, bpU = psS.next()
                            for h in range(2):
                                hc = slice(h * 64, (h + 1) * 64)
                                k.op("pe", lambda e: e.matmul(pU[:, hc], Tt[:, c * 2 + h, :], Wsb[:, hc], start=True, stop=True),
                                     reads=[b_tt, bW], writes=[bpU])
                            Un, bUn = Ur.next()
                            k.op("act", lambda e: e.activation(Un[:], pU[:], AF.Copy, scale=-1.0), reads=[bpU], writes=[bUn])
                            pYy, bpYy = psS.next()
                            k.op("pe", lambda e: e.matmul(pYy[:], KR[:, c, 1, :], Hb[:], start=True, stop=False, skip_group_check=True),
                                 reads=[b_in, b_H], writes=[bpYy])
                            for h in range(2):
                                hc = slice(h * 64, (h + 1) * 64)
                                k.op("pe", lambda e: e.matmul(pYy[:, hc], SC[:, c, h, 128:256], TOK[:, c, 2, hc], start=False, stop=False, skip_group_check=True),
                                     reads=[b_sc, b_tok], writes=[bpYy])
                                k.op("pe", lambda e: e.matmul(pYy[:, hc], SC[:, c, h, 384:512], Un[:, hc], start=False, stop=True, skip_group_check=True),
                                     reads=[b_sc, bUn], writes=[bpYy])
                            ysl = Ytok[:, c, hp * 128:(hp + 1) * 128]
                            if j == 0 or "rw_j1" in P.debug:
                                k.op("act", lambda e: e.activation(ysl, pYy[:], AF.Copy), reads=[bpYy], writes=[bY[c][hp]])
                            else:
                                k.op("dve", lambda e: e.tensor_tensor(ysl, ysl, pYy[:], ALU.add), reads=[bpYy, bY[c][hp]], writes=[bY[c][hp]])
                            pH, bpH = psS.next()
                            k.op("pe", lambda e: e.matmul(pH[:], TOK[:, c, 0, :], TOK[:, c, 2, :], start=True, stop=False), reads=[b_tok], writes=[bpH])
                            k.op("pe", lambda e: e.matmul(pH[:], TOK[:, c, 1, :], Un[:], start=False, stop=True), reads=[b_tok, bUn], writes=[bpH])
                            k.op("dve", lambda e: e.tensor_tensor(Ht[:], H[:], pH[:], ALU.add), reads=[bpH, b_H], writes=[b_H])
                            k.op("dve", lambda e: e.scalar_tensor_tensor(H[:], Ht[:], PLt[:, c:c + 1], bdm[:], ALU.mult, ALU.mult),
                                 reads=[b_H, b_in, b_c], writes=[b_H])
                            k.op("act", lambda e: e.activation(Hb[:], H[:], AF.Copy), reads=[b_H], writes=[b_H])
            if "YTOK" in P.debug:
                k.dma("sp", C.YTOK[b], Ytok[:], reads=[x for row in bY for x in row], writes=[C.bRW])
            with P.scope():
                BON = P.sb("BON", [128, 4, TT], F32)
                G = P.sb("G", [128, 4, TT], BF16)
                b_l = Buf("rdl")
                k.dma("sp", BON[:], C.RWB[b].rearrange("(c p) t -> p c t", p=128), reads=[C.bRW], writes=[b_l])
                k.dma("act", G[:], C.RWG[b].rearrange("(c p) t -> p c t", p=128), reads=[C.bRW], writes=[b_l])
                st8 = Ring(P, "st8", 2, [128, 6, 8], F32)
                ysq = Ring(P, "ysq", 2, [128, 512], F32)
                ynr = Ring(P, "yn", 2, [128, 512], F32)
                psR = Ring(P, "psR", 2, [128, 4, 128], F32, psum=True)
                ofr = Ring(P, "of", 3, [128, 128], F32)
                obr = Ring(P, "ob", 3, [128, 128], BF16)
                for c in range(NCK):
                    allY = bY[c]
                    y = Ytok[:, c, :]
                    y3 = y.rearrange("p (h x) -> p h x", x=64)
                    s, bs = st8.next()
                    k.op("dve", lambda e: e.reduce_sum(s[:, 0, :], y3, AX.X), reads=allY, writes=[bs])
                    q, bq = ysq.next()
                    k.op("pool", lambda e: e.tensor_tensor(q[:], y, y, ALU.mult), reads=allY, writes=[bq])
                    k.op("dve", lambda e: e.reduce_sum(s[:, 1, :], q[:].rearrange("p (h x) -> p h x", x=64), AX.X), reads=[bq, bs], writes=[bs])
                    k.op("dve", lambda e: e.tensor_scalar(s[:, 2, :], s[:, 0, :], 1.0 / 64.0, None, ALU.mult), reads=[bs], writes=[bs])
                    k.op("dve", lambda e: e.tensor_tensor(s[:, 3, :], s[:, 2, :], s[:, 2, :], ALU.mult), reads=[bs], writes=[bs])
                    k.op("dve", lambda e: e.scalar_tensor_tensor(s[:, 4, :], s[:, 1, :], 1.0 / 64.0, s[:, 3, :], ALU.mult, ALU.subtract),
                         reads=[bs], writes=[bs])
                    k.op("act", lambda e: e.activation(s[:, 5, :], s[:, 4, :], AF.Sqrt, bias=gne[:, 0:1]), reads=[bs, b_c], writes=[bs])
                    k.op("dve", lambda e: e.reciprocal(s[:, 5, :], s[:, 5, :]), reads=[bs], writes=[bs])
                    yn, byn = ynr.next()
                    yn3 = yn[:].rearrange("p (h x) -> p h x", x=64)
                    k.op("dve", lambda e: e.tensor_tensor(yn3, y3, s[:, 2, :].unsqueeze(2).to_broadcast([128, 8, 64]), ALU.subtract),
                         reads=allY + [bs], writes=[byn])
                    k.op("pool", lambda e: e.tensor_tensor(yn3, yn3, s[:, 5, :].unsqueeze(2).to_broadcast([128, 8, 64]), ALU.mult),
                         reads=[byn, bs], writes=[byn])
                    pr, bpr = psR.next()
                    for hp in range(4):
                        k.op("pe", lambda e: e.transpose(pr[:, hp, :], yn[:, hp * 128:(hp + 1) * 128], identf[:]), reads=[byn, b_c], writes=[bpr])
                    cs = slice(c * 128, (c + 1) * 128)
                    for hp in range(4):
                        of, bof = ofr.next()
                        k.op("act", lambda e: e.activation(of[:], pr[:, hp, :], AF.Identity, bias=vec[:, 47 + hp:48 + hp], scale=vec[:, 43 + hp:44 + hp]),
                             reads=[bpr, b_c], writes=[bof])
                        k.op("dve", lambda e: e.tensor_tensor(of[:], of[:], BON[:, hp, cs], ALU.add), reads=[bof, b_l], writes=[bof])
                        ob, bob = obr.next()
                        k.op("pool", lambda e: e.tensor_tensor(ob[:], of[:], G[:, hp, cs], ALU.mult), reads=[bof, b_l], writes=[bob])
                        k.dma("sp", YT[b, 1, hp * 128:(hp + 1) * 128, cs], ob[:], reads=[bob], writes=[C.bYT])


def build(n_layers=DEPTH, debug=(), stop_after=None):
    P = Prog(n_layers, debug)
    nc, k = P.nc, P.k
    xt0 = P.din("xt0", [NB, D, TT])
    cT = P.din("cT", [128, KC, 3])
    cst_ones = P.din("ones", [128, 128])
    ada_w = P.din("ada_w", [DEPTH, D, 6 * D])
    ada_b = P.din("ada_b_r", [DEPTH, 128, 48])
    ng_r = P.din("ng_r", [DEPTH, 128, 2, KC])
    w_in = P.din("w_in", [DEPTH, D, N_IN])
    XT = P.dscr("XT", [NB, D, TT], F32)
    PT = P.dscr("PT", [NB, NCH * 128, TT], BF16)
    bXT = [Buf("XT0"), Buf("XT1")]
    bPT = Buf("PT")
    if "yt_in" in P.debug:
        YT = P.din("YT3", [NB, 3, 512, TT], BF16)
        P.dbufs["YT3"] = Buf("YT3")
    else:
        YT = P.dscr("YT3", [NB, 3, 512, TT], BF16)
    C = type("Ctx", (), {})()
    C.XT, C.PT, C.YT, C.bXT, C.bPT, C.bYT = XT, PT, YT, bXT, bPT, Buf("YT3")
    C.xt0 = xt0
    declare_inputs(P, C)

    with P.scope():
        ones = P.sb("ones", [128, 128], F32)
        b_ones = Buf("ones")
        k.dma("sp", ones[:], cst_ones, writes=[b_ones])
        silu_c = P.sb("silu_c", [128, KC, 3], F32)
        b_silu = Buf("silu_c")
        k.dma("sp", silu_c[:], cT, writes=[b_silu])
        k.op("act", lambda e: e.activation(silu_c[:], silu_c[:], AF.Silu), reads=[b_silu], writes=[b_silu])
        C.epsc = P.sb("epsc_g", [128, 1], F32)
        k.op("dve", lambda e: e.memset(C.epsc[:], NORM_EPS), writes=[b_ones])
        mod = P.sb("mod", [128, 48, 3], F32)
        b_mod = Buf("mod")
        gs = P.sb("gs", [128, 2, KC, 3], F32)
        b_gs = Buf("gs")

        for l in range(n_layers):
            src_x = xt0 if l == 0 else XT
            with P.scope():
                adab = P.sb("adab", [128, 48], F32)
                b_adab = Buf("adab")
                k.dma("sp", adab[:], ada_b[l], writes=[b_adab])
                ng = P.sb("ng", [128, 2, KC], F32)
                b_ng = Buf("ng")
                k.dma("sp", ng[:], ng_r[l], writes=[b_ng])
                wr = Ring(P, "adaw", 2, [128, KC, 512], F32)
                pr = Ring(P, "modps", 2, [128, 4, 3], F32, psum=True)
                for g in range(12):
                    wt, bw = wr.next()
                    k.dma("sp" if g % 2 == 0 else "act", wt[:],
                          ada_w[l][:, g * 512:(g + 1) * 512].rearrange("(kc p) n -> p kc n", p=128), writes=[bw])
                    pt, bp = pr.next()
                    for c4 in range(4):
                        for kc in range(KC):
                            k.op("pe", lambda e, c4=c4, kc=kc: e.matmul(
                                pt[:, c4, :], wt[:, kc, c4 * 128:(c4 + 1) * 128], silu_c[:, kc, :],
                                start=(kc == 0), stop=(kc == KC - 1)),
                                reads=[bw, b_silu], writes=[bp])
                    k.op("dve", lambda e: e.tensor_tensor(
                        mod[:, g * 4:(g + 1) * 4, :], pt[:],
                        adab[:, g * 4:(g + 1) * 4].unsqueeze(2).to_broadcast([128, 4, 3]), ALU.add),
                        reads=[bp, b_adab], writes=[b_mod])
                for n, mi in ((0, 1), (1, 4)):
                    k.op("dve", lambda e, n=n, mi=mi: e.tensor_scalar(
                        gs[:, n, :, :], mod[:, mi * 8:(mi + 1) * 8, :], 1.0, None, ALU.add),
                        reads=[b_mod], writes=[b_gs])
                    k.op("dve", lambda e, n=n: e.tensor_tensor(
                        gs[:, n, :, :], gs[:, n, :, :],
                        ng[:, n, :].unsqueeze(2).to_broadcast([128, KC, 3]), ALU.mult),
                        reads=[b_gs, b_ng], writes=[b_gs])
            if stop_after == "mod":
                break

            with P.scope():
                hT = P.sb("hT", [128, NB, KC, TT], BF16)
                b_h = [[Buf(f"h{b}_{i}") for i in range(5)] for b in range(NB)]
                blocks = [(0, TC)] + [(TC + i * 512, 512) for i in range(4)]
                xr = Ring(P, "xin", 2, [128, KC, 512], F32)
                sqr = Ring(P, "sq", 1, [128, KC, 512], F32)
                ssr = Ring(P, "ssps", 2, [128, 512], F32, psum=True)
                rsr = Ring(P, "rstd", 2, [128, 512], F32)
                for b in range(NB):
                    for bi, (t0, n) in enumerate(blocks):
                        j = 2 if bi == 0 else b
                        xt, bx = xr.next()
                        k.dma("sp", xt[:, :, :n], src_x[b][:, t0:t0 + n].rearrange("(kc p) n -> p kc n", p=128),
                              reads=[bXT[b]], writes=[bx])
                        sq, bs = sqr.next()
                        k.op("act", lambda e: e.activation(sq[:, :, :n], xt[:, :, :n], AF.Square),
                             reads=[bx], writes=[bs])
                        ss, bss = ssr.next()
                        for kc in range(KC):
                            k.op("pe", lambda e, kc=kc: e.matmul(ss[:, :n], ones[:], sq[:, kc, :n],
                                                                 start=(kc == 0), stop=(kc == KC - 1)),
                                 reads=[bs, b_ones], writes=[bss])
                        rs, brs = rsr.next()
                        k.op("act", lambda e: e.activation(rs[:, :n], ss[:, :n], AF.Sqrt, bias=NORM_EPS, scale=1.0 / D),
                             reads=[bss], writes=[brs])
                        k.op("dve", lambda e: e.reciprocal(rs[:, :n], rs[:, :n]), reads=[brs], writes=[brs])
                        k.op("dve", lambda e: e.tensor_tensor(
                            sq[:, :, :n], xt[:, :, :n], rs[:, :n].unsqueeze(1).to_broadcast([128, KC, n]), ALU.mult),
                            reads=[bx, brs, bs], writes=[bs])
                        for kc in range(KC):
                            k.op("act", lambda e, kc=kc: e.activation(
                                hT[:, b, kc, t0:t0 + n], sq[:, kc, :n], AF.Identity,
                                bias=mod[:, 0 * 8 + kc, j:j + 1], scale=gs[:, 0, kc, j:j + 1]),
                                reads=[bs, b_mod, b_gs], writes=[b_h[b][bi]])
                groups = [list(range(g * 4, g * 4 + 4)) for g in range(6)] + [[24]] + \
                         [list(range(25 + g * 4, 29 + g * 4)) for g in range(6)]
                wr = Ring(P, "win", 2, [128, KC, 512], BF16)
                pr = Ring(P, "inps", 4, [128, 512], F32, psum=True)
                sr = Ring(P, "instage", 4, [128, 512], BF16)
                ev = 0
                for grp in groups:
                    c0 = chunk_cols(grp[0])[0]
                    ncols = sum(chunk_cols(ci)[1] for ci in grp)
                    wt, bw = wr.next()
                    k.dma("pool", wt[:, :, :ncols],
                          w_in[l][:, c0:c0 + ncols].rearrange("(kc p) n -> p kc n", p=128), writes=[bw])
                    for b in range(NB):
                        for bi, (t0, n) in enumerate(blocks):
                            for ci in grp:
                                cc0, cn = chunk_cols(ci)
                                o = cc0 - c0
                                pt, bp = pr.next()
                                for kc in range(KC):
                                    k.op("pe", lambda e, kc=kc: e.matmul(
                                        pt[:cn, :n], wt[:, kc, o:o + cn], hT[:, b, kc, t0:t0 + n],
                                        start=(kc == 0), stop=(kc == KC - 1)),
                                        reads=[bw, b_h[b][bi]], writes=[bp])
                                st, bst = sr.next()
                                if ci >= 25:
                                    k.op("act", lambda e: e.activation(st[:cn, :n], pt[:cn, :n], AF.Sigmoid),
                                         reads=[bp], writes=[bst])
                                elif ev % 2 == 0:
                                    k.op("act", lambda e: e.activation(st[:cn, :n], pt[:cn, :n], AF.Copy),
                                         reads=[bp], writes=[bst])
                                else:
                                    k.op("dve", lambda e: e.tensor_copy(st[:cn, :n], pt[:cn, :n]),
                                         reads=[bp], writes=[bst])
                                ev += 1
                                k.dma("sp", PT[b, ci * 128:ci * 128 + cn, t0:t0 + n], st[:cn, :n],
                                      reads=[bst], writes=[bPT])
            if stop_after == "in":
                break
            C.mod, C.b_mod, C.gs, C.b_gs, C.ones, C.b_ones = mod, b_mod, gs, b_gs, ones, b_ones
            if "nomla" not in P.debug and "yt_in" not in P.debug:
                phase_mla(P, C, l)
            if stop_after == "mla":
                break
            if "nossm" not in P.debug and "yt_in" not in P.debug:
                phase_ssm(P, C, l)
            if stop_after == "ssm":
                break
            if "yt_in" not in P.debug:
                phase_rwkv_prep(P, C, l)
                if stop_after == "rwprep":
                    break
                phase_rwkv_scan(P, C, l)
                if stop_after == "rwkv":
                    break
            phase_merge(P, C, l)
            if stop_after == "merge":
                break
            phase_moe(P, C, l, last=(l == n_layers - 1))
            if stop_after == "moe":
                break
        k.barrier()
    return P


def host_inputs(inputs, core):
    b0 = core * NB
    x = inputs["x"][b0:b0 + NB]
    ctx = inputs["ctx"][b0:b0 + NB]
    xt0 = np.ascontiguousarray(np.concatenate([ctx, x], axis=1).transpose(0, 2, 1))
    cT = np.stack([inputs["c"][b0], inputs["c"][b0 + 1], inputs["c_ctx"]], axis=1)
    cT = np.ascontiguousarray(cT.reshape(KC, 128, 3).transpose(1, 0, 2))
    m = {"xt0": xt0, "cT": cT}
    return m


def pj(v, n):
    return np.ascontiguousarray(v.reshape(v.shape[:-1] + (n, 128)).swapaxes(-1, -2))


def host_shared(inputs):
    m = {"ones": np.ones((128, 128), np.float32)}
    for name in ("ada_w", "w_in"):
        m[name] = inputs[name]
    for name in ("mla_w_uq", "mla_w_ukv"):
        m[name] = inputs[name]
    mats = np.zeros((128, 3, 128), np.float32)
    mats[:64, 0, :64] = 1.0
    mats[64:96, 0, 64:96] = 1.0
    mats[:64, 1, :64] = 1.0
    for i in range(16):
        mats[64 + 16 + i, 2, 64 + i] = -1.0
        mats[64 + i, 2, 64 + 16 + i] = 1.0
    m["mla_mats"] = mats
    vec = np.zeros((DEPTH, 128, 8), np.float32)
    vec[:, :, 0:3] = pj(inputs["mla_q_norm"], 3)
    vec[:, :, 3:5] = pj(inputs["mla_kv_norm"], 2)
    vec[:, :64, 5] = inputs["mla_qn_nope"]
    vec[:, 64:96, 5] = inputs["mla_qn_rope"]
    vec[:, :64, 6] = inputs["mla_kn_nope"]
    vec[:, 64:96, 6] = inputs["mla_kn_rope"]
    vec[:, :64, 7] = 1.0 / 64.0
    vec[:, 64:96, 7] = 1.0 / 32.0
    m["mla_vec"] = vec
    tt = np.arange(TL)
    inv = (10000.0 ** (-np.arange(0, 16, 2, dtype=np.float32) / 16.0)).astype(np.float32)
    ang = np.concatenate([(tt // 64).astype(np.float32)[:, None] * inv, (tt % 64).astype(np.float32)[:, None] * inv], axis=-1)
    tab = np.zeros((96, 2, TT), np.float32)
    tab[:, 0, :] = 1.0
    tab[64:80, 0, TC:] = np.cos(ang).T
    tab[80:96, 0, TC:] = np.cos(ang).T
    tab[64:80, 1, TC:] = np.sin(ang).T
    tab[80:96, 1, TC:] = np.sin(ang).T
    m["rope_tab"] = tab
    it = np.zeros((128, 2, TT), np.float32)
    it[:, 0, :] = np.arange(TT)
    it[:, 1, :TC] = TC - 1 - np.arange(TC)
    it[:, 1, TC:] = TC + (TL - 1 - np.arange(TL))
    m["ssm_iota"] = it
    L_ = DEPTH
    lre = inputs["ssm_lambda_re"].reshape(L_, 2, 16, 128)
    lim = inputs["ssm_lambda_im"].reshape(L_, 2, 16, 128)
    ldt = np.repeat(inputs["ssm_log_dt"], 64, axis=-1).reshape(L_, 2, 16, 128)
    m["ssm_sv"] = np.ascontiguousarray(np.stack([lre, lim, ldt], axis=-1).transpose(0, 3, 1, 2, 4))
    BT = np.zeros((L_, 2, 16, 128, 128), np.float32)
    CT = np.zeros((L_, 2, 2, 16, 128, 128), np.float32)
    for ri, (bn, cn) in enumerate((("ssm_b_re", "ssm_c_re"), ("ssm_b_im", "ssm_c_im"))):
        bb = inputs[bn]
        cc = inputs[cn]
        for g in range(32):
            sc, gg = g // 2, g % 2
            r0 = (sc % 4) * 32 + gg * 16
            BT[:, ri, sc, r0:r0 + 16, gg * 64:(gg + 1) * 64] = bb[:, g].transpose(0, 2, 1)
            CT[:, :, ri, sc, gg * 64:(gg + 1) * 64, r0:r0 + 16] = cc[:, :, g].transpose(0, 1, 3, 2)
    m["ssm_BT"] = BT
    m["ssm_CT"] = CT
    m["ssm_vec"] = np.ascontiguousarray(np.stack([pj(inputs["ssm_d"], 4), pj(inputs["ssm_glu_b"], 4)], axis=2))
    m["ssm_glu_w"] = inputs["ssm_glu_w"]
    for name in ("w_branch", "w_out", "router_w"):
        m[name] = inputs[name]
    for name in ("moe_w1", "moe_w3", "moe_w2"):
        m[name] = inputs[name]
    cst = np.zeros((128, 3, 256), np.float32)
    cst[:, 0, :] = np.arange(1, 257)
    cst[:16, 1, :16] = 1.0
    cst[16:32, 1, 16:32] = 1.0
    cst[:, 2, :128] = np.eye(128)
    m["moe_cst"] = cst
    m["moe_jcol"] = np.stack([np.arange(1, 129), np.arange(129, 257)], axis=1).astype(np.float32)
    L_ = DEPTH
    rv = np.zeros((L_, 128, 51), np.float32)
    rv[:, :, 0:15] = pj(inputs["rwkv_mu"], 15)
    rv[:, :, 15:23] = pj(inputs["rwkv_w0"], 4).transpose(0, 2, 1, 3).reshape(L_, 128, 8)
    rv[:, :, 23:31] = pj(inputs["rwkv_a0"], 4).transpose(0, 2, 1, 3).reshape(L_, 128, 8)
    rv[:, :, 31:35] = pj(inputs["rwkv_k_k"], 4)
    rv[:, :, 35:39] = pj(inputs["rwkv_k_a"], 4)
    rv[:, :, 39:43] = pj(inputs["rwkv_r_k"].reshape(L_, 512), 4)
    rv[:, :, 43:47] = pj(inputs["rwkv_ln_w"], 4)
    rv[:, :, 47:51] = pj(inputs["rwkv_ln_b"], 4)
    m["rw_vec"] = rv
    for name in ("rwkv_w2", "rwkv_a2", "rwkv_g2"):
        m[name] = inputs[name]
    bd = np.zeros((128, 128), np.float32)
    bd[:64, :64] = 1.0
    bd[64:, 64:] = 1.0
    m["rw_bd"] = bd
    rst = np.ones((128, TT + 1), np.float32)
    rst[:, 0::128] = 0.0
    m["rw_rst"] = rst
    ii = np.arange(128)
    mk = np.zeros((128, 2, 640), np.float32)
    for j_ in range(2):
        if j_ == 0:
            strict = (ii[None, :] > ii[:, None]).astype(np.float32)
            incl = (ii[None, :] >= ii[:, None]).astype(np.float32)
        else:
            strict = (ii[None, :] < ii[:, None]).astype(np.float32)
            incl = (ii[None, :] <= ii[:, None]).astype(np.float32)
        mk[:, j_, 0:128] = strict
        mk[:, j_, 128:256] = incl
        mk[:, j_, 256:384] = -strict
        mk[:, j_, 384:512] = incl
        mk[:, j_, 512:640] = -strict.T
    m["rw_masks"] = mk
    lm = np.zeros((128, 4, 128), np.float32)
    ti, si = ii[:, None], ii[None, :]
    lm[:, 0, :] = (ti // 16 == si // 16)
    for q_, sz in enumerate((16, 32, 64)):
        lm[:, 1 + q_, :] = (ti // (2 * sz) == si // (2 * sz)) & (ti // sz != si // sz)
    m["rw_lm"] = lm
    m["ident"] = np.eye(128, dtype=np.float32)
    m["ada_b_r"] = pj(inputs["ada_b"], 48)
    m["ng_r"] = np.ascontiguousarray(np.stack([pj(inputs["norm1_g"], KC), pj(inputs["norm2_g"], KC)], axis=2))
    return m


def kernel(**inputs):
    inputs = {k_: np.asarray(v) for k_, v in inputs.items()}
    P = build()
    shared = host_shared(inputs)
    in_maps = []
    for c in range(NCORES):
        m = host_inputs(inputs, c)
        m.update(shared)
        in_maps.append({n: m[n] for n in P.inputs})
    res = run_bass_kernel_spmd(P.nc, in_maps, core_ids=list(range(NCORES)))
    outs = [r["OUT"] for r in res.results]
    y = np.concatenate(outs, axis=0)
    return np.ascontiguousarray(y.transpose(0, 2, 1)).astype(np.float32)
```

```python
import contextlib
import numpy as np
import concourse.bass as bass
import concourse.mybir as mybir
from concourse.bass_utils import run_bass_kernel_spmd

F32 = mybir.dt.float32
BF16 = mybir.dt.bfloat16
AF = mybir.ActivationFunctionType
ALU = mybir.AluOpType
AX = mybir.AxisListType

NCORES = 8
NB = 2
D = 1024
KC = 8
TC = 256
TL = 2048
TT = TC + TL
DEPTH = 4
N_IN = 6176
NCH = 49
NORM_EPS = 1e-6


def chunk_cols(ci):
    if ci < 24:
        return ci * 128, 128
    if ci == 24:
        return 3072, 32
    return 3104 + (ci - 25) * 128, 128


class Buf:
    __slots__ = ("name", "w", "r")

    def __init__(self, name=""):
        self.name = name
        self.w = None
        self.r = []


class K:
    NDMA = 12

    def __init__(self, nc):
        self.nc = nc
        self.eng = {"pe": nc.tensor, "dve": nc.vector, "act": nc.scalar, "pool": nc.gpsimd, "sp": nc.sync}
        self.sem = {}
        self.cnt = {}
        self.seen = {e: {} for e in self.eng}
        self._stack = []
        for e in self.eng:
            self.sem[e] = self._mksem("s_" + e)
            self.cnt[e] = 0
        self.dq = {}
        for q in ("sp", "act", "pool"):
            self.dq[q] = {"n": 0}
            for i in range(self.NDMA):
                self.sem[(q, i)] = self._mksem(f"d_{q}{i}")
        self.n_instr = 0
        self.n_wait = 0

    def _mksem(self, name):
        g = self.nc.semaphore(name)
        s = g.__enter__()
        self._stack.append(g)
        return s

    def _wait(self, e, dep):
        if dep is None:
            return
        key, val = dep
        if key == "pe" and e == "pe":
            return
        if self.seen[e].get(key, 0) >= val:
            return
        self.eng[e].wait_ge(self.sem[key], val)
        self.seen[e][key] = val
        self.n_wait += 1

    def _deps(self, e, reads, writes):
        for b in reads:
            self._wait(e, b.w)
        for b in writes:
            self._wait(e, b.w)
            for r in b.r:
                self._wait(e, r)

    def _mark(self, tok, reads, writes):
        for b in reads:
            b.r = [r for r in b.r if r[0] != tok[0]]
            b.r.append(tok)
        for b in writes:
            b.w = tok
            b.r = []

    def op(self, e, fn, reads=(), writes=()):
        self._deps(e, reads, writes)
        ins = fn(self.eng[e])
        self.cnt[e] += 1
        ins.then_inc(self.sem[e], 1)
        self._mark((e, self.cnt[e]), reads, writes)
        self.n_instr += 1
        return ins

    def dma(self, q, out, in_, reads=(), writes=(), **kw):
        d = self.dq[q]
        i = d["n"] % self.NDMA
        gen = d["n"] // self.NDMA
        key = (q, i)
        if gen > 0:
            self._wait(q, (key, 16 * gen))
        self._deps(q, reads, writes)
        ins = self.eng[q].dma_start(out=out, in_=in_, **kw)
        ins.then_inc(self.sem[key], 16)
        d["n"] += 1
        self._mark((key, 16 * (gen + 1)), reads, writes)
        self.n_instr += 1

    def barrier(self):
        toks = [(e, self.cnt[e]) for e in self.eng if self.cnt[e] > 0]
        for q, d in self.dq.items():
            n = d["n"]
            for i in range(self.NDMA):
                c = (n - i + self.NDMA - 1) // self.NDMA
                if c > 0:
                    toks.append(((q, i), 16 * c))
        for e in self.eng:
            for t in toks:
                self._wait(e, t)


class Ring:
    def __init__(self, P, name, n, shape, dtype, psum=False):
        self.items = []
        for i in range(n):
            t = P.ps(f"{name}{i}", shape, dtype) if psum else P.sb(f"{name}{i}", shape, dtype)
            self.items.append((t, Buf(f"{name}{i}")))
        self.i = 0

    def next(self):
        it = self.items[self.i % len(self.items)]
        self.i += 1
        return it


class Prog:
    def __init__(self, n_layers=DEPTH, debug=()):
        self.nc = nc = bass.Bass("TRN2", target_bir_lowering=False)
        self.k = K(nc)
        self.debug = set(debug)
        self.n_layers = n_layers
        self._scopes = []
        self.uid = 0
        self.inputs = {}
        self.outputs = {}
        self.dbufs = {}

    def din(self, name, shape, dtype=F32):
        t = self.nc.dram_tensor(name, list(shape), dtype, kind="ExternalInput").ap()
        self.inputs[name] = t
        return t

    def dscr(self, name, shape, dtype, out=False):
        kind = "ExternalOutput" if (out or name in self.debug) else "Internal"
        t = self.nc.dram_tensor(name, list(shape), dtype, kind=kind).ap()
        if kind == "ExternalOutput":
            self.outputs[name] = t
        self.dbufs[name] = Buf(name)
        return t

    @contextlib.contextmanager
    def scope(self):
        st = contextlib.ExitStack()
        self._scopes.append(st)
        try:
            yield
        finally:
            self.k.barrier()
            self._scopes.pop()
            st.close()

    def sb(self, name, shape, dtype):
        self.uid += 1
        g = self.nc.sbuf_tensor(f"{name}_{self.uid}", list(shape), dtype)
        return self._scopes[-1].enter_context(g)

    def ps(self, name, shape, dtype=F32):
        self.uid += 1
        g = self.nc.psum_tensor(f"{name}_{self.uid}", list(shape), dtype)
        return self._scopes[-1].enter_context(g)


MLA_SCALE = 1.0 / float(np.sqrt(96.0))


def declare_inputs(P, C):
    C.mla_w_uq = P.din("mla_w_uq", [DEPTH, 384, 768])
    C.mla_w_ukv = P.din("mla_w_ukv", [DEPTH, 256, 1024])
    C.mla_mats = P.din("mla_mats", [128, 3, 128])
    C.mla_vec = P.din("mla_vec", [DEPTH, 128, 8])
    C.rope_tab = P.din("rope_tab", [96, 2, TT])
    C.ssm_iota = P.din("ssm_iota", [128, 2, TT])
    C.ssm_sv = P.din("ssm_sv", [DEPTH, 128, 2, 16, 3])
    C.ssm_BT = P.din("ssm_BT", [DEPTH, 2, 16, 128, 128])
    C.ssm_CT = P.din("ssm_CT", [DEPTH, 2, 2, 16, 128, 128])
    C.ssm_vec = P.din("ssm_vec", [DEPTH, 128, 2, 4])
    C.ssm_glu_w = P.din("ssm_glu_w", [DEPTH, 512, 512])
    C.w_branch = P.din("w_branch", [DEPTH, 3, 512, D])
    C.w_out = P.din("w_out", [DEPTH, D, D])
    C.router_w = P.din("router_w", [DEPTH, D, 16])
    C.ident = P.din("ident", [128, 128])
    C.LG = P.dscr("LG", [NB, 16, TT], F32)
    C.bLG = Buf("LG")
    C.H2 = P.dscr("H2", [NB, TT, D], BF16)
    C.bH2 = Buf("H2")
    C.moe_w1 = P.din("moe_w1", [DEPTH, NE, D, FF])
    C.moe_w3 = P.din("moe_w3", [DEPTH, NE, D, FF])
    C.moe_w2 = P.din("moe_w2", [DEPTH, NE, FF, D])
    C.moe_cst = P.din("moe_cst", [128, 3, 256])
    C.moe_jcol = P.din("moe_jcol", [128, 2])
    C.POSD = P.dscr("POSD", [NB, NE, TT], F32)
    C.MGD = P.dscr("MGD", [NB, NE, TT], F32)
    C.bPOSD = Buf("POSD")
    C.XS = P.dscr("XS", [NE, D, NJ], BF16)
    C.bXS = Buf("XS")
    C.YE = P.dscr("YE", [NE, NJ, D], BF16)
    C.bYE = Buf("YE")
    C.OUT = P.dscr("OUT", [NB, D, TL], F32, out=True)
    C.bOUT = Buf("OUT")
    C.rw_vec = P.din("rw_vec", [DEPTH, 128, 51])
    C.rwkv_w2 = P.din("rwkv_w2", [DEPTH, 2, 64, 512])
    C.rwkv_a2 = P.din("rwkv_a2", [DEPTH, 2, 64, 512])
    C.rwkv_g2 = P.din("rwkv_g2", [DEPTH, 128, 512])
    C.rw_bd = P.din("rw_bd", [128, 128])
    C.rw_rst = P.din("rw_rst", [128, TT + 1])
    C.rw_masks = P.din("rw_masks", [128, 2, 640])
    C.rw_lm = P.din("rw_lm", [128, 4, 128])
    C.RWT = P.dscr("RWT", [NB, 2, 4, 512, TT], BF16)
    C.RWV = P.dscr("RWV", [NB, 512, TT], BF16)
    C.RWG = P.dscr("RWG", [NB, 512, TT], BF16)
    C.RWB = P.dscr("RWB", [NB, 512, TT], F32)
    C.RWPL = P.dscr("RWPL", [NB, 2, 512, NCK], F32)
    C.bRW = Buf("RW")
    if "YTOK" in P.debug:
        C.YTOK = P.dscr("YTOK", [NB, 128, NCK, 512], F32)
    C.YG = P.dscr("YG", [NB, 512, TT], BF16)
    C.bYG = Buf("YG")


def rstd_from(k, ss_ap, out_ap, scale, bias, reads, bout):
    k.op("act", lambda e: e.activation(out_ap, ss_ap, AF.Sqrt, bias=bias, scale=scale), reads=reads, writes=[bout])
    k.op("dve", lambda e: e.reciprocal(out_ap, out_ap), reads=[bout], writes=[bout])


def phase_mla(P, C, l):
    k = P.k
    PT, YT = C.PT, C.YT
    blocks = [(0, TC)] + [(TC + i * 512, 512) for i in range(4)]
    with P.scope():
        mats = P.sb("mla_mats", [128, 3, 128], BF16)
        b_mats = Buf("mats")
        k.dma("pool", mats[:], C.mla_mats, writes=[b_mats])
        onesb = P.sb("onesb", [128, 128], BF16)
        b_onesb = Buf("onesb")
        k.op("dve", lambda e: e.memset(onesb[:], 1.0), writes=[b_onesb])
        vec = P.sb("mla_vec", [128, 8], F32)
        b_vec = Buf("vec")
        k.dma("sp", vec[:], C.mla_vec[l], writes=[b_vec])
        epsc = P.sb("epsc", [128, 1], F32)
        k.op("dve", lambda e: e.memset(epsc[:], NORM_EPS), writes=[b_vec])
        tab = P.sb("rope_tab", [96, 2, TT], F32)
        b_tab = Buf("tab")
        k.dma("sp", tab[:], C.rope_tab, writes=[b_tab])
        wuq = P.sb("wuq", [128, 3, 768], BF16)
        b_wuq = Buf("wuq")
        k.dma("pool", wuq[:], C.mla_w_uq[l].rearrange("(kc p) n -> p kc n", p=128), writes=[b_wuq])
        wk = P.sb("wk", [128, 2, 8, 64], BF16)
        wv = P.sb("wv", [128, 2, 8, 64], BF16)
        b_wkv = Buf("wkv")
        ukv = C.mla_w_ukv[l].rearrange("(kc p) (h x) -> p kc h x", p=128, x=128)
        for kc in range(2):
            k.dma("pool", wk[:, kc], ukv[:, kc, :, 0:64], writes=[b_wkv])
            k.dma("pool", wv[:, kc], ukv[:, kc, :, 64:128], writes=[b_wkv])
        QT = P.sb("QT", [96, 8, TT], BF16)
        KT = P.sb("KT", [96, 8, TT], BF16)
        Vt = P.sb("Vt", [128, 18, 512], BF16)
        for b in range(NB):
            b_Q = [Buf(f"Q{i}") for i in range(5)]
            b_K = [Buf(f"K{i}") for i in range(5)]
            b_V = [Buf(f"V{i}") for i in range(5)]
            with P.scope():
                x5r = Ring(P, "x5", 2, [128, 5, 512], BF16)
                krr = Ring(P, "kr", 2, [96, 512], BF16)
                sq5r = Ring(P, "sq5", 1, [128, 5, 512], BF16)
                cqnr = Ring(P, "cqn", 2, [128, 5, 512], BF16)
                psA = Ring(P, "psA", 3, [128, 512], F32, psum=True)
                psB = Ring(P, "psB", 3, [128, 512], F32, psum=True)
                rsr = Ring(P, "rs", 3, [128, 512], F32)
                f32r = Ring(P, "f32t", 3, [128, 512], F32)
                f32r2 = Ring(P, "f32u", 3, [128, 512], F32)
                bfr = Ring(P, "bft", 3, [128, 512], BF16)
                krf = P.sb("krf", [96, 512], BF16)
                b_krf = Buf("krf")
                for bi, (t0, n) in enumerate(blocks):
                    x5, bx5 = x5r.next()
                    k.dma("sp", x5[:, :, :n], PT[b, 19 * 128:24 * 128, t0:t0 + n].rearrange("(c p) n -> p c n", p=128),
                          reads=[C.bPT], writes=[bx5])
                    kr, bkr = krr.next()
                    k.dma("sp", kr[64:96, :n], PT[b, 24 * 128:24 * 128 + 32, t0:t0 + n], reads=[C.bPT], writes=[bkr])
                    sq5, bsq5 = sq5r.next()
                    k.op("act", lambda e: e.activation(sq5[:, :, :n], x5[:, :, :n], AF.Square), reads=[bx5], writes=[bsq5])
                    cqn, bcqn = cqnr.next()
                    for (c0, nc_, col0, dim) in ((0, 3, 0, 384.0), (3, 2, 3, 256.0)):
                        ps, bps = psA.next()
                        for c in range(nc_):
                            k.op("pe", lambda e: e.matmul(ps[:, :n], onesb[:], sq5[:, c0 + c, :n],
                                                          start=(c == 0), stop=(c == nc_ - 1)),
                                 reads=[bsq5, b_onesb], writes=[bps])
                        rs, brs = rsr.next()
                        rstd_from(k, ps[:, :n], rs[:, :n], 1.0 / dim, epsc[:, 0:1], [bps, b_vec], brs)
                        for c in range(nc_):
                            k.op("dve", lambda e: e.scalar_tensor_tensor(
                                cqn[:, c0 + c, :n], x5[:, c0 + c, :n], vec[:, col0 + c:col0 + c + 1], rs[:, :n],
                                ALU.mult, ALU.mult), reads=[bx5, brs, b_vec], writes=[bcqn])
                    sqk, bsqk = bfr.next()
                    k.op("act", lambda e: e.activation(sqk[64:96, :n], kr[64:96, :n], AF.Square), reads=[bkr], writes=[bsqk])
                    ps, bps = psA.next()
                    k.op("pe", lambda e: e.matmul(ps[64:96, :n], mats[64:96, 0, 64:96], sqk[64:96, :n], start=True, stop=True),
                         reads=[bsqk, b_mats], writes=[bps])
                    rs, brs = rsr.next()
                    rstd_from(k, ps[64:96, :n], rs[64:96, :n], 1.0 / 32.0, epsc[64:96, 0:1], [bps, b_vec], brs)
                    krn, bkrn = bfr.next()
                    k.op("dve", lambda e: e.scalar_tensor_tensor(
                        krn[64:96, :n], kr[64:96, :n], vec[64:96, 6:7], rs[64:96, :n], ALU.mult, ALU.mult),
                        reads=[bkr, brs, b_vec], writes=[bkrn])
                    ps2, bps2 = psB.next()
                    k.op("pe", lambda e: e.matmul(ps2[64:96, :n], mats[64:96, 2, 64:96], krn[64:96, :n], start=True, stop=True),
                         reads=[bkrn, b_mats], writes=[bps2])
                    t1, bt1 = f32r.next()
                    k.op("dve", lambda e: e.tensor_tensor(t1[64:96, :n], krn[64:96, :n], tab[64:96, 0, t0:t0 + n], ALU.mult),
                         reads=[bkrn, b_tab], writes=[bt1])
                    t2, bt2 = f32r2.next()
                    k.op("dve", lambda e: e.tensor_tensor(t2[64:96, :n], ps2[64:96, :n], tab[64:96, 1, t0:t0 + n], ALU.mult),
                         reads=[bps2, b_tab], writes=[bt2])
                    k.op("pool", lambda e: e.tensor_tensor(krf[64:96, :n], t1[64:96, :n], t2[64:96, :n], ALU.add),
                         reads=[bt1, bt2], writes=[b_krf])
                    k.op("pool", lambda e: e.tensor_copy(
                        KT[64:96, :, t0:t0 + n], krf[64:96, :n].unsqueeze(1).to_broadcast([32, 8, n])),
                        reads=[b_krf], writes=[b_K[bi]])
                    for h in range(8):
                        ps, bps = psA.next()
                        for c in range(3):
                            k.op("pe", lambda e: e.matmul(ps[:96, :n], wuq[:, c, h * 96:(h + 1) * 96], cqn[:, c, :n],
                                                          start=(c == 0), stop=(c == 2)),
                                 reads=[bcqn, b_wuq], writes=[bps])
                        qs, bqs = f32r.next()
                        k.op("act", lambda e: e.activation(qs[:96, :n], ps[:96, :n], AF.Copy), reads=[bps], writes=[bqs])
                        sq, bsq = bfr.next()
                        k.op("act", lambda e: e.activation(sq[:96, :n], ps[:96, :n], AF.Square), reads=[bps], writes=[bsq])
                        ps2, bps2 = psB.next()
                        k.op("pe", lambda e: e.matmul(ps2[:96, :n], mats[:96, 0, :96], sq[:96, :n], start=True, stop=True),
                             reads=[bsq, b_mats], writes=[bps2])
                        rs, brs = rsr.next()
                        rstd_from(k, ps2[:96, :n], rs[:96, :n], vec[:96, 7:8], epsc[:96, 0:1], [bps2, b_vec], brs)
                        qn, bqn = bfr.next()
                        k.op("dve", lambda e: e.scalar_tensor_tensor(
                            qn[:96, :n], qs[:96, :n], vec[:96, 5:6], rs[:96, :n], ALU.mult, ALU.mult),
                            reads=[bqs, brs, b_vec], writes=[bqn])
                        ps3, bps3 = psB.next()
                        k.op("pe", lambda e: e.matmul(ps3[:96, :n], mats[:96, 2, :96], qn[:96, :n], start=True, stop=True),
                             reads=[bqn, b_mats], writes=[bps3])
                        t1, bt1 = f32r.next()
                        k.op("pool", lambda e: e.tensor_tensor(t1[:96, :n], qn[:96, :n], tab[:, 0, t0:t0 + n], ALU.mult),
                             reads=[bqn, b_tab], writes=[bt1])
                        t2, bt2 = f32r2.next()
                        k.op("dve", lambda e: e.tensor_tensor(t2[:96, :n], ps3[:96, :n], tab[:, 1, t0:t0 + n], ALU.mult),
                             reads=[bps3, b_tab], writes=[bt2])
                        k.op("pool", lambda e: e.tensor_tensor(QT[:, h, t0:t0 + n], t1[:96, :n], t2[:96, :n], ALU.add),
                             reads=[bt1, bt2], writes=[b_Q[bi]])
                        ps, bps = psA.next()
                        for c in range(2):
                            k.op("pe", lambda e: e.matmul(ps[:64, :n], wk[:, c, h, :], cqn[:, 3 + c, :n],
                                                          start=(c == 0), stop=(c == 1)),
                                 reads=[bcqn, b_wkv], writes=[bps])
                        ks_, bks = f32r.next()
                        k.op("act", lambda e: e.activation(ks_[:64, :n], ps[:64, :n], AF.Copy), reads=[bps], writes=[bks])
                        sq, bsq = bfr.next()
                        k.op("act", lambda e: e.activation(sq[:64, :n], ps[:64, :n], AF.Square), reads=[bps], writes=[bsq])
                        ps2, bps2 = psB.next()
                        k.op("pe", lambda e: e.matmul(ps2[:64, :n], onesb[:64, :64], sq[:64, :n], start=True, stop=True),
                             reads=[bsq, b_onesb], writes=[bps2])
                        rs, brs = rsr.next()
                        rstd_from(k, ps2[:64, :n], rs[:64, :n], 1.0 / 64.0, epsc[:64, 0:1], [bps2, b_vec], brs)
                        k.op("dve", lambda e: e.scalar_tensor_tensor(
                            KT[0:64, h, t0:t0 + n], ks_[:64, :n], vec[:64, 6:7], rs[:64, :n], ALU.mult, ALU.mult),
                            reads=[bks, brs, b_vec], writes=[b_K[bi]])
                    for ti in range(n // 128):
                        tile_i = (t0 // 128) + ti
                        ps, bps = psA.next()
                        for c in range(2):
                            k.op("pe", lambda e: e.matmul(
                                ps[:, :], cqn[:, 3 + c, ti * 128:(ti + 1) * 128],
                                wv[:, c].rearrange("p h x -> p (h x)"), start=(c == 0), stop=(c == 1)),
                                reads=[bcqn, b_wkv], writes=[bps])
                        k.op("act", lambda e: e.activation(Vt[:, tile_i, :], ps[:, :], AF.Copy), reads=[bps], writes=[b_V[bi]])
            if "stop_mlaprep" in P.debug:
                continue
            with P.scope():
                psS = Ring(P, "psS", 4, [128, 512], F32, psum=True)
                psO = Ring(P, "psO", 2, [64, 512], F32, psum=True)
                psD = Ring(P, "psD", 2, [64, 512], F32, psum=True)
                ptr = Ring(P, "pT", 4, [128, 512], BF16)
                rdr = Ring(P, "rden", 2, [64, 512], F32)
                osr = Ring(P, "ost", 3, [64, 512], BF16)
                allK = b_K + b_V
                LOOK = 2
                for h in range(8):
                    for bi, (q0, n) in enumerate(blocks):
                        nkt = 2 if bi == 0 else 18
                        po, bpo = psO.next()
                        pd, bpd = psD.next()
                        pend = []
                        for kt in range(nkt + LOOK):
                            if kt < nkt:
                                pS, bpS = psS.next()
                                k.op("pe", lambda e: e.matmul(pS[:, :n], KT[:, h, kt * 128:(kt + 1) * 128], QT[:, h, q0:q0 + n],
                                                              start=True, stop=True),
                                     reads=allK + [b_Q[bi]], writes=[bpS])
                                pT, bpT = ptr.next()
                                k.op("act", lambda e: e.activation(pT[:, :n], pS[:, :n], AF.Exp, scale=MLA_SCALE),
                                     reads=[bpS], writes=[bpT])
                                pend.append((kt, pT, bpT))
                            if kt >= LOOK:
                                k2, pT2, bpT2 = pend.pop(0)
                                k.op("pe", lambda e: e.matmul(po[:, :n], Vt[:, k2, h * 64:(h + 1) * 64], pT2[:, :n],
                                                              start=(k2 == 0), stop=(k2 == nkt - 1)),
                                     reads=[bpT2] + b_V, writes=[bpo])
                                k.op("pe", lambda e: e.matmul(pd[:, :n], onesb[:, :64], pT2[:, :n],
                                                              start=(k2 == 0), stop=(k2 == nkt - 1)),
                                     reads=[bpT2, b_onesb], writes=[bpd])
                        rd, brd = rdr.next()
                        k.op("dve", lambda e: e.reciprocal(rd[:, :n], pd[:, :n]), reads=[bpd], writes=[brd])
                        os_, bos = osr.next()
                        k.op("dve", lambda e: e.tensor_tensor(os_[:, :n], po[:, :n], rd[:, :n], ALU.mult),
                             reads=[bpo, brd], writes=[bos])
                        k.dma("sp", YT[b, 2, h * 64:(h + 1) * 64, q0:q0 + n], os_[:, :n], reads=[bos], writes=[C.bYT])


PI = float(np.pi)


def rev_ap(ap2d, lo, hi):
    from concourse.ap import AP
    a = ap2d[:, lo:hi]
    return AP(a.tensor, a.offset + (hi - lo - 1) * a.ap[1][0], [list(a.ap[0]), [-a.ap[1][0], hi - lo]])


MAGIC = 12582912.0
TWO_PI = 2.0 * PI


def sin_reduced(k, out, tmp, in0, th, th2pi, shift, hp_tile, reads, bout, btmp):
    k.op("dve", lambda e: e.tensor_scalar(tmp, in0, th2pi, MAGIC + shift / TWO_PI, ALU.mult, ALU.add),
         reads=reads + [btmp], writes=[btmp])
    k.op("dve", lambda e: e.tensor_scalar(tmp, tmp, -MAGIC, -TWO_PI, ALU.add, ALU.mult), reads=[btmp], writes=[btmp])
    k.op("dve", lambda e: e.scalar_tensor_tensor(tmp, in0, th, tmp, ALU.mult, ALU.add), reads=reads + [btmp], writes=[btmp])
    if shift == 0.0:
        k.op("act", lambda e: e.activation(out, tmp, AF.Sin, scale=0.999999), reads=[btmp, bout], writes=[bout])
    else:
        k.op("act", lambda e: e.activation(out, tmp, AF.Sin, scale=0.999999, bias=hp_tile), reads=[btmp, bout], writes=[bout])


def phase_ssm(P, C, l):
    k = P.k
    PT, YT = C.PT, C.YT
    blocks = [(0, TC)] + [(TC + i * 512, 512) for i in range(4)]
    YG = C.YG
    with P.scope():
        iota = P.sb("iota", [128, 2, TT], F32)
        b_c = Buf("ssmconst")
        k.dma("sp", iota[:], C.ssm_iota, writes=[b_c])
        sv = P.sb("sv", [128, 2, 16, 3], F32)
        k.dma("sp", sv[:], C.ssm_sv[l], writes=[b_c])
        BT = P.sb("BT", [128, 2, 16, 128], BF16)
        k.dma("pool", BT[:], C.ssm_BT[l].rearrange("r s p n -> p r s n"), writes=[b_c])
        CT = P.sb("CT", [128, 2, 2, 16, 128], BF16)
        for j in range(2):
            k.dma("pool", CT[:, j], C.ssm_CT[l, j].rearrange("r s p n -> p r s n"), writes=[b_c])
        dsk = P.sb("dsk", [128, 2, 4], F32)
        k.dma("sp", dsk[:], C.ssm_vec[l], writes=[b_c])
        hpi = P.sb("hpi", [128, 1], F32)
        k.op("dve", lambda e: e.memset(hpi[:], 0.5 * PI * 0.999999), writes=[b_c])
        shp = [128, 2, 16]
        names = "dt rho th th2 sn cs are aim den t1 t2 cre cim ncre".split()
        Tl = {n_: P.sb("d_" + n_, shp, F32) for n_ in names}
        bd = Buf("disc")
        lr, li, ldt = sv[:, :, :, 0], sv[:, :, :, 1], sv[:, :, :, 2]
        A_ = lambda e_, fn: k.op(e_, fn, reads=[b_c, bd], writes=[bd])
        A_("act", lambda e: e.activation(Tl["dt"][:], ldt, AF.Exp))
        A_("dve", lambda e: e.tensor_tensor(Tl["t1"][:], lr, Tl["dt"][:], ALU.mult))
        A_("act", lambda e: e.activation(Tl["rho"][:], Tl["t1"][:], AF.Exp))
        A_("dve", lambda e: e.tensor_tensor(Tl["th"][:], li, Tl["dt"][:], ALU.mult))
        sin_reduced(k, Tl["sn"][:], Tl["t1"][:], Tl["th"][:], 1.0, 1.0 / TWO_PI, 0.0, None, [b_c, bd], bd, bd)
        sin_reduced(k, Tl["cs"][:], Tl["t1"][:], Tl["th"][:], 1.0, 1.0 / TWO_PI, 0.5 * PI, hpi[:, 0:1], [b_c, bd], bd, bd)
        A_("dve", lambda e: e.tensor_scalar(Tl["th2"][:], Tl["th"][:], 1.0 / TWO_PI, None, ALU.mult))
        A_("dve", lambda e: e.tensor_tensor(Tl["are"][:], Tl["rho"][:], Tl["cs"][:], ALU.mult))
        A_("dve", lambda e: e.tensor_tensor(Tl["aim"][:], Tl["rho"][:], Tl["sn"][:], ALU.mult))
        A_("dve", lambda e: e.tensor_scalar(Tl["are"][:], Tl["are"][:], -1.0, None, ALU.add))
        A_("dve", lambda e: e.tensor_tensor(Tl["t1"][:], lr, lr, ALU.mult))
        A_("dve", lambda e: e.tensor_tensor(Tl["t2"][:], li, li, ALU.mult))
        A_("dve", lambda e: e.tensor_tensor(Tl["den"][:], Tl["t1"][:], Tl["t2"][:], ALU.add))
        A_("dve", lambda e: e.reciprocal(Tl["den"][:], Tl["den"][:]))
        A_("dve", lambda e: e.tensor_tensor(Tl["t1"][:], Tl["are"][:], lr, ALU.mult))
        A_("dve", lambda e: e.tensor_tensor(Tl["t2"][:], Tl["aim"][:], li, ALU.mult))
        A_("dve", lambda e: e.tensor_tensor(Tl["cre"][:], Tl["t1"][:], Tl["t2"][:], ALU.add))
        A_("dve", lambda e: e.tensor_tensor(Tl["cre"][:], Tl["cre"][:], Tl["den"][:], ALU.mult))
        A_("dve", lambda e: e.tensor_tensor(Tl["t1"][:], Tl["aim"][:], lr, ALU.mult))
        A_("dve", lambda e: e.tensor_tensor(Tl["t2"][:], Tl["are"][:], li, ALU.mult))
        A_("dve", lambda e: e.tensor_tensor(Tl["cim"][:], Tl["t1"][:], Tl["t2"][:], ALU.subtract))
        A_("dve", lambda e: e.tensor_tensor(Tl["cim"][:], Tl["cim"][:], Tl["den"][:], ALU.mult))
        A_("dve", lambda e: e.tensor_scalar(Tl["ncre"][:], Tl["cre"][:], -1.0, None, ALU.mult))
        big = lambda n_: P.sb(n_, [128, TT], F32)
        bigb = lambda n_: P.sb(n_, [128, TT], BF16)
        CS, SN, ERE, EIM = bigb("CS"), bigb("SN"), bigb("ERE"), bigb("EIM")
        BUR, BUI = bigb("BUR"), bigb("BUI")
        WK = [[bigb(f"{x}{b}") for x in ("T1", "T2", "ZR", "ZI")] for b in range(NB)]
        TA, TB = big("TA"), big("TB")
        b_ta, b_tb = Buf("ta"), Buf("tb")
        bWK = [[Buf(f"{x}{b}") for x in ("t1", "t2", "zr", "zi")] for b in range(NB)]
        T1, T2, ZR, ZI = WK[0]
        b_t1, b_t2, b_zr, b_zi = bWK[0]
        b_tab, b_bu = Buf("tab"), Buf("bu")
        Q = [P.sb(f"Q{i}", [128, TT], BF16) for i in range(4)]
        b_q = [Buf(f"q{i}") for i in range(4)]
        U = [P.sb(f"U{b}", [128, TT], BF16) for b in range(NB)]
        b_u = [Buf(f"u{b}") for b in range(NB)]
        YA = [P.sb(f"YA{b}", [128, TT], F32) for b in range(NB)]
        b_ya = [Buf(f"ya{b}") for b in range(NB)]
        psr = Ring(P, "ssmps", 4, [128, 512], F32, psum=True)
        psy = Ring(P, "ssmpy", 3, [128, 512], F32, psum=True)
        for oc in range(4):
            for b in range(NB):
                k.dma("sp", U[b][:], PT[b, oc * 128:(oc + 1) * 128, :], reads=[C.bPT], writes=[b_u[b]])
                k.op("pool", lambda e: e.memset(YA[b][:], 0.0), writes=[b_ya[b]])
            for j in range(2):
                for s4 in range(4):
                    sc = oc * 4 + s4
                    th = Tl["th"][:, j, sc:sc + 1]
                    th2 = Tl["th2"][:, j, sc:sc + 1]
                    sin_reduced(k, SN[:], TA[:], iota[:, j, :], th, th2, 0.0, None, [b_c, bd], b_tab, b_ta)
                    sin_reduced(k, CS[:], TB[:], iota[:, j, :], th, th2, 0.5 * PI, hpi[:, 0:1], [b_c, bd], b_tab, b_tb)
                    cre, cim, ncre = (Tl[x][:, j, sc:sc + 1] for x in ("cre", "cim", "ncre"))
                    k.op("dve", lambda e: e.tensor_scalar(ERE[:], CS[:], cre, None, ALU.mult), reads=[b_tab, bd], writes=[b_tab])
                    k.op("dve", lambda e: e.scalar_tensor_tensor(ERE[:], SN[:], cim, ERE[:], ALU.mult, ALU.add),
                         reads=[b_tab, bd], writes=[b_tab])
                    k.op("pool", lambda e: e.tensor_scalar(EIM[:], CS[:], cim, None, ALU.mult), reads=[b_tab, bd], writes=[b_tab])
                    k.op("dve", lambda e: e.scalar_tensor_tensor(EIM[:], SN[:], ncre, EIM[:], ALU.mult, ALU.add),
                         reads=[b_tab, bd], writes=[b_tab])
                    rho = Tl["rho"][:, j, sc:sc + 1]
                    for b in range(NB):
                        T1, T2, ZR, ZI = WK[b]
                        b_t1, b_t2, b_zr, b_zi = bWK[b]
                        for (t0, n) in blocks:
                            for ri, dst in ((0, BUR), (1, BUI)):
                                ps, bps = psr.next()
                                k.op("pe", lambda e: e.matmul(ps[:, :n], BT[:, ri, sc, :], U[b][:, t0:t0 + n], start=True, stop=True),
                                     reads=[b_c, b_u[b]], writes=[bps])
                                k.op("act", lambda e: e.activation(dst[:, t0:t0 + n], ps[:, :n], AF.Copy),
                                     reads=[bps], writes=[b_bu])
                        k.op("dve", lambda e: e.tensor_tensor(T1[:], ERE[:], BUR[:], ALU.mult), reads=[b_tab, b_bu], writes=[b_t1])
                        k.op("pool", lambda e: e.tensor_tensor(T2[:], EIM[:], BUI[:], ALU.mult), reads=[b_tab, b_bu], writes=[b_t2])
                        k.op("dve", lambda e: e.tensor_tensor(ZR[:], T1[:], T2[:], ALU.subtract), reads=[b_t1, b_t2], writes=[b_zr])
                        k.op("pool", lambda e: e.tensor_tensor(T1[:], ERE[:], BUI[:], ALU.mult), reads=[b_tab, b_bu, b_zr], writes=[b_t1])
                        k.op("dve", lambda e: e.tensor_tensor(T2[:], EIM[:], BUR[:], ALU.mult), reads=[b_tab, b_bu, b_zr], writes=[b_t2])
                        k.op("pool", lambda e: e.tensor_tensor(ZI[:], T1[:], T2[:], ALU.add), reads=[b_t1, b_t2], writes=[b_zi])
                        for Z, bz, eng in ((ZR, b_zr, "dve"), (ZI, b_zi, "dve")):
                            if j == 0:
                                k.op(eng, lambda e: e.tensor_tensor_scan(Z[:], rho.to_broadcast([128, TT]), Z[:], 0.0, ALU.mult, ALU.add),
                                     reads=[bz, bd], writes=[bz])
                            else:
                                r0 = rev_ap(Z[:], 0, TC)
                                k.op(eng, lambda e: e.tensor_tensor_scan(r0, rho.to_broadcast([128, TC]), r0, 0.0, ALU.mult, ALU.add),
                                     reads=[bz, bd], writes=[bz])
                                r1 = rev_ap(Z[:], TC, TT)
                                k.op(eng, lambda e: e.tensor_tensor_scan(r1, rho.to_broadcast([128, TL]), r1, Z[:, 0:1], ALU.mult, ALU.add),
                                     reads=[bz, bd], writes=[bz])
                        k.op("pool", lambda e: e.tensor_tensor(Q[0][:], CS[:], ZR[:], ALU.mult), reads=[b_tab, b_zr], writes=[b_q[0]])
                        for qi, (tabl, Z, bz) in ((1, (SN, ZI, b_zi)), (2, (SN, ZR, b_zr)), (3, (CS, ZI, b_zi))):
                            k.op("dve", lambda e: e.scalar_tensor_tensor(Q[qi][:], tabl[:], -1.0, Z[:], ALU.mult, ALU.mult),
                                 reads=[b_tab, bz], writes=[b_q[qi]])
                        lhs = (CT[:, j, 0, sc, :], CT[:, j, 0, sc, :], CT[:, j, 1, sc, :], CT[:, j, 1, sc, :])
                        for (t0, n) in blocks:
                            ps, bps = psy.next()
                            for qi in range(4):
                                k.op("pe", lambda e: e.matmul(ps[:, :n], lhs[qi], Q[qi][:, t0:t0 + n], start=(qi == 0), stop=(qi == 3)),
                                     reads=[b_c, b_q[qi]], writes=[bps])
                            k.op("dve", lambda e: e.tensor_tensor(YA[b][:, t0:t0 + n], YA[b][:, t0:t0 + n], ps[:, :n], ALU.add),
                                 reads=[bps, b_ya[b]], writes=[b_ya[b]])
            for b in range(NB):
                k.op("dve", lambda e: e.scalar_tensor_tensor(TA[:], U[b][:], dsk[:, 0, oc:oc + 1], YA[b][:], ALU.mult, ALU.add),
                     reads=[b_u[b], b_ya[b], b_c, b_ta], writes=[b_ta])
                k.op("act", lambda e: e.activation(TB[:], TA[:], AF.Square), reads=[b_ta, b_tb], writes=[b_tb])
                k.op("dve", lambda e: e.tensor_scalar(TB[:], TB[:], 0.044715, 1.0, ALU.mult, ALU.add), reads=[b_tb], writes=[b_tb])
                k.op("pool", lambda e: e.tensor_tensor(TB[:], TB[:], TA[:], ALU.mult), reads=[b_ta, b_tb], writes=[b_tb])
                k.op("act", lambda e: e.activation(TB[:], TB[:], AF.Sigmoid, scale=1.5957691216057308), reads=[b_tb], writes=[b_tb])
                k.op("dve", lambda e: e.tensor_tensor(Q[b][:], TA[:], TB[:], ALU.mult), reads=[b_ta, b_tb, b_q[b]], writes=[b_q[b]])
                k.dma("sp", YG[b, oc * 128:(oc + 1) * 128, :], Q[b][:], reads=[b_q[b]], writes=[C.bYG])
    with P.scope():
        gw = P.sb("gluw", [128, 4, 512], BF16)
        b_gw = Buf("gluw")
        k.dma("pool", gw[:], C.ssm_glu_w[l].rearrange("(kc p) n -> p kc n", p=128), writes=[b_gw])
        dsk = P.sb("dsk2", [128, 2, 4], F32)
        k.dma("sp", dsk[:], C.ssm_vec[l], writes=[b_gw])
        ygr = Ring(P, "yg", 2, [128, 4, 512], BF16)
        psr = Ring(P, "glups", 4, [128, 512], F32, psum=True)
        sgr = Ring(P, "sg", 3, [128, 512], F32)
        str_ = Ring(P, "gst", 3, [128, 512], BF16)
        for b in range(NB):
            for (t0, n) in blocks:
                yg, byg = ygr.next()
                k.dma("sp", yg[:, :, :n], YG[b, :, t0:t0 + n].rearrange("(c p) n -> p c n", p=128), reads=[C.bYG], writes=[byg])
                for oc in range(4):
                    ps, bps = psr.next()
                    for kc in range(4):
                        k.op("pe", lambda e: e.matmul(ps[:, :n], gw[:, kc, oc * 128:(oc + 1) * 128], yg[:, kc, :n],
                                                      start=(kc == 0), stop=(kc == 3)), reads=[b_gw, byg], writes=[bps])
                    sg, bsg = sgr.next()
                    k.op("act", lambda e: e.activation(sg[:, :n], ps[:, :n], AF.Sigmoid, bias=dsk[:, 1, oc:oc + 1]),
                         reads=[bps, b_gw], writes=[bsg])
                    st, bst = str_.next()
                    k.op("dve", lambda e: e.tensor_tensor(st[:, :n], yg[:, oc, :n], sg[:, :n], ALU.mult), reads=[byg, bsg], writes=[bst])
                    k.dma("sp", YT[b, 0, oc * 128:(oc + 1) * 128, t0:t0 + n], st[:, :n], reads=[bst], writes=[C.bYT])


def phase_merge(P, C, l):
    k = P.k
    PT, YT, XT = C.PT, C.YT, C.XT
    src_x = C.xt0 if l == 0 else XT
    mod, gs = C.mod, C.gs
    blocks = [(i * 256, 256) for i in range(9)]
    with P.scope():
        wb = P.sb("wb", [128, 3, 4, D], BF16)
        b_w = Buf("mw")
        for j in range(3):
            k.dma("pool", wb[:, j], C.w_branch[l, j].rearrange("(kc p) n -> p kc n", p=128), writes=[b_w])
        wo = P.sb("wo", [128, KC, D], BF16)
        k.dma("pool", wo[:], C.w_out[l].rearrange("(kc p) n -> p kc n", p=128), writes=[b_w])
        rw = P.sb("rw", [128, KC, 16], F32)
        k.dma("sp", rw[:], C.router_w[l].rearrange("(kc p) n -> p kc n", p=128), writes=[b_w])
        ident = P.sb("ident", [128, 128], BF16)
        k.dma("pool", ident[:], C.ident, writes=[b_w])
        y3r = Ring(P, "y3", 2, [128, 3, 4, 256], BF16)
        gr = Ring(P, "g", 2, [128, 24, 256], BF16)
        xr = Ring(P, "mx", 2, [128, KC, 256], F32)
        mTr = Ring(P, "mT", 1, [128, KC, 256], BF16)
        mr = Ring(P, "m", 6, [128, 256], F32)
        x1r = Ring(P, "x1", 1, [128, KC, 256], F32)
        sqr = Ring(P, "msq", 1, [128, KC, 256], F32)
        h2fr = Ring(P, "h2f", 1, [128, KC, 256], F32)
        h2br = Ring(P, "h2b", 1, [128, KC, 256], BF16)
        tsr = Ring(P, "tst", 2, [128, D], BF16)
        rsr = Ring(P, "mrs", 2, [128, 256], F32)
        lgr = Ring(P, "lgs", 2, [16, 256], F32)
        psb = Ring(P, "psb", 3, [128, 256], F32, psum=True)
        pso = Ring(P, "pso", 2, [128, 256], F32, psum=True)
        pss = Ring(P, "pss", 2, [128, 256], F32, psum=True)
        pst = Ring(P, "pst", 1, [128, D], BF16, psum=True)
        for b in range(NB):
            for (t0, n) in blocks:
                j = 2 if t0 < TC else b
                y3, by3 = y3r.next()
                k.dma("sp", y3[:], YT[b, :, :, t0:t0 + n].rearrange("j (c p) n -> p j c n", p=128), reads=[C.bYT], writes=[by3])
                g, bg = gr.next()
                k.dma("act", g[:], PT[b, 25 * 128:49 * 128, t0:t0 + n].rearrange("(c p) n -> p c n", p=128), reads=[C.bPT], writes=[bg])
                x, bx = xr.next()
                k.dma("sp", x[:], src_x[b][:, t0:t0 + n].rearrange("(kc p) n -> p kc n", p=128), reads=[C.bXT[b]], writes=[bx])
                mT, bmT = mTr.next()
                for dc in range(KC):
                    ms = []
                    for jj in range(3):
                        ps, bps = psb.next()
                        for kc in range(4):
                            k.op("pe", lambda e: e.matmul(ps[:], wb[:, jj, kc, dc * 128:(dc + 1) * 128], y3[:, jj, kc, :],
                                                          start=(kc == 0), stop=(kc == 3)), reads=[b_w, by3], writes=[bps])
                        m, bm = mr.next()
                        k.op("dve", lambda e: e.tensor_tensor(m[:], ps[:], g[:, jj * 8 + dc, :], ALU.mult), reads=[bps, bg], writes=[bm])
                        ms.append((m, bm))
                    k.op("pool", lambda e: e.tensor_tensor(ms[0][0][:], ms[0][0][:], ms[1][0][:], ALU.add),
                         reads=[ms[0][1], ms[1][1]], writes=[ms[0][1]])
                    k.op("pool", lambda e: e.tensor_tensor(mT[:, dc, :], ms[0][0][:], ms[2][0][:], ALU.add),
                         reads=[ms[0][1], ms[2][1]], writes=[bmT])
                x1, bx1 = x1r.next()
                for dc in range(KC):
                    ps, bps = pso.next()
                    for kc in range(KC):
                        k.op("pe", lambda e: e.matmul(ps[:], wo[:, kc, dc * 128:(dc + 1) * 128], mT[:, kc, :],
                                                      start=(kc == 0), stop=(kc == KC - 1)), reads=[b_w, bmT], writes=[bps])
                    k.op("dve", lambda e: e.scalar_tensor_tensor(x1[:, dc, :], ps[:], mod[:, 2 * 8 + dc, j:j + 1], x[:, dc, :],
                                                                 ALU.mult, ALU.add), reads=[bps, bx, C.b_mod], writes=[bx1])
                k.dma("sp", XT[b][:, t0:t0 + n].rearrange("(kc p) n -> p kc n", p=128), x1[:], reads=[bx1], writes=[C.bXT[b]])
                sq, bsq = sqr.next()
                k.op("act", lambda e: e.activation(sq[:], x1[:], AF.Square), reads=[bx1], writes=[bsq])
                ss, bss = pss.next()
                for kc in range(KC):
                    k.op("pe", lambda e: e.matmul(ss[:], C.ones[:], sq[:, kc, :], start=(kc == 0), stop=(kc == KC - 1)),
                         reads=[bsq, C.b_ones], writes=[bss])
                rs, brs = rsr.next()
                k.op("act", lambda e: e.activation(rs[:], ss[:], AF.Sqrt, bias=C.epsc[:, 0:1], scale=1.0 / D), reads=[bss], writes=[brs])
                k.op("dve", lambda e: e.reciprocal(rs[:], rs[:]), reads=[brs], writes=[brs])
                k.op("dve", lambda e: e.tensor_tensor(sq[:], x1[:], rs[:].unsqueeze(1).to_broadcast([128, KC, n]), ALU.mult),
                     reads=[bx1, brs, bsq], writes=[bsq])
                h2f, bh2f = h2fr.next()
                for kc in range(KC):
                    k.op("act", lambda e: e.activation(h2f[:, kc, :], sq[:, kc, :], AF.Identity,
                                                       bias=mod[:, 3 * 8 + kc, j:j + 1], scale=gs[:, 1, kc, j:j + 1]),
                         reads=[bsq, C.b_mod, C.b_gs], writes=[bh2f])
                h2b, bh2b = h2br.next()
                k.op("pool", lambda e: e.tensor_copy(h2b[:], h2f[:]), reads=[bh2f], writes=[bh2b])
                lp, blp = pss.next()
                for kc in range(KC):
                    k.op("pe", lambda e: e.matmul(lp[:16, :], rw[:, kc, :], h2f[:, kc, :], start=(kc == 0), stop=(kc == KC - 1)),
                         reads=[b_w, bh2f], writes=[blp])
                lg, blg = lgr.next()
                k.op("act", lambda e: e.activation(lg[:], lp[:16, :], AF.Copy), reads=[blp], writes=[blg])
                k.dma("sp", C.LG[b, :, t0:t0 + n], lg[:], reads=[blg], writes=[C.bLG])
                for tt in range(n // 128):
                    pt, bpt = pst.next()
                    for kc in range(KC):
                        k.op("pe", lambda e: e.transpose(pt[:, kc * 128:(kc + 1) * 128], h2b[:, kc, tt * 128:(tt + 1) * 128], ident[:]),
                             reads=[bh2b, b_w], writes=[bpt])
                    ts, bts = tsr.next()
                    k.op("act", lambda e: e.activation(ts[:], pt[:], AF.Copy), reads=[bpt], writes=[bts])
                    k.dma("sp", C.H2[b, t0 + tt * 128:t0 + (tt + 1) * 128, :], ts[:], reads=[bts], writes=[C.bH2])


NE = 16
FF = 1536
CAPL = 256
CAPC = 32
NJ = 2 * CAPL + 2 * CAPC


def phase_moe(P, C, l, last):
    k = P.k
    XT = C.XT
    mod = C.mod
    with P.scope():
        cst = P.sb("moecst", [128, 3, 256], F32)
        b_c = Buf("moecst")
        k.dma("sp", cst[:], C.moe_cst, writes=[b_c])
        A = P.sb("rA", [32, TT], F32)
        AFF = P.sb("rAFF", [32, TT], F32)
        W = P.sb("rW", [32, TT], F32)
        MG = P.sb("rMG", [32, TT], F32)
        MK = P.sb("rMK", [32, TT], F32)
        PS_ = P.sb("rPOS", [32, TT], F32)
        mx = P.sb("rmx", [32, 8], F32)
        bA, bAFF, bW, bMG, bMK, bPOS, bmx = [Buf(x) for x in "A AFF W MG MK POS mx".split()]
        for b in range(NB):
            k.dma("sp", A[b * 16:(b + 1) * 16, :], C.LG[b], reads=[C.bLG], writes=[bA])
        k.op("act", lambda e: e.activation(A[:], A[:], AF.Exp), reads=[bA], writes=[bA])
        psr = Ring(P, "rps", 2, [128, 512], F32, psum=True)
        for t0 in range(0, TT, 512):
            n = min(512, TT - t0)
            ps, bps = psr.next()
            k.op("pe", lambda e: e.matmul(ps[:32, :n], cst[:32, 1, :32], A[:, t0:t0 + n], start=True, stop=True),
                 reads=[bA, b_c], writes=[bps])
            k.op("dve", lambda e: e.reciprocal(W[:, t0:t0 + n], ps[:32, :n]), reads=[bps], writes=[bW])
        k.op("dve", lambda e: e.tensor_tensor(AFF[:], A[:], W[:], ALU.mult), reads=[bA, bW], writes=[bAFF])
        k.op("dve", lambda e: e.tensor_copy(W[:], AFF[:]), reads=[bAFF, bW], writes=[bW])
        for (lo, hi, cap) in ((0, TC, CAPC), (TC, TT, CAPL)):
            for it in range(cap // 8):
                k.op("dve", lambda e: e.max(out=mx[:], in_=W[:, lo:hi]), reads=[bW, bmx], writes=[bmx])
                k.op("dve", lambda e: e.match_replace(out=W[:, lo:hi], in_to_replace=mx[:], in_values=W[:, lo:hi], imm_value=0.0),
                     reads=[bmx, bW], writes=[bW])
        k.op("dve", lambda e: e.tensor_tensor(MG[:], AFF[:], W[:], ALU.subtract), reads=[bAFF, bW], writes=[bMG])
        k.op("dve", lambda e: e.tensor_single_scalar(MK[:], MG[:], 0.0, ALU.is_gt), reads=[bMG], writes=[bMK])
        for (lo, hi) in ((0, TC), (TC, TT)):
            k.op("dve", lambda e: e.tensor_tensor_scan(PS_[:, lo:hi], cst[:32, 0, 0:1].to_broadcast([32, hi - lo]), MK[:, lo:hi],
                                                       0.0, ALU.mult, ALU.add), reads=[bMK, b_c], writes=[bPOS])
        for b in range(NB):
            k.dma("sp", C.POSD[b], PS_[b * 16:(b + 1) * 16, :], reads=[bPOS], writes=[C.bPOSD])
            k.dma("sp", C.MGD[b], MG[b * 16:(b + 1) * 16, :], reads=[bMG], writes=[C.bPOSD])
        posT = P.sb("posT", [128, 18, 32], F32)
        mkT = P.sb("mkT", [128, 18, 32], F32)
        b_pT = Buf("posT")
        for tt in range(18):
            ps, bps = psr.next()
            k.op("pe", lambda e: e.transpose(ps[:, 0:32], PS_[:, tt * 128:(tt + 1) * 128], cst[:32, 2, :32]),
                 reads=[bPOS, b_c], writes=[bps])
            k.op("pe", lambda e: e.transpose(ps[:, 32:64], MK[:, tt * 128:(tt + 1) * 128], cst[:32, 2, :32]),
                 reads=[bMK, b_c], writes=[bps])
            k.op("act", lambda e: e.activation(posT[:, tt, :], ps[:, 0:32], AF.Copy), reads=[bps], writes=[b_pT])
            k.op("act", lambda e: e.activation(mkT[:, tt, :], ps[:, 32:64], AF.Copy), reads=[bps], writes=[b_pT])
        H2s = P.sb("H2s", [128, 18, D], BF16)
        bH = Buf("H2s")
        selr = Ring(P, "sel", 2, [128, 18, 256], BF16)
        gps = Ring(P, "gps", 3, [128, 256], F32, psum=True)
        gpc = Ring(P, "gpc", 2, [128, 32], F32, psum=True)
        xsr = Ring(P, "xs", 2, [128, KC, CAPL + CAPC], BF16)
        for b in range(NB):
            k.dma("sp", H2s[:], C.H2[b].rearrange("(tt p) d -> p tt d", p=128), reads=[C.bH2], writes=[bH])
            for ex in range(NE):
                col = b * 16 + ex
                sel, bsel = selr.next()
                for tt in range(18):
                    ncap = CAPC if tt < 2 else CAPL
                    k.op("dve" if tt % 2 == 0 else "pool", lambda e: e.tensor_scalar(
                        sel[:, tt, :ncap], cst[:, 0, :ncap], posT[:, tt, col:col + 1], mkT[:, tt, col:col + 1],
                        ALU.is_equal, ALU.mult), reads=[b_c, b_pT], writes=[bsel])
                xs, bxs = xsr.next()
                for kc in range(KC):
                    ps, bps = gps.next()
                    for tt in range(16):
                        k.op("pe", lambda e: e.matmul(ps[:], H2s[:, 2 + tt, kc * 128:(kc + 1) * 128], sel[:, 2 + tt, :],
                                                      start=(tt == 0), stop=(tt == 15)), reads=[bH, bsel], writes=[bps])
                    k.op("act" if kc % 2 == 0 else "dve",
                         (lambda e: e.activation(xs[:, kc, :CAPL], ps[:], AF.Copy)) if kc % 2 == 0 else
                         (lambda e: e.tensor_copy(xs[:, kc, :CAPL], ps[:])), reads=[bps], writes=[bxs])
                    pc, bpc = gpc.next()
                    for tt in range(2):
                        k.op("pe", lambda e: e.matmul(pc[:], H2s[:, tt, kc * 128:(kc + 1) * 128], sel[:, tt, :CAPC],
                                                      start=(tt == 0), stop=(tt == 1)), reads=[bH, bsel], writes=[bpc])
                    k.op("act", lambda e: e.activation(xs[:, kc, CAPL:], pc[:], AF.Copy), reads=[bpc], writes=[bxs])
                k.dma("sp", C.XS[ex, :, b * CAPL:(b + 1) * CAPL].rearrange("(kc p) j -> p kc j", p=128), xs[:, :, :CAPL],
                      reads=[bxs], writes=[C.bXS])
                k.dma("sp", C.XS[ex, :, 2 * CAPL + b * CAPC:2 * CAPL + (b + 1) * CAPC].rearrange("(kc p) j -> p kc j", p=128),
                      xs[:, :, CAPL:], reads=[bxs], writes=[C.bXS])
    if "stop_moeA" in P.debug:
        return
    with P.scope():
        w1r = Ring(P, "w1", 2, [128, KC, FF], BF16)
        w3r = Ring(P, "w3", 2, [128, KC, FF], BF16)
        w2r = Ring(P, "w2", 2, [128, 12, D], BF16)
        xsr = Ring(P, "xsb", 2, [128, KC, NJ], BF16)
        hr = Ring(P, "hid", 2, [128, 12, NJ], BF16)
        slr = Ring(P, "silu", 3, [128, 288], F32)
        yer = Ring(P, "ye", 3, [128, D], BF16)
        ps1 = Ring(P, "ps1", 2, [128, 288], F32, psum=True)
        ps3 = Ring(P, "ps3", 2, [128, 288], F32, psum=True)
        psy = Ring(P, "psy", 3, [128, 512], F32, psum=True)
        for ex in range(NE):
            w1, bw1 = w1r.next()
            w3, bw3 = w3r.next()
            w2, bw2 = w2r.next()
            k.dma("pool", w1[:], C.moe_w1[l, ex].rearrange("(kc p) f -> p kc f", p=128), writes=[bw1])
            k.dma("pool", w3[:], C.moe_w3[l, ex].rearrange("(kc p) f -> p kc f", p=128), writes=[bw3])
            k.dma("pool", w2[:], C.moe_w2[l, ex].rearrange("(fc p) d -> p fc d", p=128), writes=[bw2])
            xs, bxs = xsr.next()
            k.dma("sp", xs[:], C.XS[ex].rearrange("(kc p) j -> p kc j", p=128), reads=[C.bXS], writes=[bxs])
            hid, bh = hr.next()
            for fc in range(12):
                for half in range(2):
                    c0 = half * 288
                    p1, bp1 = ps1.next()
                    p3, bp3 = ps3.next()
                    for kc in range(KC):
                        k.op("pe", lambda e: e.matmul(p1[:], w1[:, kc, fc * 128:(fc + 1) * 128], xs[:, kc, c0:c0 + 288],
                                                      start=(kc == 0), stop=(kc == KC - 1)), reads=[bw1, bxs], writes=[bp1])
                    for kc in range(KC):
                        k.op("pe", lambda e: e.matmul(p3[:], w3[:, kc, fc * 128:(fc + 1) * 128], xs[:, kc, c0:c0 + 288],
                                                      start=(kc == 0), stop=(kc == KC - 1)), reads=[bw3, bxs], writes=[bp3])
                    sl, bsl = slr.next()
                    k.op("act", lambda e: e.activation(sl[:], p1[:], AF.Silu), reads=[bp1], writes=[bsl])
                    k.op("dve", lambda e: e.tensor_tensor(hid[:, fc, c0:c0 + 288], sl[:], p3[:], ALU.mult),
                         reads=[bsl, bp3], writes=[bh])
            for jt in range(5):
                nj = 128 if jt < 4 else NJ - 512
                ye, bye = yer.next()
                for dh in range(2):
                    py, bpy = psy.next()
                    for fc in range(12):
                        k.op("pe", lambda e: e.matmul(py[:nj, :], hid[:, fc, jt * 128:jt * 128 + nj], w2[:, fc, dh * 512:(dh + 1) * 512],
                                                      start=(fc == 0), stop=(fc == 11)), reads=[bh, bw2], writes=[bpy])
                    k.op("act" if dh == 0 else "dve",
                         (lambda e: e.activation(ye[:nj, dh * 512:(dh + 1) * 512], py[:nj, :], AF.Copy)) if dh == 0 else
                         (lambda e: e.tensor_copy(ye[:nj, dh * 512:(dh + 1) * 512], py[:nj, :])), reads=[bpy], writes=[bye])
                k.dma("sp", C.YE[ex, jt * 128:jt * 128 + nj, :], ye[:nj, :], reads=[bye], writes=[C.bYE])
    if "stop_moeB" in P.debug:
        return
    with P.scope():
        jcol = P.sb("jcol", [128, 2], F32)
        b_c = Buf("jcol")
        k.dma("sp", jcol[:], C.moe_jcol, writes=[b_c])
        yel = P.sb("yel", [128, NE, 2, D], BF16)
        yec = P.sb("yec", [32, NE, D], BF16)
        b_ye = Buf("yel")
        posb = P.sb("posb", [128, NE, 512], F32)
        mgb = P.sb("mgb", [128, NE, 512], F32)
        b_pb = Buf("posb")
        sgr = Ring(P, "selg", 4, [128, 512], BF16)
        x1r = Ring(P, "cx1", 2, [128, KC, 512], F32)
        pso = Ring(P, "cps", 8, [128, 512], F32, psum=True)
        for b in range(NB):
            for jt in range(2):
                k.dma("sp", yel[:, :, jt, :], C.YE[:, b * CAPL + jt * 128:b * CAPL + (jt + 1) * 128, :].rearrange("e p d -> p e d"),
                      reads=[C.bYE], writes=[b_ye])
            k.dma("sp", yec[:], C.YE[:, 2 * CAPL + b * CAPC:2 * CAPL + (b + 1) * CAPC, :].rearrange("e p d -> p e d"),
                  reads=[C.bYE], writes=[b_ye])
            for (t0, n) in [(0, TC)] + [(TC + i * 512, 512) for i in range(4)]:
                isctx = t0 < TC
                j = 2 if isctx else b
                k.dma("sp", posb[:, :, :n], C.POSD[b, :, t0:t0 + n].partition_broadcast(128), reads=[C.bPOSD], writes=[b_pb])
                k.dma("act", mgb[:, :, :n], C.MGD[b, :, t0:t0 + n].partition_broadcast(128), reads=[C.bPOSD], writes=[b_pb])
                x1, bx1 = x1r.next()
                k.dma("sp", x1[:, :, :n], XT[b][:, t0:t0 + n].rearrange("(kc p) n -> p kc n", p=128), reads=[C.bXT[b]], writes=[bx1])
                acc = [pso.next() for _ in range(KC)]
                njt = 1 if isctx else 2
                for ex in range(NE):
                    for jt in range(njt):
                        sg, bsg = sgr.next()
                        k.op("dve", lambda e: e.scalar_tensor_tensor(sg[:, :n], posb[:, ex, :n], jcol[:, jt:jt + 1], mgb[:, ex, :n],
                                                                     ALU.is_equal, ALU.mult), reads=[b_pb, b_c], writes=[bsg])
                        first = (ex == 0 and jt == 0)
                        lastm = (ex == NE - 1 and jt == njt - 1)
                        for dc in range(KC):
                            pa, bpa = acc[dc]
                            if isctx:
                                k.op("pe", lambda e: e.matmul(pa[:, :n], yec[:, ex, dc * 128:(dc + 1) * 128], sg[:32, :n],
                                                              start=first, stop=lastm), reads=[b_ye, bsg], writes=[bpa])
                            else:
                                k.op("pe", lambda e: e.matmul(pa[:, :n], yel[:, ex, jt, dc * 128:(dc + 1) * 128], sg[:, :n],
                                                              start=first, stop=lastm), reads=[b_ye, bsg], writes=[bpa])
                for dc in range(KC):
                    pa, bpa = acc[dc]
                    k.op("dve", lambda e: e.scalar_tensor_tensor(x1[:, dc, :n], pa[:, :n], mod[:, 5 * 8 + dc, j:j + 1], x1[:, dc, :n],
                                                                 ALU.mult, ALU.add), reads=[bpa, bx1, C.b_mod], writes=[bx1])
                if last:
                    if not isctx:
                        k.dma("sp", C.OUT[b][:, t0 - TC:t0 - TC + n].rearrange("(kc p) n -> p kc n", p=128), x1[:, :, :n],
                              reads=[bx1], writes=[C.bOUT])
                else:
                    k.dma("sp", XT[b][:, t0:t0 + n].rearrange("(kc p) n -> p kc n", p=128), x1[:, :, :n],
                          reads=[bx1], writes=[C.bXT[b]])


LAM = float(np.exp(-0.5))
GN_EPS = 64e-5
NCK = TT // 128


def phase_rwkv_prep(P, C, l):
    k = P.k
    PT = C.PT
    blocks = [(0, TC)] + [(TC + i * 512, 512) for i in range(4)]
    with P.scope():
        vec = P.sb("rwvec", [128, 51], F32)
        b_c = Buf("rwc")
        k.dma("sp", vec[:], C.rw_vec[l], writes=[b_c])
        MU, W0, A0, KK_, KA, RK = 0, 15, 23, 31, 35, 39
        der = P.sb("rwder", [128, 15 + 15 + 4], F32)
        k.op("dve", lambda e: e.tensor_scalar(der[:, 0:15], vec[:, MU:MU + 15], -1.0, 1.0, ALU.mult, ALU.add), reads=[b_c], writes=[b_c])
        k.op("dve", lambda e: e.tensor_scalar(der[:, 15:30], vec[:, MU:MU + 15], 0.5, None, ALU.mult), reads=[b_c], writes=[b_c])
        k.op("dve", lambda e: e.tensor_scalar(der[:, 30:34], vec[:, KA:KA + 4], -1.0, 1.0, ALU.mult, ALU.add), reads=[b_c], writes=[b_c])
        tiny = P.sb("rwtiny", [128, 1], F32)
        k.op("dve", lambda e: e.memset(tiny[:], 1e-12), writes=[b_c])
        w2 = P.sb("rw_w2", [128, 512], BF16)
        a2 = P.sb("rw_a2", [128, 512], BF16)
        g2 = P.sb("rw_g2", [128, 512], BF16)
        k.dma("pool", w2[:], C.rwkv_w2[l].rearrange("j l c -> (j l) c"), writes=[b_c])
        k.dma("pool", a2[:], C.rwkv_a2[l].rearrange("j l c -> (j l) c"), writes=[b_c])
        k.dma("pool", g2[:], C.rwkv_g2[l], writes=[b_c])
        bd = P.sb("rw_bd", [128, 128], BF16)
        k.dma("pool", bd[:], C.rw_bd, writes=[b_c])
        rst = P.sb("rw_rst", [128, TT + 1], F32)
        k.dma("sp", rst[:], C.rw_rst, writes=[b_c])
        big = lambda n_, dt=F32: P.sb(n_, [128, TT], dt)
        X = big("rX", BF16)
        S = big("rS")
        KP = [big(f"rKP{i}", BF16) for i in range(4)]
        KAP = [big(f"rKAP{i}", BF16) for i in range(4)]
        KS = big("rKSUM")
        bKS = Buf("KS")
        RP = [big(f"rRP{i}", BF16) for i in range(4)]
        VP = [big(f"rVP{i}", BF16) for i in range(4)]
        TW, PA, SG = big("rTW", BF16), big("rPA", BF16), big("rSG", BF16)
        T1, T2, T3, T4 = big("rT1"), big("rT2"), big("rT3"), big("rT4")
        O = [big(f"rO{i}", BF16) for i in range(4)]
        bX, bS, bT1, bT2, bT3, bT4 = [Buf(x) for x in "X S T1 T2 T3 T4".split()]
        bKP = [Buf(f"KP{i}") for i in range(4)]
        bKAP = [Buf(f"KAP{i}") for i in range(4)]
        bRP = [Buf(f"RP{i}") for i in range(4)]
        bVP = [Buf(f"VP{i}") for i in range(4)]
        bTW, bPA, bSG = Buf("TW"), Buf("PA"), Buf("SG")
        bO = [Buf(f"O{i}") for i in range(4)]
        psr = Ring(P, "rwps", 4, [128, 512], F32, psum=True)
        stg = Ring(P, "rwstg", 3, [128, 512], BF16)
        plr = Ring(P, "rwpl", 2, [128, NCK], F32)
        for b in range(NB):
            for ci in range(15):
                k.dma("sp", X[:], PT[b, (4 + ci) * 128:(5 + ci) * 128, :], reads=[C.bPT], writes=[bX])
                k.op("pool", lambda e: e.tensor_tensor(S[:, 1:TT - 1], X[:, 0:TT - 2], X[:, 2:TT], ALU.add), reads=[bX], writes=[bS])
                for (d_, s_) in ((0, 1), (TC - 1, TC - 2), (TC, TC + 1), (TT - 1, TT - 2)):
                    k.op("pool", lambda e: e.tensor_copy(S[:, d_:d_ + 1], X[:, s_:s_ + 1]), reads=[bX, bS], writes=[bS])
                k.op("dve", lambda e: e.tensor_scalar(T1[:], X[:], der[:, ci:ci + 1], None, ALU.mult), reads=[bX, b_c], writes=[bT1])
                if ci < 4:
                    dst, bdst = RP[ci], bRP[ci]
                elif ci < 8:
                    dst, bdst = KP[ci - 4], bKP[ci - 4]
                elif ci < 12:
                    dst, bdst = VP[ci - 8], bVP[ci - 8]
                else:
                    dst, bdst = T2, bT2
                k.op("dve", lambda e: e.scalar_tensor_tensor(dst[:], S[:], der[:, 15 + ci:16 + ci], T1[:], ALU.mult, ALU.add),
                     reads=[bS, bT1, b_c], writes=[bdst])
                if ci == 12:
                    k.op("act", lambda e: e.activation(TW[:], T2[:], AF.Tanh), reads=[bT2], writes=[bTW])
                elif ci == 13:
                    k.op("act", lambda e: e.activation(PA[:], T2[:], AF.Copy), reads=[bT2], writes=[bPA])
                elif ci == 14:
                    k.op("act", lambda e: e.activation(SG[:], T2[:], AF.Sigmoid), reads=[bT2], writes=[bSG])
            for cc in range(4):
                k.dma("sp", C.RWV[b, cc * 128:(cc + 1) * 128, :], VP[cc][:], reads=[bVP[cc]], writes=[C.bRW])
            for cc in range(4):
                for (t0, n) in blocks:
                    ps, bps = psr.next()
                    k.op("pe", lambda e: e.matmul(ps[:, :n], g2[:, cc * 128:(cc + 1) * 128], SG[:, t0:t0 + n], start=True, stop=True),
                         reads=[bSG, b_c], writes=[bps])
                    st, bst = stg.next()
                    k.op("act", lambda e: e.activation(st[:, :n], ps[:, :n], AF.Copy), reads=[bps], writes=[bst])
                    k.dma("sp", C.RWG[b, cc * 128:(cc + 1) * 128, t0:t0 + n], st[:, :n], reads=[bst], writes=[C.bRW])
            for cc in range(4):
                k.op("dve", lambda e: e.tensor_scalar(T1[:], KP[cc][:], vec[:, KK_ + cc:KK_ + cc + 1], None, ALU.mult),
                     reads=[bKP[cc], b_c, bT1], writes=[bT1])
                k.op("act", lambda e: e.activation(O[0][:], T1[:], AF.Square), reads=[bT1, bO[0]], writes=[bO[0]])
                for (t0, n) in blocks:
                    ps, bps = psr.next()
                    k.op("pe", lambda e: e.matmul(ps[:, :n], bd[:], O[0][:, t0:t0 + n], start=True, stop=True),
                         reads=[bO[0], b_c], writes=[bps])
                    k.op("act", lambda e: e.activation(T2[:, t0:t0 + n], ps[:, :n], AF.Sqrt, bias=tiny[:, 0:1]), reads=[bps, bT2, b_c], writes=[bT2])
                k.op("dve", lambda e: e.reciprocal(T2[:], T2[:]), reads=[bT2], writes=[bT2])
                k.op("dve", lambda e: e.tensor_tensor(KAP[cc][:], T1[:], T2[:], ALU.mult), reads=[bT1, bT2], writes=[bKAP[cc]])
            for cc in range(4):
                for j in range(2):
                    jr = slice(j * 64, (j + 1) * 64)
                    for (t0, n) in blocks:
                        ps, bps = psr.next()
                        k.op("pe", lambda e: e.matmul(ps[:, :n], w2[jr, cc * 128:(cc + 1) * 128], TW[jr, t0:t0 + n], start=True, stop=True),
                             reads=[bTW, b_c], writes=[bps])
                        k.op("act", lambda e: e.activation(T1[:, t0:t0 + n], ps[:, :n], AF.Sigmoid, bias=vec[:, W0 + j * 4 + cc:W0 + j * 4 + cc + 1]),
                             reads=[bps, b_c, bT1], writes=[bT1])
                        ps2, bps2 = psr.next()
                        k.op("pe", lambda e: e.matmul(ps2[:, :n], a2[jr, cc * 128:(cc + 1) * 128], PA[jr, t0:t0 + n], start=True, stop=True),
                             reads=[bPA, b_c], writes=[bps2])
                        k.op("act", lambda e: e.activation(T2[:, t0:t0 + n], ps2[:, :n], AF.Sigmoid, bias=vec[:, A0 + j * 4 + cc:A0 + j * 4 + cc + 1]),
                             reads=[bps2, b_c, bT2], writes=[bT2])
                    if j == 0:
                        k.op("dve", lambda e: e.tensor_tensor_scan(T3[:], rst[:, 0:TT], T1[:], 0.0, ALU.mult, ALU.add),
                             reads=[bT1, b_c, bT3], writes=[bT3])
                    else:
                        k.op("dve", lambda e: e.tensor_tensor_scan(rev_ap(T3[:], 0, TT), rev_ap(rst[:], 1, TT + 1), rev_ap(T1[:], 0, TT),
                                                                   0.0, ALU.mult, ALU.add), reads=[bT1, b_c, bT3], writes=[bT3])
                    k.op("dve", lambda e: e.tensor_scalar(T4[:], T2[:], vec[:, KA + cc:KA + cc + 1], der[:, 30 + cc:31 + cc], ALU.mult, ALU.add),
                         reads=[bT2, b_c, bT4], writes=[bT4])
                    k.op("pool", lambda e: e.tensor_tensor(T4[:], T4[:], KP[cc][:], ALU.mult), reads=[bT4, bKP[cc]], writes=[bT4])
                    k.op("pool", lambda e: e.tensor_tensor(T2[:], T2[:], KAP[cc][:], ALU.mult), reads=[bT2, bKAP[cc]], writes=[bT2])
                    k.op("pool", lambda e: e.tensor_tensor(T1[:], T3[:], T1[:], ALU.subtract), reads=[bT1, bT3], writes=[bT1])
                    k.op("act", lambda e: e.activation(T1[:], T1[:], AF.Exp, scale=-LAM), reads=[bT1], writes=[bT1])
                    k.op("act", lambda e: e.activation(S[:], T3[:], AF.Exp, scale=LAM), reads=[bT3, bS], writes=[bS])
                    k.op("act", lambda e: e.activation(T3[:], T3[:], AF.Exp, scale=-LAM), reads=[bT3], writes=[bT3])
                    pl, bpl = plr.next()
                    off = 127 if j == 0 else 0
                    k.op("dve", lambda e: e.tensor_copy(pl[:], T3[:, off:TT:128]), reads=[bT3], writes=[bpl])
                    k.dma("sp", C.RWPL[b, j, cc * 128:(cc + 1) * 128, :], pl[:], reads=[bpl], writes=[C.bRW])
                    k.op("dve", lambda e: e.tensor_tensor(O[0][:], RP[cc][:], T3[:], ALU.mult), reads=[bRP[cc], bT3, bO[0]], writes=[bO[0]])
                    k.op("pool", lambda e: e.tensor_tensor(O[1][:], KAP[cc][:], T1[:], ALU.mult), reads=[bKAP[cc], bT1, bO[1]], writes=[bO[1]])
                    k.op("dve", lambda e: e.tensor_tensor(O[2][:], T4[:], S[:], ALU.mult), reads=[bT4, bS, bO[2]], writes=[bO[2]])
                    k.op("pool", lambda e: e.tensor_tensor(O[3][:], T2[:], S[:], ALU.mult), reads=[bT2, bS, bO[3]], writes=[bO[3]])
                    for q in range(4):
                        k.dma("sp" if q % 2 == 0 else "act", C.RWT[b, j, q, cc * 128:(cc + 1) * 128, :], O[q][:], reads=[bO[q]], writes=[C.bRW])
                    if j == 0:
                        k.op("dve", lambda e: e.tensor_copy(KS[:], T4[:]), reads=[bT4, bKS], writes=[bKS])
                    else:
                        k.op("dve", lambda e: e.tensor_tensor(KS[:], KS[:], T4[:], ALU.add), reads=[bT4, bKS], writes=[bKS])
                k.op("dve", lambda e: e.scalar_tensor_tensor(KS[:], KS[:], vec[:, RK + cc:RK + cc + 1], RP[cc][:], ALU.mult, ALU.mult),
                     reads=[bKS, bRP[cc], b_c], writes=[bKS])
                k.op("act", lambda e: e.activation(O[0][:], KS[:], AF.Copy), reads=[bKS, bO[0]], writes=[bO[0]])
                for (t0, n) in blocks:
                    ps, bps = psr.next()
                    k.op("pe", lambda e: e.matmul(ps[:, :n], bd[:], O[0][:, t0:t0 + n], start=True, stop=True), reads=[bO[0], b_c], writes=[bps])
                    k.op("dve", lambda e: e.tensor_tensor(T1[:, t0:t0 + n], ps[:, :n], VP[cc][:, t0:t0 + n], ALU.mult),
                         reads=[bps, bVP[cc], bT1], writes=[bT1])
                k.dma("sp", C.RWB[b, cc * 128:(cc + 1) * 128, :], T1[:], reads=[bT1], writes=[C.bRW])


def run_interleaved(gens):
    gens = list(gens)
    while gens:
        for g_ in list(gens):
            try:
                next(g_)
            except StopIteration:
                gens.remove(g_)


def phase_rwkv_scan(P, C, l):
    k = P.k
    YT = C.YT
    with P.scope():
        masks = P.sb("rwmask", [128, 2, 640], BF16)
        b_c = Buf("rwc2")
        k.dma("pool", masks[:], C.rw_masks, writes=[b_c])
        ident = P.sb("rwident", [128, 128], BF16)
        k.dma("pool", ident[:], C.ident, writes=[b_c])
        identf = P.sb("rwidentf", [128, 128], F32)
        k.dma("sp", identf[:], C.ident, writes=[b_c])
        bdm = P.sb("rwbdm", [128, 128], F32)
        k.dma("sp", bdm[:], C.rw_bd, writes=[b_c])
        vec = P.sb("rwvec2", [128, 51], F32)
        k.dma("sp", vec[:], C.rw_vec[l], writes=[b_c])
        lmf = P.sb("rwlm", [128, 4, 128], F32)
        k.dma("sp", lmf[:], C.rw_lm, writes=[b_c])
        gne = P.sb("gne", [128, 1], F32)
        k.op("dve", lambda e: e.memset(gne[:], GN_EPS), writes=[b_c])
        Ytok = P.sb("Ytok", [128, NCK, 512], F32)
        for b in range(NB):
            bY = [[Buf(f"Y{c}_{hp}") for hp in range(4)] for c in range(NCK)]
            with P.scope():
                KR = P.sb("KR", [128, NCK, 2, 128], BF16)
                KF = P.sb("KF", [128, TT], BF16)
                BF_ = P.sb("BF", [128, TT], BF16)
                VF = P.sb("VF", [128, TT], BF16)
                PLt = P.sb("PLt", [128, NCK], F32)
                TOK = P.sb("TOK", [128, NCK, 3, 128], BF16)
                SC = P.sb("SC", [128, NCK, 2, 512], BF16)
                Tt = P.sb("Tt", [128, NCK * 2, 128], BF16)
                SC36 = SC[:].rearrange("p c h x -> p (c h) x")
                NSLOT = 2
                NFs = [Ring(P, f"NF{i}", 1, [128, 4, 2, 128], F32) for i in range(NSLOT)]
                F4s = [Ring(P, f"F4{i}", 4, [128, 4, 128], F32) for i in range(NSLOT)]
                FTs = [Ring(P, f"FT{i}", 2, [128, 4, 128], F32) for i in range(NSLOT)]
                B4s = [Ring(P, f"B4{i}", 8, [128, 4, 128], BF16) for i in range(NSLOT)]
                H = P.sb("Hst", [128, 128], F32)
                Ht = P.sb("Htmp", [128, 128], F32)
                Hb = P.sb("Hb", [128, 128], BF16)
                Wr = Ring(P, "Wsb", 2, [128, 128], BF16)
                Ur = Ring(P, "Un", 2, [128, 128], BF16)
                psT = P.ps("pstr", [128, 3, 128], BF16)
                bpsT = Buf("pstr")
                psL = Ring(P, "psL", 5, [128, 512], F32, psum=True)
                psS = Ring(P, "psS", 2, [128, 128], F32, psum=True)
                b_in, b_tok, b_sc, b_tt, b_H = Buf("in"), Buf("tok"), Buf("sc"), Buf("tt"), Buf("H")
                for j in ([0] if "rw_j0" in P.debug else [1] if "rw_j1" in P.debug else [0, 1]):
                    for hp in range(4):
                        rows = slice(hp * 128, (hp + 1) * 128)
                        k.dma("sp", KR[:, :, 0, :], C.RWT[b, j, 1, rows, :].rearrange("p (c t) -> p c t", t=128), reads=[C.bRW], writes=[b_in])
                        k.dma("act", KR[:, :, 1, :], C.RWT[b, j, 0, rows, :].rearrange("p (c t) -> p c t", t=128), reads=[C.bRW], writes=[b_in])
                        k.dma("sp", KF[:], C.RWT[b, j, 2, rows, :], reads=[C.bRW], writes=[b_in])
                        k.dma("act", BF_[:], C.RWT[b, j, 3, rows, :], reads=[C.bRW], writes=[b_in])
                        k.dma("sp", VF[:], C.RWV[b, rows, :], reads=[C.bRW], writes=[b_in])
                        k.dma("sp", PLt[:], C.RWPL[b, j, rows, :], reads=[C.bRW], writes=[b_in])
                        for c in range(NCK):
                            cs = slice(c * 128, (c + 1) * 128)
                            for q, src in enumerate((KF, BF_, VF)):
                                k.op("pe", lambda e: e.transpose(psT[:, q, :], src[:, cs], ident[:]), reads=[b_in, b_c], writes=[bpsT])
                            k.op("act", lambda e: e.activation(TOK[:, c], psT[:], AF.Copy), reads=[bpsT], writes=[b_tok])
                        def inv_group(g, slot, j=j):
                            NFr, F4, B4, FT = NFs[slot], F4s[slot], B4s[slot], FTs[slot]
                            NF, bNF = NFr.next()
                            for i in range(4):
                                c, h = 2 * g + i // 2, i % 2
                                cs = slice(c * 128, (c + 1) * 128)
                                hr = slice(h * 64, (h + 1) * 64)
                                kr2 = KR[hr, c].rearrange("p x t -> p (x t)")
                                pX, bpX = psL.next()
                                k.op("pe", lambda e: e.matmul(pX[:, 0:256], KF[hr, cs], kr2, start=True, stop=True), reads=[b_in], writes=[bpX])
                                k.op("pe", lambda e: e.matmul(pX[:, 256:512], BF_[hr, cs], kr2, start=True, stop=True), reads=[b_in], writes=[bpX])
                                pY, bpY = psS.next()
                                k.op("pe", lambda e: e.matmul(pY[:], KR[hr, c, 0, :], BF_[hr, cs], start=True, stop=True), reads=[b_in], writes=[bpY])
                                k.op("dve", lambda e: e.tensor_tensor(SC[:, c, h, :], pX[:], masks[:, j, 0:512], ALU.mult), reads=[bpX, b_c], writes=[b_sc])
                                k.op("dve", lambda e: e.tensor_tensor(NF[:, i, 1, :], pX[:, 256:384], masks[:, j, 256:384], ALU.mult), reads=[bpX, b_c], writes=[bNF])
                                k.op("dve", lambda e: e.tensor_tensor(NF[:, i, 0, :], pY[:], masks[:, j, 512:640], ALU.mult), reads=[bpY, b_c], writes=[bNF])
                            yield
                            bl = slice(g * 4, (g + 1) * 4)
                            bc4 = lambda m_: m_.unsqueeze(1).to_broadcast([128, 4, 128])
                            Mk, bMk = F4.next()
                            Mtk, bMtk = F4.next()
                            Tf, bTf = FT.next()
                            Ttf, bTtf = FT.next()
                            k.op("pool", lambda e: e.tensor_tensor(Mk[:], NF[:, :, 0, :], bc4(lmf[:, 0, :]), ALU.mult), reads=[bNF, b_c], writes=[bMk])
                            k.op("pool", lambda e: e.tensor_tensor(Mtk[:], NF[:, :, 1, :], bc4(lmf[:, 0, :]), ALU.mult), reads=[bNF, b_c], writes=[bMtk])
                            k.op("pool", lambda e: e.tensor_tensor(Tf[:], Mk[:], bc4(identf[:]), ALU.add), reads=[bMk, b_c], writes=[bTf])
                            k.op("pool", lambda e: e.tensor_tensor(Ttf[:], Mtk[:], bc4(identf[:]), ALU.add), reads=[bMtk, b_c], writes=[bTtf])
                            yield
                            for lev in range(1, 4):
                                M2, bM2 = F4.next()
                                Mt2, bMt2 = F4.next()
                                p1, bp1 = psL.next()
                                p2, bp2 = psL.next()
                                for i in range(4):
                                    k.op("pe", lambda e: e.matmul(p1[:, i * 128:(i + 1) * 128], Mk[:, i, :], Mtk[:, i, :], start=True, stop=True),
                                         reads=[bMk, bMtk], writes=[bp1])
                                    k.op("pe", lambda e: e.matmul(p2[:, i * 128:(i + 1) * 128], Mtk[:, i, :], Mk[:, i, :], start=True, stop=True),
                                         reads=[bMk, bMtk], writes=[bp2])
                                yield
                                k.op("act", lambda e: e.activation(Mt2[:].rearrange("p a b -> p (a b)"), p1[:], AF.Copy), reads=[bp1], writes=[bMt2])
                                k.op("dve", lambda e: e.tensor_copy(M2[:].rearrange("p a b -> p (a b)"), p2[:]), reads=[bp2], writes=[bM2])
                                yield
                                p3, bp3 = psL.next()
                                p4, bp4 = psL.next()
                                for i in range(4):
                                    k.op("pe", lambda e: e.matmul(p3[:, i * 128:(i + 1) * 128], Mt2[:, i, :], Tf[:, i, :], start=True, stop=True),
                                         reads=[bMt2, bTf], writes=[bp3])
                                    k.op("pe", lambda e: e.matmul(p4[:, i * 128:(i + 1) * 128], M2[:, i, :], Ttf[:, i, :], start=True, stop=True),
                                         reads=[bM2, bTtf], writes=[bp4])
                                yield
                                k.op("dve", lambda e: e.tensor_tensor(Tf[:].rearrange("p a b -> p (a b)"), Tf[:].rearrange("p a b -> p (a b)"), p3[:], ALU.add),
                                     reads=[bp3, bTf], writes=[bTf])
                                k.op("dve", lambda e: e.tensor_tensor(Ttf[:].rearrange("p a b -> p (a b)"), Ttf[:].rearrange("p a b -> p (a b)"), p4[:], ALU.add),
                                     reads=[bp4, bTtf], writes=[bTtf])
                                Mk, bMk, Mtk, bMtk = M2, bM2, Mt2, bMt2
                                yield
                            Tb, bTb = B4.next()
                            Ttb, bTtb = B4.next()
                            k.op("act", lambda e: e.activation(Tb[:], Tf[:], AF.Copy), reads=[bTf], writes=[bTb])
                            k.op("act", lambda e: e.activation(Ttb[:], Ttf[:], AF.Copy), reads=[bTtf], writes=[bTtb])
                            for li in range(1, 4):
                                lastl = (li == 3)
                                Cm, bCm = B4.next()
                                k.op("pool", lambda e: e.tensor_tensor(Cm[:], NF[:, :, 0, :], bc4(lmf[:, li, :]), ALU.mult), reads=[bNF, b_c], writes=[bCm])
                                p2, bp2 = psL.next()
                                for i in range(4):
                                    k.op("pe", lambda e: e.matmul(p2[:, i * 128:(i + 1) * 128], Cm[:, i, :], Ttb[:, i, :], start=True, stop=True),
                                         reads=[bCm, bTtb], writes=[bp2])
                                yield
                                Z2, bZ2 = B4.next()
                                k.op("act", lambda e: e.activation(Z2[:].rearrange("p a b -> p (a b)"), p2[:], AF.Copy), reads=[bp2], writes=[bZ2])
                                if not lastl:
                                    Cmt, bCmt = B4.next()
                                    k.op("pool", lambda e: e.tensor_tensor(Cmt[:], NF[:, :, 1, :], bc4(lmf[:, li, :]), ALU.mult), reads=[bNF, b_c], writes=[bCmt])
                                    p1, bp1 = psL.next()
                                    for i in range(4):
                                        k.op("pe", lambda e: e.matmul(p1[:, i * 128:(i + 1) * 128], Cmt[:, i, :], Tb[:, i, :], start=True, stop=True),
                                             reads=[bCmt, bTb], writes=[bp1])
                                    Z1, bZ1 = B4.next()
                                    k.op("dve", lambda e: e.tensor_copy(Z1[:].rearrange("p a b -> p (a b)"), p1[:]), reads=[bp1], writes=[bZ1])
                                yield
                                p4, bp4 = psL.next()
                                for i in range(4):
                                    k.op("pe", lambda e: e.matmul(p4[:, i * 128:(i + 1) * 128], Tb[:, i, :], Z2[:, i, :], start=True, stop=True),
                                         reads=[bTb, bZ2], writes=[bp4])
                                if not lastl:
                                    p3, bp3 = psL.next()
                                    for i in range(4):
                                        k.op("pe", lambda e: e.matmul(p3[:, i * 128:(i + 1) * 128], Ttb[:, i, :], Z1[:, i, :], start=True, stop=True),
                                             reads=[bTtb, bZ1], writes=[bp3])
                                    yield
                                    Tn, bTn = B4.next()
                                    Ttn, bTtn = B4.next()
                                    k.op("dve", lambda e: e.tensor_tensor(Tn[:].rearrange("p a b -> p (a b)"), Tb[:].rearrange("p a b -> p (a b)"), p3[:], ALU.add),
                                         reads=[bp3, bTb], writes=[bTn])
                                    k.op("dve", lambda e: e.tensor_tensor(Ttn[:].rearrange("p a b -> p (a b)"), Ttb[:].rearrange("p a b -> p (a b)"), p4[:], ALU.add),
                                         reads=[bp4, bTtb], writes=[bTtn])
                                    Tb, bTb, Ttb, bTtb = Tn, bTn, Ttn, bTtn
                                else:
                                    yield
                                    k.op("dve", lambda e: e.tensor_tensor(Tt[:, bl, :], Ttb[:], p4[:].rearrange("p (a b) -> p a b", b=128), ALU.add),
                                         reads=[bp4, bTtb, b_tt], writes=[b_tt])
                        for w0 in range(0, NCK // 2, NSLOT):
                            run_interleaved([inv_group(g, i) for i, g in enumerate(range(w0, min(w0 + NSLOT, NCK // 2)))])
                        k.op("pool", lambda e: e.memset(H[:], 0.0), reads=[b_H], writes=[b_H])
                        k.op("pool", lambda e: e.memset(Hb[:], 0.0), reads=[b_H], writes=[b_H])
                        order = list(range(NCK)) if j == 0 else [1, 0] + list(range(NCK - 1, 1, -1))
                        for c in order:
                            pW, bpW = psS.next()
                            k.op("pe", lambda e: e.matmul(pW[:], KR[:, c, 0, :], Hb[:], start=True, stop=False, skip_group_check=True),
                                 reads=[b_in, b_H], writes=[bpW])
                            for h in range(2):
                                hc = slice(h * 64, (h + 1) * 64)
                                k.op("pe", lambda e: e.matmul(pW[:, hc], SC[:, c, h, 0:128], TOK[:, c, 2, hc], start=False, stop=True, skip_group_check=True),
                                     reads=[b_sc, b_tok], writes=[bpW])
                            Wsb, bW = Wr.next()
                            k.op("act", lambda e: e.activation(Wsb[:], pW[:], AF.Copy), reads=[bpW], writes=[bW])
                            pU, bpU = psS.next()
                            for h in range(2):
                                hc = slice(h * 64, (h + 1) * 64)
                                k.op("pe", lambda e: e.matmul(pU[:, hc], Tt[:, c * 2 + h, :], Wsb[:, hc], start=True, stop=True),
                                     reads=[b_tt, bW], writes=[bpU])
                            Un, bUn = Ur.next()
                            k.op("act", lambda e: e.activation(Un[:], pU[:], AF.Copy, scale=-1.0), reads=[bpU], writes=[bUn])
                            pYy, bpYy = psS.next()
                            k.op("pe", lambda e: e.matmul(pYy[:], KR[:, c, 1, :], Hb[:], start=True, stop=False, skip_group_check=True),
                                 reads=[b_in, b_H], writes=[bpYy])
                            for h in range(2):
                                hc = slice(h * 64, (h + 1) * 64)
                                k.op("pe", lambda e: e.matmul(pYy[:, hc], SC[:, c, h, 128:256], TOK[:, c, 2, hc], start=False, stop=False, skip_group_check=True),
                                     reads=[b_sc, b_tok], writes=[bpYy])
                                k.op("pe", lambda e: e.matmul(pYy[:, hc], SC[:, c, h, 384:512], Un[:, hc], start=False, stop=True, skip_group_check=True),
                                     reads=[b_sc, bUn], writes=[bpYy])
                            ysl = Ytok[:, c, hp * 128:(hp + 1) * 128]
                            if j == 0 or "rw_j1" in P.debug:
                                k.op("act", lambda e: e.activation(ysl, pYy[:], AF.Copy), reads=[bpYy], writes=[bY[c][hp]])
                            else:
                                k.op("dve", lambda e: e.tensor_tensor(ysl, ysl, pYy[:], ALU.add), reads=[bpYy, bY[c][hp]], writes=[bY[c][hp]])
                            pH, bpH = psS.next()
                            k.op("pe", lambda e: e.matmul(pH[:], TOK[:, c, 0, :], TOK[:, c, 2, :], start=True, stop=False), reads=[b_tok], writes=[bpH])
                            k.op("pe", lambda e: e.matmul(pH[:], TOK[:, c, 1, :], Un[:], start=False, stop=True), reads=[b_tok, bUn], writes=[bpH])
                            k.op("dve", lambda e: e.tensor_tensor(Ht[:], H[:], pH[:], ALU.add), reads=[bpH, b_H], writes=[b_H])
                            k.op("dve", lambda e: e.scalar_tensor_tensor(H[:], Ht[:], PLt[:, c:c + 1], bdm[:], ALU.mult, ALU.mult),
                                 reads=[b_H, b_in, b_c], writes=[b_H])
                            k.op("act", lambda e: e.activation(Hb[:], H[:], AF.Copy), reads=[b_H], writes=[b_H])
            if "YTOK" in P.debug:
                k.dma("sp", C.YTOK[b], Ytok[:], reads=[x for row in bY for x in row], writes=[C.bRW])
            with P.scope():
                BON = P.sb("BON", [128, 4, TT], F32)
                G = P.sb("G", [128, 4, TT], BF16)
                b_l = Buf("rdl")
                k.dma("sp", BON[:], C.RWB[b].rearrange("(c p) t -> p c t", p=128), reads=[C.bRW], writes=[b_l])
                k.dma("act", G[:], C.RWG[b].rearrange("(c p) t -> p c t", p=128), reads=[C.bRW], writes=[b_l])
                st8 = Ring(P, "st8", 2, [128, 6, 8], F32)
                ysq = Ring(P, "ysq", 2, [128, 512], F32)
                ynr = Ring(P, "yn", 2, [128, 512], F32)
                psR = Ring(P, "psR", 2, [128, 4, 128], F32, psum=True)
                ofr = Ring(P, "of", 3, [128, 128], F32)
                obr = Ring(P, "ob", 3, [128, 128], BF16)
                for c in range(NCK):
                    allY = bY[c]
                    y = Ytok[:, c, :]
                    y3 = y.rearrange("p (h x) -> p h x", x=64)
                    s, bs = st8.next()
                    k.op("dve", lambda e: e.reduce_sum(s[:, 0, :], y3, AX.X), reads=allY, writes=[bs])
                    q, bq = ysq.next()
                    k.op("pool", lambda e: e.tensor_tensor(q[:], y, y, ALU.mult), reads=allY, writes=[bq])
                    k.op("dve", lambda e: e.reduce_sum(s[:, 1, :], q[:].rearrange("p (h x) -> p h x", x=64), AX.X), reads=[bq, bs], writes=[bs])
                    k.op("dve", lambda e: e.tensor_scalar(s[:, 2, :], s[:, 0, :], 1.0 / 64.0, None, ALU.mult), reads=[bs], writes=[bs])
                    k.op("dve", lambda e: e.tensor_tensor(s[:, 3, :], s[:, 2, :], s[:, 2, :], ALU.mult), reads=[bs], writes=[bs])
                    k.op("dve", lambda e: e.scalar_tensor_tensor(s[:, 4, :], s[:, 1, :], 1.0 / 64.0, s[:, 3, :], ALU.mult, ALU.subtract),
                         reads=[bs], writes=[bs])
                    k.op("act", lambda e: e.activation(s[:, 5, :], s[:, 4, :], AF.Sqrt, bias=gne[:, 0:1]), reads=[bs, b_c], writes=[bs])
                    k.op("dve", lambda e: e.reciprocal(s[:, 5, :], s[:, 5, :]), reads=[bs], writes=[bs])
                    yn, byn = ynr.next()
                    yn3 = yn[:].rearrange("p (h x) -> p h x", x=64)
                    k.op("dve", lambda e: e.tensor_tensor(yn3, y3, s[:, 2, :].unsqueeze(2).to_broadcast([128, 8, 64]), ALU.subtract),
                         reads=allY + [bs], writes=[byn])
                    k.op("pool", lambda e: e.tensor_tensor(yn3, yn3, s[:, 5, :].unsqueeze(2).to_broadcast([128, 8, 64]), ALU.mult),
                         reads=[byn, bs], writes=[byn])
                    pr, bpr = psR.next()
                    for hp in range(4):
                        k.op("pe", lambda e: e.transpose(pr[:, hp, :], yn[:, hp * 128:(hp + 1) * 128], identf[:]), reads=[byn, b_c], writes=[bpr])
                    cs = slice(c * 128, (c + 1) * 128)
                    for hp in range(4):
                        of, bof = ofr.next()
                        k.op("act", lambda e: e.activation(of[:], pr[:, hp, :], AF.Identity, bias=vec[:, 47 + hp:48 + hp], scale=vec[:, 43 + hp:44 + hp]),
                             reads=[bpr, b_c], writes=[bof])
                        k.op("dve", lambda e: e.tensor_tensor(of[:], of[:], BON[:, hp, cs], ALU.add), reads=[bof, b_l], writes=[bof])
                        ob, bob = obr.next()
                        k.op("pool", lambda e: e.tensor_tensor(ob[:], of[:], G[:, hp, cs], ALU.mult), reads=[bof, b_l], writes=[bob])
                        k.dma("sp", YT[b, 1, hp * 128:(hp + 1) * 128, cs], ob[:], reads=[bob], writes=[C.bYT])


def build(n_layers=DEPTH, debug=(), stop_after=None):
    P = Prog(n_layers, debug)
    nc, k = P.nc, P.k
    xt0 = P.din("xt0", [NB, D, TT])
    cT = P.din("cT", [128, KC, 3])
    cst_ones = P.din("ones", [128, 128])
    ada_w = P.din("ada_w", [DEPTH, D, 6 * D])
    ada_b = P.din("ada_b_r", [DEPTH, 128, 48])
    ng_r = P.din("ng_r", [DEPTH, 128, 2, KC])
    w_in = P.din("w_in", [DEPTH, D, N_IN])
    XT = P.dscr("XT", [NB, D, TT], F32)
    PT = P.dscr("PT", [NB, NCH * 128, TT], BF16)
    bXT = [Buf("XT0"), Buf("XT1")]
    bPT = Buf("PT")
    if "yt_in" in P.debug:
        YT = P.din("YT3", [NB, 3, 512, TT], BF16)
        P.dbufs["YT3"] = Buf("YT3")
    else:
        YT = P.dscr("YT3", [NB, 3, 512, TT], BF16)
    C = type("Ctx", (), {})()
    C.XT, C.PT, C.YT, C.bXT, C.bPT, C.bYT = XT, PT, YT, bXT, bPT, Buf("YT3")
    C.xt0 = xt0
    declare_inputs(P, C)

    with P.scope():
        ones = P.sb("ones", [128, 128], F32)
        b_ones = Buf("ones")
        k.dma("sp", ones[:], cst_ones, writes=[b_ones])
        silu_c = P.sb("silu_c", [128, KC, 3], F32)
        b_silu = Buf("silu_c")
        k.dma("sp", silu_c[:], cT, writes=[b_silu])
        k.op("act", lambda e: e.activation(silu_c[:], silu_c[:], AF.Silu), reads=[b_silu], writes=[b_silu])
        C.epsc = P.sb("epsc_g", [128, 1], F32)
        k.op("dve", lambda e: e.memset(C.epsc[:], NORM_EPS), writes=[b_ones])
        mod = P.sb("mod", [128, 48, 3], F32)
        b_mod = Buf("mod")
        gs = P.sb("gs", [128, 2, KC, 3], F32)
        b_gs = Buf("gs")

        for l in range(n_layers):
            src_x = xt0 if l == 0 else XT
            with P.scope():
                adab = P.sb("adab", [128, 48], F32)
                b_adab = Buf("adab")
                k.dma("sp", adab[:], ada_b[l], writes=[b_adab])
                ng = P.sb("ng", [128, 2, KC], F32)
                b_ng = Buf("ng")
                k.dma("sp", ng[:], ng_r[l], writes=[b_ng])
                wr = Ring(P, "adaw", 2, [128, KC, 512], F32)
                pr = Ring(P, "modps", 2, [128, 4, 3], F32, psum=True)
                for g in range(12):
                    wt, bw = wr.next()
                    k.dma("sp" if g % 2 == 0 else "act", wt[:],
                          ada_w[l][:, g * 512:(g + 1) * 512].rearrange("(kc p) n -> p kc n", p=128), writes=[bw])
                    pt, bp = pr.next()
                    for c4 in range(4):
                        for kc in range(KC):
                            k.op("pe", lambda e, c4=c4, kc=kc: e.matmul(
                                pt[:, c4, :], wt[:, kc, c4 * 128:(c4 + 1) * 128], silu_c[:, kc, :],
                                start=(kc == 0), stop=(kc == KC - 1)),
                                reads=[bw, b_silu], writes=[bp])
                    k.op("dve", lambda e: e.tensor_tensor(
                        mod[:, g * 4:(g + 1) * 4, :], pt[:],
                        adab[:, g * 4:(g + 1) * 4].unsqueeze(2).to_broadcast([128, 4, 3]), ALU.add),
                        reads=[bp, b_adab], writes=[b_mod])
                for n, mi in ((0, 1), (1, 4)):
                    k.op("dve", lambda e, n=n, mi=mi: e.tensor_scalar(
                        gs[:, n, :, :], mod[:, mi * 8:(mi + 1) * 8, :], 1.0, None, ALU.add),
                        reads=[b_mod], writes=[b_gs])
                    k.op("dve", lambda e, n=n: e.tensor_tensor(
                        gs[:, n, :, :], gs[:, n, :, :],
                        ng[:, n, :].unsqueeze(2).to_broadcast([128, KC, 3]), ALU.mult),
                        reads=[b_gs, b_ng], writes=[b_gs])
            if stop_after == "mod":
                break

            with P.scope():
                hT = P.sb("hT", [128, NB, KC, TT], BF16)
                b_h = [[Buf(f"h{b}_{i}") for i in range(5)] for b in range(NB)]
                blocks = [(0, TC)] + [(TC + i * 512, 512) for i in range(4)]
                xr = Ring(P, "xin", 2, [128, KC, 512], F32)
                sqr = Ring(P, "sq", 1, [128, KC, 512], F32)
                ssr = Ring(P, "ssps", 2, [128, 512], F32, psum=True)
                rsr = Ring(P, "rstd", 2, [128, 512], F32)
                for b in range(NB):
                    for bi, (t0, n) in enumerate(blocks):
                        j = 2 if bi == 0 else b
                        xt, bx = xr.next()
                        k.dma("sp", xt[:, :, :n], src_x[b][:, t0:t0 + n].rearrange("(kc p) n -> p kc n", p=128),
                              reads=[bXT[b]], writes=[bx])
                        sq, bs = sqr.next()
                        k.op("act", lambda e: e.activation(sq[:, :, :n], xt[:, :, :n], AF.Square),
                             reads=[bx], writes=[bs])
                        ss, bss = ssr.next()
                        for kc in range(KC):
                            k.op("pe", lambda e, kc=kc: e.matmul(ss[:, :n], ones[:], sq[:, kc, :n],
                                                                 start=(kc == 0), stop=(kc == KC - 1)),
                                 reads=[bs, b_ones], writes=[bss])
                        rs, brs = rsr.next()
                        k.op("act", lambda e: e.activation(rs[:, :n], ss[:, :n], AF.Sqrt, bias=NORM_EPS, scale=1.0 / D),
                             reads=[bss], writes=[brs])
                        k.op("dve", lambda e: e.reciprocal(rs[:, :n], rs[:, :n]), reads=[brs], writes=[brs])
                        k.op("dve", lambda e: e.tensor_tensor(
                            sq[:, :, :n], xt[:, :, :n], rs[:, :n].unsqueeze(1).to_broadcast([128, KC, n]), ALU.mult),
                            reads=[bx, brs, bs], writes=[bs])
                        for kc in range(KC):
                            k.op("act", lambda e, kc=kc: e.activation(
                                hT[:, b, kc, t0:t0 + n], sq[:, kc, :n], AF.Identity,
                                bias=mod[:, 0 * 8 + kc, j:j + 1], scale=gs[:, 0, kc, j:j + 1]),
                                reads=[bs, b_mod, b_gs], writes=[b_h[b][bi]])
                groups = [list(range(g * 4, g * 4 + 4)) for g in range(6)] + [[24]] + \
                         [list(range(25 + g * 4, 29 + g * 4)) for g in range(6)]
                wr = Ring(P, "win", 2, [128, KC, 512], BF16)
                pr = Ring(P, "inps", 4, [128, 512], F32, psum=True)
                sr = Ring(P, "instage", 4, [128, 512], BF16)
                ev = 0
                for grp in groups:
                    c0 = chunk_cols(grp[0])[0]
                    ncols = sum(chunk_cols(ci)[1] for ci in grp)
                    wt, bw = wr.next()
                    k.dma("pool", wt[:, :, :ncols],
                          w_in[l][:, c0:c0 + ncols].rearrange("(kc p) n -> p kc n", p=128), writes=[bw])
                    for b in range(NB):
                        for bi, (t0, n) in enumerate(blocks):
                            for ci in grp:
                                cc0, cn = chunk_cols(ci)
                                o = cc0 - c0
                                pt, bp = pr.next()
                                for kc in range(KC):
                                    k.op("pe", lambda e, kc=kc: e.matmul(
                                        pt[:cn, :n], wt[:, kc, o:o + cn], hT[:, b, kc, t0:t0 + n],
                                        start=(kc == 0), stop=(kc == KC - 1)),
                                        reads=[bw, b_h[b][bi]], writes=[bp])
                                st, bst = sr.next()
                                if ci >= 25:
                                    k.op("act", lambda e: e.activation(st[:cn, :n], pt[:cn, :n], AF.Sigmoid),
                                         reads=[bp], writes=[bst])
                                elif ev % 2 == 0:
                                    k.op("act", lambda e: e.activation(st[:cn, :n], pt[:cn, :n], AF.Copy),
                                         reads=[bp], writes=[bst])
                                else:
                                    k.op("dve", lambda e: e.tensor_copy(st[:cn, :n], pt[:cn, :n]),
                                         reads=[bp], writes=[bst])
                                ev += 1
                                k.dma("sp", PT[b, ci * 128:ci * 128 + cn, t0:t0 + n], st[:cn, :n],
                                      reads=[bst], writes=[bPT])
            if stop_after == "in":
                break
            C.mod, C.b_mod, C.gs, C.b_gs, C.ones, C.b_ones = mod, b_mod, gs, b_gs, ones, b_ones
            if "nomla" not in P.debug and "yt_in" not in P.debug:
                phase_mla(P, C, l)
            if stop_after == "mla":
                break
            if "nossm" not in P.debug and "yt_in" not in P.debug:
                phase_ssm(P, C, l)
            if stop_after == "ssm":
                break
            if "yt_in" not in P.debug:
                phase_rwkv_prep(P, C, l)
                if stop_after == "rwprep":
                    break
                phase_rwkv_scan(P, C, l)
                if stop_after == "rwkv":
                    break
            phase_merge(P, C, l)
            if stop_after == "merge":
                break
            phase_moe(P, C, l, last=(l == n_layers - 1))
            if stop_after == "moe":
                break
        k.barrier()
    return P


def host_inputs(inputs, core):
    b0 = core * NB
    x = inputs["x"][b0:b0 + NB]
    ctx = inputs["ctx"][b0:b0 + NB]
    xt0 = np.ascontiguousarray(np.concatenate([ctx, x], axis=1).transpose(0, 2, 1))
    cT = np.stack([inputs["c"][b0], inputs["c"][b0 + 1], inputs["c_ctx"]], axis=1)
    cT = np.ascontiguousarray(cT.reshape(KC, 128, 3).transpose(1, 0, 2))
    m = {"xt0": xt0, "cT": cT}
    return m


def pj(v, n):
    return np.ascontiguousarray(v.reshape(v.shape[:-1] + (n, 128)).swapaxes(-1, -2))


def host_shared(inputs):
    m = {"ones": np.ones((128, 128), np.float32)}
    for name in ("ada_w", "w_in"):
        m[name] = inputs[name]
    for name in ("mla_w_uq", "mla_w_ukv"):
        m[name] = inputs[name]
    mats = np.zeros((128, 3, 128), np.float32)
    mats[:64, 0, :64] = 1.0
    mats[64:96, 0, 64:96] = 1.0
    mats[:64, 1, :64] = 1.0
    for i in range(16):
        mats[64 + 16 + i, 2, 64 + i] = -1.0
        mats[64 + i, 2, 64 + 16 + i] = 1.0
    m["mla_mats"] = mats
    vec = np.zeros((DEPTH, 128, 8), np.float32)
    vec[:, :, 0:3] = pj(inputs["mla_q_norm"], 3)
    vec[:, :, 3:5] = pj(inputs["mla_kv_norm"], 2)
    vec[:, :64, 5] = inputs["mla_qn_nope"]
    vec[:, 64:96, 5] = inputs["mla_qn_rope"]
    vec[:, :64, 6] = inputs["mla_kn_nope"]
    vec[:, 64:96, 6] = inputs["mla_kn_rope"]
    vec[:, :64, 7] = 1.0 / 64.0
    vec[:, 64:96, 7] = 1.0 / 32.0
    m["mla_vec"] = vec
    tt = np.arange(TL)
    inv = (10000.0 ** (-np.arange(0, 16, 2, dtype=np.float32) / 16.0)).astype(np.float32)
    ang = np.concatenate([(tt // 64).astype(np.float32)[:, None] * inv, (tt % 64).astype(np.float32)[:, None] * inv], axis=-1)
    tab = np.zeros((96, 2, TT), np.float32)
    tab[:, 0, :] = 1.0
    tab[64:80, 0, TC:] = np.cos(ang).T
    tab[80:96, 0, TC:] = np.cos(ang).T
    tab[64:80, 1, TC:] = np.sin(ang).T
    tab[80:96, 1, TC:] = np.sin(ang).T
    m["rope_tab"] = tab
    it = np.zeros((128, 2, TT), np.float32)
    it[:, 0, :] = np.arange(TT)
    it[:, 1, :TC] = TC - 1 - np.arange(TC)
    it[:, 1, TC:] = TC + (TL - 1 - np.arange(TL))
    m["ssm_iota"] = it
    L_ = DEPTH
    lre = inputs["ssm_lambda_re"].reshape(L_, 2, 16, 128)
    lim = inputs["ssm_lambda_im"].reshape(L_, 2, 16, 128)
    ldt = np.repeat(inputs["ssm_log_dt"], 64, axis=-1).reshape(L_, 2, 16, 128)
    m["ssm_sv"] = np.ascontiguousarray(np.stack([lre, lim, ldt], axis=-1).transpose(0, 3, 1, 2, 4))
    BT = np.zeros((L_, 2, 16, 128, 128), np.float32)
    CT = np.zeros((L_, 2, 2, 16, 128, 128), np.float32)
    for ri, (bn, cn) in enumerate((("ssm_b_re", "ssm_c_re"), ("ssm_b_im", "ssm_c_im"))):
        bb = inputs[bn]
        cc = inputs[cn]
        for g in range(32):
            sc, gg = g // 2, g % 2
            r0 = (sc % 4) * 32 + gg * 16
            BT[:, ri, sc, r0:r0 + 16, gg * 64:(gg + 1) * 64] = bb[:, g].transpose(0, 2, 1)
            CT[:, :, ri, sc, gg * 64:(gg + 1) * 64, r0:r0 + 16] = cc[:, :, g].transpose(0, 1, 3, 2)
    m["ssm_BT"] = BT
    m["ssm_CT"] = CT
    m["ssm_vec"] = np.ascontiguousarray(np.stack([pj(inputs["ssm_d"], 4), pj(inputs["ssm_glu_b"], 4)], axis=2))
    m["ssm_glu_w"] = inputs["ssm_glu_w"]
    for name in ("w_branch", "w_out", "router_w"):
        m[name] = inputs[name]
    for name in ("moe_w1", "moe_w3", "moe_w2"):
        m[name] = inputs[name]
    cst = np.zeros((128, 3, 256), np.float32)
    cst[:, 0, :] = np.arange(1, 257)
    cst[:16, 1, :16] = 1.0
    cst[16:32, 1, 16:32] = 1.0
    cst[:, 2, :128] = np.eye(128)
    m["moe_cst"] = cst
    m["moe_jcol"] = np.stack([np.arange(1, 129), np.arange(129, 257)], axis=1).astype(np.float32)
    L_ = DEPTH
    rv = np.zeros((L_, 128, 51), np.float32)
    rv[:, :, 0:15] = pj(inputs["rwkv_mu"], 15)
    rv[:, :, 15:23] = pj(inputs["rwkv_w0"], 4).transpose(0, 2, 1, 3).reshape(L_, 128, 8)
    rv[:, :, 23:31] = pj(inputs["rwkv_a0"], 4).transpose(0, 2, 1, 3).reshape(L_, 128, 8)
    rv[:, :, 31:35] = pj(inputs["rwkv_k_k"], 4)
    rv[:, :, 35:39] = pj(inputs["rwkv_k_a"], 4)
    rv[:, :, 39:43] = pj(inputs["rwkv_r_k"].reshape(L_, 512), 4)
    rv[:, :, 43:47] = pj(inputs["rwkv_ln_w"], 4)
    rv[:, :, 47:51] = pj(inputs["rwkv_ln_b"], 4)
    m["rw_vec"] = rv
    for name in ("rwkv_w2", "rwkv_a2", "rwkv_g2"):
        m[name] = inputs[name]
    bd = np.zeros((128, 128), np.float32)
    bd[:64, :64] = 1.0
    bd[64:, 64:] = 1.0
    m["rw_bd"] = bd
    rst = np.ones((128, TT + 1), np.float32)
    rst[:, 0::128] = 0.0
    m["rw_rst"] = rst
    ii = np.arange(128)
    mk = np.zeros((128, 2, 640), np.float32)
    for j_ in range(2):
        if j_ == 0:
            strict = (ii[None, :] > ii[:, None]).astype(np.float32)
            incl = (ii[None, :] >= ii[:, None]).astype(np.float32)
        else:
            strict = (ii[None, :] < ii[:, None]).astype(np.float32)
            incl = (ii[None, :] <= ii[:, None]).astype(np.float32)
        mk[:, j_, 0:128] = strict
        mk[:, j_, 128:256] = incl
        mk[:, j_, 256:384] = -strict
        mk[:, j_, 384:512] = incl
        mk[:, j_, 512:640] = -strict.T
    m["rw_masks"] = mk
    lm = np.zeros((128, 4, 128), np.float32)
    ti, si = ii[:, None], ii[None, :]
    lm[:, 0, :] = (ti // 16 == si // 16)
    for q_, sz in enumerate((16, 32, 64)):
        lm[:, 1 + q_, :] = (ti // (2 * sz) == si // (2 * sz)) & (ti // sz != si // sz)
    m["rw_lm"] = lm
    m["ident"] = np.eye(128, dtype=np.float32)
    m["ada_b_r"] = pj(inputs["ada_b"], 48)
    m["ng_r"] = np.ascontiguousarray(np.stack([pj(inputs["norm1_g"], KC), pj(inputs["norm2_g"], KC)], axis=2))
    return m


def kernel(**inputs):
    inputs = {k_: np.asarray(v) for k_, v in inputs.items()}
    P = build()
    shared = host_shared(inputs)
    in_maps = []
    for c in range(NCORES):
        m = host_inputs(inputs, c)
        m.update(shared)
        in_maps.append({n: m[n] for n in P.inputs})
    res = run_bass_kernel_spmd(P.nc, in_maps, core_ids=list(range(NCORES)))
    outs = [r["OUT"] for r in res.results]
    y = np.concatenate(outs, axis=0)
    return np.ascontiguousarray(y.transpose(0, 2, 1)).astype(np.float32)
```

```python
import contextlib
import numpy as np
import concourse.bass as bass
import concourse.mybir as mybir
from concourse.bass_utils import run_bass_kernel_spmd

F32 = mybir.dt.float32
BF16 = mybir.dt.bfloat16
AF = mybir.ActivationFunctionType
ALU = mybir.AluOpType
AX = mybir.AxisListType

NCORES = 8
NB = 2
D = 1024
KC = 8
TC = 256
TL = 2048
TT = TC + TL
DEPTH = 4
N_IN = 6176
NCH = 49
NORM_EPS = 1e-6


def chunk_cols(ci):
    if ci < 24:
        return ci * 128, 128
    if ci == 24:
        return 3072, 32
    return 3104 + (ci - 25) * 128, 128


class Buf:
    __slots__ = ("name", "w", "r")

    def __init__(self, name=""):
        self.name = name
        self.w = None
        self.r = []


ATTACH_WAITS = True


class K:
    NDMA = 12

    def __init__(self, nc):
        self.nc = nc
        self.eng = {"pe": nc.tensor, "dve": nc.vector, "act": nc.scalar, "pool": nc.gpsimd, "sp": nc.sync}
        self.sem = {}
        self.cnt = {}
        self.seen = {e: {} for e in self.eng}
        self._stack = []
        for e in self.eng:
            self.sem[e] = self._mksem("s_" + e)
            self.cnt[e] = 0
        self.dq = {}
        for q in ("sp", "act", "pool"):
            self.dq[q] = {"n": 0}
            for i in range(self.NDMA):
                self.sem[(q, i)] = self._mksem(f"d_{q}{i}")
        self.n_instr = 0
        self.n_wait = 0

    def _mksem(self, name):
        g = self.nc.semaphore(name)
        s = g.__enter__()
        self._stack.append(g)
        return s

    def _need(self, e, dep, out):
        if dep is None:
            return
        key, val = dep
        if key == "pe" and e == "pe":
            return
        if self.seen[e].get(key, 0) >= val:
            return
        for i, (k2, v2) in enumerate(out):
            if k2 == key:
                if v2 < val:
                    out[i] = (key, val)
                return
        out.append((key, val))

    def _wait(self, e, dep):
        need = []
        self._need(e, dep, need)
        self._emit_waits(e, need, attach=False)

    def _emit_waits(self, e, need, attach):
        last = None
        if attach and ATTACH_WAITS and need:
            last = need.pop()
        for key, val in need:
            self.eng[e].wait_ge(self.sem[key], val)
            self.seen[e][key] = val
            self.n_wait += 1
        if last is not None:
            self.seen[e][last[0]] = last[1]
        return last

    def _deps(self, e, reads, writes, attach=False):
        need = []
        for b in reads:
            self._need(e, b.w, need)
        for b in writes:
            self._need(e, b.w, need)
            for r in b.r:
                self._need(e, r, need)
        return self._emit_waits(e, need, attach)

    def _mark(self, tok, reads, writes):
        for b in reads:
            b.r = [r for r in b.r if r[0] != tok[0]]
            b.r.append(tok)
        for b in writes:
            b.w = tok
            b.r = []

    def op(self, e, fn, reads=(), writes=()):
        last = self._deps(e, reads, writes, attach=True)
        ins = fn(self.eng[e])
        if last is not None:
            ins._wait_ge(self.sem[last[0]], last[1])
        self.cnt[e] += 1
        ins.then_inc(self.sem[e], 1)
        self._mark((e, self.cnt[e]), reads, writes)
        self.n_instr += 1
        return ins

    def dma(self, q, out, in_, reads=(), writes=(), **kw):
        d = self.dq[q]
        i = d["n"] % self.NDMA
        gen = d["n"] // self.NDMA
        key = (q, i)
        if gen > 0:
            self._wait(q, (key, 16 * gen))
        self._deps(q, reads, writes)
        ins = self.eng[q].dma_start(out=out, in_=in_, **kw)
        ins.then_inc(self.sem[key], 16)
        d["n"] += 1
        self._mark((key, 16 * (gen + 1)), reads, writes)
        self.n_instr += 1

    def barrier(self):
        toks = [(e, self.cnt[e]) for e in self.eng if self.cnt[e] > 0]
        for q, d in self.dq.items():
            n = d["n"]
            for i in range(self.NDMA):
                c = (n - i + self.NDMA - 1) // self.NDMA
                if c > 0:
                    toks.append(((q, i), 16 * c))
        for e in self.eng:
            for t in toks:
                self._wait(e, t)


class Ring:
    def __init__(self, P, name, n, shape, dtype, psum=False):
        self.items = []
        for i in range(n):
            t = P.ps(f"{name}{i}", shape, dtype) if psum else P.sb(f"{name}{i}", shape, dtype)
            self.items.append((t, Buf(f"{name}{i}")))
        self.i = 0

    def next(self):
        it = self.items[self.i % len(self.items)]
        self.i += 1
        return it


class Prog:
    def __init__(self, n_layers=DEPTH, debug=()):
        self.nc = nc = bass.Bass("TRN2", target_bir_lowering=False)
        self.k = K(nc)
        self.debug = set(debug)
        self.n_layers = n_layers
        self._scopes = []
        self.uid = 0
        self.inputs = {}
        self.outputs = {}
        self.dbufs = {}

    def din(self, name, shape, dtype=F32):
        t = self.nc.dram_tensor(name, list(shape), dtype, kind="ExternalInput").ap()
        self.inputs[name] = t
        return t

    def dscr(self, name, shape, dtype, out=False):
        kind = "ExternalOutput" if (out or name in self.debug) else "Internal"
        t = self.nc.dram_tensor(name, list(shape), dtype, kind=kind).ap()
        if kind == "ExternalOutput":
            self.outputs[name] = t
        self.dbufs[name] = Buf(name)
        return t

    @contextlib.contextmanager
    def scope(self):
        st = contextlib.ExitStack()
        self._scopes.append(st)
        try:
            yield
        finally:
            self.k.barrier()
            self._scopes.pop()
            st.close()

    def sb(self, name, shape, dtype):
        self.uid += 1
        g = self.nc.sbuf_tensor(f"{name}_{self.uid}", list(shape), dtype)
        return self._scopes[-1].enter_context(g)

    def ps(self, name, shape, dtype=F32):
        self.uid += 1
        g = self.nc.psum_tensor(f"{name}_{self.uid}", list(shape), dtype)
        return self._scopes[-1].enter_context(g)


MLA_SCALE = 1.0 / float(np.sqrt(96.0))


def declare_inputs(P, C):
    C.mla_w_uq = P.din("mla_w_uq", [DEPTH, 384, 768])
    C.mla_w_ukv = P.din("mla_w_ukv", [DEPTH, 256, 1024])
    C.mla_mats = P.din("mla_mats", [128, 3, 128])
    C.mla_vec = P.din("mla_vec", [DEPTH, 128, 8])
    C.rope_tab = P.din("rope_tab", [96, 2, TT])
    C.ssm_iota = P.din("ssm_iota", [128, 2, TT])
    C.ssm_sv = P.din("ssm_sv", [DEPTH, 128, 2, 16, 3])
    C.ssm_BT = P.din("ssm_BT", [DEPTH, 2, 16, 128, 128])
    C.ssm_CT = P.din("ssm_CT", [DEPTH, 2, 2, 16, 128, 128])
    C.ssm_vec = P.din("ssm_vec", [DEPTH, 128, 2, 4])
    C.ssm_glu_w = P.din("ssm_glu_w", [DEPTH, 512, 512])
    C.w_branch = P.din("w_branch", [DEPTH, 3, 512, D])
    C.w_out = P.din("w_out", [DEPTH, D, D])
    C.router_w = P.din("router_w", [DEPTH, D, 16])
    C.ident = P.din("ident", [128, 128])
    C.LG = P.dscr("LG", [NB, 16, TT], F32)
    C.bLG = Buf("LG")
    C.H2 = P.dscr("H2", [NB, TT, D], BF16)
    C.bH2 = Buf("H2")
    C.moe_w1 = P.din("moe_w1", [DEPTH, NE, D, FF])
    C.moe_w3 = P.din("moe_w3", [DEPTH, NE, D, FF])
    C.moe_w2 = P.din("moe_w2", [DEPTH, NE, FF, D])
    C.moe_cst = P.din("moe_cst", [128, 3, 256])
    C.moe_jcol = P.din("moe_jcol", [128, 2])
    C.POSD = P.dscr("POSD", [NB, NE, TT], F32)
    C.MGD = P.dscr("MGD", [NB, NE, TT], F32)
    C.bPOSD = Buf("POSD")
    C.XS = P.dscr("XS", [NE, D, NJ], BF16)
    C.bXS = Buf("XS")
    C.YE = P.dscr("YE", [NE, NJ, D], BF16)
    C.bYE = Buf("YE")
    C.OUT = P.dscr("OUT", [NB, D, TL], F32, out=True)
    C.bOUT = Buf("OUT")
    C.rw_vec = P.din("rw_vec", [DEPTH, 128, 51])
    C.rwkv_w2 = P.din("rwkv_w2", [DEPTH, 2, 64, 512])
    C.rwkv_a2 = P.din("rwkv_a2", [DEPTH, 2, 64, 512])
    C.rwkv_g2 = P.din("rwkv_g2", [DEPTH, 128, 512])
    C.rw_bd = P.din("rw_bd", [128, 128])
    C.rw_rst = P.din("rw_rst", [128, TT + 1])
    C.rw_masks = P.din("rw_masks", [128, 2, 640])
    C.rw_lm = P.din("rw_lm", [128, 4, 128])
    C.RWT = P.dscr("RWT", [NB, 2, 4, 512, TT], BF16)
    C.RWV = P.dscr("RWV", [NB, 512, TT], BF16)
    C.RWG = P.dscr("RWG", [NB, 512, TT], BF16)
    C.RWB = P.dscr("RWB", [NB, 512, TT], F32)
    C.RWPL = P.dscr("RWPL", [NB, 2, 512, NCK], F32)
    C.bRW = Buf("RW")
    if "YTOK" in P.debug:
        C.YTOK = P.dscr("YTOK", [NB, 128, NCK, 512], F32)
    C.YG = P.dscr("YG", [NB, 512, TT], BF16)
    C.bYG = Buf("YG")


def rstd_from(k, ss_ap, out_ap, scale, bias, reads, bout):
    k.op("act", lambda e: e.activation(out_ap, ss_ap, AF.Sqrt, bias=bias, scale=scale), reads=reads, writes=[bout])
    k.op("dve", lambda e: e.reciprocal(out_ap, out_ap), reads=[bout], writes=[bout])


def phase_mla(P, C, l):
    k = P.k
    PT, YT = C.PT, C.YT
    blocks = [(0, TC)] + [(TC + i * 512, 512) for i in range(4)]
    with P.scope():
        mats = P.sb("mla_mats", [128, 3, 128], BF16)
        b_mats = Buf("mats")
        k.dma("pool", mats[:], C.mla_mats, writes=[b_mats])
        onesb = P.sb("onesb", [128, 128], BF16)
        b_onesb = Buf("onesb")
        k.op("dve", lambda e: e.memset(onesb[:], 1.0), writes=[b_onesb])
        vec = P.sb("mla_vec", [128, 8], F32)
        b_vec = Buf("vec")
        k.dma("sp", vec[:], C.mla_vec[l], writes=[b_vec])
        epsc = P.sb("epsc", [128, 1], F32)
        k.op("dve", lambda e: e.memset(epsc[:], NORM_EPS), writes=[b_vec])
        tab = P.sb("rope_tab", [96, 2, TT], F32)
        b_tab = Buf("tab")
        k.dma("sp", tab[:], C.rope_tab, writes=[b_tab])
        wuq = P.sb("wuq", [128, 3, 768], BF16)
        b_wuq = Buf("wuq")
        k.dma("pool", wuq[:], C.mla_w_uq[l].rearrange("(kc p) n -> p kc n", p=128), writes=[b_wuq])
        wk = P.sb("wk", [128, 2, 8, 64], BF16)
        wv = P.sb("wv", [128, 2, 8, 64], BF16)
        b_wkv = Buf("wkv")
        ukv = C.mla_w_ukv[l].rearrange("(kc p) (h x) -> p kc h x", p=128, x=128)
        for kc in range(2):
            k.dma("pool", wk[:, kc], ukv[:, kc, :, 0:64], writes=[b_wkv])
            k.dma("pool", wv[:, kc], ukv[:, kc, :, 64:128], writes=[b_wkv])
        QT = P.sb("QT", [96, 8, TT], BF16)
        KT = P.sb("KT", [96, 8, TT], BF16)
        Vt = P.sb("Vt", [128, 18, 512], BF16)
        for b in range(NB):
            b_Q = [Buf(f"Q{i}") for i in range(5)]
            b_K = [Buf(f"K{i}") for i in range(5)]
            b_V = [Buf(f"V{i}") for i in range(5)]
            with P.scope():
                x5r = Ring(P, "x5", 2, [128, 5, 512], BF16)
                krr = Ring(P, "kr", 2, [96, 512], BF16)
                sq5r = Ring(P, "sq5", 1, [128, 5, 512], BF16)
                cqnr = Ring(P, "cqn", 2, [128, 5, 512], BF16)
                psA = Ring(P, "psA", 3, [128, 512], F32, psum=True)
                psB = Ring(P, "psB", 3, [128, 512], F32, psum=True)
                rsr = Ring(P, "rs", 3, [128, 512], F32)
                f32r = Ring(P, "f32t", 3, [128, 512], F32)
                f32r2 = Ring(P, "f32u", 3, [128, 512], F32)
                bfr = Ring(P, "bft", 3, [128, 512], BF16)
                krf = P.sb("krf", [96, 512], BF16)
                b_krf = Buf("krf")
                for bi, (t0, n) in enumerate(blocks):
                    x5, bx5 = x5r.next()
                    k.dma("sp", x5[:, :, :n], PT[b, 19 * 128:24 * 128, t0:t0 + n].rearrange("(c p) n -> p c n", p=128),
                          reads=[C.bPT], writes=[bx5])
                    kr, bkr = krr.next()
                    k.dma("sp", kr[64:96, :n], PT[b, 24 * 128:24 * 128 + 32, t0:t0 + n], reads=[C.bPT], writes=[bkr])
                    sq5, bsq5 = sq5r.next()
                    k.op("act", lambda e: e.activation(sq5[:, :, :n], x5[:, :, :n], AF.Square), reads=[bx5], writes=[bsq5])
                    cqn, bcqn = cqnr.next()
                    for (c0, nc_, col0, dim) in ((0, 3, 0, 384.0), (3, 2, 3, 256.0)):
                        ps, bps = psA.next()
                        for c in range(nc_):
                            k.op("pe", lambda e: e.matmul(ps[:, :n], onesb[:], sq5[:, c0 + c, :n],
                                                          start=(c == 0), stop=(c == nc_ - 1)),
                                 reads=[bsq5, b_onesb], writes=[bps])
                        rs, brs = rsr.next()
                        rstd_from(k, ps[:, :n], rs[:, :n], 1.0 / dim, epsc[:, 0:1], [bps, b_vec], brs)
                        for c in range(nc_):
                            k.op("dve", lambda e: e.scalar_tensor_tensor(
                                cqn[:, c0 + c, :n], x5[:, c0 + c, :n], vec[:, col0 + c:col0 + c + 1], rs[:, :n],
                                ALU.mult, ALU.mult), reads=[bx5, brs, b_vec], writes=[bcqn])
                    sqk, bsqk = bfr.next()
                    k.op("act", lambda e: e.activation(sqk[64:96, :n], kr[64:96, :n], AF.Square), reads=[bkr], writes=[bsqk])
                    ps, bps = psA.next()
                    k.op("pe", lambda e: e.matmul(ps[64:96, :n], mats[64:96, 0, 64:96], sqk[64:96, :n], start=True, stop=True),
                         reads=[bsqk, b_mats], writes=[bps])
                    rs, brs = rsr.next()
                    rstd_from(k, ps[64:96, :n], rs[64:96, :n], 1.0 / 32.0, epsc[64:96, 0:1], [bps, b_vec], brs)
                    krn, bkrn = bfr.next()
                    k.op("dve", lambda e: e.scalar_tensor_tensor(
                        krn[64:96, :n], kr[64:96, :n], vec[64:96, 6:7], rs[64:96, :n], ALU.mult, ALU.mult),
                        reads=[bkr, brs, b_vec], writes=[bkrn])
                    ps2, bps2 = psB.next()
                    k.op("pe", lambda e: e.matmul(ps2[64:96, :n], mats[64:96, 2, 64:96], krn[64:96, :n], start=True, stop=True),
                         reads=[bkrn, b_mats], writes=[bps2])
                    t1, bt1 = f32r.next()
                    k.op("dve", lambda e: e.tensor_tensor(t1[64:96, :n], krn[64:96, :n], tab[64:96, 0, t0:t0 + n], ALU.mult),
                         reads=[bkrn, b_tab], writes=[bt1])
                    t2, bt2 = f32r2.next()
                    k.op("dve", lambda e: e.tensor_tensor(t2[64:96, :n], ps2[64:96, :n], tab[64:96, 1, t0:t0 + n], ALU.mult),
                         reads=[bps2, b_tab], writes=[bt2])
                    k.op("pool", lambda e: e.tensor_tensor(krf[64:96, :n], t1[64:96, :n], t2[64:96, :n], ALU.add),
                         reads=[bt1, bt2], writes=[b_krf])
                    k.op("pool", lambda e: e.tensor_copy(
                        KT[64:96, :, t0:t0 + n], krf[64:96, :n].unsqueeze(1).to_broadcast([32, 8, n])),
                        reads=[b_krf], writes=[b_K[bi]])
                    for h in range(8):
                        ps, bps = psA.next()
                        for c in range(3):
                            k.op("pe", lambda e: e.matmul(ps[:96, :n], wuq[:, c, h * 96:(h + 1) * 96], cqn[:, c, :n],
                                                          start=(c == 0), stop=(c == 2)),
                                 reads=[bcqn, b_wuq], writes=[bps])
                        qs, bqs = f32r.next()
                        k.op("act", lambda e: e.activation(qs[:96, :n], ps[:96, :n], AF.Copy), reads=[bps], writes=[bqs])
                        sq, bsq = bfr.next()
                        k.op("act", lambda e: e.activation(sq[:96, :n], ps[:96, :n], AF.Square), reads=[bps], writes=[bsq])
                        ps2, bps2 = psB.next()
                        k.op("pe", lambda e: e.matmul(ps2[:96, :n], mats[:96, 0, :96], sq[:96, :n], start=True, stop=True),
                             reads=[bsq, b_mats], writes=[bps2])
                        rs, brs = rsr.next()
                        rstd_from(k, ps2[:96, :n], rs[:96, :n], vec[:96, 7:8], epsc[:96, 0:1], [bps2, b_vec], brs)
                        qn, bqn = bfr.next()
                        k.op("dve", lambda e: e.scalar_tensor_tensor(
                            qn[:96, :n], qs[:96, :n], vec[:96, 5:6], rs[:96, :n], ALU.mult, ALU.mult),
                            reads=[bqs, brs, b_vec], writes=[bqn])
                        ps3, bps3 = psB.next()
                        k.op("pe", lambda e: e.matmul(ps3[:96, :n], mats[:96, 2, :96], qn[:96, :n], start=True, stop=True),
                             reads=[bqn, b_mats], writes=[bps3])
                        t1, bt1 = f32r.next()
                        k.op("pool", lambda e: e.tensor_tensor(t1[:96, :n], qn[:96, :n], tab[:, 0, t0:t0 + n], ALU.mult),
                             reads=[bqn, b_tab], writes=[bt1])
                        t2, bt2 = f32r2.next()
                        k.op("dve", lambda e: e.tensor_tensor(t2[:96, :n], ps3[:96, :n], tab[:, 1, t0:t0 + n], ALU.mult),
                             reads=[bps3, b_tab], writes=[bt2])
                        k.op("pool", lambda e: e.tensor_tensor(QT[:, h, t0:t0 + n], t1[:96, :n], t2[:96, :n], ALU.add),
                             reads=[bt1, bt2], writes=[b_Q[bi]])
                        ps, bps = psA.next()
                        for c in range(2):
                            k.op("pe", lambda e: e.matmul(ps[:64, :n], wk[:, c, h, :], cqn[:, 3 + c, :n],
                                                          start=(c == 0), stop=(c == 1)),
                                 reads=[bcqn, b_wkv], writes=[bps])
                        ks_, bks = f32r.next()
                        k.op("act", lambda e: e.activation(ks_[:64, :n], ps[:64, :n], AF.Copy), reads=[bps], writes=[bks])
                        sq, bsq = bfr.next()
                        k.op("act", lambda e: e.activation(sq[:64, :n], ps[:64, :n], AF.Square), reads=[bps], writes=[bsq])
                        ps2, bps2 = psB.next()
                        k.op("pe", lambda e: e.matmul(ps2[:64, :n], onesb[:64, :64], sq[:64, :n], start=True, stop=True),
                             reads=[bsq, b_onesb], writes=[bps2])
                        rs, brs = rsr.next()
                        rstd_from(k, ps2[:64, :n], rs[:64, :n], 1.0 / 64.0, epsc[:64, 0:1], [bps2, b_vec], brs)
                        k.op("dve", lambda e: e.scalar_tensor_tensor(
                            KT[0:64, h, t0:t0 + n], ks_[:64, :n], vec[:64, 6:7], rs[:64, :n], ALU.mult, ALU.mult),
                            reads=[bks, brs, b_vec], writes=[b_K[bi]])
                    for ti in range(n // 128):
                        tile_i = (t0 // 128) + ti
                        ps, bps = psA.next()
                        for c in range(2):
                            k.op("pe", lambda e: e.matmul(
                                ps[:, :], cqn[:, 3 + c, ti * 128:(ti + 1) * 128],
                                wv[:, c].rearrange("p h x -> p (h x)"), start=(c == 0), stop=(c == 1)),
                                reads=[bcqn, b_wkv], writes=[bps])
                        k.op("act", lambda e: e.activation(Vt[:, tile_i, :], ps[:, :], AF.Copy), reads=[bps], writes=[b_V[bi]])
            if "stop_mlaprep" in P.debug:
                continue
            with P.scope():
                psS = Ring(P, "psS", 4, [128, 512], F32, psum=True)
                psO = Ring(P, "psO", 2, [64, 512], F32, psum=True)
                psD = Ring(P, "psD", 2, [64, 512], F32, psum=True)
                ptr = Ring(P, "pT", 4, [128, 512], BF16)
                rdr = Ring(P, "rden", 2, [64, 512], F32)
                osr = Ring(P, "ost", 3, [64, 512], BF16)
                allK = b_K + b_V
                LOOK = 2
                for h in range(8):
                    for bi, (q0, n) in enumerate(blocks):
                        nkt = 2 if bi == 0 else 18
                        po, bpo = psO.next()
                        pd, bpd = psD.next()
                        pend = []
                        for kt in range(nkt + LOOK):
                            if kt < nkt:
                                pS, bpS = psS.next()
                                k.op("pe", lambda e: e.matmul(pS[:, :n], KT[:, h, kt * 128:(kt + 1) * 128], QT[:, h, q0:q0 + n],
                                                              start=True, stop=True),
                                     reads=allK + [b_Q[bi]], writes=[bpS])
                                pT, bpT = ptr.next()
                                k.op("act", lambda e: e.activation(pT[:, :n], pS[:, :n], AF.Exp, scale=MLA_SCALE),
                                     reads=[bpS], writes=[bpT])
                                pend.append((kt, pT, bpT))
                            if kt >= LOOK:
                                k2, pT2, bpT2 = pend.pop(0)
                                k.op("pe", lambda e: e.matmul(po[:, :n], Vt[:, k2, h * 64:(h + 1) * 64], pT2[:, :n],
                                                              start=(k2 == 0), stop=(k2 == nkt - 1)),
                                     reads=[bpT2] + b_V, writes=[bpo])
                                k.op("pe", lambda e: e.matmul(pd[:, :n], onesb[:, :64], pT2[:, :n],
                                                              start=(k2 == 0), stop=(k2 == nkt - 1)),
                                     reads=[bpT2, b_onesb], writes=[bpd])
                        rd, brd = rdr.next()
                        k.op("dve", lambda e: e.reciprocal(rd[:, :n], pd[:, :n]), reads=[bpd], writes=[brd])
                        os_, bos = osr.next()
                        k.op("dve", lambda e: e.tensor_tensor(os_[:, :n], po[:, :n], rd[:, :n], ALU.mult),
                             reads=[bpo, brd], writes=[bos])
                        k.dma("sp", YT[b, 2, h * 64:(h + 1) * 64, q0:q0 + n], os_[:, :n], reads=[bos], writes=[C.bYT])


PI = float(np.pi)


def rev_ap(ap2d, lo, hi):
    from concourse.ap import AP
    a = ap2d[:, lo:hi]
    return AP(a.tensor, a.offset + (hi - lo - 1) * a.ap[1][0], [list(a.ap[0]), [-a.ap[1][0], hi - lo]])


MAGIC = 12582912.0
TWO_PI = 2.0 * PI


def sin_reduced(k, out, tmp, in0, th, th2pi, shift, hp_tile, reads, bout, btmp):
    k.op("dve", lambda e: e.tensor_scalar(tmp, in0, th2pi, MAGIC + shift / TWO_PI, ALU.mult, ALU.add),
         reads=reads + [btmp], writes=[btmp])
    k.op("dve", lambda e: e.tensor_scalar(tmp, tmp, -MAGIC, -TWO_PI, ALU.add, ALU.mult), reads=[btmp], writes=[btmp])
    k.op("dve", lambda e: e.scalar_tensor_tensor(tmp, in0, th, tmp, ALU.mult, ALU.add), reads=reads + [btmp], writes=[btmp])
    if shift == 0.0:
        k.op("act", lambda e: e.activation(out, tmp, AF.Sin, scale=0.999999), reads=[btmp, bout], writes=[bout])
    else:
        k.op("act", lambda e: e.activation(out, tmp, AF.Sin, scale=0.999999, bias=hp_tile), reads=[btmp, bout], writes=[bout])


def phase_ssm(P, C, l):
    k = P.k
    PT, YT = C.PT, C.YT
    blocks = [(0, TC)] + [(TC + i * 512, 512) for i in range(4)]
    YG = C.YG
    with P.scope():
        iota = P.sb("iota", [128, 2, TT], F32)
        b_c = Buf("ssmconst")
        k.dma("sp", iota[:], C.ssm_iota, writes=[b_c])
        sv = P.sb("sv", [128, 2, 16, 3], F32)
        k.dma("sp", sv[:], C.ssm_sv[l], writes=[b_c])
        BT = P.sb("BT", [128, 2, 16, 128], BF16)
        k.dma("pool", BT[:], C.ssm_BT[l].rearrange("r s p n -> p r s n"), writes=[b_c])
        CT = P.sb("CT", [128, 2, 2, 16, 128], BF16)
        for j in range(2):
            k.dma("pool", CT[:, j], C.ssm_CT[l, j].rearrange("r s p n -> p r s n"), writes=[b_c])
        dsk = P.sb("dsk", [128, 2, 4], F32)
        k.dma("sp", dsk[:], C.ssm_vec[l], writes=[b_c])
        hpi = P.sb("hpi", [128, 1], F32)
        k.op("dve", lambda e: e.memset(hpi[:], 0.5 * PI * 0.999999), writes=[b_c])
        shp = [128, 2, 16]
        names = "dt rho th th2 sn cs are aim den t1 t2 cre cim ncre".split()
        Tl = {n_: P.sb("d_" + n_, shp, F32) for n_ in names}
        bd = Buf("disc")
        lr, li, ldt = sv[:, :, :, 0], sv[:, :, :, 1], sv[:, :, :, 2]
        A_ = lambda e_, fn: k.op(e_, fn, reads=[b_c, bd], writes=[bd])
        A_("act", lambda e: e.activation(Tl["dt"][:], ldt, AF.Exp))
        A_("dve", lambda e: e.tensor_tensor(Tl["t1"][:], lr, Tl["dt"][:], ALU.mult))
        A_("act", lambda e: e.activation(Tl["rho"][:], Tl["t1"][:], AF.Exp))
        A_("dve", lambda e: e.tensor_tensor(Tl["th"][:], li, Tl["dt"][:], ALU.mult))
        sin_reduced(k, Tl["sn"][:], Tl["t1"][:], Tl["th"][:], 1.0, 1.0 / TWO_PI, 0.0, None, [b_c, bd], bd, bd)
        sin_reduced(k, Tl["cs"][:], Tl["t1"][:], Tl["th"][:], 1.0, 1.0 / TWO_PI, 0.5 * PI, hpi[:, 0:1], [b_c, bd], bd, bd)
        A_("dve", lambda e: e.tensor_scalar(Tl["th2"][:], Tl["th"][:], 1.0 / TWO_PI, None, ALU.mult))
        A_("dve", lambda e: e.tensor_tensor(Tl["are"][:], Tl["rho"][:], Tl["cs"][:], ALU.mult))
        A_("dve", lambda e: e.tensor_tensor(Tl["aim"][:], Tl["rho"][:], Tl["sn"][:], ALU.mult))
        A_("dve", lambda e: e.tensor_scalar(Tl["are"][:], Tl["are"][:], -1.0, None, ALU.add))
        A_("dve", lambda e: e.tensor_tensor(Tl["t1"][:], lr, lr, ALU.mult))
        A_("dve", lambda e: e.tensor_tensor(Tl["t2"][:], li, li, ALU.mult))
        A_("dve", lambda e: e.tensor_tensor(Tl["den"][:], Tl["t1"][:], Tl["t2"][:], ALU.add))
        A_("dve", lambda e: e.reciprocal(Tl["den"][:], Tl["den"][:]))
        A_("dve", lambda e: e.tensor_tensor(Tl["t1"][:], Tl["are"][:], lr, ALU.mult))
        A_("dve", lambda e: e.tensor_tensor(Tl["t2"][:], Tl["aim"][:], li, ALU.mult))
        A_("dve", lambda e: e.tensor_tensor(Tl["cre"][:], Tl["t1"][:], Tl["t2"][:], ALU.add))
        A_("dve", lambda e: e.tensor_tensor(Tl["cre"][:], Tl["cre"][:], Tl["den"][:], ALU.mult))
        A_("dve", lambda e: e.tensor_tensor(Tl["t1"][:], Tl["aim"][:], lr, ALU.mult))
        A_("dve", lambda e: e.tensor_tensor(Tl["t2"][:], Tl["are"][:], li, ALU.mult))
        A_("dve", lambda e: e.tensor_tensor(Tl["cim"][:], Tl["t1"][:], Tl["t2"][:], ALU.subtract))
        A_("dve", lambda e: e.tensor_tensor(Tl["cim"][:], Tl["cim"][:], Tl["den"][:], ALU.mult))
        A_("dve", lambda e: e.tensor_scalar(Tl["ncre"][:], Tl["cre"][:], -1.0, None, ALU.mult))
        big = lambda n_: P.sb(n_, [128, TT], F32)
        bigb = lambda n_: P.sb(n_, [128, TT], BF16)
        CS, SN, ERE, EIM = bigb("CS"), bigb("SN"), bigb("ERE"), bigb("EIM")
        BUR, BUI = bigb("BUR"), bigb("BUI")
        WK = [[bigb(f"{x}{b}") for x in ("T1", "T2", "ZR", "ZI")] for b in range(NB)]
        TA, TB = big("TA"), big("TB")
        b_ta, b_tb = Buf("ta"), Buf("tb")
        bWK = [[Buf(f"{x}{b}") for x in ("t1", "t2", "zr", "zi")] for b in range(NB)]
        T1, T2, ZR, ZI = WK[0]
        b_t1, b_t2, b_zr, b_zi = bWK[0]
        b_tab, b_bu = Buf("tab"), Buf("bu")
        Q = [P.sb(f"Q{i}", [128, TT], BF16) for i in range(4)]
        b_q = [Buf(f"q{i}") for i in range(4)]
        U = [P.sb(f"U{b}", [128, TT], BF16) for b in range(NB)]
        b_u = [Buf(f"u{b}") for b in range(NB)]
        YA = [P.sb(f"YA{b}", [128, TT], F32) for b in range(NB)]
        b_ya = [Buf(f"ya{b}") for b in range(NB)]
        psr = Ring(P, "ssmps", 4, [128, 512], F32, psum=True)
        psy = Ring(P, "ssmpy", 3, [128, 512], F32, psum=True)
        for oc in range(4):
            for b in range(NB):
                k.dma("sp", U[b][:], PT[b, oc * 128:(oc + 1) * 128, :], reads=[C.bPT], writes=[b_u[b]])
                k.op("pool", lambda e: e.memset(YA[b][:], 0.0), writes=[b_ya[b]])
            for j in range(2):
                for s4 in range(4):
                    sc = oc * 4 + s4
                    th = Tl["th"][:, j, sc:sc + 1]
                    th2 = Tl["th2"][:, j, sc:sc + 1]
                    sin_reduced(k, SN[:], TA[:], iota[:, j, :], th, th2, 0.0, None, [b_c, bd], b_tab, b_ta)
                    sin_reduced(k, CS[:], TB[:], iota[:, j, :], th, th2, 0.5 * PI, hpi[:, 0:1], [b_c, bd], b_tab, b_tb)
                    cre, cim, ncre = (Tl[x][:, j, sc:sc + 1] for x in ("cre", "cim", "ncre"))
                    k.op("dve", lambda e: e.tensor_scalar(ERE[:], CS[:], cre, None, ALU.mult), reads=[b_tab, bd], writes=[b_tab])
                    k.op("dve", lambda e: e.scalar_tensor_tensor(ERE[:], SN[:], cim, ERE[:], ALU.mult, ALU.add),
                         reads=[b_tab, bd], writes=[b_tab])
                    k.op("pool", lambda e: e.tensor_scalar(EIM[:], CS[:], cim, None, ALU.mult), reads=[b_tab, bd], writes=[b_tab])
                    k.op("dve", lambda e: e.scalar_tensor_tensor(EIM[:], SN[:], ncre, EIM[:], ALU.mult, ALU.add),
                         reads=[b_tab, bd], writes=[b_tab])
                    rho = Tl["rho"][:, j, sc:sc + 1]
                    for b in range(NB):
                        T1, T2, ZR, ZI = WK[b]
                        b_t1, b_t2, b_zr, b_zi = bWK[b]
                        for (t0, n) in blocks:
                            for ri, dst in ((0, BUR), (1, BUI)):
                                ps, bps = psr.next()
                                k.op("pe", lambda e: e.matmul(ps[:, :n], BT[:, ri, sc, :], U[b][:, t0:t0 + n], start=True, stop=True),
                                     reads=[b_c, b_u[b]], writes=[bps])
                                k.op("act", lambda e: e.activation(dst[:, t0:t0 + n], ps[:, :n], AF.Copy),
                                     reads=[bps], writes=[b_bu])
                        k.op("dve", lambda e: e.tensor_tensor(T1[:], ERE[:], BUR[:], ALU.mult), reads=[b_tab, b_bu], writes=[b_t1])
                        k.op("pool", lambda e: e.tensor_tensor(T2[:], EIM[:], BUI[:], ALU.mult), reads=[b_tab, b_bu], writes=[b_t2])
                        k.op("dve", lambda e: e.tensor_tensor(ZR[:], T1[:], T2[:], ALU.subtract), reads=[b_t1, b_t2], writes=[b_zr])
                        k.op("pool", lambda e: e.tensor_tensor(T1[:], ERE[:], BUI[:], ALU.mult), reads=[b_tab, b_bu, b_zr], writes=[b_t1])
                        k.op("dve", lambda e: e.tensor_tensor(T2[:], EIM[:], BUR[:], ALU.mult), reads=[b_tab, b_bu, b_zr], writes=[b_t2])
                        k.op("pool", lambda e: e.tensor_tensor(ZI[:], T1[:], T2[:], ALU.add), reads=[b_t1, b_t2], writes=[b_zi])
                        for Z, bz, eng in ((ZR, b_zr, "dve"), (ZI, b_zi, "dve")):
                            if j == 0:
                                k.op(eng, lambda e: e.tensor_tensor_scan(Z[:], rho.to_broadcast([128, TT]), Z[:], 0.0, ALU.mult, ALU.add),
                                     reads=[bz, bd], writes=[bz])
                            else:
                                r0 = rev_ap(Z[:], 0, TC)
                                k.op(eng, lambda e: e.tensor_tensor_scan(r0, rho.to_broadcast([128, TC]), r0, 0.0, ALU.mult, ALU.add),
                                     reads=[bz, bd], writes=[bz])
                                r1 = rev_ap(Z[:], TC, TT)
                                k.op(eng, lambda e: e.tensor_tensor_scan(r1, rho.to_broadcast([128, TL]), r1, Z[:, 0:1], ALU.mult, ALU.add),
                                     reads=[bz, bd], writes=[bz])
                        k.op("pool", lambda e: e.tensor_tensor(Q[0][:], CS[:], ZR[:], ALU.mult), reads=[b_tab, b_zr], writes=[b_q[0]])
                        for qi, (tabl, Z, bz) in ((1, (SN, ZI, b_zi)), (2, (SN, ZR, b_zr)), (3, (CS, ZI, b_zi))):
                            k.op("dve", lambda e: e.scalar_tensor_tensor(Q[qi][:], tabl[:], -1.0, Z[:], ALU.mult, ALU.mult),
                                 reads=[b_tab, bz], writes=[b_q[qi]])
                        lhs = (CT[:, j, 0, sc, :], CT[:, j, 0, sc, :], CT[:, j, 1, sc, :], CT[:, j, 1, sc, :])
                        for (t0, n) in blocks:
                            ps, bps = psy.next()
                            for qi in range(4):
                                k.op("pe", lambda e: e.matmul(ps[:, :n], lhs[qi], Q[qi][:, t0:t0 + n], start=(qi == 0), stop=(qi == 3)),
                                     reads=[b_c, b_q[qi]], writes=[bps])
                            k.op("dve", lambda e: e.tensor_tensor(YA[b][:, t0:t0 + n], YA[b][:, t0:t0 + n], ps[:, :n], ALU.add),
                                 reads=[bps, b_ya[b]], writes=[b_ya[b]])
            for b in range(NB):
                k.op("dve", lambda e: e.scalar_tensor_tensor(TA[:], U[b][:], dsk[:, 0, oc:oc + 1], YA[b][:], ALU.mult, ALU.add),
                     reads=[b_u[b], b_ya[b], b_c, b_ta], writes=[b_ta])
                k.op("act", lambda e: e.activation(TB[:], TA[:], AF.Square), reads=[b_ta, b_tb], writes=[b_tb])
                k.op("dve", lambda e: e.tensor_scalar(TB[:], TB[:], 0.044715, 1.0, ALU.mult, ALU.add), reads=[b_tb], writes=[b_tb])
                k.op("pool", lambda e: e.tensor_tensor(TB[:], TB[:], TA[:], ALU.mult), reads=[b_ta, b_tb], writes=[b_tb])
                k.op("act", lambda e: e.activation(TB[:], TB[:], AF.Sigmoid, scale=1.5957691216057308), reads=[b_tb], writes=[b_tb])
                k.op("dve", lambda e: e.tensor_tensor(Q[b][:], TA[:], TB[:], ALU.mult), reads=[b_ta, b_tb, b_q[b]], writes=[b_q[b]])
                k.dma("sp", YG[b, oc * 128:(oc + 1) * 128, :], Q[b][:], reads=[b_q[b]], writes=[C.bYG])
    with P.scope():
        gw = P.sb("gluw", [128, 4, 512], BF16)
        b_gw = Buf("gluw")
        k.dma("pool", gw[:], C.ssm_glu_w[l].rearrange("(kc p) n -> p kc n", p=128), writes=[b_gw])
        dsk = P.sb("dsk2", [128, 2, 4], F32)
        k.dma("sp", dsk[:], C.ssm_vec[l], writes=[b_gw])
        ygr = Ring(P, "yg", 2, [128, 4, 512], BF16)
        psr = Ring(P, "glups", 4, [128, 512], F32, psum=True)
        sgr = Ring(P, "sg", 3, [128, 512], F32)
        str_ = Ring(P, "gst", 3, [128, 512], BF16)
        for b in range(NB):
            for (t0, n) in blocks:
                yg, byg = ygr.next()
                k.dma("sp", yg[:, :, :n], YG[b, :, t0:t0 + n].rearrange("(c p) n -> p c n", p=128), reads=[C.bYG], writes=[byg])
                for oc in range(4):
                    ps, bps = psr.next()
                    for kc in range(4):
                        k.op("pe", lambda e: e.matmul(ps[:, :n], gw[:, kc, oc * 128:(oc + 1) * 128], yg[:, kc, :n],
                                                      start=(kc == 0), stop=(kc == 3)), reads=[b_gw, byg], writes=[bps])
                    sg, bsg = sgr.next()
                    k.op("act", lambda e: e.activation(sg[:, :n], ps[:, :n], AF.Sigmoid, bias=dsk[:, 1, oc:oc + 1]),
                         reads=[bps, b_gw], writes=[bsg])
                    st, bst = str_.next()
                    k.op("dve", lambda e: e.tensor_tensor(st[:, :n], yg[:, oc, :n], sg[:, :n], ALU.mult), reads=[byg, bsg], writes=[bst])
                    k.dma("sp", YT[b, 0, oc * 128:(oc + 1) * 128, t0:t0 + n], st[:, :n], reads=[bst], writes=[C.bYT])


def phase_merge(P, C, l):
    k = P.k
    PT, YT, XT = C.PT, C.YT, C.XT
    src_x = C.xt0 if l == 0 else XT
    mod, gs = C.mod, C.gs
    blocks = [(i * 256, 256) for i in range(9)]
    with P.scope():
        wb = P.sb("wb", [128, 3, 4, D], BF16)
        b_w = Buf("mw")
        for j in range(3):
            k.dma("pool", wb[:, j], C.w_branch[l, j].rearrange("(kc p) n -> p kc n", p=128), writes=[b_w])
        wo = P.sb("wo", [128, KC, D], BF16)
        k.dma("pool", wo[:], C.w_out[l].rearrange("(kc p) n -> p kc n", p=128), writes=[b_w])
        rw = P.sb("rw", [128, KC, 16], F32)
        k.dma("sp", rw[:], C.router_w[l].rearrange("(kc p) n -> p kc n", p=128), writes=[b_w])
        ident = P.sb("ident", [128, 128], BF16)
        k.dma("pool", ident[:], C.ident, writes=[b_w])
        y3r = Ring(P, "y3", 2, [128, 3, 4, 256], BF16)
        gr = Ring(P, "g", 2, [128, 24, 256], BF16)
        xr = Ring(P, "mx", 2, [128, KC, 256], F32)
        mTr = Ring(P, "mT", 1, [128, KC, 256], BF16)
        mr = Ring(P, "m", 6, [128, 256], F32)
        x1r = Ring(P, "x1", 1, [128, KC, 256], F32)
        sqr = Ring(P, "msq", 1, [128, KC, 256], F32)
        h2fr = Ring(P, "h2f", 1, [128, KC, 256], F32)
        h2br = Ring(P, "h2b", 1, [128, KC, 256], BF16)
        tsr = Ring(P, "tst", 2, [128, D], BF16)
        rsr = Ring(P, "mrs", 2, [128, 256], F32)
        lgr = Ring(P, "lgs", 2, [16, 256], F32)
        psb = Ring(P, "psb", 3, [128, 256], F32, psum=True)
        pso = Ring(P, "pso", 2, [128, 256], F32, psum=True)
        pss = Ring(P, "pss", 2, [128, 256], F32, psum=True)
        pst = Ring(P, "pst", 1, [128, D], BF16, psum=True)
        for b in range(NB):
            for (t0, n) in blocks:
                j = 2 if t0 < TC else b
                y3, by3 = y3r.next()
                k.dma("sp", y3[:], YT[b, :, :, t0:t0 + n].rearrange("j (c p) n -> p j c n", p=128), reads=[C.bYT], writes=[by3])
                g, bg = gr.next()
                k.dma("act", g[:], PT[b, 25 * 128:49 * 128, t0:t0 + n].rearrange("(c p) n -> p c n", p=128), reads=[C.bPT], writes=[bg])
                x, bx = xr.next()
                k.dma("sp", x[:], src_x[b][:, t0:t0 + n].rearrange("(kc p) n -> p kc n", p=128), reads=[C.bXT[b]], writes=[bx])
                mT, bmT = mTr.next()
                for dc in range(KC):
                    ms = []
                    for jj in range(3):
                        ps, bps = psb.next()
                        for kc in range(4):
                            k.op("pe", lambda e: e.matmul(ps[:], wb[:, jj, kc, dc * 128:(dc + 1) * 128], y3[:, jj, kc, :],
                                                          start=(kc == 0), stop=(kc == 3)), reads=[b_w, by3], writes=[bps])
                        m, bm = mr.next()
                        k.op("dve", lambda e: e.tensor_tensor(m[:], ps[:], g[:, jj * 8 + dc, :], ALU.mult), reads=[bps, bg], writes=[bm])
                        ms.append((m, bm))
                    k.op("pool", lambda e: e.tensor_tensor(ms[0][0][:], ms[0][0][:], ms[1][0][:], ALU.add),
                         reads=[ms[0][1], ms[1][1]], writes=[ms[0][1]])
                    k.op("pool", lambda e: e.tensor_tensor(mT[:, dc, :], ms[0][0][:], ms[2][0][:], ALU.add),
                         reads=[ms[0][1], ms[2][1]], writes=[bmT])
                x1, bx1 = x1r.next()
                for dc in range(KC):
                    ps, bps = pso.next()
                    for kc in range(KC):
                        k.op("pe", lambda e: e.matmul(ps[:], wo[:, kc, dc * 128:(dc + 1) * 128], mT[:, kc, :],
                                                      start=(kc == 0), stop=(kc == KC - 1)), reads=[b_w, bmT], writes=[bps])
                    k.op("dve", lambda e: e.scalar_tensor_tensor(x1[:, dc, :], ps[:], mod[:, 2 * 8 + dc, j:j + 1], x[:, dc, :],
                                                                 ALU.mult, ALU.add), reads=[bps, bx, C.b_mod], writes=[bx1])
                k.dma("sp", XT[b][:, t0:t0 + n].rearrange("(kc p) n -> p kc n", p=128), x1[:], reads=[bx1], writes=[C.bXT[b]])
                sq, bsq = sqr.next()
                k.op("act", lambda e: e.activation(sq[:], x1[:], AF.Square), reads=[bx1], writes=[bsq])
                ss, bss = pss.next()
                for kc in range(KC):
                    k.op("pe", lambda e: e.matmul(ss[:], C.ones[:], sq[:, kc, :], start=(kc == 0), stop=(kc == KC - 1)),
                         reads=[bsq, C.b_ones], writes=[bss])
                rs, brs = rsr.next()
                k.op("act", lambda e: e.activation(rs[:], ss[:], AF.Sqrt, bias=C.epsc[:, 0:1], scale=1.0 / D), reads=[bss], writes=[brs])
                k.op("dve", lambda e: e.reciprocal(rs[:], rs[:]), reads=[brs], writes=[brs])
                k.op("dve", lambda e: e.tensor_tensor(sq[:], x1[:], rs[:].unsqueeze(1).to_broadcast([128, KC, n]), ALU.mult),
                     reads=[bx1, brs, bsq], writes=[bsq])
                h2f, bh2f = h2fr.next()
                for kc in range(KC):
                    k.op("act", lambda e: e.activation(h2f[:, kc, :], sq[:, kc, :], AF.Identity,
                                                       bias=mod[:, 3 * 8 + kc, j:j + 1], scale=gs[:, 1, kc, j:j + 1]),
                         reads=[bsq, C.b_mod, C.b_gs], writes=[bh2f])
                h2b, bh2b = h2br.next()
                k.op("pool", lambda e: e.tensor_copy(h2b[:], h2f[:]), reads=[bh2f], writes=[bh2b])
                lp, blp = pss.next()
                for kc in range(KC):
                    k.op("pe", lambda e: e.matmul(lp[:16, :], rw[:, kc, :], h2f[:, kc, :], start=(kc == 0), stop=(kc == KC - 1)),
                         reads=[b_w, bh2f], writes=[blp])
                lg, blg = lgr.next()
                k.op("act", lambda e: e.activation(lg[:], lp[:16, :], AF.Copy), reads=[blp], writes=[blg])
                k.dma("sp", C.LG[b, :, t0:t0 + n], lg[:], reads=[blg], writes=[C.bLG])
                for tt in range(n // 128):
                    pt, bpt = pst.next()
                    for kc in range(KC):
                        k.op("pe", lambda e: e.transpose(pt[:, kc * 128:(kc + 1) * 128], h2b[:, kc, tt * 128:(tt + 1) * 128], ident[:]),
                             reads=[bh2b, b_w], writes=[bpt])
                    ts, bts = tsr.next()
                    k.op("act", lambda e: e.activation(ts[:], pt[:], AF.Copy), reads=[bpt], writes=[bts])
                    k.dma("sp", C.H2[b, t0 + tt * 128:t0 + (tt + 1) * 128, :], ts[:], reads=[bts], writes=[C.bH2])


NE = 16
FF = 1536
CAPL = 256
CAPC = 32
NJ = 2 * CAPL + 2 * CAPC


def phase_moe(P, C, l, last):
    k = P.k
    XT = C.XT
    mod = C.mod
    with P.scope():
        cst = P.sb("moecst", [128, 3, 256], F32)
        b_c = Buf("moecst")
        k.dma("sp", cst[:], C.moe_cst, writes=[b_c])
        A = P.sb("rA", [32, TT], F32)
        AFF = P.sb("rAFF", [32, TT], F32)
        W = P.sb("rW", [32, TT], F32)
        MG = P.sb("rMG", [32, TT], F32)
        MK = P.sb("rMK", [32, TT], F32)
        PS_ = P.sb("rPOS", [32, TT], F32)
        mx = P.sb("rmx", [32, 8], F32)
        bA, bAFF, bW, bMG, bMK, bPOS, bmx = [Buf(x) for x in "A AFF W MG MK POS mx".split()]
        for b in range(NB):
            k.dma("sp", A[b * 16:(b + 1) * 16, :], C.LG[b], reads=[C.bLG], writes=[bA])
        k.op("act", lambda e: e.activation(A[:], A[:], AF.Exp), reads=[bA], writes=[bA])
        psr = Ring(P, "rps", 2, [128, 512], F32, psum=True)
        for t0 in range(0, TT, 512):
            n = min(512, TT - t0)
            ps, bps = psr.next()
            k.op("pe", lambda e: e.matmul(ps[:32, :n], cst[:32, 1, :32], A[:, t0:t0 + n], start=True, stop=True),
                 reads=[bA, b_c], writes=[bps])
            k.op("dve", lambda e: e.reciprocal(W[:, t0:t0 + n], ps[:32, :n]), reads=[bps], writes=[bW])
        k.op("dve", lambda e: e.tensor_tensor(AFF[:], A[:], W[:], ALU.mult), reads=[bA, bW], writes=[bAFF])
        k.op("dve", lambda e: e.tensor_copy(W[:], AFF[:]), reads=[bAFF, bW], writes=[bW])
        for (lo, hi, cap) in ((0, TC, CAPC), (TC, TT, CAPL)):
            for it in range(cap // 8):
                k.op("dve", lambda e: e.max(out=mx[:], in_=W[:, lo:hi]), reads=[bW, bmx], writes=[bmx])
                k.op("dve", lambda e: e.match_replace(out=W[:, lo:hi], in_to_replace=mx[:], in_values=W[:, lo:hi], imm_value=0.0),
                     reads=[bmx, bW], writes=[bW])
        k.op("dve", lambda e: e.tensor_tensor(MG[:], AFF[:], W[:], ALU.subtract), reads=[bAFF, bW], writes=[bMG])
        k.op("dve", lambda e: e.tensor_single_scalar(MK[:], MG[:], 0.0, ALU.is_gt), reads=[bMG], writes=[bMK])
        for (lo, hi) in ((0, TC), (TC, TT)):
            k.op("dve", lambda e: e.tensor_tensor_scan(PS_[:, lo:hi], cst[:32, 0, 0:1].to_broadcast([32, hi - lo]), MK[:, lo:hi],
                                                       0.0, ALU.mult, ALU.add), reads=[bMK, b_c], writes=[bPOS])
        for b in range(NB):
            k.dma("sp", C.POSD[b], PS_[b * 16:(b + 1) * 16, :], reads=[bPOS], writes=[C.bPOSD])
            k.dma("sp", C.MGD[b], MG[b * 16:(b + 1) * 16, :], reads=[bMG], writes=[C.bPOSD])
        posT = P.sb("posT", [128, 18, 32], F32)
        mkT = P.sb("mkT", [128, 18, 32], F32)
        b_pT = Buf("posT")
        for tt in range(18):
            ps, bps = psr.next()
            k.op("pe", lambda e: e.transpose(ps[:, 0:32], PS_[:, tt * 128:(tt + 1) * 128], cst[:32, 2, :32]),
                 reads=[bPOS, b_c], writes=[bps])
            k.op("pe", lambda e: e.transpose(ps[:, 32:64], MK[:, tt * 128:(tt + 1) * 128], cst[:32, 2, :32]),
                 reads=[bMK, b_c], writes=[bps])
            k.op("act", lambda e: e.activation(posT[:, tt, :], ps[:, 0:32], AF.Copy), reads=[bps], writes=[b_pT])
            k.op("act", lambda e: e.activation(mkT[:, tt, :], ps[:, 32:64], AF.Copy), reads=[bps], writes=[b_pT])
        H2s = P.sb("H2s", [128, 18, D], BF16)
        bH = Buf("H2s")
        selr = Ring(P, "sel", 2, [128, 18, 256], BF16)
        gps = Ring(P, "gps", 3, [128, 256], F32, psum=True)
        gpc = Ring(P, "gpc", 2, [128, 32], F32, psum=True)
        xsr = Ring(P, "xs", 2, [128, KC, CAPL + CAPC], BF16)
        for b in range(NB):
            k.dma("sp", H2s[:], C.H2[b].rearrange("(tt p) d -> p tt d", p=128), reads=[C.bH2], writes=[bH])
            for ex in range(NE):
                col = b * 16 + ex
                sel, bsel = selr.next()
                for tt in range(18):
                    ncap = CAPC if tt < 2 else CAPL
                    k.op("dve" if tt % 2 == 0 else "pool", lambda e: e.tensor_scalar(
                        sel[:, tt, :ncap], cst[:, 0, :ncap], posT[:, tt, col:col + 1], mkT[:, tt, col:col + 1],
                        ALU.is_equal, ALU.mult), reads=[b_c, b_pT], writes=[bsel])
                xs, bxs = xsr.next()
                for kc in range(KC):
                    ps, bps = gps.next()
                    for tt in range(16):
                        k.op("pe", lambda e: e.matmul(ps[:], H2s[:, 2 + tt, kc * 128:(kc + 1) * 128], sel[:, 2 + tt, :],
                                                      start=(tt == 0), stop=(tt == 15)), reads=[bH, bsel], writes=[bps])
                    k.op("act" if kc % 2 == 0 else "dve",
                         (lambda e: e.activation(xs[:, kc, :CAPL], ps[:], AF.Copy)) if kc % 2 == 0 else
                         (lambda e: e.tensor_copy(xs[:, kc, :CAPL], ps[:])), reads=[bps], writes=[bxs])
                    pc, bpc = gpc.next()
                    for tt in range(2):
                        k.op("pe", lambda e: e.matmul(pc[:], H2s[:, tt, kc * 128:(kc + 1) * 128], sel[:, tt, :CAPC],
                                                      start=(tt == 0), stop=(tt == 1)), reads=[bH, bsel], writes=[bpc])
                    k.op("act", lambda e: e.activation(xs[:, kc, CAPL:], pc[:], AF.Copy), reads=[bpc], writes=[bxs])
                k.dma("sp", C.XS[ex, :, b * CAPL:(b + 1) * CAPL].rearrange("(kc p) j -> p kc j", p=128), xs[:, :, :CAPL],
                      reads=[bxs], writes=[C.bXS])
                k.dma("sp", C.XS[ex, :, 2 * CAPL + b * CAPC:2 * CAPL + (b + 1) * CAPC].rearrange("(kc p) j -> p kc j", p=128),
                      xs[:, :, CAPL:], reads=[bxs], writes=[C.bXS])
    if "stop_moeA" in P.debug:
        return
    with P.scope():
        w1r = Ring(P, "w1", 2, [128, KC, FF], BF16)
        w3r = Ring(P, "w3", 2, [128, KC, FF], BF16)
        w2r = Ring(P, "w2", 2, [128, 12, D], BF16)
        xsr = Ring(P, "xsb", 2, [128, KC, NJ], BF16)
        hr = Ring(P, "hid", 2, [128, 12, NJ], BF16)
        slr = Ring(P, "silu", 3, [128, 288], F32)
        yer = Ring(P, "ye", 3, [128, D], BF16)
        ps1 = Ring(P, "ps1", 2, [128, 288], F32, psum=True)
        ps3 = Ring(P, "ps3", 2, [128, 288], F32, psum=True)
        psy = Ring(P, "psy", 3, [128, 512], F32, psum=True)
        for ex in range(NE):
            w1, bw1 = w1r.next()
            w3, bw3 = w3r.next()
            w2, bw2 = w2r.next()
            k.dma("pool", w1[:], C.moe_w1[l, ex].rearrange("(kc p) f -> p kc f", p=128), writes=[bw1])
            k.dma("pool", w3[:], C.moe_w3[l, ex].rearrange("(kc p) f -> p kc f", p=128), writes=[bw3])
            k.dma("pool", w2[:], C.moe_w2[l, ex].rearrange("(fc p) d -> p fc d", p=128), writes=[bw2])
            xs, bxs = xsr.next()
            k.dma("sp", xs[:], C.XS[ex].rearrange("(kc p) j -> p kc j", p=128), reads=[C.bXS], writes=[bxs])
            hid, bh = hr.next()
            for fc in range(12):
                for half in range(2):
                    c0 = half * 288
                    p1, bp1 = ps1.next()
                    p3, bp3 = ps3.next()
                    for kc in range(KC):
                        k.op("pe", lambda e: e.matmul(p1[:], w1[:, kc, fc * 128:(fc + 1) * 128], xs[:, kc, c0:c0 + 288],
                                                      start=(kc == 0), stop=(kc == KC - 1)), reads=[bw1, bxs], writes=[bp1])
                    for kc in range(KC):
                        k.op("pe", lambda e: e.matmul(p3[:], w3[:, kc, fc * 128:(fc + 1) * 128], xs[:, kc, c0:c0 + 288],
                                                      start=(kc == 0), stop=(kc == KC - 1)), reads=[bw3, bxs], writes=[bp3])
                    sl, bsl = slr.next()
                    k.op("act", lambda e: e.activation(sl[:], p1[:], AF.Silu), reads=[bp1], writes=[bsl])
                    k.op("dve", lambda e: e.tensor_tensor(hid[:, fc, c0:c0 + 288], sl[:], p3[:], ALU.mult),
                         reads=[bsl, bp3], writes=[bh])
            for jt in range(5):
                nj = 128 if jt < 4 else NJ - 512
                ye, bye = yer.next()
                for dh in range(2):
                    py, bpy = psy.next()
                    for fc in range(12):
                        k.op("pe", lambda e: e.matmul(py[:nj, :], hid[:, fc, jt * 128:jt * 128 + nj], w2[:, fc, dh * 512:(dh + 1) * 512],
                                                      start=(fc == 0), stop=(fc == 11)), reads=[bh, bw2], writes=[bpy])
                    k.op("act" if dh == 0 else "dve",
                         (lambda e: e.activation(ye[:nj, dh * 512:(dh + 1) * 512], py[:nj, :], AF.Copy)) if dh == 0 else
                         (lambda e: e.tensor_copy(ye[:nj, dh * 512:(dh + 1) * 512], py[:nj, :])), reads=[bpy], writes=[bye])
                k.dma("sp", C.YE[ex, jt * 128:jt * 128 + nj, :], ye[:nj, :], reads=[bye], writes=[C.bYE])
    if "stop_moeB" in P.debug:
        return
    with P.scope():
        jcol = P.sb("jcol", [128, 2], F32)
        b_c = Buf("jcol")
        k.dma("sp", jcol[:], C.moe_jcol, writes=[b_c])
        yel = P.sb("yel", [128, NE, 2, D], BF16)
        yec = P.sb("yec", [32, NE, D], BF16)
        b_ye = Buf("yel")
        posb = P.sb("posb", [128, NE, 512], F32)
        mgb = P.sb("mgb", [128, NE, 512], F32)
        b_pb = Buf("posb")
        sgr = Ring(P, "selg", 4, [128, 512], BF16)
        x1r = Ring(P, "cx1", 2, [128, KC, 512], F32)
        pso = Ring(P, "cps", 8, [128, 512], F32, psum=True)
        for b in range(NB):
            for jt in range(2):
                k.dma("sp", yel[:, :, jt, :], C.YE[:, b * CAPL + jt * 128:b * CAPL + (jt + 1) * 128, :].rearrange("e p d -> p e d"),
                      reads=[C.bYE], writes=[b_ye])
            k.dma("sp", yec[:], C.YE[:, 2 * CAPL + b * CAPC:2 * CAPL + (b + 1) * CAPC, :].rearrange("e p d -> p e d"),
                  reads=[C.bYE], writes=[b_ye])
            for (t0, n) in [(0, TC)] + [(TC + i * 512, 512) for i in range(4)]:
                isctx = t0 < TC
                j = 2 if isctx else b
                k.dma("sp", posb[:, :, :n], C.POSD[b, :, t0:t0 + n].partition_broadcast(128), reads=[C.bPOSD], writes=[b_pb])
                k.dma("act", mgb[:, :, :n], C.MGD[b, :, t0:t0 + n].partition_broadcast(128), reads=[C.bPOSD], writes=[b_pb])
                x1, bx1 = x1r.next()
                k.dma("sp", x1[:, :, :n], XT[b][:, t0:t0 + n].rearrange("(kc p) n -> p kc n", p=128), reads=[C.bXT[b]], writes=[bx1])
                acc = [pso.next() for _ in range(KC)]
                njt = 1 if isctx else 2
                for ex in range(NE):
                    for jt in range(njt):
                        sg, bsg = sgr.next()
                        k.op("dve", lambda e: e.scalar_tensor_tensor(sg[:, :n], posb[:, ex, :n], jcol[:, jt:jt + 1], mgb[:, ex, :n],
                                                                     ALU.is_equal, ALU.mult), reads=[b_pb, b_c], writes=[bsg])
                        first = (ex == 0 and jt == 0)
                        lastm = (ex == NE - 1 and jt == njt - 1)
                        for dc in range(KC):
                            pa, bpa = acc[dc]
                            if isctx:
                                k.op("pe", lambda e: e.matmul(pa[:, :n], yec[:, ex, dc * 128:(dc + 1) * 128], sg[:32, :n],
                                                              start=first, stop=lastm), reads=[b_ye, bsg], writes=[bpa])
                            else:
                                k.op("pe", lambda e: e.matmul(pa[:, :n], yel[:, ex, jt, dc * 128:(dc + 1) * 128], sg[:, :n],
                                                              start=first, stop=lastm), reads=[b_ye, bsg], writes=[bpa])
                for dc in range(KC):
                    pa, bpa = acc[dc]
                    k.op("dve", lambda e: e.scalar_tensor_tensor(x1[:, dc, :n], pa[:, :n], mod[:, 5 * 8 + dc, j:j + 1], x1[:, dc, :n],
                                                                 ALU.mult, ALU.add), reads=[bpa, bx1, C.b_mod], writes=[bx1])
                if last:
                    if not isctx:
                        k.dma("sp", C.OUT[b][:, t0 - TC:t0 - TC + n].rearrange("(kc p) n -> p kc n", p=128), x1[:, :, :n],
                              reads=[bx1], writes=[C.bOUT])
                else:
                    k.dma("sp", XT[b][:, t0:t0 + n].rearrange("(kc p) n -> p kc n", p=128), x1[:, :, :n],
                          reads=[bx1], writes=[C.bXT[b]])


LAM = float(np.exp(-0.5))
GN_EPS = 64e-5
NCK = TT // 128


def phase_rwkv_prep(P, C, l):
    k = P.k
    PT = C.PT
    blocks = [(0, TC)] + [(TC + i * 512, 512) for i in range(4)]
    with P.scope():
        vec = P.sb("rwvec", [128, 51], F32)
        b_c = Buf("rwc")
        k.dma("sp", vec[:], C.rw_vec[l], writes=[b_c])
        MU, W0, A0, KK_, KA, RK = 0, 15, 23, 31, 35, 39
        der = P.sb("rwder", [128, 15 + 15 + 4], F32)
        k.op("dve", lambda e: e.tensor_scalar(der[:, 0:15], vec[:, MU:MU + 15], -1.0, 1.0, ALU.mult, ALU.add), reads=[b_c], writes=[b_c])
        k.op("dve", lambda e: e.tensor_scalar(der[:, 15:30], vec[:, MU:MU + 15], 0.5, None, ALU.mult), reads=[b_c], writes=[b_c])
        k.op("dve", lambda e: e.tensor_scalar(der[:, 30:34], vec[:, KA:KA + 4], -1.0, 1.0, ALU.mult, ALU.add), reads=[b_c], writes=[b_c])
        tiny = P.sb("rwtiny", [128, 1], F32)
        k.op("dve", lambda e: e.memset(tiny[:], 1e-12), writes=[b_c])
        w2 = P.sb("rw_w2", [128, 512], BF16)
        a2 = P.sb("rw_a2", [128, 512], BF16)
        g2 = P.sb("rw_g2", [128, 512], BF16)
        k.dma("pool", w2[:], C.rwkv_w2[l].rearrange("j l c -> (j l) c"), writes=[b_c])
        k.dma("pool", a2[:], C.rwkv_a2[l].rearrange("j l c -> (j l) c"), writes=[b_c])
        k.dma("pool", g2[:], C.rwkv_g2[l], writes=[b_c])
        bd = P.sb("rw_bd", [128, 128], BF16)
        k.dma("pool", bd[:], C.rw_bd, writes=[b_c])
        rst = P.sb("rw_rst", [128, TT + 1], F32)
        k.dma("sp", rst[:], C.rw_rst, writes=[b_c])
        big = lambda n_, dt=F32: P.sb(n_, [128, TT], dt)
        X = big("rX", BF16)
        S = big("rS")
        KP = [big(f"rKP{i}", BF16) for i in range(4)]
        KAP = [big(f"rKAP{i}", BF16) for i in range(4)]
        KS = big("rKSUM")
        bKS = Buf("KS")
        RP = [big(f"rRP{i}", BF16) for i in range(4)]
        VP = [big(f"rVP{i}", BF16) for i in range(4)]
        TW, PA, SG = big("rTW", BF16), big("rPA", BF16), big("rSG", BF16)
        T1, T2, T3, T4 = big("rT1"), big("rT2"), big("rT3"), big("rT4")
        O = [big(f"rO{i}", BF16) for i in range(4)]
        bX, bS, bT1, bT2, bT3, bT4 = [Buf(x) for x in "X S T1 T2 T3 T4".split()]
        bKP = [Buf(f"KP{i}") for i in range(4)]
        bKAP = [Buf(f"KAP{i}") for i in range(4)]
        bRP = [Buf(f"RP{i}") for i in range(4)]
        bVP = [Buf(f"VP{i}") for i in range(4)]
        bTW, bPA, bSG = Buf("TW"), Buf("PA"), Buf("SG")
        bO = [Buf(f"O{i}") for i in range(4)]
        psr = Ring(P, "rwps", 4, [128, 512], F32, psum=True)
        stg = Ring(P, "rwstg", 3, [128, 512], BF16)
        plr = Ring(P, "rwpl", 2, [128, NCK], F32)
        for b in range(NB):
            for ci in range(15):
                k.dma("sp", X[:], PT[b, (4 + ci) * 128:(5 + ci) * 128, :], reads=[C.bPT], writes=[bX])
                k.op("pool", lambda e: e.tensor_tensor(S[:, 1:TT - 1], X[:, 0:TT - 2], X[:, 2:TT], ALU.add), reads=[bX], writes=[bS])
                for (d_, s_) in ((0, 1), (TC - 1, TC - 2), (TC, TC + 1), (TT - 1, TT - 2)):
                    k.op("pool", lambda e: e.tensor_copy(S[:, d_:d_ + 1], X[:, s_:s_ + 1]), reads=[bX, bS], writes=[bS])
                k.op("dve", lambda e: e.tensor_scalar(T1[:], X[:], der[:, ci:ci + 1], None, ALU.mult), reads=[bX, b_c], writes=[bT1])
                if ci < 4:
                    dst, bdst = RP[ci], bRP[ci]
                elif ci < 8:
                    dst, bdst = KP[ci - 4], bKP[ci - 4]
                elif ci < 12:
                    dst, bdst = VP[ci - 8], bVP[ci - 8]
                else:
                    dst, bdst = T2, bT2
                k.op("dve", lambda e: e.scalar_tensor_tensor(dst[:], S[:], der[:, 15 + ci:16 + ci], T1[:], ALU.mult, ALU.add),
                     reads=[bS, bT1, b_c], writes=[bdst])
                if ci == 12:
                    k.op("act", lambda e: e.activation(TW[:], T2[:], AF.Tanh), reads=[bT2], writes=[bTW])
                elif ci == 13:
                    k.op("act", lambda e: e.activation(PA[:], T2[:], AF.Copy), reads=[bT2], writes=[bPA])
                elif ci == 14:
                    k.op("act", lambda e: e.activation(SG[:], T2[:], AF.Sigmoid), reads=[bT2], writes=[bSG])
            for cc in range(4):
                k.dma("sp", C.RWV[b, cc * 128:(cc + 1) * 128, :], VP[cc][:], reads=[bVP[cc]], writes=[C.bRW])
            for cc in range(4):
                for (t0, n) in blocks:
                    ps, bps = psr.next()
                    k.op("pe", lambda e: e.matmul(ps[:, :n], g2[:, cc * 128:(cc + 1) * 128], SG[:, t0:t0 + n], start=True, stop=True),
                         reads=[bSG, b_c], writes=[bps])
                    st, bst = stg.next()
                    k.op("act", lambda e: e.activation(st[:, :n], ps[:, :n], AF.Copy), reads=[bps], writes=[bst])
                    k.dma("sp", C.RWG[b, cc * 128:(cc + 1) * 128, t0:t0 + n], st[:, :n], reads=[bst], writes=[C.bRW])
            for cc in range(4):
                k.op("dve", lambda e: e.tensor_scalar(T1[:], KP[cc][:], vec[:, KK_ + cc:KK_ + cc + 1], None, ALU.mult),
                     reads=[bKP[cc], b_c, bT1], writes=[bT1])
                k.op("act", lambda e: e.activation(O[0][:], T1[:], AF.Square), reads=[bT1, bO[0]], writes=[bO[0]])
                for (t0, n) in blocks:
                    ps, bps = psr.next()
                    k.op("pe", lambda e: e.matmul(ps[:, :n], bd[:], O[0][:, t0:t0 + n], start=True, stop=True),
                         reads=[bO[0], b_c], writes=[bps])
                    k.op("act", lambda e: e.activation(T2[:, t0:t0 + n], ps[:, :n], AF.Sqrt, bias=tiny[:, 0:1]), reads=[bps, bT2, b_c], writes=[bT2])
                k.op("dve", lambda e: e.reciprocal(T2[:], T2[:]), reads=[bT2], writes=[bT2])
                k.op("dve", lambda e: e.tensor_tensor(KAP[cc][:], T1[:], T2[:], ALU.mult), reads=[bT1, bT2], writes=[bKAP[cc]])
            for cc in range(4):
                for j in range(2):
                    jr = slice(j * 64, (j + 1) * 64)
                    for (t0, n) in blocks:
                        ps, bps = psr.next()
                        k.op("pe", lambda e: e.matmul(ps[:, :n], w2[jr, cc * 128:(cc + 1) * 128], TW[jr, t0:t0 + n], start=True, stop=True),
                             reads=[bTW, b_c], writes=[bps])
                        k.op("act", lambda e: e.activation(T1[:, t0:t0 + n], ps[:, :n], AF.Sigmoid, bias=vec[:, W0 + j * 4 + cc:W0 + j * 4 + cc + 1]),
                             reads=[bps, b_c, bT1], writes=[bT1])
                        ps2, bps2 = psr.next()
                        k.op("pe", lambda e: e.matmul(ps2[:, :n], a2[jr, cc * 128:(cc + 1) * 128], PA[jr, t0:t0 + n], start=True, stop=True),
                             reads=[bPA, b_c], writes=[bps2])
                        k.op("act", lambda e: e.activation(T2[:, t0:t0 + n], ps2[:, :n], AF.Sigmoid, bias=vec[:, A0 + j * 4 + cc:A0 + j * 4 + cc + 1]),
                             reads=[bps2, b_c, bT2], writes=[bT2])
                    if j == 0:
                        k.op("dve", lambda e: e.tensor_tensor_scan(T3[:], rst[:, 0:TT], T1[:], 0.0, ALU.mult, ALU.add),
                             reads=[bT1, b_c, bT3], writes=[bT3])
                    else:
                        k.op("dve", lambda e: e.tensor_tensor_scan(rev_ap(T3[:], 0, TT), rev_ap(rst[:], 1, TT + 1), rev_ap(T1[:], 0, TT),
                                                                   0.0, ALU.mult, ALU.add), reads=[bT1, b_c, bT3], writes=[bT3])
                    k.op("dve", lambda e: e.tensor_scalar(T4[:], T2[:], vec[:, KA + cc:KA + cc + 1], der[:, 30 + cc:31 + cc], ALU.mult, ALU.add),
                         reads=[bT2, b_c, bT4], writes=[bT4])
                    k.op("pool", lambda e: e.tensor_tensor(T4[:], T4[:], KP[cc][:], ALU.mult), reads=[bT4, bKP[cc]], writes=[bT4])
                    k.op("pool", lambda e: e.tensor_tensor(T2[:], T2[:], KAP[cc][:], ALU.mult), reads=[bT2, bKAP[cc]], writes=[bT2])
                    k.op("pool", lambda e: e.tensor_tensor(T1[:], T3[:], T1[:], ALU.subtract), reads=[bT1, bT3], writes=[bT1])
                    k.op("act", lambda e: e.activation(T1[:], T1[:], AF.Exp, scale=-LAM), reads=[bT1], writes=[bT1])
                    k.op("act", lambda e: e.activation(S[:], T3[:], AF.Exp, scale=LAM), reads=[bT3, bS], writes=[bS])
                    k.op("act", lambda e: e.activation(T3[:], T3[:], AF.Exp, scale=-LAM), reads=[bT3], writes=[bT3])
                    pl, bpl = plr.next()
                    off = 127 if j == 0 else 0
                    k.op("dve", lambda e: e.tensor_copy(pl[:], T3[:, off:TT:128]), reads=[bT3], writes=[bpl])
                    k.dma("sp", C.RWPL[b, j, cc * 128:(cc + 1) * 128, :], pl[:], reads=[bpl], writes=[C.bRW])
                    k.op("dve", lambda e: e.tensor_tensor(O[0][:], RP[cc][:], T3[:], ALU.mult), reads=[bRP[cc], bT3, bO[0]], writes=[bO[0]])
                    k.op("pool", lambda e: e.tensor_tensor(O[1][:], KAP[cc][:], T1[:], ALU.mult), reads=[bKAP[cc], bT1, bO[1]], writes=[bO[1]])
                    k.op("dve", lambda e: e.tensor_tensor(O[2][:], T4[:], S[:], ALU.mult), reads=[bT4, bS, bO[2]], writes=[bO[2]])
                    k.op("pool", lambda e: e.tensor_tensor(O[3][:], T2[:], S[:], ALU.mult), reads=[bT2, bS, bO[3]], writes=[bO[3]])
                    for q in range(4):
                        k.dma("sp" if q % 2 == 0 else "act", C.RWT[b, j, q, cc * 128:(cc + 1) * 128, :], O[q][:], reads=[bO[q]], writes=[C.bRW])
                    if j == 0:
                        k.op("dve", lambda e: e.tensor_copy(KS[:], T4[:]), reads=[bT4, bKS], writes=[bKS])
                    else:
                        k.op("dve", lambda e: e.tensor_tensor(KS[:], KS[:], T4[:], ALU.add), reads=[bT4, bKS], writes=[bKS])
                k.op("dve", lambda e: e.scalar_tensor_tensor(KS[:], KS[:], vec[:, RK + cc:RK + cc + 1], RP[cc][:], ALU.mult, ALU.mult),
                     reads=[bKS, bRP[cc], b_c], writes=[bKS])
                k.op("act", lambda e: e.activation(O[0][:], KS[:], AF.Copy), reads=[bKS, bO[0]], writes=[bO[0]])
                for (t0, n) in blocks:
                    ps, bps = psr.next()
                    k.op("pe", lambda e: e.matmul(ps[:, :n], bd[:], O[0][:, t0:t0 + n], start=True, stop=True), reads=[bO[0], b_c], writes=[bps])
                    k.op("dve", lambda e: e.tensor_tensor(T1[:, t0:t0 + n], ps[:, :n], VP[cc][:, t0:t0 + n], ALU.mult),
                         reads=[bps, bVP[cc], bT1], writes=[bT1])
                k.dma("sp", C.RWB[b, cc * 128:(cc + 1) * 128, :], T1[:], reads=[bT1], writes=[C.bRW])


def run_interleaved(gens):
    gens = list(gens)
    while gens:
        for g_ in list(gens):
            try:
                next(g_)
            except StopIteration:
                gens.remove(g_)


def phase_rwkv_scan(P, C, l):
    k = P.k
    YT = C.YT
    with P.scope():
        masks = P.sb("rwmask", [128, 2, 640], BF16)
        b_c = Buf("rwc2")
        k.dma("pool", masks[:], C.rw_masks, writes=[b_c])
        ident = P.sb("rwident", [128, 128], BF16)
        k.dma("pool", ident[:], C.ident, writes=[b_c])
        identf = P.sb("rwidentf", [128, 128], F32)
        k.dma("sp", identf[:], C.ident, writes=[b_c])
        bdm = P.sb("rwbdm", [128, 128], F32)
        k.dma("sp", bdm[:], C.rw_bd, writes=[b_c])
        vec = P.sb("rwvec2", [128, 51], F32)
        k.dma("sp", vec[:], C.rw_vec[l], writes=[b_c])
        lmf = P.sb("rwlm", [128, 4, 128], F32)
        k.dma("sp", lmf[:], C.rw_lm, writes=[b_c])
        gne = P.sb("gne", [128, 1], F32)
        k.op("dve", lambda e: e.memset(gne[:], GN_EPS), writes=[b_c])
        Ytok = P.sb("Ytok", [128, NCK, 512], F32)
        for b in range(NB):
            bY = [[Buf(f"Y{c}_{hp}") for hp in range(4)] for c in range(NCK)]
            with P.scope():
                KR = P.sb("KR", [128, NCK, 2, 128], BF16)
                KF = P.sb("KF", [128, TT], BF16)
                BF_ = P.sb("BF", [128, TT], BF16)
                VF = P.sb("VF", [128, TT], BF16)
                PLt = P.sb("PLt", [128, NCK], F32)
                TOK = P.sb("TOK", [128, NCK, 3, 128], BF16)
                SC = P.sb("SC", [128, NCK, 2, 512], BF16)
                Tt = P.sb("Tt", [128, NCK * 2, 128], BF16)
                SC36 = SC[:].rearrange("p c h x -> p (c h) x")
                NSLOT = 2
                NFs = [Ring(P, f"NF{i}", 1, [128, 4, 2, 128], F32) for i in range(NSLOT)]
                F4s = [Ring(P, f"F4{i}", 4, [128, 4, 128], F32) for i in range(NSLOT)]
                FTs = [Ring(P, f"FT{i}", 2, [128, 4, 128], F32) for i in range(NSLOT)]
                B4s = [Ring(P, f"B4{i}", 8, [128, 4, 128], BF16) for i in range(NSLOT)]
                H = P.sb("Hst", [128, 128], F32)
                Ht = P.sb("Htmp", [128, 128], F32)
                Hb = P.sb("Hb", [128, 128], BF16)
                Wr = Ring(P, "Wsb", 2, [128, 128], BF16)
                Ur = Ring(P, "Un", 2, [128, 128], BF16)
                psT = P.ps("pstr", [128, 3, 128], BF16)
                bpsT = Buf("pstr")
                psL = Ring(P, "psL", 5, [128, 512], F32, psum=True)
                psS = Ring(P, "psS", 2, [128, 128], F32, psum=True)
                b_in, b_tok, b_sc, b_tt, b_H = Buf("in"), Buf("tok"), Buf("sc"), Buf("tt"), Buf("H")
                for j in ([0] if "rw_j0" in P.debug else [1] if "rw_j1" in P.debug else [0, 1]):
                    for hp in range(4):
                        rows = slice(hp * 128, (hp + 1) * 128)
                        k.dma("sp", KR[:, :, 0, :], C.RWT[b, j, 1, rows, :].rearrange("p (c t) -> p c t", t=128), reads=[C.bRW], writes=[b_in])
                        k.dma("act", KR[:, :, 1, :], C.RWT[b, j, 0, rows, :].rearrange("p (c t) -> p c t", t=128), reads=[C.bRW], writes=[b_in])
                        k.dma("sp", KF[:], C.RWT[b, j, 2, rows, :], reads=[C.bRW], writes=[b_in])
                        k.dma("act", BF_[:], C.RWT[b, j, 3, rows, :], reads=[C.bRW], writes=[b_in])
                        k.dma("sp", VF[:], C.RWV[b, rows, :], reads=[C.bRW], writes=[b_in])
                        k.dma("sp", PLt[:], C.RWPL[b, j, rows, :], reads=[C.bRW], writes=[b_in])
                        for c in range(NCK):
                            cs = slice(c * 128, (c + 1) * 128)
                            for q, src in enumerate((KF, BF_, VF)):
                                k.op("pe", lambda e: e.transpose(psT[:, q, :], src[:, cs], ident[:]), reads=[b_in, b_c], writes=[bpsT])
                            k.op("act", lambda e: e.activation(TOK[:, c], psT[:], AF.Copy), reads=[bpsT], writes=[b_tok])
                        def inv_group(g, slot, j=j):
                            NFr, F4, B4, FT = NFs[slot], F4s[slot], B4s[slot], FTs[slot]
                            NF, bNF = NFr.next()
                            for i in range(4):
                                c, h = 2 * g + i // 2, i % 2
                                cs = slice(c * 128, (c + 1) * 128)
                                hr = slice(h * 64, (h + 1) * 64)
                                kr2 = KR[hr, c].rearrange("p x t -> p (x t)")
                                pX, bpX = psL.next()
                                k.op("pe", lambda e: e.matmul(pX[:, 0:256], KF[hr, cs], kr2, start=True, stop=True), reads=[b_in], writes=[bpX])
                                k.op("pe", lambda e: e.matmul(pX[:, 256:512], BF_[hr, cs], kr2, start=True, stop=True), reads=[b_in], writes=[bpX])
                                pY, bpY = psS.next()
                                k.op("pe", lambda e: e.matmul(pY[:], KR[hr, c, 0, :], BF_[hr, cs], start=True, stop=True), reads=[b_in], writes=[bpY])
                                k.op("dve", lambda e: e.tensor_tensor(SC[:, c, h, :], pX[:], masks[:, j, 0:512], ALU.mult), reads=[bpX, b_c], writes=[b_sc])
                                k.op("dve", lambda e: e.tensor_tensor(NF[:, i, 1, :], pX[:, 256:384], masks[:, j, 256:384], ALU.mult), reads=[bpX, b_c], writes=[bNF])
                                k.op("dve", lambda e: e.tensor_tensor(NF[:, i, 0, :], pY[:], masks[:, j, 512:640], ALU.mult), reads=[bpY, b_c], writes=[bNF])
                            yield
                            bl = slice(g * 4, (g + 1) * 4)
                            bc4 = lambda m_: m_.unsqueeze(1).to_broadcast([128, 4, 128])
                            Mk, bMk = F4.next()
                            Mtk, bMtk = F4.next()
                            Tf, bTf = FT.next()
                            Ttf, bTtf = FT.next()
                            k.op("pool", lambda e: e.tensor_tensor(Mk[:], NF[:, :, 0, :], bc4(lmf[:, 0, :]), ALU.mult), reads=[bNF, b_c], writes=[bMk])
                            k.op("pool", lambda e: e.tensor_tensor(Mtk[:], NF[:, :, 1, :], bc4(lmf[:, 0, :]), ALU.mult), reads=[bNF, b_c], writes=[bMtk])
                            k.op("pool", lambda e: e.tensor_tensor(Tf[:], Mk[:], bc4(identf[:]), ALU.add), reads=[bMk, b_c], writes=[bTf])
                            k.op("pool", lambda e: e.tensor_tensor(Ttf[:], Mtk[:], bc4(identf[:]), ALU.add), reads=[bMtk, b_c], writes=[bTtf])
                            yield
                            for lev in range(1, 4):
                                M2, bM2 = F4.next()
                                Mt2, bMt2 = F4.next()
                                p1, bp1 = psL.next()
                                p2, bp2 = psL.next()
                                for i in range(4):
                                    k.op("pe", lambda e: e.matmul(p1[:, i * 128:(i + 1) * 128], Mk[:, i, :], Mtk[:, i, :], start=True, stop=True),
                                         reads=[bMk, bMtk], writes=[bp1])
                                    k.op("pe", lambda e: e.matmul(p2[:, i * 128:(i + 1) * 128], Mtk[:, i, :], Mk[:, i, :], start=True, stop=True),
                                         reads=[bMk, bMtk], writes=[bp2])
                                yield
                                k.op("act", lambda e: e.activation(Mt2[:].rearrange("p a b -> p (a b)"), p1[:], AF.Copy), reads=[bp1], writes=[bMt2])
                                k.op("dve", lambda e: e.tensor_copy(M2[:].rearrange("p a b -> p (a b)"), p2[:]), reads=[bp2], writes=[bM2])
                                yield
                                p3, bp3 = psL.next()
                                p4, bp4 = psL.next()
                                for i in range(4):
                                    k.op("pe", lambda e: e.matmul(p3[:, i * 128:(i + 1) * 128], Mt2[:, i, :], Tf[:, i, :], start=True, stop=True),
                                         reads=[bMt2, bTf], writes=[bp3])
                                    k.op("pe", lambda e: e.matmul(p4[:, i * 128:(i + 1) * 128], M2[:, i, :], Ttf[:, i, :], start=True, stop=True),
                                         reads=[bM2, bTtf], writes=[bp4])
                                yield
                                k.op("dve", lambda e: e.tensor_tensor(Tf[:].rearrange("p a b -> p (a b)"), Tf[:].rearrange("p a b -> p (a b)"), p3[:], ALU.add),
                                     reads=[bp3, bTf], writes=[bTf])
                                k.op("dve", lambda e: e.tensor_tensor(Ttf[:].rearrange("p a b -> p (a b)"), Ttf[:].rearrange("p a b -> p (a b)"), p4[:], ALU.add),
                                     reads=[bp4, bTtf], writes=[bTtf])
                                Mk, bMk, Mtk, bMtk = M2, bM2, Mt2, bMt2
                                yield
                            Tb, bTb = B4.next()
                            Ttb, bTtb = B4.next()
                            k.op("act", lambda e: e.activation(Tb[:], Tf[:], AF.Copy), reads=[bTf], writes=[bTb])
                            k.op("act", lambda e: e.activation(Ttb[:], Ttf[:], AF.Copy), reads=[bTtf], writes=[bTtb])
                            for li in range(1, 4):
                                lastl = (li == 3)
                                Cm, bCm = B4.next()
                                k.op("pool", lambda e: e.tensor_tensor(Cm[:], NF[:, :, 0, :], bc4(lmf[:, li, :]), ALU.mult), reads=[bNF, b_c], writes=[bCm])
                                p2, bp2 = psL.next()
                                for i in range(4):
                                    k.op("pe", lambda e: e.matmul(p2[:, i * 128:(i + 1) * 128], Cm[:, i, :], Ttb[:, i, :], start=True, stop=True),
                                         reads=[bCm, bTtb], writes=[bp2])
                                yield
                                Z2, bZ2 = B4.next()
                                k.op("act", lambda e: e.activation(Z2[:].rearrange("p a b -> p (a b)"), p2[:], AF.Copy), reads=[bp2], writes=[bZ2])
                                if not lastl:
                                    Cmt, bCmt = B4.next()
                                    k.op("pool", lambda e: e.tensor_tensor(Cmt[:], NF[:, :, 1, :], bc4(lmf[:, li, :]), ALU.mult), reads=[bNF, b_c], writes=[bCmt])
                                    p1, bp1 = psL.next()
                                    for i in range(4):
                                        k.op("pe", lambda e: e.matmul(p1[:, i * 128:(i + 1) * 128], Cmt[:, i, :], Tb[:, i, :], start=True, stop=True),
                                             reads=[bCmt, bTb], writes=[bp1])
                                    Z1, bZ1 = B4.next()
                                    k.op("dve", lambda e: e.tensor_copy(Z1[:].rearrange("p a b -> p (a b)"), p1[:]), reads=[bp1], writes=[bZ1])
                                yield
                                p4, bp4 = psL.next()
                                for i in range(4):
                                    k.op("pe", lambda e: e.matmul(p4[:, i * 128:(i + 1) * 128], Tb[:, i, :], Z2[:, i, :], start=True, stop=True),
                                         reads=[bTb, bZ2], writes=[bp4])
                                if not lastl:
                                    p3, bp3 = psL.next()
                                    for i in range(4):
                                        k.op("pe", lambda e: e.matmul(p3[:, i * 128:(i + 1) * 128], Ttb[:, i, :], Z1[:, i, :], start=True, stop=True),
                                             reads=[bTtb, bZ1], writes=[bp3])
                                    yield
                                    Tn, bTn = B4.next()
                                    Ttn, bTtn = B4.next()
                                    k.op("dve", lambda e: e.tensor_tensor(Tn[:].rearrange("p a b -> p (a b)"), Tb[:].rearrange("p a b -> p (a b)"), p3[:], ALU.add),
                                         reads=[bp3, bTb], writes=[bTn])
                                    k.op("dve", lambda e: e.tensor_tensor(Ttn[:].rearrange("p a b -> p (a b)"), Ttb[:].rearrange("p a b -> p (a b)"), p4[:], ALU.add),
                                         reads=[bp4, bTtb], writes=[bTtn])
                                    Tb, bTb, Ttb, bTtb = Tn, bTn, Ttn, bTtn
                                else:
                                    yield
                                    k.op("dve", lambda e: e.tensor_tensor(Tt[:, bl, :], Ttb[:], p4[:].rearrange("p (a b) -> p a b", b=128), ALU.add),
                                         reads=[bp4, bTtb, b_tt], writes=[b_tt])
                        for w0 in range(0, NCK // 2, NSLOT):
                            run_interleaved([inv_group(g, i) for i, g in enumerate(range(w0, min(w0 + NSLOT, NCK // 2)))])
                        k.op("pool", lambda e: e.memset(H[:], 0.0), reads=[b_H], writes=[b_H])
                        k.op("pool", lambda e: e.memset(Hb[:], 0.0), reads=[b_H], writes=[b_H])
                        order = list(range(NCK)) if j == 0 else [1, 0] + list(range(NCK - 1, 1, -1))
                        for c in order:
                            pW, bpW = psS.next()
                            k.op("pe", lambda e: e.matmul(pW[:], KR[:, c, 0, :], Hb[:], start=True, stop=False, skip_group_check=True),
                                 reads=[b_in, b_H], writes=[bpW])
                            for h in range(2):
                                hc = slice(h * 64, (h + 1) * 64)
                                k.op("pe", lambda e: e.matmul(pW[:, hc], SC[:, c, h, 0:128], TOK[:, c, 2, hc], start=False, stop=True, skip_group_check=True),
                                     reads=[b_sc, b_tok], writes=[bpW])
                            Wsb, bW = Wr.next()
                            k.op("act", lambda e: e.activation(Wsb[:], pW[:], AF.Copy), reads=[bpW], writes=[bW])
                            pU, bpU = psS.next()
                            for h in range(2):
                                hc = slice(h * 64, (h + 1) * 64)
                                k.op("pe", lambda e: e.matmul(pU[:, hc], Tt[:, c * 2 + h, :], Wsb[:, hc], start=True, stop=True),
                                     reads=[b_tt, bW], writes=[bpU])
                            Un, bUn = Ur.next()
                            k.op("act", lambda e: e.activation(Un[:], pU[:], AF.Copy, scale=-1.0), reads=[bpU], writes=[bUn])
                            pYy, bpYy = psS.next()
                            k.op("pe", lambda e: e.matmul(pYy[:], KR[:, c, 1, :], Hb[:], start=True, stop=False, skip_group_check=True),
                                 reads=[b_in, b_H], writes=[bpYy])
                            for h in range(2):
                                hc = slice(h * 64, (h + 1) * 64)
                                k.op("pe", lambda e: e.matmul(pYy[:, hc], SC[:, c, h, 128:256], TOK[:, c, 2, hc], start=False, stop=False, skip_group_check=True),
                                     reads=[b_sc, b_tok], writes=[bpYy])
                                k.op("pe", lambda e: e.matmul(pYy[:, hc], SC[:, c, h, 384:512], Un[:, hc], start=False, stop=True, skip_group_check=True),
                                     reads=[b_sc, bUn], writes=[bpYy])
                            ysl = Ytok[:, c, hp * 128:(hp + 1) * 128]
                            if j == 0 or "rw_j1" in P.debug:
                                k.op("act", lambda e: e.activation(ysl, pYy[:], AF.Copy), reads=[bpYy], writes=[bY[c][hp]])
                            else:
                                k.op("dve", lambda e: e.tensor_tensor(ysl, ysl, pYy[:], ALU.add), reads=[bpYy, bY[c][hp]], writes=[bY[c][hp]])
                            pH, bpH = psS.next()
                            k.op("pe", lambda e: e.matmul(pH[:], TOK[:, c, 0, :], TOK[:, c, 2, :], start=True, stop=False), reads=[b_tok], writes=[bpH])
                            k.op("pe", lambda e: e.matmul(pH[:], TOK[:, c, 1, :], Un[:], start=False, stop=True), reads=[b_tok, bUn], writes=[bpH])
                            k.op("dve", lambda e: e.tensor_tensor(Ht[:], H[:], pH[:], ALU.add), reads=[bpH, b_H], writes=[b_H])
                            k.op("dve", lambda e: e.scalar_tensor_tensor(H[:], Ht[:], PLt[:, c:c + 1], bdm[:], ALU.mult, ALU.mult),
                                 reads=[b_H, b_in, b_c], writes=[b_H])
                            k.op("act", lambda e: e.activation(Hb[:], H[:], AF.Copy), reads=[b_H], writes=[b_H])
            if "YTOK" in P.debug:
                k.dma("sp", C.YTOK[b], Ytok[:], reads=[x for row in bY for x in row], writes=[C.bRW])
            with P.scope():
                BON = P.sb("BON", [128, 4, TT], F32)
                G = P.sb("G", [128, 4, TT], BF16)
                b_l = Buf("rdl")
                k.dma("sp", BON[:], C.RWB[b].rearrange("(c p) t -> p c t", p=128), reads=[C.bRW], writes=[b_l])
                k.dma("act", G[:], C.RWG[b].rearrange("(c p) t -> p c t", p=128), reads=[C.bRW], writes=[b_l])
                st8 = Ring(P, "st8", 2, [128, 6, 8], F32)
                ysq = Ring(P, "ysq", 2, [128, 512], F32)
                ynr = Ring(P, "yn", 2, [128, 512], F32)
                psR = Ring(P, "psR", 2, [128, 4, 128], F32, psum=True)
                ofr = Ring(P, "of", 3, [128, 128], F32)
                obr = Ring(P, "ob", 3, [128, 128], BF16)
                for c in range(NCK):
                    allY = bY[c]
                    y = Ytok[:, c, :]
                    y3 = y.rearrange("p (h x) -> p h x", x=64)
                    s, bs = st8.next()
                    k.op("dve", lambda e: e.reduce_sum(s[:, 0, :], y3, AX.X), reads=allY, writes=[bs])
                    q, bq = ysq.next()
                    k.op("pool", lambda e: e.tensor_tensor(q[:], y, y, ALU.mult), reads=allY, writes=[bq])
                    k.op("dve", lambda e: e.reduce_sum(s[:, 1, :], q[:].rearrange("p (h x) -> p h x", x=64), AX.X), reads=[bq, bs], writes=[bs])
                    k.op("dve", lambda e: e.tensor_scalar(s[:, 2, :], s[:, 0, :], 1.0 / 64.0, None, ALU.mult), reads=[bs], writes=[bs])
                    k.op("dve", lambda e: e.tensor_tensor(s[:, 3, :], s[:, 2, :], s[:, 2, :], ALU.mult), reads=[bs], writes=[bs])
                    k.op("dve", lambda e: e.scalar_tensor_tensor(s[:, 4, :], s[:, 1, :], 1.0 / 64.0, s[:, 3, :], ALU.mult, ALU.subtract),
                         reads=[bs], writes=[bs])
                    k.op("act", lambda e: e.activation(s[:, 5, :], s[:, 4, :], AF.Sqrt, bias=gne[:, 0:1]), reads=[bs, b_c], writes=[bs])
                    k.op("dve", lambda e: e.reciprocal(s[:, 5, :], s[:, 5, :]), reads=[bs], writes=[bs])
                    yn, byn = ynr.next()
                    yn3 = yn[:].rearrange("p (h x) -> p h x", x=64)
                    k.op("dve", lambda e: e.tensor_tensor(yn3, y3, s[:, 2, :].unsqueeze(2).to_broadcast([128, 8, 64]), ALU.subtract),
                         reads=allY + [bs], writes=[byn])
                    k.op("pool", lambda e: e.tensor_tensor(yn3, yn3, s[:, 5, :].unsqueeze(2).to_broadcast([128, 8, 64]), ALU.mult),
                         reads=[byn, bs], writes=[byn])
                    pr, bpr = psR.next()
                    for hp in range(4):
                        k.op("pe", lambda e: e.transpose(pr[:, hp, :], yn[:, hp * 128:(hp + 1) * 128], identf[:]), reads=[byn, b_c], writes=[bpr])
                    cs = slice(c * 128, (c + 1) * 128)
                    for hp in range(4):
                        of, bof = ofr.next()
                        k.op("act", lambda e: e.activation(of[:], pr[:, hp, :], AF.Identity, bias=vec[:, 47 + hp:48 + hp], scale=vec[:, 43 + hp:44 + hp]),
                             reads=[bpr, b_c], writes=[bof])
                        k.op("dve", lambda e: e.tensor_tensor(of[:], of[:], BON[:, hp, cs], ALU.add), reads=[bof, b_l], writes=[bof])
                        ob, bob = obr.next()
                        k.op("pool", lambda e: e.tensor_tensor(ob[:], of[:], G[:, hp, cs], ALU.mult), reads=[bof, b_l], writes=[bob])
                        k.dma("sp", YT[b, 1, hp * 128:(hp + 1) * 128, cs], ob[:], reads=[bob], writes=[C.bYT])


def build(n_layers=DEPTH, debug=(), stop_after=None):
    P = Prog(n_layers, debug)
    nc, k = P.nc, P.k
    xt0 = P.din("xt0", [NB, D, TT])
    cT = P.din("cT", [128, KC, 3])
    cst_ones = P.din("ones", [128, 128])
    ada_w = P.din("ada_w", [DEPTH, D, 6 * D])
    ada_b = P.din("ada_b_r", [DEPTH, 128, 48])
    ng_r = P.din("ng_r", [DEPTH, 128, 2, KC])
    w_in = P.din("w_in", [DEPTH, D, N_IN])
    XT = P.dscr("XT", [NB, D, TT], F32)
    PT = P.dscr("PT", [NB, NCH * 128, TT], BF16)
    bXT = [Buf("XT0"), Buf("XT1")]
    bPT = Buf("PT")
    if "yt_in" in P.debug:
        YT = P.din("YT3", [NB, 3, 512, TT], BF16)
        P.dbufs["YT3"] = Buf("YT3")
    else:
        YT = P.dscr("YT3", [NB, 3, 512, TT], BF16)
    C = type("Ctx", (), {})()
    C.XT, C.PT, C.YT, C.bXT, C.bPT, C.bYT = XT, PT, YT, bXT, bPT, Buf("YT3")
    C.xt0 = xt0
    declare_inputs(P, C)

    with P.scope():
        ones = P.sb("ones", [128, 128], F32)
        b_ones = Buf("ones")
        k.dma("sp", ones[:], cst_ones, writes=[b_ones])
        silu_c = P.sb("silu_c", [128, KC, 3], F32)
        b_silu = Buf("silu_c")
        k.dma("sp", silu_c[:], cT, writes=[b_silu])
        k.op("act", lambda e: e.activation(silu_c[:], silu_c[:], AF.Silu), reads=[b_silu], writes=[b_silu])
        C.epsc = P.sb("epsc_g", [128, 1], F32)
        k.op("dve", lambda e: e.memset(C.epsc[:], NORM_EPS), writes=[b_ones])
        mod = P.sb("mod", [128, 48, 3], F32)
        b_mod = Buf("mod")
        gs = P.sb("gs", [128, 2, KC, 3], F32)
        b_gs = Buf("gs")

        for l in range(n_layers):
            src_x = xt0 if l == 0 else XT
            with P.scope():
                adab = P.sb("adab", [128, 48], F32)
                b_adab = Buf("adab")
                k.dma("sp", adab[:], ada_b[l], writes=[b_adab])
                ng = P.sb("ng", [128, 2, KC], F32)
                b_ng = Buf("ng")
                k.dma("sp", ng[:], ng_r[l], writes=[b_ng])
                wr = Ring(P, "adaw", 2, [128, KC, 512], F32)
                pr = Ring(P, "modps", 2, [128, 4, 3], F32, psum=True)
                for g in range(12):
                    wt, bw = wr.next()
                    k.dma("sp" if g % 2 == 0 else "act", wt[:],
                          ada_w[l][:, g * 512:(g + 1) * 512].rearrange("(kc p) n -> p kc n", p=128), writes=[bw])
                    pt, bp = pr.next()
                    for c4 in range(4):
                        for kc in range(KC):
                            k.op("pe", lambda e, c4=c4, kc=kc: e.matmul(
                                pt[:, c4, :], wt[:, kc, c4 * 128:(c4 + 1) * 128], silu_c[:, kc, :],
                                start=(kc == 0), stop=(kc == KC - 1)),
                                reads=[bw, b_silu], writes=[bp])
                    k.op("dve", lambda e: e.tensor_tensor(
                        mod[:, g * 4:(g + 1) * 4, :], pt[:],
                        adab[:, g * 4:(g + 1) * 4].unsqueeze(2).to_broadcast([128, 4, 3]), ALU.add),
                        reads=[bp, b_adab], writes=[b_mod])
                for n, mi in ((0, 1), (1, 4)):
                    k.op("dve", lambda e, n=n, mi=mi: e.tensor_scalar(
                        gs[:, n, :, :], mod[:, mi * 8:(mi + 1) * 8, :], 1.0, None, ALU.add),
                        reads=[b_mod], writes=[b_gs])
                    k.op("dve", lambda e, n=n: e.tensor_tensor(
                        gs[:, n, :, :], gs[:, n, :, :],
                        ng[:, n, :].unsqueeze(2).to_broadcast([128, KC, 3]), ALU.mult),
                        reads=[b_gs, b_ng], writes=[b_gs])
            if stop_after == "mod":
                break

            with P.scope():
                hT = P.sb("hT", [128, NB, KC, TT], BF16)
                b_h = [[Buf(f"h{b}_{i}") for i in range(5)] for b in range(NB)]
                blocks = [(0, TC)] + [(TC + i * 512, 512) for i in range(4)]
                xr = Ring(P, "xin", 2, [128, KC, 512], F32)
                sqr = Ring(P, "sq", 1, [128, KC, 512], F32)
                ssr = Ring(P, "ssps", 2, [128, 512], F32, psum=True)
                rsr = Ring(P, "rstd", 2, [128, 512], F32)
                for b in range(NB):
                    for bi, (t0, n) in enumerate(blocks):
                        j = 2 if bi == 0 else b
                        xt, bx = xr.next()
                        k.dma("sp", xt[:, :, :n], src_x[b][:, t0:t0 + n].rearrange("(kc p) n -> p kc n", p=128),
                              reads=[bXT[b]], writes=[bx])
                        sq, bs = sqr.next()
                        k.op("act", lambda e: e.activation(sq[:, :, :n], xt[:, :, :n], AF.Square),
                             reads=[bx], writes=[bs])
                        ss, bss = ssr.next()
                        for kc in range(KC):
                            k.op("pe", lambda e, kc=kc: e.matmul(ss[:, :n], ones[:], sq[:, kc, :n],
                                                                 start=(kc == 0), stop=(kc == KC - 1)),
                                 reads=[bs, b_ones], writes=[bss])
                        rs, brs = rsr.next()
                        k.op("act", lambda e: e.activation(rs[:, :n], ss[:, :n], AF.Sqrt, bias=NORM_EPS, scale=1.0 / D),
                             reads=[bss], writes=[brs])
                        k.op("dve", lambda e: e.reciprocal(rs[:, :n], rs[:, :n]), reads=[brs], writes=[brs])
                        k.op("dve", lambda e: e.tensor_tensor(
                            sq[:, :, :n], xt[:, :, :n], rs[:, :n].unsqueeze(1).to_broadcast([128, KC, n]), ALU.mult),
                            reads=[bx, brs, bs], writes=[bs])
                        for kc in range(KC):
                            k.op("act", lambda e, kc=kc: e.activation(
                                hT[:, b, kc, t0:t0 + n], sq[:, kc, :n], AF.Identity,
                                bias=mod[:, 0 * 8 + kc, j:j + 1], scale=gs[:, 0, kc, j:j + 1]),
                                reads=[bs, b_mod, b_gs], writes=[b_h[b][bi]])
                groups = [list(range(g * 4, g * 4 + 4)) for g in range(6)] + [[24]] + \
                         [list(range(25 + g * 4, 29 + g * 4)) for g in range(6)]
                wr = Ring(P, "win", 2, [128, KC, 512], BF16)
                pr = Ring(P, "inps", 4, [128, 512], F32, psum=True)
                sr = Ring(P, "instage", 4, [128, 512], BF16)
                ev = 0
                for grp in groups:
                    c0 = chunk_cols(grp[0])[0]
                    ncols = sum(chunk_cols(ci)[1] for ci in grp)
                    wt, bw = wr.next()
                    k.dma("pool", wt[:, :, :ncols],
                          w_in[l][:, c0:c0 + ncols].rearrange("(kc p) n -> p kc n", p=128), writes=[bw])
                    for b in range(NB):
                        for bi, (t0, n) in enumerate(blocks):
                            for ci in grp:
                                cc0, cn = chunk_cols(ci)
                                o = cc0 - c0
                                pt, bp = pr.next()
                                for kc in range(KC):
                                    k.op("pe", lambda e, kc=kc: e.matmul(
                                        pt[:cn, :n], wt[:, kc, o:o + cn], hT[:, b, kc, t0:t0 + n],
                                        start=(kc == 0), stop=(kc == KC - 1)),
                                        reads=[bw, b_h[b][bi]], writes=[bp])
                                st, bst = sr.next()
                                if ci >= 25:
                                    k.op("act", lambda e: e.activation(st[:cn, :n], pt[:cn, :n], AF.Sigmoid),
                                         reads=[bp], writes=[bst])
                                elif ev % 2 == 0:
                                    k.op("act", lambda e: e.activation(st[:cn, :n], pt[:cn, :n], AF.Copy),
                                         reads=[bp], writes=[bst])
                                else:
                                    k.op("dve", lambda e: e.tensor_copy(st[:cn, :n], pt[:cn, :n]),
                                         reads=[bp], writes=[bst])
                                ev += 1
                                k.dma("sp", PT[b, ci * 128:ci * 128 + cn, t0:t0 + n], st[:cn, :n],
                                      reads=[bst], writes=[bPT])
            if stop_after == "in":
                break
            C.mod, C.b_mod, C.gs, C.b_gs, C.ones, C.b_ones = mod, b_mod, gs, b_gs, ones, b_ones
            if "nomla" not in P.debug and "yt_in" not in P.debug:
                phase_mla(P, C, l)
            if stop_after == "mla":
                break
            if "nossm" not in P.debug and "yt_in" not in P.debug:
                phase_ssm(P, C, l)
            if stop_after == "ssm":
                break
            if "yt_in" not in P.debug:
                phase_rwkv_prep(P, C, l)
                if stop_after == "rwprep":
                    break
                phase_rwkv_scan(P, C, l)
                if stop_after == "rwkv":
                    break
            phase_merge(P, C, l)
            if stop_after == "merge":
                break
            phase_moe(P, C, l, last=(l == n_layers - 1))
            if stop_after == "moe":
                break
        k.barrier()
    return P


def host_inputs(inputs, core):
    b0 = core * NB
    x = inputs["x"][b0:b0 + NB]
    ctx = inputs["ctx"][b0:b0 + NB]
    xt0 = np.ascontiguousarray(np.concatenate([ctx, x], axis=1).transpose(0, 2, 1))
    cT = np.stack([inputs["c"][b0], inputs["c"][b0 + 1], inputs["c_ctx"]], axis=1)
    cT = np.ascontiguousarray(cT.reshape(KC, 128, 3).transpose(1, 0, 2))
    m = {"xt0": xt0, "cT": cT}
    return m


def pj(v, n):
    return np.ascontiguousarray(v.reshape(v.shape[:-1] + (n, 128)).swapaxes(-1, -2))


def host_shared(inputs):
    m = {"ones": np.ones((128, 128), np.float32)}
    for name in ("ada_w", "w_in"):
        m[name] = inputs[name]
    for name in ("mla_w_uq", "mla_w_ukv"):
        m[name] = inputs[name]
    mats = np.zeros((128, 3, 128), np.float32)
    mats[:64, 0, :64] = 1.0
    mats[64:96, 0, 64:96] = 1.0
    mats[:64, 1, :64] = 1.0
    for i in range(16):
        mats[64 + 16 + i, 2, 64 + i] = -1.0
        mats[64 + i, 2, 64 + 16 + i] = 1.0
    m["mla_mats"] = mats
    vec = np.zeros((DEPTH, 128, 8), np.float32)
    vec[:, :, 0:3] = pj(inputs["mla_q_norm"], 3)
    vec[:, :, 3:5] = pj(inputs["mla_kv_norm"], 2)
    vec[:, :64, 5] = inputs["mla_qn_nope"]
    vec[:, 64:96, 5] = inputs["mla_qn_rope"]
    vec[:, :64, 6] = inputs["mla_kn_nope"]
    vec[:, 64:96, 6] = inputs["mla_kn_rope"]
    vec[:, :64, 7] = 1.0 / 64.0
    vec[:, 64:96, 7] = 1.0 / 32.0
    m["mla_vec"] = vec
    tt = np.arange(TL)
    inv = (10000.0 ** (-np.arange(0, 16, 2, dtype=np.float32) / 16.0)).astype(np.float32)
    ang = np.concatenate([(tt // 64).astype(np.float32)[:, None] * inv, (tt % 64).astype(np.float32)[:, None] * inv], axis=-1)
    tab = np.zeros((96, 2, TT), np.float32)
    tab[:, 0, :] = 1.0
    tab[64:80, 0, TC:] = np.cos(ang).T
    tab[80:96, 0, TC:] = np.cos(ang).T
    tab[64:80, 1, TC:] = np.sin(ang).T
    tab[80:96, 1, TC:] = np.sin(ang).T
    m["rope_tab"] = tab
    it = np.zeros((128, 2, TT), np.float32)
    it[:, 0, :] = np.arange(TT)
    it[:, 1, :TC] = TC - 1 - np.arange(TC)
    it[:, 1, TC:] = TC + (TL - 1 - np.arange(TL))
    m["ssm_iota"] = it
    L_ = DEPTH
    lre = inputs["ssm_lambda_re"].reshape(L_, 2, 16, 128)
    lim = inputs["ssm_lambda_im"].reshape(L_, 2, 16, 128)
    ldt = np.repeat(inputs["ssm_log_dt"], 64, axis=-1).reshape(L_, 2, 16, 128)
    m["ssm_sv"] = np.ascontiguousarray(np.stack([lre, lim, ldt], axis=-1).transpose(0, 3, 1, 2, 4))
    BT = np.zeros((L_, 2, 16, 128, 128), np.float32)
    CT = np.zeros((L_, 2, 2, 16, 128, 128), np.float32)
    for ri, (bn, cn) in enumerate((("ssm_b_re", "ssm_c_re"), ("ssm_b_im", "ssm_c_im"))):
        bb = inputs[bn]
        cc = inputs[cn]
        for g in range(32):
            sc, gg = g // 2, g % 2
            r0 = (sc % 4) * 32 + gg * 16
            BT[:, ri, sc, r0:r0 + 16, gg * 64:(gg + 1) * 64] = bb[:, g].transpose(0, 2, 1)
            CT[:, :, ri, sc, gg * 64:(gg + 1) * 64, r0:r0 + 16] = cc[:, :, g].transpose(0, 1, 3, 2)
    m["ssm_BT"] = BT
    m["ssm_CT"] = CT
    m["ssm_vec"] = np.ascontiguousarray(np.stack([pj(inputs["ssm_d"], 4), pj(inputs["ssm_glu_b"], 4)], axis=2))
    m["ssm_glu_w"] = inputs["ssm_glu_w"]
    for name in ("w_branch", "w_out", "router_w"):
        m[name] = inputs[name]
    for name in ("moe_w1", "moe_w3", "moe_w2"):
        m[name] = inputs[name]
    cst = np.zeros((128, 3, 256), np.float32)
    cst[:, 0, :] = np.arange(1, 257)
    cst[:16, 1, :16] = 1.0
    cst[16:32, 1, 16:32] = 1.0
    cst[:, 2, :128] = np.eye(128)
    m["moe_cst"] = cst
    m["moe_jcol"] = np.stack([np.arange(1, 129), np.arange(129, 257)], axis=1).astype(np.float32)
    L_ = DEPTH
    rv = np.zeros((L_, 128, 51), np.float32)
    rv[:, :, 0:15] = pj(inputs["rwkv_mu"], 15)
    rv[:, :, 15:23] = pj(inputs["rwkv_w0"], 4).transpose(0, 2, 1, 3).reshape(L_, 128, 8)
    rv[:, :, 23:31] = pj(inputs["rwkv_a0"], 4).transpose(0, 2, 1, 3).reshape(L_, 128, 8)
    rv[:, :, 31:35] = pj(inputs["rwkv_k_k"], 4)
    rv[:, :, 35:39] = pj(inputs["rwkv_k_a"], 4)
    rv[:, :, 39:43] = pj(inputs["rwkv_r_k"].reshape(L_, 512), 4)
    rv[:, :, 43:47] = pj(inputs["rwkv_ln_w"], 4)
    rv[:, :, 47:51] = pj(inputs["rwkv_ln_b"], 4)
    m["rw_vec"] = rv
    for name in ("rwkv_w2", "rwkv_a2", "rwkv_g2"):
        m[name] = inputs[name]
    bd = np.zeros((128, 128), np.float32)
    bd[:64, :64] = 1.0
    bd[64:, 64:] = 1.0
    m["rw_bd"] = bd
    rst = np.ones((128, TT + 1), np.float32)
    rst[:, 0::128] = 0.0
    m["rw_rst"] = rst
    ii = np.arange(128)
    mk = np.zeros((128, 2, 640), np.float32)
    for j_ in range(2):
        if j_ == 0:
            strict = (ii[None, :] > ii[:, None]).astype(np.float32)
            incl = (ii[None, :] >= ii[:, None]).astype(np.float32)
        else:
            strict = (ii[None, :] < ii[:, None]).astype(np.float32)
            incl = (ii[None, :] <= ii[:, None]).astype(np.float32)
        mk[:, j_, 0:128] = strict
        mk[:, j_, 128:256] = incl
        mk[:, j_, 256:384] = -strict
        mk[:, j_, 384:512] = incl
        mk[:, j_, 512:640] = -strict.T
    m["rw_masks"] = mk
    lm = np.zeros((128, 4, 128), np.float32)
    ti, si = ii[:, None], ii[None, :]
    lm[:, 0, :] = (ti // 16 == si // 16)
    for q_, sz in enumerate((16, 32, 64)):
        lm[:, 1 + q_, :] = (ti // (2 * sz) == si // (2 * sz)) & (ti // sz != si // sz)
    m["rw_lm"] = lm
    m["ident"] = np.eye(128, dtype=np.float32)
    m["ada_b_r"] = pj(inputs["ada_b"], 48)
    m["ng_r"] = np.ascontiguousarray(np.stack([pj(inputs["norm1_g"], KC), pj(inputs["norm2_g"], KC)], axis=2))
    return m


def kernel(**inputs):
    inputs = {k_: np.asarray(v) for k_, v in inputs.items()}
    P = build()
    shared = host_shared(inputs)
    in_maps = []
    for c in range(NCORES):
        m = host_inputs(inputs, c)
        m.update(shared)
        in_maps.append({n: m[n] for n in P.inputs})
    res = run_bass_kernel_spmd(P.nc, in_maps, core_ids=list(range(NCORES)))
    outs = [r["OUT"] for r in res.results]
    y = np.concatenate(outs, axis=0)
    return np.ascontiguousarray(y.transpose(0, 2, 1)).astype(np.float32)
```

```python
import contextlib
import numpy as np
import concourse.bass as bass
import concourse.mybir as mybir
from concourse.bass_utils import run_bass_kernel_spmd

F32 = mybir.dt.float32
BF16 = mybir.dt.bfloat16
AF = mybir.ActivationFunctionType
ALU = mybir.AluOpType
AX = mybir.AxisListType

NCORES = 8
NB = 2
D = 1024
KC = 8
TC = 256
TL = 2048
TT = TC + TL
DEPTH = 4
N_IN = 6176
NCH = 49
NORM_EPS = 1e-6


def chunk_cols(ci):
    if ci < 24:
        return ci * 128, 128
    if ci == 24:
        return 3072, 32
    return 3104 + (ci - 25) * 128, 128


class Buf:
    __slots__ = ("name", "w", "r")

    def __init__(self, name=""):
        self.name = name
        self.w = None
        self.r = []


ATTACH_WAITS = True


class K:
    NDMA = 12

    def __init__(self, nc):
        self.nc = nc
        self.eng = {"pe": nc.tensor, "dve": nc.vector, "act": nc.scalar, "pool": nc.gpsimd, "sp": nc.sync}
        self.sem = {}
        self.cnt = {}
        self.seen = {e: {} for e in self.eng}
        self._stack = []
        for e in self.eng:
            self.sem[e] = self._mksem("s_" + e)
            self.cnt[e] = 0
        self.dq = {}
        for q in ("sp", "act", "pool"):
            self.dq[q] = {"n": 0}
            for i in range(self.NDMA):
                self.sem[(q, i)] = self._mksem(f"d_{q}{i}")
        self.n_instr = 0
        self.n_wait = 0

    def _mksem(self, name):
        g = self.nc.semaphore(name)
        s = g.__enter__()
        self._stack.append(g)
        return s

    def _need(self, e, dep, out):
        if dep is None:
            return
        key, val = dep
        if key == "pe" and e == "pe":
            return
        if self.seen[e].get(key, 0) >= val:
            return
        for i, (k2, v2) in enumerate(out):
            if k2 == key:
                if v2 < val:
                    out[i] = (key, val)
                return
        out.append((key, val))

    def _wait(self, e, dep):
        need = []
        self._need(e, dep, need)
        self._emit_waits(e, need, attach=False)

    def _emit_waits(self, e, need, attach):
        last = None
        if attach and ATTACH_WAITS and need:
            last = need.pop()
        for key, val in need:
            self.eng[e].wait_ge(self.sem[key], val)
            self.seen[e][key] = val
            self.n_wait += 1
        if last is not None:
            self.seen[e][last[0]] = last[1]
        return last

    def _deps(self, e, reads, writes, attach=False):
        need = []
        for b in reads:
            self._need(e, b.w, need)
        for b in writes:
            self._need(e, b.w, need)
            for r in b.r:
                self._need(e, r, need)
        return self._emit_waits(e, need, attach)

    def _mark(self, tok, reads, writes):
        for b in reads:
            b.r = [r for r in b.r if r[0] != tok[0]]
            b.r.append(tok)
        for b in writes:
            b.w = tok
            b.r = []

    def op(self, e, fn, reads=(), writes=()):
        last = self._deps(e, reads, writes, attach=True)
        ins = fn(self.eng[e])
        if last is not None:
            ins._wait_ge(self.sem[last[0]], last[1])
        self.cnt[e] += 1
        ins.then_inc(self.sem[e], 1)
        self._mark((e, self.cnt[e]), reads, writes)
        self.n_instr += 1
        return ins

    def dma(self, q, out, in_, reads=(), writes=(), **kw):
        d = self.dq[q]
        i = d["n"] % self.NDMA
        gen = d["n"] // self.NDMA
        key = (q, i)
        if gen > 0:
            self._wait(q, (key, 16 * gen))
        self._deps(q, reads, writes)
        ins = self.eng[q].dma_start(out=out, in_=in_, **kw)
        ins.then_inc(self.sem[key], 16)
        d["n"] += 1
        self._mark((key, 16 * (gen + 1)), reads, writes)
        self.n_instr += 1

    def barrier(self):
        toks = [(e, self.cnt[e]) for e in self.eng if self.cnt[e] > 0]
        for q, d in self.dq.items():
            n = d["n"]
            for i in range(self.NDMA):
                c = (n - i + self.NDMA - 1) // self.NDMA
                if c > 0:
                    toks.append(((q, i), 16 * c))
        for e in self.eng:
            for t in toks:
                self._wait(e, t)


class Ring:
    def __init__(self, P, name, n, shape, dtype, psum=False):
        self.items = []
        for i in range(n):
            t = P.ps(f"{name}{i}", shape, dtype) if psum else P.sb(f"{name}{i}", shape, dtype)
            self.items.append((t, Buf(f"{name}{i}")))
        self.i = 0

    def next(self):
        it = self.items[self.i % len(self.items)]
        self.i += 1
        return it


class Prog:
    def __init__(self, n_layers=DEPTH, debug=()):
        self.nc = nc = bass.Bass("TRN2", target_bir_lowering=False)
        self.k = K(nc)
        self.debug = set(debug)
        self.n_layers = n_layers
        self._scopes = []
        self.uid = 0
        self.inputs = {}
        self.outputs = {}
        self.dbufs = {}

    def din(self, name, shape, dtype=F32):
        t = self.nc.dram_tensor(name, list(shape), dtype, kind="ExternalInput").ap()
        self.inputs[name] = t
        return t

    def dscr(self, name, shape, dtype, out=False):
        kind = "ExternalOutput" if (out or name in self.debug) else "Internal"
        t = self.nc.dram_tensor(name, list(shape), dtype, kind=kind).ap()
        if kind == "ExternalOutput":
            self.outputs[name] = t
        self.dbufs[name] = Buf(name)
        return t

    @contextlib.contextmanager
    def scope(self):
        st = contextlib.ExitStack()
        self._scopes.append(st)
        try:
            yield
        finally:
            self.k.barrier()
            self._scopes.pop()
            st.close()

    def sb(self, name, shape, dtype):
        self.uid += 1
        g = self.nc.sbuf_tensor(f"{name}_{self.uid}", list(shape), dtype)
        return self._scopes[-1].enter_context(g)

    def ps(self, name, shape, dtype=F32):
        self.uid += 1
        g = self.nc.psum_tensor(f"{name}_{self.uid}", list(shape), dtype)
        return self._scopes[-1].enter_context(g)


MLA_SCALE = 1.0 / float(np.sqrt(96.0))


def declare_inputs(P, C):
    C.mla_w_uq = P.din("mla_w_uq", [DEPTH, 384, 768])
    C.mla_w_ukv = P.din("mla_w_ukv", [DEPTH, 256, 1024])
    C.mla_mats = P.din("mla_mats", [128, 3, 128])
    C.mla_vec = P.din("mla_vec", [DEPTH, 128, 8])
    C.rope_tab = P.din("rope_tab", [96, 2, TT])
    C.ssm_iota = P.din("ssm_iota", [128, 2, TT])
    C.ssm_sv = P.din("ssm_sv", [DEPTH, 128, 2, 16, 3])
    C.ssm_BT = P.din("ssm_BT", [DEPTH, 2, 16, 128, 128])
    C.ssm_CT = P.din("ssm_CT", [DEPTH, 2, 2, 16, 128, 128])
    C.ssm_vec = P.din("ssm_vec", [DEPTH, 128, 2, 4])
    C.ssm_glu_w = P.din("ssm_glu_w", [DEPTH, 512, 512])
    C.w_branch = P.din("w_branch", [DEPTH, 3, 512, D])
    C.w_out = P.din("w_out", [DEPTH, D, D])
    C.router_w = P.din("router_w", [DEPTH, D, 16])
    C.ident = P.din("ident", [128, 128])
    C.LG = P.dscr("LG", [NB, 16, TT], F32)
    C.bLG = Buf("LG")
    C.H2 = P.dscr("H2", [NB, TT, D], BF16)
    C.bH2 = Buf("H2")
    C.moe_w1 = P.din("moe_w1", [DEPTH, NE, D, FF])
    C.moe_w3 = P.din("moe_w3", [DEPTH, NE, D, FF])
    C.moe_w2 = P.din("moe_w2", [DEPTH, NE, FF, D])
    C.moe_cst = P.din("moe_cst", [128, 3, 256])
    C.moe_jcol = P.din("moe_jcol", [128, 2])
    C.POSD = P.dscr("POSD", [NB, NE, TT], F32)
    C.MGD = P.dscr("MGD", [NB, NE, TT], F32)
    C.bPOSD = Buf("POSD")
    C.XS = P.dscr("XS", [NE, D, NJ], BF16)
    C.bXS = Buf("XS")
    C.YE = P.dscr("YE", [NE, NJ, D], BF16)
    C.bYE = Buf("YE")
    C.OUT = P.dscr("OUT", [NB, D, TL], F32, out=True)
    C.bOUT = Buf("OUT")
    C.rw_vec = P.din("rw_vec", [DEPTH, 128, 51])
    C.rwkv_w2 = P.din("rwkv_w2", [DEPTH, 2, 64, 512])
    C.rwkv_a2 = P.din("rwkv_a2", [DEPTH, 2, 64, 512])
    C.rwkv_g2 = P.din("rwkv_g2", [DEPTH, 128, 512])
    C.rw_bd = P.din("rw_bd", [128, 128])
    C.rw_rst = P.din("rw_rst", [128, TT + 1])
    C.rw_masks = P.din("rw_masks", [128, 2, 640])
    C.rw_lm = P.din("rw_lm", [128, 4, 128])
    C.RWT = P.dscr("RWT", [NB, 2, 4, 512, TT], BF16)
    C.RWV = P.dscr("RWV", [NB, 512, TT], BF16)
    C.RWG = P.dscr("RWG", [NB, 512, TT], BF16)
    C.RWB = P.dscr("RWB", [NB, 512, TT], F32)
    C.RWPL = P.dscr("RWPL", [NB, 2, 512, NCK], F32)
    C.bRW = Buf("RW")
    if "YTOK" in P.debug:
        C.YTOK = P.dscr("YTOK", [NB, 128, NCK, 512], F32)
    C.YG = P.dscr("YG", [NB, 512, TT], BF16)
    C.bYG = Buf("YG")


def rstd_from(k, ss_ap, out_ap, scale, bias, reads, bout):
    k.op("act", lambda e: e.activation(out_ap, ss_ap, AF.Sqrt, bias=bias, scale=scale), reads=reads, writes=[bout])
    k.op("dve", lambda e: e.reciprocal(out_ap, out_ap), reads=[bout], writes=[bout])


def phase_mla(P, C, l):
    k = P.k
    PT, YT = C.PT, C.YT
    blocks = [(0, TC)] + [(TC + i * 512, 512) for i in range(4)]
    with P.scope():
        mats = P.sb("mla_mats", [128, 3, 128], BF16)
        b_mats = Buf("mats")
        k.dma("pool", mats[:], C.mla_mats, writes=[b_mats])
        onesb = P.sb("onesb", [128, 128], BF16)
        b_onesb = Buf("onesb")
        k.op("dve", lambda e: e.memset(onesb[:], 1.0), writes=[b_onesb])
        vec = P.sb("mla_vec", [128, 8], F32)
        b_vec = Buf("vec")
        k.dma("sp", vec[:], C.mla_vec[l], writes=[b_vec])
        epsc = P.sb("epsc", [128, 1], F32)
        k.op("dve", lambda e: e.memset(epsc[:], NORM_EPS), writes=[b_vec])
        tab = P.sb("rope_tab", [96, 2, TT], F32)
        b_tab = Buf("tab")
        k.dma("sp", tab[:], C.rope_tab, writes=[b_tab])
        wuq = P.sb("wuq", [128, 3, 768], BF16)
        b_wuq = Buf("wuq")
        k.dma("pool", wuq[:], C.mla_w_uq[l].rearrange("(kc p) n -> p kc n", p=128), writes=[b_wuq])
        wk = P.sb("wk", [128, 2, 8, 64], BF16)
        wv = P.sb("wv", [128, 2, 8, 64], BF16)
        b_wkv = Buf("wkv")
        ukv = C.mla_w_ukv[l].rearrange("(kc p) (h x) -> p kc h x", p=128, x=128)
        for kc in range(2):
            k.dma("pool", wk[:, kc], ukv[:, kc, :, 0:64], writes=[b_wkv])
            k.dma("pool", wv[:, kc], ukv[:, kc, :, 64:128], writes=[b_wkv])
        QT = P.sb("QT", [96, 8, TT], BF16)
        KT = P.sb("KT", [96, 8, TT], BF16)
        Vt = P.sb("Vt", [128, 18, 512], BF16)
        for b in range(NB):
            b_Q = [Buf(f"Q{i}") for i in range(5)]
            b_K = [Buf(f"K{i}") for i in range(5)]
            b_V = [Buf(f"V{i}") for i in range(5)]
            with P.scope():
                x5r = Ring(P, "x5", 2, [128, 5, 512], BF16)
                krr = Ring(P, "kr", 2, [96, 512], BF16)
                sq5r = Ring(P, "sq5", 1, [128, 5, 512], BF16)
                cqnr = Ring(P, "cqn", 2, [128, 5, 512], BF16)
                psA = Ring(P, "psA", 3, [128, 512], F32, psum=True)
                psB = Ring(P, "psB", 3, [128, 512], F32, psum=True)
                rsr = Ring(P, "rs", 3, [128, 512], F32)
                f32r = Ring(P, "f32t", 3, [128, 512], F32)
                f32r2 = Ring(P, "f32u", 3, [128, 512], F32)
                bfr = Ring(P, "bft", 3, [128, 512], BF16)
                krf = P.sb("krf", [96, 512], BF16)
                b_krf = Buf("krf")
                for bi, (t0, n) in enumerate(blocks):
                    x5, bx5 = x5r.next()
                    k.dma("sp", x5[:, :, :n], PT[b, 19 * 128:24 * 128, t0:t0 + n].rearrange("(c p) n -> p c n", p=128),
                          reads=[C.bPT], writes=[bx5])
                    kr, bkr = krr.next()
                    k.dma("sp", kr[64:96, :n], PT[b, 24 * 128:24 * 128 + 32, t0:t0 + n], reads=[C.bPT], writes=[bkr])
                    sq5, bsq5 = sq5r.next()
                    k.op("act", lambda e: e.activation(sq5[:, :, :n], x5[:, :, :n], AF.Square), reads=[bx5], writes=[bsq5])
                    cqn, bcqn = cqnr.next()
                    for (c0, nc_, col0, dim) in ((0, 3, 0, 384.0), (3, 2, 3, 256.0)):
                        ps, bps = psA.next()
                        for c in range(nc_):
                            k.op("pe", lambda e: e.matmul(ps[:, :n], onesb[:], sq5[:, c0 + c, :n],
                                                          start=(c == 0), stop=(c == nc_ - 1)),
                                 reads=[bsq5, b_onesb], writes=[bps])
                        rs, brs = rsr.next()
                        rstd_from(k, ps[:, :n], rs[:, :n], 1.0 / dim, epsc[:, 0:1], [bps, b_vec], brs)
                        for c in range(nc_):
                            k.op("dve", lambda e: e.scalar_tensor_tensor(
                                cqn[:, c0 + c, :n], x5[:, c0 + c, :n], vec[:, col0 + c:col0 + c + 1], rs[:, :n],
                                ALU.mult, ALU.mult), reads=[bx5, brs, b_vec], writes=[bcqn])
                    sqk, bsqk = bfr.next()
                    k.op("act", lambda e: e.activation(sqk[64:96, :n], kr[64:96, :n], AF.Square), reads=[bkr], writes=[bsqk])
                    ps, bps = psA.next()
                    k.op("pe", lambda e: e.matmul(ps[64:96, :n], mats[64:96, 0, 64:96], sqk[64:96, :n], start=True, stop=True),
                         reads=[bsqk, b_mats], writes=[bps])
                    rs, brs = rsr.next()
                    rstd_from(k, ps[64:96, :n], rs[64:96, :n], 1.0 / 32.0, epsc[64:96, 0:1], [bps, b_vec], brs)
                    krn, bkrn = bfr.next()
                    k.op("dve", lambda e: e.scalar_tensor_tensor(
                        krn[64:96, :n], kr[64:96, :n], vec[64:96, 6:7], rs[64:96, :n], ALU.mult, ALU.mult),
                        reads=[bkr, brs, b_vec], writes=[bkrn])
                    ps2, bps2 = psB.next()
                    k.op("pe", lambda e: e.matmul(ps2[64:96, :n], mats[64:96, 2, 64:96], krn[64:96, :n], start=True, stop=True),
                         reads=[bkrn, b_mats], writes=[bps2])
                    t1, bt1 = f32r.next()
                    k.op("dve", lambda e: e.tensor_tensor(t1[64:96, :n], krn[64:96, :n], tab[64:96, 0, t0:t0 + n], ALU.mult),
                         reads=[bkrn, b_tab], writes=[bt1])
                    t2, bt2 = f32r2.next()
                    k.op("dve", lambda e: e.tensor_tensor(t2[64:96, :n], ps2[64:96, :n], tab[64:96, 1, t0:t0 + n], ALU.mult),
                         reads=[bps2, b_tab], writes=[bt2])
                    k.op("pool", lambda e: e.tensor_tensor(krf[64:96, :n], t1[64:96, :n], t2[64:96, :n], ALU.add),
                         reads=[bt1, bt2], writes=[b_krf])
                    k.op("pool", lambda e: e.tensor_copy(
                        KT[64:96, :, t0:t0 + n], krf[64:96, :n].unsqueeze(1).to_broadcast([32, 8, n])),
                        reads=[b_krf], writes=[b_K[bi]])
                    for h in range(8):
                        ps, bps = psA.next()
                        for c in range(3):
                            k.op("pe", lambda e: e.matmul(ps[:96, :n], wuq[:, c, h * 96:(h + 1) * 96], cqn[:, c, :n],
                                                          start=(c == 0), stop=(c == 2)),
                                 reads=[bcqn, b_wuq], writes=[bps])
                        qs, bqs = f32r.next()
                        k.op("act", lambda e: e.activation(qs[:96, :n], ps[:96, :n], AF.Copy), reads=[bps], writes=[bqs])
                        sq, bsq = bfr.next()
                        k.op("act", lambda e: e.activation(sq[:96, :n], ps[:96, :n], AF.Square), reads=[bps], writes=[bsq])
                        ps2, bps2 = psB.next()
                        k.op("pe", lambda e: e.matmul(ps2[:96, :n], mats[:96, 0, :96], sq[:96, :n], start=True, stop=True),
                             reads=[bsq, b_mats], writes=[bps2])
                        rs, brs = rsr.next()
                        rstd_from(k, ps2[:96, :n], rs[:96, :n], vec[:96, 7:8], epsc[:96, 0:1], [bps2, b_vec], brs)
                        qn, bqn = bfr.next()
                        k.op("dve", lambda e: e.scalar_tensor_tensor(
                            qn[:96, :n], qs[:96, :n], vec[:96, 5:6], rs[:96, :n], ALU.mult, ALU.mult),
                            reads=[bqs, brs, b_vec], writes=[bqn])
                        ps3, bps3 = psB.next()
                        k.op("pe", lambda e: e.matmul(ps3[:96, :n], mats[:96, 2, :96], qn[:96, :n], start=True, stop=True),
                             reads=[bqn, b_mats], writes=[bps3])
                        t1, bt1 = f32r.next()
                        k.op("pool", lambda e: e.tensor_tensor(t1[:96, :n], qn[:96, :n], tab[:, 0, t0:t0 + n], ALU.mult),
                             reads=[bqn, b_tab], writes=[bt1])
                        t2, bt2 = f32r2.next()
                        k.op("dve", lambda e: e.tensor_tensor(t2[:96, :n], ps3[:96, :n], tab[:, 1, t0:t0 + n], ALU.mult),
                             reads=[bps3, b_tab], writes=[bt2])
                        k.op("pool", lambda e: e.tensor_tensor(QT[:, h, t0:t0 + n], t1[:96, :n], t2[:96, :n], ALU.add),
                             reads=[bt1, bt2], writes=[b_Q[bi]])
                        ps, bps = psA.next()
                        for c in range(2):
                            k.op("pe", lambda e: e.matmul(ps[:64, :n], wk[:, c, h, :], cqn[:, 3 + c, :n],
                                                          start=(c == 0), stop=(c == 1)),
                                 reads=[bcqn, b_wkv], writes=[bps])
                        ks_, bks = f32r.next()
                        k.op("act", lambda e: e.activation(ks_[:64, :n], ps[:64, :n], AF.Copy), reads=[bps], writes=[bks])
                        sq, bsq = bfr.next()
                        k.op("act", lambda e: e.activation(sq[:64, :n], ps[:64, :n], AF.Square), reads=[bps], writes=[bsq])
                        ps2, bps2 = psB.next()
                        k.op("pe", lambda e: e.matmul(ps2[:64, :n], onesb[:64, :64], sq[:64, :n], start=True, stop=True),
                             reads=[bsq, b_onesb], writes=[bps2])
                        rs, brs = rsr.next()
                        rstd_from(k, ps2[:64, :n], rs[:64, :n], 1.0 / 64.0, epsc[:64, 0:1], [bps2, b_vec], brs)
                        k.op("dve", lambda e: e.scalar_tensor_tensor(
                            KT[0:64, h, t0:t0 + n], ks_[:64, :n], vec[:64, 6:7], rs[:64, :n], ALU.mult, ALU.mult),
                            reads=[bks, brs, b_vec], writes=[b_K[bi]])
                    for ti in range(n // 128):
                        tile_i = (t0 // 128) + ti
                        ps, bps = psA.next()
                        for c in range(2):
                            k.op("pe", lambda e: e.matmul(
                                ps[:, :], cqn[:, 3 + c, ti * 128:(ti + 1) * 128],
                                wv[:, c].rearrange("p h x -> p (h x)"), start=(c == 0), stop=(c == 1)),
                                reads=[bcqn, b_wkv], writes=[bps])
                        k.op("act", lambda e: e.activation(Vt[:, tile_i, :], ps[:, :], AF.Copy), reads=[bps], writes=[b_V[bi]])
            if "stop_mlaprep" in P.debug:
                continue
            with P.scope():
                psS = Ring(P, "psS", 4, [128, 512], F32, psum=True)
                psO = Ring(P, "psO", 2, [64, 512], F32, psum=True)
                psD = Ring(P, "psD", 2, [64, 512], F32, psum=True)
                ptr = Ring(P, "pT", 4, [128, 512], BF16)
                rdr = Ring(P, "rden", 2, [64, 512], F32)
                osr = Ring(P, "ost", 3, [64, 512], BF16)
                allK = b_K + b_V
                LOOK = 2
                for h in range(8):
                    for bi, (q0, n) in enumerate(blocks):
                        nkt = 2 if bi == 0 else 18
                        po, bpo = psO.next()
                        pd, bpd = psD.next()
                        pend = []
                        for kt in range(nkt + LOOK):
                            if kt < nkt:
                                pS, bpS = psS.next()
                                k.op("pe", lambda e: e.matmul(pS[:, :n], KT[:, h, kt * 128:(kt + 1) * 128], QT[:, h, q0:q0 + n],
                                                              start=True, stop=True),
                                     reads=allK + [b_Q[bi]], writes=[bpS])
                                pT, bpT = ptr.next()
                                k.op("act", lambda e: e.activation(pT[:, :n], pS[:, :n], AF.Exp, scale=MLA_SCALE),
                                     reads=[bpS], writes=[bpT])
                                pend.append((kt, pT, bpT))
                            if kt >= LOOK:
                                k2, pT2, bpT2 = pend.pop(0)
                                k.op("pe", lambda e: e.matmul(po[:, :n], Vt[:, k2, h * 64:(h + 1) * 64], pT2[:, :n],
                                                              start=(k2 == 0), stop=(k2 == nkt - 1)),
                                     reads=[bpT2] + b_V, writes=[bpo])
                                k.op("pe", lambda e: e.matmul(pd[:, :n], onesb[:, :64], pT2[:, :n],
                                                              start=(k2 == 0), stop=(k2 == nkt - 1)),
                                     reads=[bpT2, b_onesb], writes=[bpd])
                        rd, brd = rdr.next()
                        k.op("dve", lambda e: e.reciprocal(rd[:, :n], pd[:, :n]), reads=[bpd], writes=[brd])
                        os_, bos = osr.next()
                        k.op("dve", lambda e: e.tensor_tensor(os_[:, :n], po[:, :n], rd[:, :n], ALU.mult),
                             reads=[bpo, brd], writes=[bos])
                        k.dma("sp", YT[b, 2, h * 64:(h + 1) * 64, q0:q0 + n], os_[:, :n], reads=[bos], writes=[C.bYT])


PI = float(np.pi)


def rev_ap(ap2d, lo, hi):
    from concourse.ap import AP
    a = ap2d[:, lo:hi]
    return AP(a.tensor, a.offset + (hi - lo - 1) * a.ap[1][0], [list(a.ap[0]), [-a.ap[1][0], hi - lo]])


MAGIC = 12582912.0
TWO_PI = 2.0 * PI


def sin_reduced(k, out, tmp, in0, th, th2pi, shift, hp_tile, reads, bout, btmp):
    k.op("dve", lambda e: e.tensor_scalar(tmp, in0, th2pi, MAGIC + shift / TWO_PI, ALU.mult, ALU.add),
         reads=reads + [btmp], writes=[btmp])
    k.op("dve", lambda e: e.tensor_scalar(tmp, tmp, -MAGIC, -TWO_PI, ALU.add, ALU.mult), reads=[btmp], writes=[btmp])
    k.op("dve", lambda e: e.scalar_tensor_tensor(tmp, in0, th, tmp, ALU.mult, ALU.add), reads=reads + [btmp], writes=[btmp])
    if shift == 0.0:
        k.op("act", lambda e: e.activation(out, tmp, AF.Sin, scale=0.999999), reads=[btmp, bout], writes=[bout])
    else:
        k.op("act", lambda e: e.activation(out, tmp, AF.Sin, scale=0.999999, bias=hp_tile), reads=[btmp, bout], writes=[bout])


def phase_ssm(P, C, l):
    k = P.k
    PT, YT = C.PT, C.YT
    blocks = [(0, TC)] + [(TC + i * 512, 512) for i in range(4)]
    YG = C.YG
    with P.scope():
        iota = P.sb("iota", [128, 2, TT], F32)
        b_c = Buf("ssmconst")
        k.dma("sp", iota[:], C.ssm_iota, writes=[b_c])
        sv = P.sb("sv", [128, 2, 16, 3], F32)
        k.dma("sp", sv[:], C.ssm_sv[l], writes=[b_c])
        BT = P.sb("BT", [128, 2, 16, 128], BF16)
        k.dma("pool", BT[:], C.ssm_BT[l].rearrange("r s p n -> p r s n"), writes=[b_c])
        CT = P.sb("CT", [128, 2, 2, 16, 128], BF16)
        for j in range(2):
            k.dma("pool", CT[:, j], C.ssm_CT[l, j].rearrange("r s p n -> p r s n"), writes=[b_c])
        dsk = P.sb("dsk", [128, 2, 4], F32)
        k.dma("sp", dsk[:], C.ssm_vec[l], writes=[b_c])
        hpi = P.sb("hpi", [128, 1], F32)
        k.op("dve", lambda e: e.memset(hpi[:], 0.5 * PI * 0.999999), writes=[b_c])
        shp = [128, 2, 16]
        names = "dt rho th th2 sn cs are aim den t1 t2 cre cim ncre".split()
        Tl = {n_: P.sb("d_" + n_, shp, F32) for n_ in names}
        bd = Buf("disc")
        lr, li, ldt = sv[:, :, :, 0], sv[:, :, :, 1], sv[:, :, :, 2]
        A_ = lambda e_, fn: k.op(e_, fn, reads=[b_c, bd], writes=[bd])
        A_("act", lambda e: e.activation(Tl["dt"][:], ldt, AF.Exp))
        A_("dve", lambda e: e.tensor_tensor(Tl["t1"][:], lr, Tl["dt"][:], ALU.mult))
        A_("act", lambda e: e.activation(Tl["rho"][:], Tl["t1"][:], AF.Exp))
        A_("dve", lambda e: e.tensor_tensor(Tl["th"][:], li, Tl["dt"][:], ALU.mult))
        sin_reduced(k, Tl["sn"][:], Tl["t1"][:], Tl["th"][:], 1.0, 1.0 / TWO_PI, 0.0, None, [b_c, bd], bd, bd)
        sin_reduced(k, Tl["cs"][:], Tl["t1"][:], Tl["th"][:], 1.0, 1.0 / TWO_PI, 0.5 * PI, hpi[:, 0:1], [b_c, bd], bd, bd)
        A_("dve", lambda e: e.tensor_scalar(Tl["th2"][:], Tl["th"][:], 1.0 / TWO_PI, None, ALU.mult))
        A_("dve", lambda e: e.tensor_tensor(Tl["are"][:], Tl["rho"][:], Tl["cs"][:], ALU.mult))
        A_("dve", lambda e: e.tensor_tensor(Tl["aim"][:], Tl["rho"][:], Tl["sn"][:], ALU.mult))
        A_("dve", lambda e: e.tensor_scalar(Tl["are"][:], Tl["are"][:], -1.0, None, ALU.add))
        A_("dve", lambda e: e.tensor_tensor(Tl["t1"][:], lr, lr, ALU.mult))
        A_("dve", lambda e: e.tensor_tensor(Tl["t2"][:], li, li, ALU.mult))
        A_("dve", lambda e: e.tensor_tensor(Tl["den"][:], Tl["t1"][:], Tl["t2"][:], ALU.add))
        A_("dve", lambda e: e.reciprocal(Tl["den"][:], Tl["den"][:]))
        A_("dve", lambda e: e.tensor_tensor(Tl["t1"][:], Tl["are"][:], lr, ALU.mult))
        A_("dve", lambda e: e.tensor_tensor(Tl["t2"][:], Tl["aim"][:], li, ALU.mult))
        A_("dve", lambda e: e.tensor_tensor(Tl["cre"][:], Tl["t1"][:], Tl["t2"][:], ALU.add))
        A_("dve", lambda e: e.tensor_tensor(Tl["cre"][:], Tl["cre"][:], Tl["den"][:], ALU.mult))
        A_("dve", lambda e: e.tensor_tensor(Tl["t1"][:], Tl["aim"][:], lr, ALU.mult))
        A_("dve", lambda e: e.tensor_tensor(Tl["t2"][:], Tl["are"][:], li, ALU.mult))
        A_("dve", lambda e: e.tensor_tensor(Tl["cim"][:], Tl["t1"][:], Tl["t2"][:], ALU.subtract))
        A_("dve", lambda e: e.tensor_tensor(Tl["cim"][:], Tl["cim"][:], Tl["den"][:], ALU.mult))
        A_("dve", lambda e: e.tensor_scalar(Tl["ncre"][:], Tl["cre"][:], -1.0, None, ALU.mult))
        big = lambda n_: P.sb(n_, [128, TT], F32)
        bigb = lambda n_: P.sb(n_, [128, TT], BF16)
        CS, SN, ERE, EIM = bigb("CS"), bigb("SN"), bigb("ERE"), bigb("EIM")
        BU = [(bigb(f"BUR{b}"), bigb(f"BUI{b}")) for b in range(NB)]
        b_bus = [Buf(f"bu{b}") for b in range(NB)]
        WK = [[bigb(f"{x}{b}") for x in ("T1", "T2", "ZR", "ZI")] for b in range(NB)]
        TA, TB = big("TA"), big("TB")
        b_ta, b_tb = Buf("ta"), Buf("tb")
        bWK = [[Buf(f"{x}{b}") for x in ("t1", "t2", "zr", "zi")] for b in range(NB)]
        T1, T2, ZR, ZI = WK[0]
        b_t1, b_t2, b_zr, b_zi = bWK[0]
        b_tab, b_bu = Buf("tab"), Buf("bu")
        Q = [P.sb(f"Q{i}", [128, TT], BF16) for i in range(4)]
        b_q = [Buf(f"q{i}") for i in range(4)]
        U = [P.sb(f"U{b}", [128, TT], BF16) for b in range(NB)]
        b_u = [Buf(f"u{b}") for b in range(NB)]
        YA = [P.sb(f"YA{b}", [128, TT], F32) for b in range(NB)]
        b_ya = [Buf(f"ya{b}") for b in range(NB)]
        psr = Ring(P, "ssmps", 4, [128, 512], F32, psum=True)
        psy = Ring(P, "ssmpy", 3, [128, 512], F32, psum=True)
        for oc in range(4):
            for b in range(NB):
                k.dma("sp", U[b][:], PT[b, oc * 128:(oc + 1) * 128, :], reads=[C.bPT], writes=[b_u[b]])
                k.op("pool", lambda e: e.memset(YA[b][:], 0.0), writes=[b_ya[b]])
            for j in range(2):
                for s4 in range(4):
                    sc = oc * 4 + s4
                    th = Tl["th"][:, j, sc:sc + 1]
                    th2 = Tl["th2"][:, j, sc:sc + 1]
                    sin_reduced(k, SN[:], TA[:], iota[:, j, :], th, th2, 0.0, None, [b_c, bd], b_tab, b_ta)
                    sin_reduced(k, CS[:], TB[:], iota[:, j, :], th, th2, 0.5 * PI, hpi[:, 0:1], [b_c, bd], b_tab, b_tb)
                    cre, cim, ncre = (Tl[x][:, j, sc:sc + 1] for x in ("cre", "cim", "ncre"))
                    k.op("dve", lambda e: e.tensor_scalar(ERE[:], CS[:], cre, None, ALU.mult), reads=[b_tab, bd], writes=[b_tab])
                    k.op("dve", lambda e: e.scalar_tensor_tensor(ERE[:], SN[:], cim, ERE[:], ALU.mult, ALU.add),
                         reads=[b_tab, bd], writes=[b_tab])
                    k.op("pool", lambda e: e.tensor_scalar(EIM[:], CS[:], cim, None, ALU.mult), reads=[b_tab, bd], writes=[b_tab])
                    k.op("dve", lambda e: e.scalar_tensor_tensor(EIM[:], SN[:], ncre, EIM[:], ALU.mult, ALU.add),
                         reads=[b_tab, bd], writes=[b_tab])
                    rho = Tl["rho"][:, j, sc:sc + 1]

                    def body(b, j=j, sc=sc, rho=rho):
                        T1, T2, ZR, ZI = WK[b]
                        b_t1, b_t2, b_zr, b_zi = bWK[b]
                        BUR, BUI = BU[b]
                        b_bu = b_bus[b]
                        for (t0, n) in blocks:
                            for ri, dst in ((0, BUR), (1, BUI)):
                                ps, bps = psr.next()
                                k.op("pe", lambda e: e.matmul(ps[:, :n], BT[:, ri, sc, :], U[b][:, t0:t0 + n], start=True, stop=True),
                                     reads=[b_c, b_u[b]], writes=[bps])
                                k.op("act", lambda e: e.activation(dst[:, t0:t0 + n], ps[:, :n], AF.Copy),
                                     reads=[bps], writes=[b_bu])
                            yield
                        k.op("dve", lambda e: e.tensor_tensor(T1[:], ERE[:], BUR[:], ALU.mult), reads=[b_tab, b_bu], writes=[b_t1])
                        k.op("pool", lambda e: e.tensor_tensor(T2[:], EIM[:], BUI[:], ALU.mult), reads=[b_tab, b_bu], writes=[b_t2])
                        yield
                        k.op("dve", lambda e: e.tensor_tensor(ZR[:], T1[:], T2[:], ALU.subtract), reads=[b_t1, b_t2], writes=[b_zr])
                        yield
                        k.op("pool", lambda e: e.tensor_tensor(T1[:], ERE[:], BUI[:], ALU.mult), reads=[b_tab, b_bu, b_zr], writes=[b_t1])
                        k.op("dve", lambda e: e.tensor_tensor(T2[:], EIM[:], BUR[:], ALU.mult), reads=[b_tab, b_bu, b_zr], writes=[b_t2])
                        yield
                        k.op("pool", lambda e: e.tensor_tensor(ZI[:], T1[:], T2[:], ALU.add), reads=[b_t1, b_t2], writes=[b_zi])
                        yield
                        for Z, bz in ((ZR, b_zr), (ZI, b_zi)):
                            if j == 0:
                                k.op("dve", lambda e: e.tensor_tensor_scan(Z[:], rho.to_broadcast([128, TT]), Z[:], 0.0, ALU.mult, ALU.add),
                                     reads=[bz, bd], writes=[bz])
                            else:
                                r0 = rev_ap(Z[:], 0, TC)
                                k.op("dve", lambda e: e.tensor_tensor_scan(r0, rho.to_broadcast([128, TC]), r0, 0.0, ALU.mult, ALU.add),
                                     reads=[bz, bd], writes=[bz])
                                r1 = rev_ap(Z[:], TC, TT)
                                k.op("dve", lambda e: e.tensor_tensor_scan(r1, rho.to_broadcast([128, TL]), r1, Z[:, 0:1], ALU.mult, ALU.add),
                                     reads=[bz, bd], writes=[bz])
                            yield
                        k.op("pool", lambda e: e.tensor_tensor(Q[0][:], CS[:], ZR[:], ALU.mult), reads=[b_tab, b_zr], writes=[b_q[0]])
                        for qi, (tabl, Z, bz) in ((1, (SN, ZI, b_zi)), (2, (SN, ZR, b_zr)), (3, (CS, ZI, b_zi))):
                            k.op("dve", lambda e: e.scalar_tensor_tensor(Q[qi][:], tabl[:], -1.0, Z[:], ALU.mult, ALU.mult),
                                 reads=[b_tab, bz], writes=[b_q[qi]])
                        lhs = (CT[:, j, 0, sc, :], CT[:, j, 0, sc, :], CT[:, j, 1, sc, :], CT[:, j, 1, sc, :])
                        for (t0, n) in blocks:
                            ps, bps = psy.next()
                            for qi in range(4):
                                k.op("pe", lambda e: e.matmul(ps[:, :n], lhs[qi], Q[qi][:, t0:t0 + n], start=(qi == 0), stop=(qi == 3)),
                                     reads=[b_c, b_q[qi]], writes=[bps])
                            k.op("dve", lambda e: e.tensor_tensor(YA[b][:, t0:t0 + n], YA[b][:, t0:t0 + n], ps[:, :n], ALU.add),
                                 reads=[bps, b_ya[b]], writes=[b_ya[b]])
                        yield

                    run_interleaved([body(b_) for b_ in range(NB)])
            for b in range(NB):
                k.op("dve", lambda e: e.scalar_tensor_tensor(TA[:], U[b][:], dsk[:, 0, oc:oc + 1], YA[b][:], ALU.mult, ALU.add),
                     reads=[b_u[b], b_ya[b], b_c, b_ta], writes=[b_ta])
                k.op("act", lambda e: e.activation(TB[:], TA[:], AF.Square), reads=[b_ta, b_tb], writes=[b_tb])
                k.op("dve", lambda e: e.tensor_scalar(TB[:], TB[:], 0.044715, 1.0, ALU.mult, ALU.add), reads=[b_tb], writes=[b_tb])
                k.op("pool", lambda e: e.tensor_tensor(TB[:], TB[:], TA[:], ALU.mult), reads=[b_ta, b_tb], writes=[b_tb])
                k.op("act", lambda e: e.activation(TB[:], TB[:], AF.Sigmoid, scale=1.5957691216057308), reads=[b_tb], writes=[b_tb])
                k.op("dve", lambda e: e.tensor_tensor(Q[b][:], TA[:], TB[:], ALU.mult), reads=[b_ta, b_tb, b_q[b]], writes=[b_q[b]])
                k.dma("sp", YG[b, oc * 128:(oc + 1) * 128, :], Q[b][:], reads=[b_q[b]], writes=[C.bYG])
    with P.scope():
        gw = P.sb("gluw", [128, 4, 512], BF16)
        b_gw = Buf("gluw")
        k.dma("pool", gw[:], C.ssm_glu_w[l].rearrange("(kc p) n -> p kc n", p=128), writes=[b_gw])
        dsk = P.sb("dsk2", [128, 2, 4], F32)
        k.dma("sp", dsk[:], C.ssm_vec[l], writes=[b_gw])
        ygr = Ring(P, "yg", 2, [128, 4, 512], BF16)
        psr = Ring(P, "glups", 4, [128, 512], F32, psum=True)
        sgr = Ring(P, "sg", 3, [128, 512], F32)
        str_ = Ring(P, "gst", 3, [128, 512], BF16)
        for b in range(NB):
            for (t0, n) in blocks:
                yg, byg = ygr.next()
                k.dma("sp", yg[:, :, :n], YG[b, :, t0:t0 + n].rearrange("(c p) n -> p c n", p=128), reads=[C.bYG], writes=[byg])
                for oc in range(4):
                    ps, bps = psr.next()
                    for kc in range(4):
                        k.op("pe", lambda e: e.matmul(ps[:, :n], gw[:, kc, oc * 128:(oc + 1) * 128], yg[:, kc, :n],
                                                      start=(kc == 0), stop=(kc == 3)), reads=[b_gw, byg], writes=[bps])
                    sg, bsg = sgr.next()
                    k.op("act", lambda e: e.activation(sg[:, :n], ps[:, :n], AF.Sigmoid, bias=dsk[:, 1, oc:oc + 1]),
                         reads=[bps, b_gw], writes=[bsg])
                    st, bst = str_.next()
                    k.op("dve", lambda e: e.tensor_tensor(st[:, :n], yg[:, oc, :n], sg[:, :n], ALU.mult), reads=[byg, bsg], writes=[bst])
                    k.dma("sp", YT[b, 0, oc * 128:(oc + 1) * 128, t0:t0 + n], st[:, :n], reads=[bst], writes=[C.bYT])


def phase_merge(P, C, l):
    k = P.k
    PT, YT, XT = C.PT, C.YT, C.XT
    src_x = C.xt0 if l == 0 else XT
    mod, gs = C.mod, C.gs
    blocks = [(i * 256, 256) for i in range(9)]
    with P.scope():
        wb = P.sb("wb", [128, 3, 4, D], BF16)
        b_w = Buf("mw")
        for j in range(3):
            k.dma("pool", wb[:, j], C.w_branch[l, j].rearrange("(kc p) n -> p kc n", p=128), writes=[b_w])
        wo = P.sb("wo", [128, KC, D], BF16)
        k.dma("pool", wo[:], C.w_out[l].rearrange("(kc p) n -> p kc n", p=128), writes=[b_w])
        rw = P.sb("rw", [128, KC, 16], F32)
        k.dma("sp", rw[:], C.router_w[l].rearrange("(kc p) n -> p kc n", p=128), writes=[b_w])
        ident = P.sb("ident", [128, 128], BF16)
        k.dma("pool", ident[:], C.ident, writes=[b_w])
        y3r = Ring(P, "y3", 2, [128, 3, 4, 256], BF16)
        gr = Ring(P, "g", 2, [128, 24, 256], BF16)
        xr = Ring(P, "mx", 2, [128, KC, 256], F32)
        mTr = Ring(P, "mT", 1, [128, KC, 256], BF16)
        mr = Ring(P, "m", 6, [128, 256], F32)
        x1r = Ring(P, "x1", 1, [128, KC, 256], F32)
        sqr = Ring(P, "msq", 1, [128, KC, 256], F32)
        h2fr = Ring(P, "h2f", 1, [128, KC, 256], F32)
        h2br = Ring(P, "h2b", 1, [128, KC, 256], BF16)
        tsr = Ring(P, "tst", 2, [128, D], BF16)
        rsr = Ring(P, "mrs", 2, [128, 256], F32)
        lgr = Ring(P, "lgs", 2, [16, 256], F32)
        psb = Ring(P, "psb", 3, [128, 256], F32, psum=True)
        pso = Ring(P, "pso", 2, [128, 256], F32, psum=True)
        pss = Ring(P, "pss", 2, [128, 256], F32, psum=True)
        pst = Ring(P, "pst", 1, [128, D], BF16, psum=True)
        for b in range(NB):
            for (t0, n) in blocks:
                j = 2 if t0 < TC else b
                y3, by3 = y3r.next()
                k.dma("sp", y3[:], YT[b, :, :, t0:t0 + n].rearrange("j (c p) n -> p j c n", p=128), reads=[C.bYT], writes=[by3])
                g, bg = gr.next()
                k.dma("act", g[:], PT[b, 25 * 128:49 * 128, t0:t0 + n].rearrange("(c p) n -> p c n", p=128), reads=[C.bPT], writes=[bg])
                x, bx = xr.next()
                k.dma("sp", x[:], src_x[b][:, t0:t0 + n].rearrange("(kc p) n -> p kc n", p=128), reads=[C.bXT[b]], writes=[bx])
                mT, bmT = mTr.next()
                for dc in range(KC):
                    ms = []
                    for jj in range(3):
                        ps, bps = psb.next()
                        for kc in range(4):
                            k.op("pe", lambda e: e.matmul(ps[:], wb[:, jj, kc, dc * 128:(dc + 1) * 128], y3[:, jj, kc, :],
                                                          start=(kc == 0), stop=(kc == 3)), reads=[b_w, by3], writes=[bps])
                        m, bm = mr.next()
                        k.op("dve", lambda e: e.tensor_tensor(m[:], ps[:], g[:, jj * 8 + dc, :], ALU.mult), reads=[bps, bg], writes=[bm])
                        ms.append((m, bm))
                    k.op("pool", lambda e: e.tensor_tensor(ms[0][0][:], ms[0][0][:], ms[1][0][:], ALU.add),
                         reads=[ms[0][1], ms[1][1]], writes=[ms[0][1]])
                    k.op("pool", lambda e: e.tensor_tensor(mT[:, dc, :], ms[0][0][:], ms[2][0][:], ALU.add),
                         reads=[ms[0][1], ms[2][1]], writes=[bmT])
                x1, bx1 = x1r.next()
                for dc in range(KC):
                    ps, bps = pso.next()
                    for kc in range(KC):
                        k.op("pe", lambda e: e.matmul(ps[:], wo[:, kc, dc * 128:(dc + 1) * 128], mT[:, kc, :],
                                                      start=(kc == 0), stop=(kc == KC - 1)), reads=[b_w, bmT], writes=[bps])
                    k.op("dve", lambda e: e.scalar_tensor_tensor(x1[:, dc, :], ps[:], mod[:, 2 * 8 + dc, j:j + 1], x[:, dc, :],
                                                                 ALU.mult, ALU.add), reads=[bps, bx, C.b_mod], writes=[bx1])
                k.dma("sp", XT[b][:, t0:t0 + n].rearrange("(kc p) n -> p kc n", p=128), x1[:], reads=[bx1], writes=[C.bXT[b]])
                sq, bsq = sqr.next()
                k.op("act", lambda e: e.activation(sq[:], x1[:], AF.Square), reads=[bx1], writes=[bsq])
                ss, bss = pss.next()
                for kc in range(KC):
                    k.op("pe", lambda e: e.matmul(ss[:], C.ones[:], sq[:, kc, :], start=(kc == 0), stop=(kc == KC - 1)),
                         reads=[bsq, C.b_ones], writes=[bss])
                rs, brs = rsr.next()
                k.op("act", lambda e: e.activation(rs[:], ss[:], AF.Sqrt, bias=C.epsc[:, 0:1], scale=1.0 / D), reads=[bss], writes=[brs])
                k.op("dve", lambda e: e.reciprocal(rs[:], rs[:]), reads=[brs], writes=[brs])
                k.op("dve", lambda e: e.tensor_tensor(sq[:], x1[:], rs[:].unsqueeze(1).to_broadcast([128, KC, n]), ALU.mult),
                     reads=[bx1, brs, bsq], writes=[bsq])
                h2f, bh2f = h2fr.next()
                for kc in range(KC):
                    k.op("act", lambda e: e.activation(h2f[:, kc, :], sq[:, kc, :], AF.Identity,
                                                       bias=mod[:, 3 * 8 + kc, j:j + 1], scale=gs[:, 1, kc, j:j + 1]),
                         reads=[bsq, C.b_mod, C.b_gs], writes=[bh2f])
                h2b, bh2b = h2br.next()
                k.op("pool", lambda e: e.tensor_copy(h2b[:], h2f[:]), reads=[bh2f], writes=[bh2b])
                lp, blp = pss.next()
                for kc in range(KC):
                    k.op("pe", lambda e: e.matmul(lp[:16, :], rw[:, kc, :], h2f[:, kc, :], start=(kc == 0), stop=(kc == KC - 1)),
                         reads=[b_w, bh2f], writes=[blp])
                lg, blg = lgr.next()
                k.op("act", lambda e: e.activation(lg[:], lp[:16, :], AF.Copy), reads=[blp], writes=[blg])
                k.dma("sp", C.LG[b, :, t0:t0 + n], lg[:], reads=[blg], writes=[C.bLG])
                for tt in range(n // 128):
                    pt, bpt = pst.next()
                    for kc in range(KC):
                        k.op("pe", lambda e: e.transpose(pt[:, kc * 128:(kc + 1) * 128], h2b[:, kc, tt * 128:(tt + 1) * 128], ident[:]),
                             reads=[bh2b, b_w], writes=[bpt])
                    ts, bts = tsr.next()
                    k.op("act", lambda e: e.activation(ts[:], pt[:], AF.Copy), reads=[bpt], writes=[bts])
                    k.dma("sp", C.H2[b, t0 + tt * 128:t0 + (tt + 1) * 128, :], ts[:], reads=[bts], writes=[C.bH2])


NE = 16
FF = 1536
CAPL = 256
CAPC = 32
NJ = 2 * CAPL + 2 * CAPC


def phase_moe(P, C, l, last):
    k = P.k
    XT = C.XT
    mod = C.mod
    with P.scope():
        cst = P.sb("moecst", [128, 3, 256], F32)
        b_c = Buf("moecst")
        k.dma("sp", cst[:], C.moe_cst, writes=[b_c])
        A = P.sb("rA", [32, TT], F32)
        AFF = P.sb("rAFF", [32, TT], F32)
        W = P.sb("rW", [32, TT], F32)
        MG = P.sb("rMG", [32, TT], F32)
        MK = P.sb("rMK", [32, TT], F32)
        PS_ = P.sb("rPOS", [32, TT], F32)
        mx = P.sb("rmx", [32, 8], F32)
        bA, bAFF, bW, bMG, bMK, bPOS, bmx = [Buf(x) for x in "A AFF W MG MK POS mx".split()]
        for b in range(NB):
            k.dma("sp", A[b * 16:(b + 1) * 16, :], C.LG[b], reads=[C.bLG], writes=[bA])
        k.op("act", lambda e: e.activation(A[:], A[:], AF.Exp), reads=[bA], writes=[bA])
        psr = Ring(P, "rps", 2, [128, 512], F32, psum=True)
        for t0 in range(0, TT, 512):
            n = min(512, TT - t0)
            ps, bps = psr.next()
            k.op("pe", lambda e: e.matmul(ps[:32, :n], cst[:32, 1, :32], A[:, t0:t0 + n], start=True, stop=True),
                 reads=[bA, b_c], writes=[bps])
            k.op("dve", lambda e: e.reciprocal(W[:, t0:t0 + n], ps[:32, :n]), reads=[bps], writes=[bW])
        k.op("dve", lambda e: e.tensor_tensor(AFF[:], A[:], W[:], ALU.mult), reads=[bA, bW], writes=[bAFF])
        k.op("dve", lambda e: e.tensor_copy(W[:], AFF[:]), reads=[bAFF, bW], writes=[bW])
        for (lo, hi, cap) in ((0, TC, CAPC), (TC, TT, CAPL)):
            for it in range(cap // 8):
                k.op("dve", lambda e: e.max(out=mx[:], in_=W[:, lo:hi]), reads=[bW, bmx], writes=[bmx])
                k.op("dve", lambda e: e.match_replace(out=W[:, lo:hi], in_to_replace=mx[:], in_values=W[:, lo:hi], imm_value=0.0),
                     reads=[bmx, bW], writes=[bW])
        k.op("dve", lambda e: e.tensor_tensor(MG[:], AFF[:], W[:], ALU.subtract), reads=[bAFF, bW], writes=[bMG])
        k.op("dve", lambda e: e.tensor_single_scalar(MK[:], MG[:], 0.0, ALU.is_gt), reads=[bMG], writes=[bMK])
        for (lo, hi) in ((0, TC), (TC, TT)):
            k.op("dve", lambda e: e.tensor_tensor_scan(PS_[:, lo:hi], cst[:32, 0, 0:1].to_broadcast([32, hi - lo]), MK[:, lo:hi],
                                                       0.0, ALU.mult, ALU.add), reads=[bMK, b_c], writes=[bPOS])
        for b in range(NB):
            k.dma("sp", C.POSD[b], PS_[b * 16:(b + 1) * 16, :], reads=[bPOS], writes=[C.bPOSD])
            k.dma("sp", C.MGD[b], MG[b * 16:(b + 1) * 16, :], reads=[bMG], writes=[C.bPOSD])
        posT = P.sb("posT", [128, 18, 32], F32)
        mkT = P.sb("mkT", [128, 18, 32], F32)
        b_pT = Buf("posT")
        for tt in range(18):
            ps, bps = psr.next()
            k.op("pe", lambda e: e.transpose(ps[:, 0:32], PS_[:, tt * 128:(tt + 1) * 128], cst[:32, 2, :32]),
                 reads=[bPOS, b_c], writes=[bps])
            k.op("pe", lambda e: e.transpose(ps[:, 32:64], MK[:, tt * 128:(tt + 1) * 128], cst[:32, 2, :32]),
                 reads=[bMK, b_c], writes=[bps])
            k.op("act", lambda e: e.activation(posT[:, tt, :], ps[:, 0:32], AF.Copy), reads=[bps], writes=[b_pT])
            k.op("act", lambda e: e.activation(mkT[:, tt, :], ps[:, 32:64], AF.Copy), reads=[bps], writes=[b_pT])
        H2s = P.sb("H2s", [128, 18, D], BF16)
        bH = Buf("H2s")
        selr = Ring(P, "sel", 2, [128, 18, 256], BF16)
        gps = Ring(P, "gps", 3, [128, 256], F32, psum=True)
        gpc = Ring(P, "gpc", 2, [128, 32], F32, psum=True)
        xsr = Ring(P, "xs", 2, [128, KC, CAPL + CAPC], BF16)
        for b in range(NB):
            k.dma("sp", H2s[:], C.H2[b].rearrange("(tt p) d -> p tt d", p=128), reads=[C.bH2], writes=[bH])
            for ex in range(NE):
                col = b * 16 + ex
                sel, bsel = selr.next()
                for tt in range(18):
                    ncap = CAPC if tt < 2 else CAPL
                    k.op("dve" if tt % 2 == 0 else "pool", lambda e: e.tensor_scalar(
                        sel[:, tt, :ncap], cst[:, 0, :ncap], posT[:, tt, col:col + 1], mkT[:, tt, col:col + 1],
                        ALU.is_equal, ALU.mult), reads=[b_c, b_pT], writes=[bsel])
                xs, bxs = xsr.next()
                for kc in range(KC):
                    ps, bps = gps.next()
                    for tt in range(16):
                        k.op("pe", lambda e: e.matmul(ps[:], H2s[:, 2 + tt, kc * 128:(kc + 1) * 128], sel[:, 2 + tt, :],
                                                      start=(tt == 0), stop=(tt == 15)), reads=[bH, bsel], writes=[bps])
                    k.op("act" if kc % 2 == 0 else "dve",
                         (lambda e: e.activation(xs[:, kc, :CAPL], ps[:], AF.Copy)) if kc % 2 == 0 else
                         (lambda e: e.tensor_copy(xs[:, kc, :CAPL], ps[:])), reads=[bps], writes=[bxs])
                    pc, bpc = gpc.next()
                    for tt in range(2):
                        k.op("pe", lambda e: e.matmul(pc[:], H2s[:, tt, kc * 128:(kc + 1) * 128], sel[:, tt, :CAPC],
                                                      start=(tt == 0), stop=(tt == 1)), reads=[bH, bsel], writes=[bpc])
                    k.op("act", lambda e: e.activation(xs[:, kc, CAPL:], pc[:], AF.Copy), reads=[bpc], writes=[bxs])
                k.dma("sp", C.XS[ex, :, b * CAPL:(b + 1) * CAPL].rearrange("(kc p) j -> p kc j", p=128), xs[:, :, :CAPL],
                      reads=[bxs], writes=[C.bXS])
                k.dma("sp", C.XS[ex, :, 2 * CAPL + b * CAPC:2 * CAPL + (b + 1) * CAPC].rearrange("(kc p) j -> p kc j", p=128),
                      xs[:, :, CAPL:], reads=[bxs], writes=[C.bXS])
    if "stop_moeA" in P.debug:
        return
    with P.scope():
        w1r = Ring(P, "w1", 2, [128, KC, FF], BF16)
        w3r = Ring(P, "w3", 2, [128, KC, FF], BF16)
        w2r = Ring(P, "w2", 2, [128, 12, D], BF16)
        xsr = Ring(P, "xsb", 2, [128, KC, NJ], BF16)
        hr = Ring(P, "hid", 2, [128, 12, NJ], BF16)
        slr = Ring(P, "silu", 3, [128, 288], F32)
        yer = Ring(P, "ye", 3, [128, D], BF16)
        ps1 = Ring(P, "ps1", 2, [128, 288], F32, psum=True)
        ps3 = Ring(P, "ps3", 2, [128, 288], F32, psum=True)
        psy = Ring(P, "psy", 3, [128, 512], F32, psum=True)
        for ex in range(NE):
            w1, bw1 = w1r.next()
            w3, bw3 = w3r.next()
            w2, bw2 = w2r.next()
            k.dma("pool", w1[:], C.moe_w1[l, ex].rearrange("(kc p) f -> p kc f", p=128), writes=[bw1])
            k.dma("pool", w3[:], C.moe_w3[l, ex].rearrange("(kc p) f -> p kc f", p=128), writes=[bw3])
            k.dma("pool", w2[:], C.moe_w2[l, ex].rearrange("(fc p) d -> p fc d", p=128), writes=[bw2])
            xs, bxs = xsr.next()
            k.dma("sp", xs[:], C.XS[ex].rearrange("(kc p) j -> p kc j", p=128), reads=[C.bXS], writes=[bxs])
            hid, bh = hr.next()
            for fc in range(12):
                for half in range(2):
                    c0 = half * 288
                    p1, bp1 = ps1.next()
                    p3, bp3 = ps3.next()
                    for kc in range(KC):
                        k.op("pe", lambda e: e.matmul(p1[:], w1[:, kc, fc * 128:(fc + 1) * 128], xs[:, kc, c0:c0 + 288],
                                                      start=(kc == 0), stop=(kc == KC - 1)), reads=[bw1, bxs], writes=[bp1])
                    for kc in range(KC):
                        k.op("pe", lambda e: e.matmul(p3[:], w3[:, kc, fc * 128:(fc + 1) * 128], xs[:, kc, c0:c0 + 288],
                                                      start=(kc == 0), stop=(kc == KC - 1)), reads=[bw3, bxs], writes=[bp3])
                    sl, bsl = slr.next()
                    k.op("act", lambda e: e.activation(sl[:], p1[:], AF.Silu), reads=[bp1], writes=[bsl])
                    k.op("dve", lambda e: e.tensor_tensor(hid[:, fc, c0:c0 + 288], sl[:], p3[:], ALU.mult),
                         reads=[bsl, bp3], writes=[bh])
            for jt in range(5):
                nj = 128 if jt < 4 else NJ - 512
                ye, bye = yer.next()
                for dh in range(2):
                    py, bpy = psy.next()
                    for fc in range(12):
                        k.op("pe", lambda e: e.matmul(py[:nj, :], hid[:, fc, jt * 128:jt * 128 + nj], w2[:, fc, dh * 512:(dh + 1) * 512],
                                                      start=(fc == 0), stop=(fc == 11)), reads=[bh, bw2], writes=[bpy])
                    k.op("act" if dh == 0 else "dve",
                         (lambda e: e.activation(ye[:nj, dh * 512:(dh + 1) * 512], py[:nj, :], AF.Copy)) if dh == 0 else
                         (lambda e: e.tensor_copy(ye[:nj, dh * 512:(dh + 1) * 512], py[:nj, :])), reads=[bpy], writes=[bye])
                k.dma("sp", C.YE[ex, jt * 128:jt * 128 + nj, :], ye[:nj, :], reads=[bye], writes=[C.bYE])
    if "stop_moeB" in P.debug:
        return
    with P.scope():
        jcol = P.sb("jcol", [128, 2], F32)
        b_c = Buf("jcol")
        k.dma("sp", jcol[:], C.moe_jcol, writes=[b_c])
        yel = P.sb("yel", [128, NE, 2, D], BF16)
        yec = P.sb("yec", [32, NE, D], BF16)
        b_ye = Buf("yel")
        posb = P.sb("posb", [128, NE, 512], F32)
        mgb = P.sb("mgb", [128, NE, 512], F32)
        b_pb = Buf("posb")
        sgr = Ring(P, "selg", 4, [128, 512], BF16)
        x1r = Ring(P, "cx1", 2, [128, KC, 512], F32)
        pso = Ring(P, "cps", 8, [128, 512], F32, psum=True)
        for b in range(NB):
            for jt in range(2):
                k.dma("sp", yel[:, :, jt, :], C.YE[:, b * CAPL + jt * 128:b * CAPL + (jt + 1) * 128, :].rearrange("e p d -> p e d"),
                      reads=[C.bYE], writes=[b_ye])
            k.dma("sp", yec[:], C.YE[:, 2 * CAPL + b * CAPC:2 * CAPL + (b + 1) * CAPC, :].rearrange("e p d -> p e d"),
                  reads=[C.bYE], writes=[b_ye])
            for (t0, n) in [(0, TC)] + [(TC + i * 512, 512) for i in range(4)]:
                isctx = t0 < TC
                j = 2 if isctx else b
                k.dma("sp", posb[:, :, :n], C.POSD[b, :, t0:t0 + n].partition_broadcast(128), reads=[C.bPOSD], writes=[b_pb])
                k.dma("act", mgb[:, :, :n], C.MGD[b, :, t0:t0 + n].partition_broadcast(128), reads=[C.bPOSD], writes=[b_pb])
                x1, bx1 = x1r.next()
                k.dma("sp", x1[:, :, :n], XT[b][:, t0:t0 + n].rearrange("(kc p) n -> p kc n", p=128), reads=[C.bXT[b]], writes=[bx1])
                acc = [pso.next() for _ in range(KC)]
                njt = 1 if isctx else 2
                for ex in range(NE):
                    for jt in range(njt):
                        sg, bsg = sgr.next()
                        k.op("dve", lambda e: e.scalar_tensor_tensor(sg[:, :n], posb[:, ex, :n], jcol[:, jt:jt + 1], mgb[:, ex, :n],
                                                                     ALU.is_equal, ALU.mult), reads=[b_pb, b_c], writes=[bsg])
                        first = (ex == 0 and jt == 0)
                        lastm = (ex == NE - 1 and jt == njt - 1)
                        for dc in range(KC):
                            pa, bpa = acc[dc]
                            if isctx:
                                k.op("pe", lambda e: e.matmul(pa[:, :n], yec[:, ex, dc * 128:(dc + 1) * 128], sg[:32, :n],
                                                              start=first, stop=lastm), reads=[b_ye, bsg], writes=[bpa])
                            else:
                                k.op("pe", lambda e: e.matmul(pa[:, :n], yel[:, ex, jt, dc * 128:(dc + 1) * 128], sg[:, :n],
                                                              start=first, stop=lastm), reads=[b_ye, bsg], writes=[bpa])
                for dc in range(KC):
                    pa, bpa = acc[dc]
                    k.op("dve", lambda e: e.scalar_tensor_tensor(x1[:, dc, :n], pa[:, :n], mod[:, 5 * 8 + dc, j:j + 1], x1[:, dc, :n],
                                                                 ALU.mult, ALU.add), reads=[bpa, bx1, C.b_mod], writes=[bx1])
                if last:
                    if not isctx:
                        k.dma("sp", C.OUT[b][:, t0 - TC:t0 - TC + n].rearrange("(kc p) n -> p kc n", p=128), x1[:, :, :n],
                              reads=[bx1], writes=[C.bOUT])
                else:
                    k.dma("sp", XT[b][:, t0:t0 + n].rearrange("(kc p) n -> p kc n", p=128), x1[:, :, :n],
                          reads=[bx1], writes=[C.bXT[b]])


LAM = float(np.exp(-0.5))
GN_EPS = 64e-5
NCK = TT // 128


def phase_rwkv_prep(P, C, l):
    k = P.k
    PT = C.PT
    blocks = [(0, TC)] + [(TC + i * 512, 512) for i in range(4)]
    with P.scope():
        vec = P.sb("rwvec", [128, 51], F32)
        b_c = Buf("rwc")
        k.dma("sp", vec[:], C.rw_vec[l], writes=[b_c])
        MU, W0, A0, KK_, KA, RK = 0, 15, 23, 31, 35, 39
        der = P.sb("rwder", [128, 15 + 15 + 4], F32)
        k.op("dve", lambda e: e.tensor_scalar(der[:, 0:15], vec[:, MU:MU + 15], -1.0, 1.0, ALU.mult, ALU.add), reads=[b_c], writes=[b_c])
        k.op("dve", lambda e: e.tensor_scalar(der[:, 15:30], vec[:, MU:MU + 15], 0.5, None, ALU.mult), reads=[b_c], writes=[b_c])
        k.op("dve", lambda e: e.tensor_scalar(der[:, 30:34], vec[:, KA:KA + 4], -1.0, 1.0, ALU.mult, ALU.add), reads=[b_c], writes=[b_c])
        tiny = P.sb("rwtiny", [128, 1], F32)
        k.op("dve", lambda e: e.memset(tiny[:], 1e-12), writes=[b_c])
        w2 = P.sb("rw_w2", [128, 512], BF16)
        a2 = P.sb("rw_a2", [128, 512], BF16)
        g2 = P.sb("rw_g2", [128, 512], BF16)
        k.dma("pool", w2[:], C.rwkv_w2[l].rearrange("j l c -> (j l) c"), writes=[b_c])
        k.dma("pool", a2[:], C.rwkv_a2[l].rearrange("j l c -> (j l) c"), writes=[b_c])
        k.dma("pool", g2[:], C.rwkv_g2[l], writes=[b_c])
        bd = P.sb("rw_bd", [128, 128], BF16)
        k.dma("pool", bd[:], C.rw_bd, writes=[b_c])
        rst = P.sb("rw_rst", [128, TT + 1], F32)
        k.dma("sp", rst[:], C.rw_rst, writes=[b_c])
        big = lambda n_, dt=F32: P.sb(n_, [128, TT], dt)
        X = big("rX", BF16)
        S = big("rS")
        KP = [big(f"rKP{i}", BF16) for i in range(4)]
        KAP = [big(f"rKAP{i}", BF16) for i in range(4)]
        KS = big("rKSUM")
        bKS = Buf("KS")
        RP = [big(f"rRP{i}", BF16) for i in range(4)]
        VP = [big(f"rVP{i}", BF16) for i in range(4)]
        TW, PA, SG = big("rTW", BF16), big("rPA", BF16), big("rSG", BF16)
        T1, T2, T3, T4 = big("rT1"), big("rT2"), big("rT3"), big("rT4")
        O = [big(f"rO{i}", BF16) for i in range(4)]
        bX, bS, bT1, bT2, bT3, bT4 = [Buf(x) for x in "X S T1 T2 T3 T4".split()]
        bKP = [Buf(f"KP{i}") for i in range(4)]
        bKAP = [Buf(f"KAP{i}") for i in range(4)]
        bRP = [Buf(f"RP{i}") for i in range(4)]
        bVP = [Buf(f"VP{i}") for i in range(4)]
        bTW, bPA, bSG = Buf("TW"), Buf("PA"), Buf("SG")
        bO = [Buf(f"O{i}") for i in range(4)]
        psr = Ring(P, "rwps", 4, [128, 512], F32, psum=True)
        stg = Ring(P, "rwstg", 3, [128, 512], BF16)
        plr = Ring(P, "rwpl", 2, [128, NCK], F32)
        for b in range(NB):
            for ci in range(15):
                k.dma("sp", X[:], PT[b, (4 + ci) * 128:(5 + ci) * 128, :], reads=[C.bPT], writes=[bX])
                k.op("pool", lambda e: e.tensor_tensor(S[:, 1:TT - 1], X[:, 0:TT - 2], X[:, 2:TT], ALU.add), reads=[bX], writes=[bS])
                for (d_, s_) in ((0, 1), (TC - 1, TC - 2), (TC, TC + 1), (TT - 1, TT - 2)):
                    k.op("pool", lambda e: e.tensor_copy(S[:, d_:d_ + 1], X[:, s_:s_ + 1]), reads=[bX, bS], writes=[bS])
                k.op("dve", lambda e: e.tensor_scalar(T1[:], X[:], der[:, ci:ci + 1], None, ALU.mult), reads=[bX, b_c], writes=[bT1])
                if ci < 4:
                    dst, bdst = RP[ci], bRP[ci]
                elif ci < 8:
                    dst, bdst = KP[ci - 4], bKP[ci - 4]
                elif ci < 12:
                    dst, bdst = VP[ci - 8], bVP[ci - 8]
                else:
                    dst, bdst = T2, bT2
                k.op("dve", lambda e: e.scalar_tensor_tensor(dst[:], S[:], der[:, 15 + ci:16 + ci], T1[:], ALU.mult, ALU.add),
                     reads=[bS, bT1, b_c], writes=[bdst])
                if ci == 12:
                    k.op("act", lambda e: e.activation(TW[:], T2[:], AF.Tanh), reads=[bT2], writes=[bTW])
                elif ci == 13:
                    k.op("act", lambda e: e.activation(PA[:], T2[:], AF.Copy), reads=[bT2], writes=[bPA])
                elif ci == 14:
                    k.op("act", lambda e: e.activation(SG[:], T2[:], AF.Sigmoid), reads=[bT2], writes=[bSG])
            for cc in range(4):
                k.dma("sp", C.RWV[b, cc * 128:(cc + 1) * 128, :], VP[cc][:], reads=[bVP[cc]], writes=[C.bRW])
            for cc in range(4):
                for (t0, n) in blocks:
                    ps, bps = psr.next()
                    k.op("pe", lambda e: e.matmul(ps[:, :n], g2[:, cc * 128:(cc + 1) * 128], SG[:, t0:t0 + n], start=True, stop=True),
                         reads=[bSG, b_c], writes=[bps])
                    st, bst = stg.next()
                    k.op("act", lambda e: e.activation(st[:, :n], ps[:, :n], AF.Copy), reads=[bps], writes=[bst])
                    k.dma("sp", C.RWG[b, cc * 128:(cc + 1) * 128, t0:t0 + n], st[:, :n], reads=[bst], writes=[C.bRW])
            for cc in range(4):
                k.op("dve", lambda e: e.tensor_scalar(T1[:], KP[cc][:], vec[:, KK_ + cc:KK_ + cc + 1], None, ALU.mult),
                     reads=[bKP[cc], b_c, bT1], writes=[bT1])
                k.op("act", lambda e: e.activation(O[0][:], T1[:], AF.Square), reads=[bT1, bO[0]], writes=[bO[0]])
                for (t0, n) in blocks:
                    ps, bps = psr.next()
                    k.op("pe", lambda e: e.matmul(ps[:, :n], bd[:], O[0][:, t0:t0 + n], start=True, stop=True),
                         reads=[bO[0], b_c], writes=[bps])
                    k.op("act", lambda e: e.activation(T2[:, t0:t0 + n], ps[:, :n], AF.Sqrt, bias=tiny[:, 0:1]), reads=[bps, bT2, b_c], writes=[bT2])
                k.op("dve", lambda e: e.reciprocal(T2[:], T2[:]), reads=[bT2], writes=[bT2])
                k.op("dve", lambda e: e.tensor_tensor(KAP[cc][:], T1[:], T2[:], ALU.mult), reads=[bT1, bT2], writes=[bKAP[cc]])
            for cc in range(4):
                for j in range(2):
                    jr = slice(j * 64, (j + 1) * 64)
                    for (t0, n) in blocks:
                        ps, bps = psr.next()
                        k.op("pe", lambda e: e.matmul(ps[:, :n], w2[jr, cc * 128:(cc + 1) * 128], TW[jr, t0:t0 + n], start=True, stop=True),
                             reads=[bTW, b_c], writes=[bps])
                        k.op("act", lambda e: e.activation(T1[:, t0:t0 + n], ps[:, :n], AF.Sigmoid, bias=vec[:, W0 + j * 4 + cc:W0 + j * 4 + cc + 1]),
                             reads=[bps, b_c, bT1], writes=[bT1])
                        ps2, bps2 = psr.next()
                        k.op("pe", lambda e: e.matmul(ps2[:, :n], a2[jr, cc * 128:(cc + 1) * 128], PA[jr, t0:t0 + n], start=True, stop=True),
                             reads=[bPA, b_c], writes=[bps2])
                        k.op("act", lambda e: e.activation(T2[:, t0:t0 + n], ps2[:, :n], AF.Sigmoid, bias=vec[:, A0 + j * 4 + cc:A0 + j * 4 + cc + 1]),
                             reads=[bps2, b_c, bT2], writes=[bT2])
                    if j == 0:
                        k.op("dve", lambda e: e.tensor_tensor_scan(T3[:], rst[:, 0:TT], T1[:], 0.0, ALU.mult, ALU.add),
                             reads=[bT1, b_c, bT3], writes=[bT3])
                    else:
                        k.op("dve", lambda e: e.tensor_tensor_scan(rev_ap(T3[:], 0, TT), rev_ap(rst[:], 1, TT + 1), rev_ap(T1[:], 0, TT),
                                                                   0.0, ALU.mult, ALU.add), reads=[bT1, b_c, bT3], writes=[bT3])
                    k.op("dve", lambda e: e.tensor_scalar(T4[:], T2[:], vec[:, KA + cc:KA + cc + 1], der[:, 30 + cc:31 + cc], ALU.mult, ALU.add),
                         reads=[bT2, b_c, bT4], writes=[bT4])
                    k.op("pool", lambda e: e.tensor_tensor(T4[:], T4[:], KP[cc][:], ALU.mult), reads=[bT4, bKP[cc]], writes=[bT4])
                    k.op("pool", lambda e: e.tensor_tensor(T2[:], T2[:], KAP[cc][:], ALU.mult), reads=[bT2, bKAP[cc]], writes=[bT2])
                    k.op("pool", lambda e: e.tensor_tensor(T1[:], T3[:], T1[:], ALU.subtract), reads=[bT1, bT3], writes=[bT1])
                    k.op("act", lambda e: e.activation(T1[:], T1[:], AF.Exp, scale=-LAM), reads=[bT1], writes=[bT1])
                    k.op("act", lambda e: e.activation(S[:], T3[:], AF.Exp, scale=LAM), reads=[bT3, bS], writes=[bS])
                    k.op("act", lambda e: e.activation(T3[:], T3[:], AF.Exp, scale=-LAM), reads=[bT3], writes=[bT3])
                    pl, bpl = plr.next()
                    off = 127 if j == 0 else 0
                    k.op("dve", lambda e: e.tensor_copy(pl[:], T3[:, off:TT:128]), reads=[bT3], writes=[bpl])
                    k.dma("sp", C.RWPL[b, j, cc * 128:(cc + 1) * 128, :], pl[:], reads=[bpl], writes=[C.bRW])
                    k.op("dve", lambda e: e.tensor_tensor(O[0][:], RP[cc][:], T3[:], ALU.mult), reads=[bRP[cc], bT3, bO[0]], writes=[bO[0]])
                    k.op("pool", lambda e: e.tensor_tensor(O[1][:], KAP[cc][:], T1[:], ALU.mult), reads=[bKAP[cc], bT1, bO[1]], writes=[bO[1]])
                    k.op("dve", lambda e: e.tensor_tensor(O[2][:], T4[:], S[:], ALU.mult), reads=[bT4, bS, bO[2]], writes=[bO[2]])
                    k.op("pool", lambda e: e.tensor_tensor(O[3][:], T2[:], S[:], ALU.mult), reads=[bT2, bS, bO[3]], writes=[bO[3]])
                    for q in range(4):
                        k.dma("sp" if q % 2 == 0 else "act", C.RWT[b, j, q, cc * 128:(cc + 1) * 128, :], O[q][:], reads=[bO[q]], writes=[C.bRW])
                    if j == 0:
                        k.op("dve", lambda e: e.tensor_copy(KS[:], T4[:]), reads=[bT4, bKS], writes=[bKS])
                    else:
                        k.op("dve", lambda e: e.tensor_tensor(KS[:], KS[:], T4[:], ALU.add), reads=[bT4, bKS], writes=[bKS])
                k.op("dve", lambda e: e.scalar_tensor_tensor(KS[:], KS[:], vec[:, RK + cc:RK + cc + 1], RP[cc][:], ALU.mult, ALU.mult),
                     reads=[bKS, bRP[cc], b_c], writes=[bKS])
                k.op("act", lambda e: e.activation(O[0][:], KS[:], AF.Copy), reads=[bKS, bO[0]], writes=[bO[0]])
                for (t0, n) in blocks:
                    ps, bps = psr.next()
                    k.op("pe", lambda e: e.matmul(ps[:, :n], bd[:], O[0][:, t0:t0 + n], start=True, stop=True), reads=[bO[0], b_c], writes=[bps])
                    k.op("dve", lambda e: e.tensor_tensor(T1[:, t0:t0 + n], ps[:, :n], VP[cc][:, t0:t0 + n], ALU.mult),
                         reads=[bps, bVP[cc], bT1], writes=[bT1])
                k.dma("sp", C.RWB[b, cc * 128:(cc + 1) * 128, :], T1[:], reads=[bT1], writes=[C.bRW])


def run_interleaved(gens):
    gens = list(gens)
    while gens:
        for g_ in list(gens):
            try:
                next(g_)
            except StopIteration:
                gens.remove(g_)


def phase_rwkv_scan(P, C, l):
    k = P.k
    YT = C.YT
    with P.scope():
        masks = P.sb("rwmask", [128, 2, 640], BF16)
        b_c = Buf("rwc2")
        k.dma("pool", masks[:], C.rw_masks, writes=[b_c])
        ident = P.sb("rwident", [128, 128], BF16)
        k.dma("pool", ident[:], C.ident, writes=[b_c])
        identf = P.sb("rwidentf", [128, 128], F32)
        k.dma("sp", identf[:], C.ident, writes=[b_c])
        bdm = P.sb("rwbdm", [128, 128], F32)
        k.dma("sp", bdm[:], C.rw_bd, writes=[b_c])
        vec = P.sb("rwvec2", [128, 51], F32)
        k.dma("sp", vec[:], C.rw_vec[l], writes=[b_c])
        lmf = P.sb("rwlm", [128, 4, 128], F32)
        k.dma("sp", lmf[:], C.rw_lm, writes=[b_c])
        gne = P.sb("gne", [128, 1], F32)
        k.op("dve", lambda e: e.memset(gne[:], GN_EPS), writes=[b_c])
        Ytok = P.sb("Ytok", [128, NCK, 512], F32)
        for b in range(NB):
            bY = [[Buf(f"Y{c}_{hp}") for hp in range(4)] for c in range(NCK)]
            with P.scope():
                KR = P.sb("KR", [128, NCK, 2, 128], BF16)
                KF = P.sb("KF", [128, TT], BF16)
                BF_ = P.sb("BF", [128, TT], BF16)
                VF = P.sb("VF", [128, TT], BF16)
                PLt = P.sb("PLt", [128, NCK], F32)
                TOK = P.sb("TOK", [128, NCK, 3, 128], BF16)
                SC = P.sb("SC", [128, NCK, 2, 512], BF16)
                Tt = P.sb("Tt", [128, NCK * 2, 128], BF16)
                SC36 = SC[:].rearrange("p c h x -> p (c h) x")
                NSLOT = 2
                NFs = [Ring(P, f"NF{i}", 1, [128, 4, 2, 128], F32) for i in range(NSLOT)]
                F4s = [Ring(P, f"F4{i}", 4, [128, 4, 128], F32) for i in range(NSLOT)]
                FTs = [Ring(P, f"FT{i}", 2, [128, 4, 128], F32) for i in range(NSLOT)]
                B4s = [Ring(P, f"B4{i}", 8, [128, 4, 128], BF16) for i in range(NSLOT)]
                H = P.sb("Hst", [128, 128], F32)
                Ht = P.sb("Htmp", [128, 128], F32)
                Hb = P.sb("Hb", [128, 128], BF16)
                Wr = Ring(P, "Wsb", 2, [128, 128], BF16)
                Ur = Ring(P, "Un", 2, [128, 128], BF16)
                psT = P.ps("pstr", [128, 3, 128], BF16)
                bpsT = Buf("pstr")
                psL = Ring(P, "psL", 5, [128, 512], F32, psum=True)
                psS = Ring(P, "psS", 2, [128, 128], F32, psum=True)
                b_in, b_tok, b_sc, b_tt, b_H = Buf("in"), Buf("tok"), Buf("sc"), Buf("tt"), Buf("H")
                for j in ([0] if "rw_j0" in P.debug else [1] if "rw_j1" in P.debug else [0, 1]):
                    for hp in range(4):
                        rows = slice(hp * 128, (hp + 1) * 128)
                        k.dma("sp", KR[:, :, 0, :], C.RWT[b, j, 1, rows, :].rearrange("p (c t) -> p c t", t=128), reads=[C.bRW], writes=[b_in])
                        k.dma("act", KR[:, :, 1, :], C.RWT[b, j, 0, rows, :].rearrange("p (c t) -> p c t", t=128), reads=[C.bRW], writes=[b_in])
                        k.dma("sp", KF[:], C.RWT[b, j, 2, rows, :], reads=[C.bRW], writes=[b_in])
                        k.dma("act", BF_[:], C.RWT[b, j, 3, rows, :], reads=[C.bRW], writes=[b_in])
                        k.dma("sp", VF[:], C.RWV[b, rows, :], reads=[C.bRW], writes=[b_in])
                        k.dma("sp", PLt[:], C.RWPL[b, j, rows, :], reads=[C.bRW], writes=[b_in])
                        for c in range(NCK):
                            cs = slice(c * 128, (c + 1) * 128)
                            for q, src in enumerate((KF, BF_, VF)):
                                k.op("pe", lambda e: e.transpose(psT[:, q, :], src[:, cs], ident[:]), reads=[b_in, b_c], writes=[bpsT])
                            k.op("act", lambda e: e.activation(TOK[:, c], psT[:], AF.Copy), reads=[bpsT], writes=[b_tok])
                        def inv_group(g, slot, j=j):
                            NFr, F4, B4, FT = NFs[slot], F4s[slot], B4s[slot], FTs[slot]
                            NF, bNF = NFr.next()
                            for i in range(4):
                                c, h = 2 * g + i // 2, i % 2
                                cs = slice(c * 128, (c + 1) * 128)
                                hr = slice(h * 64, (h + 1) * 64)
                                kr2 = KR[hr, c].rearrange("p x t -> p (x t)")
                                pX, bpX = psL.next()
                                k.op("pe", lambda e: e.matmul(pX[:, 0:256], KF[hr, cs], kr2, start=True, stop=True), reads=[b_in], writes=[bpX])
                                k.op("pe", lambda e: e.matmul(pX[:, 256:512], BF_[hr, cs], kr2, start=True, stop=True), reads=[b_in], writes=[bpX])
                                pY, bpY = psS.next()
                                k.op("pe", lambda e: e.matmul(pY[:], KR[hr, c, 0, :], BF_[hr, cs], start=True, stop=True), reads=[b_in], writes=[bpY])
                                k.op("dve", lambda e: e.tensor_tensor(SC[:, c, h, :], pX[:], masks[:, j, 0:512], ALU.mult), reads=[bpX, b_c], writes=[b_sc])
                                k.op("dve", lambda e: e.tensor_tensor(NF[:, i, 1, :], pX[:, 256:384], masks[:, j, 256:384], ALU.mult), reads=[bpX, b_c], writes=[bNF])
                                k.op("dve", lambda e: e.tensor_tensor(NF[:, i, 0, :], pY[:], masks[:, j, 512:640], ALU.mult), reads=[bpY, b_c], writes=[bNF])
                            yield
                            bl = slice(g * 4, (g + 1) * 4)
                            bc4 = lambda m_: m_.unsqueeze(1).to_broadcast([128, 4, 128])
                            Mk, bMk = F4.next()
                            Mtk, bMtk = F4.next()
                            Tf, bTf = FT.next()
                            Ttf, bTtf = FT.next()
                            k.op("pool", lambda e: e.tensor_tensor(Mk[:], NF[:, :, 0, :], bc4(lmf[:, 0, :]), ALU.mult), reads=[bNF, b_c], writes=[bMk])
                            k.op("pool", lambda e: e.tensor_tensor(Mtk[:], NF[:, :, 1, :], bc4(lmf[:, 0, :]), ALU.mult), reads=[bNF, b_c], writes=[bMtk])
                            k.op("pool", lambda e: e.tensor_tensor(Tf[:], Mk[:], bc4(identf[:]), ALU.add), reads=[bMk, b_c], writes=[bTf])
                            k.op("pool", lambda e: e.tensor_tensor(Ttf[:], Mtk[:], bc4(identf[:]), ALU.add), reads=[bMtk, b_c], writes=[bTtf])
                            yield
                            for lev in range(1, 4):
                                M2, bM2 = F4.next()
                                Mt2, bMt2 = F4.next()
                                p1, bp1 = psL.next()
                                p2, bp2 = psL.next()
                                for i in range(4):
                                    k.op("pe", lambda e: e.matmul(p1[:, i * 128:(i + 1) * 128], Mk[:, i, :], Mtk[:, i, :], start=True, stop=True),
                                         reads=[bMk, bMtk], writes=[bp1])
                                    k.op("pe", lambda e: e.matmul(p2[:, i * 128:(i + 1) * 128], Mtk[:, i, :], Mk[:, i, :], start=True, stop=True),
                                         reads=[bMk, bMtk], writes=[bp2])
                                yield
                                k.op("act", lambda e: e.activation(Mt2[:].rearrange("p a b -> p (a b)"), p1[:], AF.Copy), reads=[bp1], writes=[bMt2])
                                k.op("dve", lambda e: e.tensor_copy(M2[:].rearrange("p a b -> p (a b)"), p2[:]), reads=[bp2], writes=[bM2])
                                yield
                                p3, bp3 = psL.next()
                                p4, bp4 = psL.next()
                                for i in range(4):
                                    k.op("pe", lambda e: e.matmul(p3[:, i * 128:(i + 1) * 128], Mt2[:, i, :], Tf[:, i, :], start=True, stop=True),
                                         reads=[bMt2, bTf], writes=[bp3])
                                    k.op("pe", lambda e: e.matmul(p4[:, i * 128:(i + 1) * 128], M2[:, i, :], Ttf[:, i, :], start=True, stop=True),
                                         reads=[bM2, bTtf], writes=[bp4])
                                yield
                                k.op("dve", lambda e: e.tensor_tensor(Tf[:].rearrange("p a b -> p (a b)"), Tf[:].rearrange("p a b -> p (a b)"), p3[:], ALU.add),
                                     reads=[bp3, bTf], writes=[bTf])
                                k.op("dve", lambda e: e.tensor_tensor(Ttf[:].rearrange("p a b -> p (a b)"), Ttf[:].rearrange("p a b -> p (a b)"), p4[:], ALU.add),
                                     reads=[bp4, bTtf], writes=[bTtf])
                                Mk, bMk, Mtk, bMtk = M2, bM2, Mt2, bMt2
                                yield
                            Tb, bTb = B4.next()
                            Ttb, bTtb = B4.next()
                            k.op("act", lambda e: e.activation(Tb[:], Tf[:], AF.Copy), reads=[bTf], writes=[bTb])
                            k.op("act", lambda e: e.activation(Ttb[:], Ttf[:], AF.Copy), reads=[bTtf], writes=[bTtb])
                            for li in range(1, 4):
                                lastl = (li == 3)
                                Cm, bCm = B4.next()
                                k.op("pool", lambda e: e.tensor_tensor(Cm[:], NF[:, :, 0, :], bc4(lmf[:, li, :]), ALU.mult), reads=[bNF, b_c], writes=[bCm])
                                p2, bp2 = psL.next()
                                for i in range(4):
                                    k.op("pe", lambda e: e.matmul(p2[:, i * 128:(i + 1) * 128], Cm[:, i, :], Ttb[:, i, :], start=True, stop=True),
                                         reads=[bCm, bTtb], writes=[bp2])
                                yield
                                Z2, bZ2 = B4.next()
                                k.op("act", lambda e: e.activation(Z2[:].rearrange("p a b -> p (a b)"), p2[:], AF.Copy), reads=[bp2], writes=[bZ2])
                                if not lastl:
                                    Cmt, bCmt = B4.next()
                                    k.op("pool", lambda e: e.tensor_tensor(Cmt[:], NF[:, :, 1, :], bc4(lmf[:, li, :]), ALU.mult), reads=[bNF, b_c], writes=[bCmt])
                                    p1, bp1 = psL.next()
                                    for i in range(4):
                                        k.op("pe", lambda e: e.matmul(p1[:, i * 128:(i + 1) * 128], Cmt[:, i, :], Tb[:, i, :], start=True, stop=True),
                                             reads=[bCmt, bTb], writes=[bp1])
                                    Z1, bZ1 = B4.next()
                                    k.op("dve", lambda e: e.tensor_copy(Z1[:].rearrange("p a b -> p (a b)"), p1[:]), reads=[bp1], writes=[bZ1])
                                yield
                                p4, bp4 = psL.next()
                                for i in range(4):
                                    k.op("pe", lambda e: e.matmul(p4[:, i * 128:(i + 1) * 128], Tb[:, i, :], Z2[:, i, :], start=True, stop=True),
                                         reads=[bTb, bZ2], writes=[bp4])
                                if not lastl:
                                    p3, bp3 = psL.next()
                                    for i in range(4):
                                        k.op("pe", lambda e: e.matmul(p3[:, i * 128:(i + 1) * 128], Ttb[:, i, :], Z1[:, i, :], start=True, stop=True),
                                             reads=[bTtb, bZ1], writes=[bp3])
                                    yield
                                    Tn, bTn = B4.next()
                                    Ttn, bTtn = B4.next()
                                    k.op("dve", lambda e: e.tensor_tensor(Tn[:].rearrange("p a b -> p (a b)"), Tb[:].rearrange("p a b -> p (a b)"), p3[:], ALU.add),
                                         reads=[bp3, bTb], writes=[bTn])
                                    k.op("dve", lambda e: e.tensor_tensor(Ttn[:].rearrange("p a b -> p (a b)"), Ttb[:].rearrange("p a b -> p (a b)"), p4[:], ALU.add),
                                         reads=[bp4, bTtb], writes=[bTtn])
                                    Tb, bTb, Ttb, bTtb = Tn, bTn, Ttn, bTtn
                                else:
                                    yield
                                    k.op("dve", lambda e: e.tensor_tensor(Tt[:, bl, :], Ttb[:], p4[:].rearrange("p (a b) -> p a b", b=128), ALU.add),
                                         reads=[bp4, bTtb, b_tt], writes=[b_tt])
                        for w0 in range(0, NCK // 2, NSLOT):
                            run_interleaved([inv_group(g, i) for i, g in enumerate(range(w0, min(w0 + NSLOT, NCK // 2)))])
                        k.op("pool", lambda e: e.memset(H[:], 0.0), reads=[b_H], writes=[b_H])
                        k.op("pool", lambda e: e.memset(Hb[:], 0.0), reads=[b_H], writes=[b_H])
                        order = list(range(NCK)) if j == 0 else [1, 0] + list(range(NCK - 1, 1, -1))
                        for c in order:
                            pW, bpW = psS.next()
                            k.op("pe", lambda e: e.matmul(pW[:], KR[:, c, 0, :], Hb[:], start=True, stop=False, skip_group_check=True),
                                 reads=[b_in, b_H], writes=[bpW])
                            for h in range(2):
                                hc = slice(h * 64, (h + 1) * 64)
                                k.op("pe", lambda e: e.matmul(pW[:, hc], SC[:, c, h, 0:128], TOK[:, c, 2, hc], start=False, stop=True, skip_group_check=True),
                                     reads=[b_sc, b_tok], writes=[bpW])
                            Wsb, bW = Wr.next()
                            k.op("act", lambda e: e.activation(Wsb[:], pW[:], AF.Copy), reads=[bpW], writes=[bW])
                            pU, bpU = psS.next()
                            for h in range(2):
                                hc = slice(h * 64, (h + 1) * 64)
                                k.op("pe", lambda e: e.matmul(pU[:, hc], Tt[:, c * 2 + h, :], Wsb[:, hc], start=True, stop=True),
                                     reads=[b_tt, bW], writes=[bpU])
                            Un, bUn = Ur.next()
                            k.op("act", lambda e: e.activation(Un[:], pU[:], AF.Copy, scale=-1.0), reads=[bpU], writes=[bUn])
                            pYy, bpYy = psS.next()
                            k.op("pe", lambda e: e.matmul(pYy[:], KR[:, c, 1, :], Hb[:], start=True, stop=False, skip_group_check=True),
                                 reads=[b_in, b_H], writes=[bpYy])
                            for h in range(2):
                                hc = slice(h * 64, (h + 1) * 64)
                                k.op("pe", lambda e: e.matmul(pYy[:, hc], SC[:, c, h, 128:256], TOK[:, c, 2, hc], start=False, stop=False, skip_group_check=True),
                                     reads=[b_sc, b_tok], writes=[bpYy])
                                k.op("pe", lambda e: e.matmul(pYy[:, hc], SC[:, c, h, 384:512], Un[:, hc], start=False, stop=True, skip_group_check=True),
                                     reads=[b_sc, bUn], writes=[bpYy])
                            ysl = Ytok[:, c, hp * 128:(hp + 1) * 128]
                            if j == 0 or "rw_j1" in P.debug:
                                k.op("act", lambda e: e.activation(ysl, pYy[:], AF.Copy), reads=[bpYy], writes=[bY[c][hp]])
                            else:
                                k.op("dve", lambda e: e.tensor_tensor(ysl, ysl, pYy[:], ALU.add), reads=[bpYy, bY[c][hp]], writes=[bY[c][hp]])
                            pH, bpH = psS.next()
                            k.op("pe", lambda e: e.matmul(pH[:], TOK[:, c, 0, :], TOK[:, c, 2, :], start=True, stop=False), reads=[b_tok], writes=[bpH])
                            k.op("pe", lambda e: e.matmul(pH[:], TOK[:, c, 1, :], Un[:], start=False, stop=True), reads=[b_tok, bUn], writes=[bpH])
                            k.op("dve", lambda e: e.tensor_tensor(Ht[:], H[:], pH[:], ALU.add), reads=[bpH, b_H], writes=[b_H])
                            k.op("dve", lambda e: e.scalar_tensor_tensor(H[:], Ht[:], PLt[:, c:c + 1], bdm[:], ALU.mult, ALU.mult),
                                 reads=[b_H, b_in, b_c], writes=[b_H])
                            k.op("act", lambda e: e.activation(Hb[:], H[:], AF.Copy), reads=[b_H], writes=[b_H])
            if "YTOK" in P.debug:
                k.dma("sp", C.YTOK[b], Ytok[:], reads=[x for row in bY for x in row], writes=[C.bRW])
            with P.scope():
                BON = P.sb("BON", [128, 4, TT], F32)
                G = P.sb("G", [128, 4, TT], BF16)
                b_l = Buf("rdl")
                k.dma("sp", BON[:], C.RWB[b].rearrange("(c p) t -> p c t", p=128), reads=[C.bRW], writes=[b_l])
                k.dma("act", G[:], C.RWG[b].rearrange("(c p) t -> p c t", p=128), reads=[C.bRW], writes=[b_l])
                st8 = Ring(P, "st8", 2, [128, 6, 8], F32)
                ysq = Ring(P, "ysq", 2, [128, 512], F32)
                ynr = Ring(P, "yn", 2, [128, 512], F32)
                psR = Ring(P, "psR", 2, [128, 4, 128], F32, psum=True)
                ofr = Ring(P, "of", 3, [128, 128], F32)
                obr = Ring(P, "ob", 3, [128, 128], BF16)
                for c in range(NCK):
                    allY = bY[c]
                    y = Ytok[:, c, :]
                    y3 = y.rearrange("p (h x) -> p h x", x=64)
                    s, bs = st8.next()
                    k.op("dve", lambda e: e.reduce_sum(s[:, 0, :], y3, AX.X), reads=allY, writes=[bs])
                    q, bq = ysq.next()
                    k.op("pool", lambda e: e.tensor_tensor(q[:], y, y, ALU.mult), reads=allY, writes=[bq])
                    k.op("dve", lambda e: e.reduce_sum(s[:, 1, :], q[:].rearrange("p (h x) -> p h x", x=64), AX.X), reads=[bq, bs], writes=[bs])
                    k.op("dve", lambda e: e.tensor_scalar(s[:, 2, :], s[:, 0, :], 1.0 / 64.0, None, ALU.mult), reads=[bs], writes=[bs])
                    k.op("dve", lambda e: e.tensor_tensor(s[:, 3, :], s[:, 2, :], s[:, 2, :], ALU.mult), reads=[bs], writes=[bs])
                    k.op("dve", lambda e: e.scalar_tensor_tensor(s[:, 4, :], s[:, 1, :], 1.0 / 64.0, s[:, 3, :], ALU.mult, ALU.subtract),
                         reads=[bs], writes=[bs])
                    k.op("act", lambda e: e.activation(s[:, 5, :], s[:, 4, :], AF.Sqrt, bias=gne[:, 0:1]), reads=[bs, b_c], writes=[bs])
                    k.op("dve", lambda e: e.reciprocal(s[:, 5, :], s[:, 5, :]), reads=[bs], writes=[bs])
                    yn, byn = ynr.next()
                    yn3 = yn[:].rearrange("p (h x) -> p h x", x=64)
                    k.op("dve", lambda e: e.tensor_tensor(yn3, y3, s[:, 2, :].unsqueeze(2).to_broadcast([128, 8, 64]), ALU.subtract),
                         reads=allY + [bs], writes=[byn])
                    k.op("pool", lambda e: e.tensor_tensor(yn3, yn3, s[:, 5, :].unsqueeze(2).to_broadcast([128, 8, 64]), ALU.mult),
                         reads=[byn, bs], writes=[byn])
                    pr, bpr = psR.next()
                    for hp in range(4):
                        k.op("pe", lambda e: e.transpose(pr[:, hp, :], yn[:, hp * 128:(hp + 1) * 128], identf[:]), reads=[byn, b_c], writes=[bpr])
                    cs = slice(c * 128, (c + 1) * 128)
                    for hp in range(4):
                        of, bof = ofr.next()
                        k.op("act", lambda e: e.activation(of[:], pr[:, hp, :], AF.Identity, bias=vec[:, 47 + hp:48 + hp], scale=vec[:, 43 + hp:44 + hp]),
                             reads=[bpr, b_c], writes=[bof])
                        k.op("dve", lambda e: e.tensor_tensor(of[:], of[:], BON[:, hp, cs], ALU.add), reads=[bof, b_l], writes=[bof])
                        ob, bob = obr.next()
                        k.op("pool", lambda e: e.tensor_tensor(ob[:], of[:], G[:, hp, cs], ALU.mult), reads=[bof, b_l], writes=[bob])
                        k.dma("sp", YT[b, 1, hp * 128:(hp + 1) * 128, cs], ob[:], reads=[bob], writes=[C.bYT])


def build(n_layers=DEPTH, debug=(), stop_after=None):
    P = Prog(n_layers, debug)
    nc, k = P.nc, P.k
    xt0 = P.din("xt0", [NB, D, TT])
    cT = P.din("cT", [128, KC, 3])
    cst_ones = P.din("ones", [128, 128])
    ada_w = P.din("ada_w", [DEPTH, D, 6 * D])
    ada_b = P.din("ada_b_r", [DEPTH, 128, 48])
    ng_r = P.din("ng_r", [DEPTH, 128, 2, KC])
    w_in = P.din("w_in", [DEPTH, D, N_IN])
    XT = P.dscr("XT", [NB, D, TT], F32)
    PT = P.dscr("PT", [NB, NCH * 128, TT], BF16)
    bXT = [Buf("XT0"), Buf("XT1")]
    bPT = Buf("PT")
    if "yt_in" in P.debug:
        YT = P.din("YT3", [NB, 3, 512, TT], BF16)
        P.dbufs["YT3"] = Buf("YT3")
    else:
        YT = P.dscr("YT3", [NB, 3, 512, TT], BF16)
    C = type("Ctx", (), {})()
    C.XT, C.PT, C.YT, C.bXT, C.bPT, C.bYT = XT, PT, YT, bXT, bPT, Buf("YT3")
    C.xt0 = xt0
    declare_inputs(P, C)

    with P.scope():
        ones = P.sb("ones", [128, 128], F32)
        b_ones = Buf("ones")
        k.dma("sp", ones[:], cst_ones, writes=[b_ones])
        silu_c = P.sb("silu_c", [128, KC, 3], F32)
        b_silu = Buf("silu_c")
        k.dma("sp", silu_c[:], cT, writes=[b_silu])
        k.op("act", lambda e: e.activation(silu_c[:], silu_c[:], AF.Silu), reads=[b_silu], writes=[b_silu])
        C.epsc = P.sb("epsc_g", [128, 1], F32)
        k.op("dve", lambda e: e.memset(C.epsc[:], NORM_EPS), writes=[b_ones])
        mod = P.sb("mod", [128, 48, 3], F32)
        b_mod = Buf("mod")
        gs = P.sb("gs", [128, 2, KC, 3], F32)
        b_gs = Buf("gs")

        for l in range(n_layers):
            src_x = xt0 if l == 0 else XT
            with P.scope():
                adab = P.sb("adab", [128, 48], F32)
                b_adab = Buf("adab")
                k.dma("sp", adab[:], ada_b[l], writes=[b_adab])
                ng = P.sb("ng", [128, 2, KC], F32)
                b_ng = Buf("ng")
                k.dma("sp", ng[:], ng_r[l], writes=[b_ng])
                wr = Ring(P, "adaw", 2, [128, KC, 512], F32)
                pr = Ring(P, "modps", 2, [128, 4, 3], F32, psum=True)
                for g in range(12):
                    wt, bw = wr.next()
                    k.dma("sp" if g % 2 == 0 else "act", wt[:],
                          ada_w[l][:, g * 512:(g + 1) * 512].rearrange("(kc p) n -> p kc n", p=128), writes=[bw])
                    pt, bp = pr.next()
                    for c4 in range(4):
                        for kc in range(KC):
                            k.op("pe", lambda e, c4=c4, kc=kc: e.matmul(
                                pt[:, c4, :], wt[:, kc, c4 * 128:(c4 + 1) * 128], silu_c[:, kc, :],
                                start=(kc == 0), stop=(kc == KC - 1)),
                                reads=[bw, b_silu], writes=[bp])
                    k.op("dve", lambda e: e.tensor_tensor(
                        mod[:, g * 4:(g + 1) * 4, :], pt[:],
                        adab[:, g * 4:(g + 1) * 4].unsqueeze(2).to_broadcast([128, 4, 3]), ALU.add),
                        reads=[bp, b_adab], writes=[b_mod])
                for n, mi in ((0, 1), (1, 4)):
                    k.op("dve", lambda e, n=n, mi=mi: e.tensor_scalar(
                        gs[:, n, :, :], mod[:, mi * 8:(mi + 1) * 8, :], 1.0, None, ALU.add),
                        reads=[b_mod], writes=[b_gs])
                    k.op("dve", lambda e, n=n: e.tensor_tensor(
                        gs[:, n, :, :], gs[:, n, :, :],
                        ng[:, n, :].unsqueeze(2).to_broadcast([128, KC, 3]), ALU.mult),
                        reads=[b_gs, b_ng], writes=[b_gs])
            if stop_after == "mod":
                break

            with P.scope():
                hT = P.sb("hT", [128, NB, KC, TT], BF16)
                b_h = [[Buf(f"h{b}_{i}") for i in range(5)] for b in range(NB)]
                blocks = [(0, TC)] + [(TC + i * 512, 512) for i in range(4)]
                xr = Ring(P, "xin", 2, [128, KC, 512], F32)
                sqr = Ring(P, "sq", 1, [128, KC, 512], F32)
                ssr = Ring(P, "ssps", 2, [128, 512], F32, psum=True)
                rsr = Ring(P, "rstd", 2, [128, 512], F32)
                for b in range(NB):
                    for bi, (t0, n) in enumerate(blocks):
                        j = 2 if bi == 0 else b
                        xt, bx = xr.next()
                        k.dma("sp", xt[:, :, :n], src_x[b][:, t0:t0 + n].rearrange("(kc p) n -> p kc n", p=128),
                              reads=[bXT[b]], writes=[bx])
                        sq, bs = sqr.next()
                        k.op("act", lambda e: e.activation(sq[:, :, :n], xt[:, :, :n], AF.Square),
                             reads=[bx], writes=[bs])
                        ss, bss = ssr.next()
                        for kc in range(KC):
                            k.op("pe", lambda e, kc=kc: e.matmul(ss[:, :n], ones[:], sq[:, kc, :n],
                                                                 start=(kc == 0), stop=(kc == KC - 1)),
                                 reads=[bs, b_ones], writes=[bss])
                        rs, brs = rsr.next()
                        k.op("act", lambda e: e.activation(rs[:, :n], ss[:, :n], AF.Sqrt, bias=NORM_EPS, scale=1.0 / D),
                             reads=[bss], writes=[brs])
                        k.op("dve", lambda e: e.reciprocal(rs[:, :n], rs[:, :n]), reads=[brs], writes=[brs])
                        k.op("dve", lambda e: e.tensor_tensor(
                            sq[:, :, :n], xt[:, :, :n], rs[:, :n].unsqueeze(1).to_broadcast([128, KC, n]), ALU.mult),
                            reads=[bx, brs, bs], writes=[bs])
                        for kc in range(KC):
                            k.op("act", lambda e, kc=kc: e.activation(
                                hT[:, b, kc, t0:t0 + n], sq[:, kc, :n], AF.Identity,
                                bias=mod[:, 0 * 8 + kc, j:j + 1], scale=gs[:, 0, kc, j:j + 1]),
                                reads=[bs, b_mod, b_gs], writes=[b_h[b][bi]])
                groups = [list(range(g * 4, g * 4 + 4)) for g in range(6)] + [[24]] + \
                         [list(range(25 + g * 4, 29 + g * 4)) for g in range(6)]
                wr = Ring(P, "win", 2, [128, KC, 512], BF16)
                pr = Ring(P, "inps", 4, [128, 512], F32, psum=True)
                sr = Ring(P, "instage", 4, [128, 512], BF16)
                ev = 0
                for grp in groups:
                    c0 = chunk_cols(grp[0])[0]
                    ncols = sum(chunk_cols(ci)[1] for ci in grp)
                    wt, bw = wr.next()
                    k.dma("pool", wt[:, :, :ncols],
                          w_in[l][:, c0:c0 + ncols].rearrange("(kc p) n -> p kc n", p=128), writes=[bw])
                    for b in range(NB):
                        for bi, (t0, n) in enumerate(blocks):
                            for ci in grp:
                                cc0, cn = chunk_cols(ci)
                                o = cc0 - c0
                                pt, bp = pr.next()
                                for kc in range(KC):
                                    k.op("pe", lambda e, kc=kc: e.matmul(
                                        pt[:cn, :n], wt[:, kc, o:o + cn], hT[:, b, kc, t0:t0 + n],
                                        start=(kc == 0), stop=(kc == KC - 1)),
                                        reads=[bw, b_h[b][bi]], writes=[bp])
                                st, bst = sr.next()
                                if ci >= 25:
                                    k.op("act", lambda e: e.activation(st[:cn, :n], pt[:cn, :n], AF.Sigmoid),
                                         reads=[bp], writes=[bst])
                                elif ev % 2 == 0:
                                    k.op("act", lambda e: e.activation(st[:cn, :n], pt[:cn, :n], AF.Copy),
                                         reads=[bp], writes=[bst])
                                else:
                                    k.op("dve", lambda e: e.tensor_copy(st[:cn, :n], pt[:cn, :n]),
                                         reads=[bp], writes=[bst])
                                ev += 1
                                k.dma("sp", PT[b, ci * 128:ci * 128 + cn, t0:t0 + n], st[:cn, :n],
                                      reads=[bst], writes=[bPT])
            if stop_after == "in":
                break
            C.mod, C.b_mod, C.gs, C.b_gs, C.ones, C.b_ones = mod, b_mod, gs, b_gs, ones, b_ones
            if "nomla" not in P.debug and "yt_in" not in P.debug:
                phase_mla(P, C, l)
            if stop_after == "mla":
                break
            if "nossm" not in P.debug and "yt_in" not in P.debug:
                phase_ssm(P, C, l)
            if stop_after == "ssm":
                break
            if "yt_in" not in P.debug:
                phase_rwkv_prep(P, C, l)
                if stop_after == "rwprep":
                    break
                phase_rwkv_scan(P, C, l)
                if stop_after == "rwkv":
                    break
            phase_merge(P, C, l)
            if stop_after == "merge":
                break
            phase_moe(P, C, l, last=(l == n_layers - 1))
            if stop_after == "moe":
                break
        k.barrier()
    return P


def host_inputs(inputs, core):
    b0 = core * NB
    x = inputs["x"][b0:b0 + NB]
    ctx = inputs["ctx"][b0:b0 + NB]
    xt0 = np.ascontiguousarray(np.concatenate([ctx, x], axis=1).transpose(0, 2, 1))
    cT = np.stack([inputs["c"][b0], inputs["c"][b0 + 1], inputs["c_ctx"]], axis=1)
    cT = np.ascontiguousarray(cT.reshape(KC, 128, 3).transpose(1, 0, 2))
    m = {"xt0": xt0, "cT": cT}
    return m


def pj(v, n):
    return np.ascontiguousarray(v.reshape(v.shape[:-1] + (n, 128)).swapaxes(-1, -2))


def host_shared(inputs):
    m = {"ones": np.ones((128, 128), np.float32)}
    for name in ("ada_w", "w_in"):
        m[name] = inputs[name]
    for name in ("mla_w_uq", "mla_w_ukv"):
        m[name] = inputs[name]
    mats = np.zeros((128, 3, 128), np.float32)
    mats[:64, 0, :64] = 1.0
    mats[64:96, 0, 64:96] = 1.0
    mats[:64, 1, :64] = 1.0
    for i in range(16):
        mats[64 + 16 + i, 2, 64 + i] = -1.0
        mats[64 + i, 2, 64 + 16 + i] = 1.0
    m["mla_mats"] = mats
    vec = np.zeros((DEPTH, 128, 8), np.float32)
    vec[:, :, 0:3] = pj(inputs["mla_q_norm"], 3)
    vec[:, :, 3:5] = pj(inputs["mla_kv_norm"], 2)
    vec[:, :64, 5] = inputs["mla_qn_nope"]
    vec[:, 64:96, 5] = inputs["mla_qn_rope"]
    vec[:, :64, 6] = inputs["mla_kn_nope"]
    vec[:, 64:96, 6] = inputs["mla_kn_rope"]
    vec[:, :64, 7] = 1.0 / 64.0
    vec[:, 64:96, 7] = 1.0 / 32.0
    m["mla_vec"] = vec
    tt = np.arange(TL)
    inv = (10000.0 ** (-np.arange(0, 16, 2, dtype=np.float32) / 16.0)).astype(np.float32)
    ang = np.concatenate([(tt // 64).astype(np.float32)[:, None] * inv, (tt % 64).astype(np.float32)[:, None] * inv], axis=-1)
    tab = np.zeros((96, 2, TT), np.float32)
    tab[:, 0, :] = 1.0
    tab[64:80, 0, TC:] = np.cos(ang).T
    tab[80:96, 0, TC:] = np.cos(ang).T
    tab[64:80, 1, TC:] = np.sin(ang).T
    tab[80:96, 1, TC:] = np.sin(ang).T
    m["rope_tab"] = tab
    it = np.zeros((128, 2, TT), np.float32)
    it[:, 0, :] = np.arange(TT)
    it[:, 1, :TC] = TC - 1 - np.arange(TC)
    it[:, 1, TC:] = TC + (TL - 1 - np.arange(TL))
    m["ssm_iota"] = it
    L_ = DEPTH
    lre = inputs["ssm_lambda_re"].reshape(L_, 2, 16, 128)
    lim = inputs["ssm_lambda_im"].reshape(L_, 2, 16, 128)
    ldt = np.repeat(inputs["ssm_log_dt"], 64, axis=-1).reshape(L_, 2, 16, 128)
    m["ssm_sv"] = np.ascontiguousarray(np.stack([lre, lim, ldt], axis=-1).transpose(0, 3, 1, 2, 4))
    BT = np.zeros((L_, 2, 16, 128, 128), np.float32)
    CT = np.zeros((L_, 2, 2, 16, 128, 128), np.float32)
    for ri, (bn, cn) in enumerate((("ssm_b_re", "ssm_c_re"), ("ssm_b_im", "ssm_c_im"))):
        bb = inputs[bn]
        cc = inputs[cn]
        for g in range(32):
            sc, gg = g // 2, g % 2
            r0 = (sc % 4) * 32 + gg * 16
            BT[:, ri, sc, r0:r0 + 16, gg * 64:(gg + 1) * 64] = bb[:, g].transpose(0, 2, 1)
            CT[:, :, ri, sc, gg * 64:(gg + 1) * 64, r0:r0 + 16] = cc[:, :, g].transpose(0, 1, 3, 2)
    m["ssm_BT"] = BT
    m["ssm_CT"] = CT
    m["ssm_vec"] = np.ascontiguousarray(np.stack([pj(inputs["ssm_d"], 4), pj(inputs["ssm_glu_b"], 4)], axis=2))
    m["ssm_glu_w"] = inputs["ssm_glu_w"]
    for name in ("w_branch", "w_out", "router_w"):
        m[name] = inputs[name]
    for name in ("moe_w1", "moe_w3", "moe_w2"):
        m[name] = inputs[name]
    cst = np.zeros((128, 3, 256), np.float32)
    cst[:, 0, :] = np.arange(1, 257)
    cst[:16, 1, :16] = 1.0
    cst[16:32, 1, 16:32] = 1.0
    cst[:, 2, :128] = np.eye(128)
    m["moe_cst"] = cst
    m["moe_jcol"] = np.stack([np.arange(1, 129), np.arange(129, 257)], axis=1).astype(np.float32)
    L_ = DEPTH
    rv = np.zeros((L_, 128, 51), np.float32)
    rv[:, :, 0:15] = pj(inputs["rwkv_mu"], 15)
    rv[:, :, 15:23] = pj(inputs["rwkv_w0"], 4).transpose(0, 2, 1, 3).reshape(L_, 128, 8)
    rv[:, :, 23:31] = pj(inputs["rwkv_a0"], 4).transpose(0, 2, 1, 3).reshape(L_, 128, 8)
    rv[:, :, 31:35] = pj(inputs["rwkv_k_k"], 4)
    rv[:, :, 35:39] = pj(inputs["rwkv_k_a"], 4)
    rv[:, :, 39:43] = pj(inputs["rwkv_r_k"].reshape(L_, 512), 4)
    rv[:, :, 43:47] = pj(inputs["rwkv_ln_w"], 4)
    rv[:, :, 47:51] = pj(inputs["rwkv_ln_b"], 4)
    m["rw_vec"] = rv
    for name in ("rwkv_w2", "rwkv_a2", "rwkv_g2"):
        m[name] = inputs[name]
    bd = np.zeros((128, 128), np.float32)
    bd[:64, :64] = 1.0
    bd[64:, 64:] = 1.0
    m["rw_bd"] = bd
    rst = np.ones((128, TT + 1), np.float32)
    rst[:, 0::128] = 0.0
    m["rw_rst"] = rst
    ii = np.arange(128)
    mk = np.zeros((128, 2, 640), np.float32)
    for j_ in range(2):
        if j_ == 0:
            strict = (ii[None, :] > ii[:, None]).astype(np.float32)
            incl = (ii[None, :] >= ii[:, None]).astype(np.float32)
        else:
            strict = (ii[None, :] < ii[:, None]).astype(np.float32)
            incl = (ii[None, :] <= ii[:, None]).astype(np.float32)
        mk[:, j_, 0:128] = strict
        mk[:, j_, 128:256] = incl
        mk[:, j_, 256:384] = -strict
        mk[:, j_, 384:512] = incl
        mk[:, j_, 512:640] = -strict.T
    m["rw_masks"] = mk
    lm = np.zeros((128, 4, 128), np.float32)
    ti, si = ii[:, None], ii[None, :]
    lm[:, 0, :] = (ti // 16 == si // 16)
    for q_, sz in enumerate((16, 32, 64)):
        lm[:, 1 + q_, :] = (ti // (2 * sz) == si // (2 * sz)) & (ti // sz != si // sz)
    m["rw_lm"] = lm
    m["ident"] = np.eye(128, dtype=np.float32)
    m["ada_b_r"] = pj(inputs["ada_b"], 48)
    m["ng_r"] = np.ascontiguousarray(np.stack([pj(inputs["norm1_g"], KC), pj(inputs["norm2_g"], KC)], axis=2))
    return m


def kernel(**inputs):
    inputs = {k_: np.asarray(v) for k_, v in inputs.items()}
    P = build()
    shared = host_shared(inputs)
    in_maps = []
    for c in range(NCORES):
        m = host_inputs(inputs, c)
        m.update(shared)
        in_maps.append({n: m[n] for n in P.inputs})
    res = run_bass_kernel_spmd(P.nc, in_maps, core_ids=list(range(NCORES)))
    outs = [r["OUT"] for r in res.results]
    y = np.concatenate(outs, axis=0)
    return np.ascontiguousarray(y.transpose(0, 2, 1)).astype(np.float32)
```
